# Optimizing a Trainium2 kernel written in Bass

```python
import math
import jax, jax.numpy as jnp
from jax import lax
import numpy as np

D_MODEL = 2048
BATCH = 2
SEQ = 8192
DEPTH = 1

D_RNN = D_MODEL
LRU_BLOCK = 128
N_LRU_BLOCKS = D_RNN // LRU_BLOCK
CONV_W = 4
C_LRU = 8.0
HEAD_DIM = 128
N_HEADS = D_MODEL // (2 * HEAD_DIM)
V_DIM = 2 * HEAD_DIM
QK_W = N_HEADS * 2 * HEAD_DIM
ATTN_W = N_HEADS * V_DIM
Q_BLOCK = 128
ROPE_THETA = 10000.0
SUBLN_EPS = 1e-5
N_GROUPS = 4
EXPERTS_PER_GROUP = 8
N_EXPERTS = N_GROUPS * EXPERTS_PER_GROUP
TOP_K = 2
D_EXPERT = D_MODEL // 2
MOE_BLOCK = 128
D_IN = 2 * D_RNN + 2 * QK_W + ATTN_W + 2 * D_MODEL
NORM_EPS = 1e-6

kernel_name = 'hybrid_rglru_diffattn_hmoe'


def rmsnorm(x, g, eps=NORM_EPS):
    xf = x.astype(jnp.float32)
    y = xf * lax.rsqrt(jnp.mean(xf * xf, axis=-1, keepdims=True) + eps)
    return (y * g.astype(jnp.float32)).astype(x.dtype)


def rope(t, positions):
    inv = 1.0 / (ROPE_THETA ** (jnp.arange(0, HEAD_DIM, 2, dtype=jnp.float32) / HEAD_DIM))
    ang = positions.astype(jnp.float32)[..., None] * inv
    ang = jnp.concatenate([ang, ang], axis=-1)[:, :, None, None, :]
    cos, sin = jnp.cos(ang), jnp.sin(ang)
    tf = t.astype(jnp.float32)
    t1, t2 = jnp.split(tf, 2, axis=-1)
    rot = jnp.concatenate([-t2, t1], axis=-1)
    return (tf * cos + rot * sin).astype(t.dtype)


def causal_conv(u, w, b):
    C = u.shape[-1]
    out = lax.conv_general_dilated(
        u, w[:, None, :].astype(u.dtype), window_strides=(1,),
        padding=[(CONV_W - 1, 0)], dimension_numbers=('NWC', 'WIO', 'NWC'),
        feature_group_count=C)
    return out + b.astype(u.dtype)


def rg_lru(u, positions, w_a, b_a, w_x, b_x, lru_param):
    B, S, C = u.shape
    ub = u.reshape(B, S, N_LRU_BLOCKS, LRU_BLOCK)
    r = jax.nn.sigmoid(jnp.einsum('bsnc,ncd->bsnd', ub, w_a).reshape(B, S, C).astype(jnp.float32)
                       + b_a.astype(jnp.float32))
    i = jax.nn.sigmoid(jnp.einsum('bsnc,ncd->bsnd', ub, w_x).reshape(B, S, C).astype(jnp.float32)
                       + b_x.astype(jnp.float32))
    log_a = -C_LRU * r * jax.nn.softplus(-lru_param.astype(jnp.float32))
    reset = (positions == 0)[..., None]
    a = jnp.where(reset, 0.0, jnp.exp(log_a))
    mult = jnp.where(reset, 1.0, jnp.sqrt(-jnp.expm1(2.0 * log_a)))
    bvals = u.astype(jnp.float32) * i * mult

    def combine(left, right):
        a1, b1 = left
        a2, b2 = right
        return a1 * a2, a2 * b1 + b2

    _, h = lax.associative_scan(combine, (a, bvals), axis=1)
    return h.astype(u.dtype)


def diff_attention(q, k, v, positions, lq1, lk1, lq2, lk2, subln_g, lam_init):
    B, S, _ = q.shape
    q = rope(q.reshape(B, S, N_HEADS, 2, HEAD_DIM), positions)
    k = rope(k.reshape(B, S, N_HEADS, 2, HEAD_DIM), positions)
    v = v.reshape(B, S, N_HEADS, V_DIM)
    lam = (jnp.exp(jnp.sum(lq1.astype(jnp.float32) * lk1.astype(jnp.float32)))
           - jnp.exp(jnp.sum(lq2.astype(jnp.float32) * lk2.astype(jnp.float32))) + lam_init)
    nb = S // Q_BLOCK
    qb = q.reshape(B, nb, Q_BLOCK, N_HEADS, 2, HEAD_DIM).transpose(1, 0, 2, 3, 4, 5)
    kidx = jnp.arange(S)
    scale = HEAD_DIM ** -0.5

    def block(args):
        qi, bi = args
        s = jnp.einsum('bqhmd,bkhmd->bhmqk', qi, k).astype(jnp.float32) * scale
        qidx = bi * Q_BLOCK + jnp.arange(Q_BLOCK)
        mask = kidx[None, :] <= qidx[:, None]
        s = jnp.where(mask, s, -jnp.inf)
        p = jax.nn.softmax(s, axis=-1)
        amap = p[:, :, 0] - lam * p[:, :, 1]
        return jnp.einsum('bhqk,bkhe->bqhe', amap.astype(v.dtype), v)

    o = lax.map(block, (qb, jnp.arange(nb)))
    o = o.transpose(1, 0, 2, 3, 4).reshape(B, S, N_HEADS, V_DIM)
    o = rmsnorm(o, subln_g, eps=SUBLN_EPS) * (1.0 - lam_init)
    return o.reshape(B, S, ATTN_W)


def hier_moe(h, w_grp, w_exp, w_gate, w_up, w_down):
    B, S, D = h.shape
    N = B * S
    hf = h.reshape(N, D)
    gp = jax.nn.softmax((hf @ w_grp).astype(jnp.float32), axis=-1)
    g_idx = jnp.argmax(gp, axis=-1).astype(jnp.int32)
    g_w = jnp.take_along_axis(gp, g_idx[:, None], axis=-1)
    el = (hf @ w_exp).astype(jnp.float32).reshape(N, N_GROUPS, EXPERTS_PER_GROUP)
    el = jnp.take_along_axis(el, g_idx[:, None, None], axis=1)[:, 0]
    ep = jax.nn.softmax(el, axis=-1)
    top_v, top_i = lax.top_k(ep, TOP_K)
    top_v = top_v / jnp.sum(top_v, axis=-1, keepdims=True)
    wts = g_w * top_v
    eid = g_idx[:, None] * EXPERTS_PER_GROUP + top_i.astype(jnp.int32)
    NK = N * TOP_K
    flat_e = eid.reshape(NK)
    flat_t = jnp.repeat(jnp.arange(N, dtype=jnp.int32), TOP_K)
    flat_w = wts.reshape(NK)
    order = jnp.argsort(flat_e)
    se = flat_e[order]
    counts = jnp.bincount(flat_e, length=N_EXPERTS).astype(jnp.int32)
    offsets = jnp.cumsum(counts) - counts
    pcounts = ((counts + MOE_BLOCK - 1) // MOE_BLOCK) * MOE_BLOCK
    pends = jnp.cumsum(pcounts)
    pstarts = pends - pcounts
    dest = pstarts[se] + jnp.arange(NK, dtype=jnp.int32) - offsets[se]
    P = NK + N_EXPERTS * MOE_BLOCK
    nb = P // MOE_BLOCK
    slot_tok = jnp.full((P,), N, dtype=jnp.int32).at[dest].set(flat_t[order])
    slot_w = jnp.zeros((P,), jnp.float32).at[dest].set(flat_w[order])
    blk_e = jnp.clip(jnp.searchsorted(pends, jnp.arange(nb, dtype=jnp.int32) * MOE_BLOCK,
                                      side='right'), 0, N_EXPERTS - 1)
    xpad = jnp.concatenate([hf, jnp.zeros((1, D), hf.dtype)], axis=0)
    xs = xpad[slot_tok].reshape(nb, MOE_BLOCK, D)

    def expert_block(args):
        xb, e = args
        return (jax.nn.silu(xb @ w_gate[e]) * (xb @ w_up[e])) @ w_down[e]

    ys = lax.map(expert_block, (xs, blk_e)).reshape(P, D)
    ys = ys * slot_w[:, None].astype(ys.dtype)
    out = jnp.zeros((N + 1, D), h.dtype).at[slot_tok].add(ys)[:N]
    return out.reshape(B, S, D)


def setup_inputs(seed: int = 0) -> dict:
    key = jax.random.key(seed)
    ks = jax.random.split(key, 24)
    f32 = jnp.float32
    nrm = lambda k, shape, s: jax.random.normal(k, shape, f32) * s
    L = DEPTH
    u = jax.random.uniform(ks[8], (L, D_RNN), f32, 0.9, 0.999)
    a0 = u ** (1.0 / C_LRU)
    lru_param = jnp.log(a0) - jnp.log1p(-a0)
    return {
        'x': nrm(ks[0], (BATCH, SEQ, D_MODEL), 1.0),
        'positions': jnp.broadcast_to(jnp.arange(SEQ, dtype=jnp.int32), (BATCH, SEQ)),
        'g_mix': 1.0 + nrm(ks[1], (L, D_MODEL), 0.02),
        'w_in': nrm(ks[2], (L, D_MODEL, D_IN), D_MODEL ** -0.5),
        'conv_w': nrm(ks[3], (L, CONV_W, D_RNN), CONV_W ** -0.5),
        'conv_b': nrm(ks[4], (L, D_RNN), 0.01),
        'w_rg_a': nrm(ks[5], (L, N_LRU_BLOCKS, LRU_BLOCK, LRU_BLOCK), LRU_BLOCK ** -0.5),
        'b_rg_a': nrm(ks[6], (L, D_RNN), 0.01),
        'w_rg_x': nrm(ks[7], (L, N_LRU_BLOCKS, LRU_BLOCK, LRU_BLOCK), LRU_BLOCK ** -0.5),
        'b_rg_x': nrm(ks[9], (L, D_RNN), 0.01),
        'lru_param': lru_param,
        'lambda_q1': nrm(ks[10], (L, HEAD_DIM), 0.1),
        'lambda_k1': nrm(ks[11], (L, HEAD_DIM), 0.1),
        'lambda_q2': nrm(ks[12], (L, HEAD_DIM), 0.1),
        'lambda_k2': nrm(ks[13], (L, HEAD_DIM), 0.1),
        'subln_g': 1.0 + nrm(ks[14], (L, V_DIM), 0.02),
        'w_br_rnn': nrm(ks[15], (L, D_RNN, D_MODEL), D_RNN ** -0.5),
        'w_br_attn': nrm(ks[16], (L, ATTN_W, D_MODEL), ATTN_W ** -0.5),
        'w_out': nrm(ks[17], (L, D_MODEL, D_MODEL), D_MODEL ** -0.5),
        'g_ffn': 1.0 + nrm(ks[18], (L, D_MODEL), 0.02),
        'w_grp_router': nrm(ks[19], (L, D_MODEL, N_GROUPS), D_MODEL ** -0.5),
        'w_exp_router': nrm(ks[20], (L, D_MODEL, N_EXPERTS), D_MODEL ** -0.5),
        'w_gate': nrm(ks[21], (L, N_EXPERTS, D_MODEL, D_EXPERT), D_MODEL ** -0.5),
        'w_up': nrm(ks[22], (L, N_EXPERTS, D_MODEL, D_EXPERT), D_MODEL ** -0.5),
        'w_down': nrm(ks[23], (L, N_EXPERTS, D_EXPERT, D_MODEL), D_EXPERT ** -0.5),
        'g_final': 1.0 + nrm(jax.random.fold_in(key, 99), (D_MODEL,), 0.02),
    }


def reference(x, positions, g_mix, w_in, conv_w, conv_b, w_rg_a, b_rg_a, w_rg_x, b_rg_x,
              lru_param, lambda_q1, lambda_k1, lambda_q2, lambda_k2, subln_g, w_br_rnn,
              w_br_attn, w_out, g_ffn, w_grp_router, w_exp_router, w_gate, w_up, w_down,
              g_final):
    splits = [D_RNN, 2 * D_RNN, 2 * D_RNN + QK_W, 2 * D_RNN + 2 * QK_W,
              2 * D_RNN + 2 * QK_W + ATTN_W, 2 * D_RNN + 2 * QK_W + ATTN_W + D_MODEL]
    for l in range(DEPTH):
        lam_init = 0.8 - 0.6 * math.exp(-0.3 * l)
        h = rmsnorm(x, g_mix[l])
        proj = h @ w_in[l]
        u, gb, q, k, v, gr, ga = jnp.split(proj, splits, axis=-1)
        u = causal_conv(u, conv_w[l], conv_b[l])
        hr = rg_lru(u, positions, w_rg_a[l], b_rg_a[l], w_rg_x[l], b_rg_x[l], lru_param[l])
        y_rnn = hr * jax.nn.gelu(gb, approximate=True)
        y_attn = diff_attention(q, k, v, positions, lambda_q1[l], lambda_k1[l],
                                lambda_q2[l], lambda_k2[l], subln_g[l], lam_init)
        merged = (jax.nn.sigmoid(gr) * (y_rnn @ w_br_rnn[l])
                  + jax.nn.sigmoid(ga) * (y_attn @ w_br_attn[l]))
        x = x + merged @ w_out[l]
        x = x + hier_moe(rmsnorm(x, g_ffn[l]), w_grp_router[l], w_exp_router[l],
                         w_gate[l], w_up[l], w_down[l])
    return rmsnorm(x, g_final)
```

```python
import math
import contextlib
import numpy as np
import concourse.bass as bass
import concourse.mybir as mybir
from concourse.bass_utils import run_bass_kernel_spmd

F32 = mybir.dt.float32
BF16 = mybir.dt.bfloat16
I32 = mybir.dt.int32
ALU = mybir.AluOpType
AF = mybir.ActivationFunctionType
AX = mybir.AxisListType

D = 2048
T = 8192
OWN0 = 6144
NOWN = 2048
KC = 16
NE = 32
DE = 1024
NH = 8
TWO_PI = 2.0 * math.pi


class Buf:
    __slots__ = ("w", "r")

    def __init__(self):
        self.w = None
        self.r = {}


class Sched:
    def __init__(self, nc, es):
        self.nc = nc
        self.st = {}
        for name, h, nd in [("pe", nc.tensor, 0), ("dve", nc.vector, 0), ("act", nc.scalar, 6),
                            ("pool", nc.gpsimd, 6), ("sp", nc.sync, 10)]:
            st = dict(h=h, cnt=0, seen={}, dcnt=0, name=name)
            st["sem"] = es.enter_context(nc.semaphore("c_" + name))
            st["dsems"] = [es.enter_context(nc.semaphore(f"d_{name}{i}")) for i in range(nd)]
            self.st[name] = st

    @staticmethod
    def _add(d, ev):
        if ev is None:
            return
        sem, val, key = ev
        if key not in d or d[key][1] < val:
            d[key] = (sem, val, key)

    def _deps(self, reads, writes):
        d = {}
        for b in reads:
            self._add(d, b.w)
        for b in writes:
            self._add(d, b.w)
            for ev in b.r.values():
                self._add(d, ev)
        return d

    def _wait(self, st, d, skip=None):
        for key, (sem, val, _) in d.items():
            if key == skip:
                continue
            if st["seen"].get(key, 0) >= val:
                continue
            st["h"].wait_ge(sem, val)
            st["seen"][key] = val

    def _mark(self, reads, writes, ev):
        for b in reads:
            self._add(b.r, ev)
        for b in writes:
            b.w = ev
            b.r = {}

    def op(self, stname, fn, reads=(), writes=()):
        st = self.st[stname]
        d = self._deps(reads, writes)
        self._wait(st, d, skip=("c_pe" if stname == "pe" else None))
        ins = fn(st["h"])
        st["cnt"] += 1
        ins.then_inc(st["sem"], 1)
        self._mark(reads, writes, (st["sem"], st["cnt"], "c_" + stname))

    def dma(self, stname, out, in_, reads=(), writes=()):
        st = self.st[stname]
        d = self._deps(reads, writes)
        i = st["dcnt"]
        R = len(st["dsems"])
        k = i % R
        sem = st["dsems"][k]
        val = 16 * (i // R + 1)
        key = f"d_{stname}{k}"
        if i >= R:
            self._add(d, (sem, val - 16, key))
        self._wait(st, d)
        st["h"].dma_start(out=out, in_=in_).then_inc(sem, 16)
        st["dcnt"] += 1
        self._mark(reads, writes, (sem, val, key))

    def all_events(self):
        d = {}
        for name, st in self.st.items():
            if st["cnt"] > 0:
                self._add(d, (st["sem"], st["cnt"], "c_" + name))
            R = len(st["dsems"])
            for k in range(R):
                n = (st["dcnt"] - k + R - 1) // R if st["dcnt"] > k else 0
                if n > 0:
                    self._add(d, (st["dsems"][k], 16 * n, f"d_{name}{k}"))
        return d

    def barrier(self, only=None):
        d = self.all_events()
        for name, st in self.st.items():
            if only is not None and name not in only:
                continue
            self._wait(st, d)


def build(upto=99, debug=()):
    nc = bass.Bass("TRN2", target_bir_lowering=False)
    dbg = set(debug)

    def dram_in(name, shape, dt=F32):
        return nc.dram_tensor(name, list(shape), dt, kind="ExternalInput").ap()

    def dram_tmp(name, shape, dt):
        kind = "ExternalOutput" if name in dbg else "Internal"
        return nc.dram_tensor(name, list(shape), dt, kind=kind).ap()

    xf = dram_in("xf", [T, D])
    posf = dram_in("posf", [T], I32)
    valid = dram_in("valid", [T])
    g_mix = dram_in("g_mix", [128, KC])
    w_in = dram_in("w_in", [D, 14336])
    conv_w = dram_in("conv_w", [128, 4, KC])
    conv_b = dram_in("conv_b", [128, KC])
    w_rg_a = dram_in("w_rg_a", [16, 128, 128])
    b_rg_a = dram_in("b_rg_a", [128, KC])
    w_rg_x = dram_in("w_rg_x", [16, 128, 128])
    b_rg_x = dram_in("b_rg_x", [128, KC])
    lru_param = dram_in("lru_param", [128, KC])
    lam_in = dram_in("lam_in", [4, 128])
    subln_g = dram_in("subln_g", [256])
    w_br_rnn = dram_in("w_br_rnn", [D, D])
    w_br_attn = dram_in("w_br_attn", [D, D])
    w_out = dram_in("w_out", [D, D])
    g_ffn = dram_in("g_ffn", [128, KC])
    valid_pk = dram_in("valid_pk", [128, 64])
    w_router = dram_in("w_router", [D, 36])
    w_gate = dram_in("w_gate", [NE, D, DE])
    w_up = dram_in("w_up", [NE, D, DE])
    w_down = dram_in("w_down", [NE, DE, D])
    g_final = dram_in("g_final", [D])
    c_rope = dram_in("c_rope", [128, 2])
    c_swap = dram_in("c_swap", [128, 128])
    c_ident = dram_in("c_ident", [128, 128])
    c_tri = dram_in("c_tri", [128, 4, 512])
    out = nc.dram_tensor("out", [NOWN, D], F32, kind="ExternalOutput").ap()

    hT_d = dram_tmp("hT_d", [16, 128, KC, 512], BF16)
    uT_d = dram_tmp("uT_d", [D, T], F32)
    KT_d = dram_tmp("KT_d", [16, 128, T], BF16)
    V_d = dram_tmp("V_d", [T, D], BF16)
    gbT_d = dram_tmp("gbT_d", [D, NOWN], BF16)
    QT_d = dram_tmp("QT_d", [16, 128, NOWN], BF16)
    sg_d = dram_tmp("sg_d", [2, D, NOWN], BF16)
    cos_d = dram_tmp("cos_d", [16, 128, 512], F32)
    sin_d = dram_tmp("sin_d", [16, 128, 512], F32)
    yrT_d = dram_tmp("yrT_d", [4, 128, KC, 512], BF16)
    yaT_d = dram_tmp("yaT_d", [4, 128, KC, 512], BF16)
    m1_d = dram_tmp("m1_d", [D, NOWN], BF16)
    mT_d = dram_tmp("mT_d", [4, 128, KC, 512], BF16)
    x2_d = dram_tmp("x2_d", [NOWN, D], F32)
    hnT_d = dram_tmp("hnT_d", [4, 128, KC, 512], BF16)
    C_d = dram_tmp("C_d", [NOWN, NE], F32)

    with contextlib.ExitStack() as es:
        S = Sched(nc, es)

        uid = [0]

        def sb(name, shape, dt, stack=es):
            uid[0] += 1
            return stack.enter_context(nc.sbuf_tensor(f"{name}_{uid[0]}", list(shape), dt))

        PS = [es.enter_context(nc.psum_tensor(f"ps{i}", [128, 512], F32)) for i in range(8)]
        PSB = [Buf() for _ in range(8)]

        ident_f = sb("ident_f", [128, 128], F32)
        ident_b = sb("ident_b", [128, 128], BF16)
        swap_f = sb("swap_f", [128, 128], F32)
        swap_b = sb("swap_b", [128, 128], BF16)
        rope_c = sb("rope_c", [128, 2], F32)
        gmix_s = sb("gmix_s", [128, KC], F32)
        gffn_s = sb("gffn_s", [128, KC], F32)
        cst = Buf()
        S.dma("sp", ident_f[:], c_ident[:, :], writes=[cst])
        S.dma("sp", swap_f[:], c_swap[:, :], writes=[cst])
        S.dma("sp", rope_c[:], c_rope[:, :], writes=[cst])
        S.dma("sp", gmix_s[:], g_mix[:, :], writes=[cst])
        S.dma("sp", gffn_s[:], g_ffn[:, :], writes=[cst])
        S.op("dve", lambda e: e.tensor_copy(out=ident_b[:], in_=ident_f[:]), reads=[cst], writes=[cst])
        S.op("dve", lambda e: e.tensor_copy(out=swap_b[:], in_=swap_f[:]), reads=[cst], writes=[cst])
        S.barrier()

        def norm_transpose(src, row0, nblk, dst_d, eps):
            with contextlib.ExitStack() as ls:
                xt = [sb(f"nt_x{i}", [128, D], F32, ls) for i in range(2)]
                xtB = [Buf() for _ in range(2)]
                junk = sb("nt_junk", [128, D], BF16, ls)
                junkB = Buf()
                xn = [sb(f"nt_xn{i}", [128, D], BF16, ls) for i in range(2)]
                xnB = [Buf() for _ in range(2)]
                st_ = [sb(f"nt_s{i}", [128, 4], F32, ls) for i in range(2)]
                stB = [Buf() for _ in range(2)]
                hb = [sb(f"nt_h{i}", [128, KC, 512], BF16, ls) for i in range(2)]
                hbB = [Buf() for _ in range(2)]
                it = 0
                for blk in range(nblk):
                    h = hb[blk % 2]
                    hB = hbB[blk % 2]
                    for s in range(4):
                        i2 = it % 2
                        r0 = row0 + (blk * 4 + s) * 128
                        S.dma("sp", xt[i2][:], src[r0:r0 + 128, :], writes=[xtB[i2]])
                        S.op("act", lambda e: e.activation(out=junk[:], in_=xt[i2][:], func=AF.Square,
                                                           accum_out=st_[i2][:, 0:1]),
                             reads=[xtB[i2]], writes=[junkB, stB[i2]])
                        S.op("dve", lambda e: e.tensor_scalar(out=st_[i2][:, 1:2], in0=st_[i2][:, 0:1],
                                                              scalar1=1.0 / D, scalar2=eps,
                                                              op0=ALU.mult, op1=ALU.add),
                             reads=[stB[i2]], writes=[stB[i2]])
                        S.op("act", lambda e: e.activation(out=st_[i2][:, 3:4], in_=st_[i2][:, 1:2], func=AF.Sqrt),
                             reads=[stB[i2]], writes=[stB[i2]])
                        S.op("dve", lambda e: e.reciprocal(out=st_[i2][:, 2:3], in_=st_[i2][:, 3:4]),
                             reads=[stB[i2]], writes=[stB[i2]])
                        S.op("dve", lambda e: e.tensor_scalar(out=xn[i2][:], in0=xt[i2][:],
                                                              scalar1=st_[i2][:, 2:3], scalar2=None,
                                                              op0=ALU.mult),
                             reads=[xtB[i2], stB[i2]], writes=[xnB[i2]])
                        for g4 in range(4):
                            pb = (it * 4 + g4) % 4
                            pv = PS[pb][:].bitcast(BF16)

                            def tr(e, g4=g4, pv=pv, i2=i2):
                                ins = None
                                for q in range(4):
                                    kc = g4 * 4 + q
                                    ins = e.transpose(out=pv[:, q * 128:(q + 1) * 128],
                                                      in_=xn[i2][:, kc * 128:(kc + 1) * 128],
                                                      identity=ident_b[:])
                                return ins
                            S.op("pe", tr, reads=[xnB[i2]], writes=[PSB[pb]])
                            eng = "act" if g4 % 2 == 0 else "dve"

                            def ev(e, g4=g4, pv=pv, s=s, h=h, eng=eng):
                                o = h[:, g4 * 4:(g4 + 1) * 4, s * 128:(s + 1) * 128]
                                i_ = pv[:, 0:512].rearrange("p (k t) -> p k t", k=4)
                                if eng == "act":
                                    return e.copy(out=o, in_=i_)
                                return e.tensor_copy(out=o, in_=i_)
                            S.op(eng, ev, reads=[PSB[pb]], writes=[hB])
                        it += 1
                    S.dma("pool", dst_d[blk], h[:], reads=[hB])
                S.barrier()

        def gemm(ls, AT_d, blks, Wview_fn, ntiles, kc_n, wcols, mode, gvec, epilogue, group=2,
                 resident=None, pbanks=(0, 1, 2, 3)):
            wst = [sb(f"g_wst{i}", [128, 4096], F32, ls) for i in range(2)]
            wstB = [Buf() for _ in range(2)]
            nwb = 2 * group
            wbf = [sb(f"g_wbf{i}", [128, 4096], BF16, ls) for i in range(nwb)]
            wbfB = [Buf() for _ in range(nwb)]
            if resident is None:
                at = [sb(f"g_at{i}", [128, KC * 512], BF16, ls) for i in range(2)]
                atB = [Buf() for _ in range(2)]
            wi = 0
            ai = 0
            pi = 0
            for t0 in range(0, ntiles, group):
                tiles = list(range(t0, min(ntiles, t0 + group)))
                wsl = {}
                for ti in tiles:
                    a = wi % 2
                    b = wi % nwb
                    wv = wst[a][:, 0:kc_n * wcols].rearrange("p (k c) -> p k c", k=kc_n)
                    wsrc = Wview_fn(ti)
                    kq = max(1, kc_n // 4)
                    for k0 in range(0, kc_n, kq):
                        S.dma("sp", wv[:, k0:k0 + kq, :], wsrc[:, k0:k0 + kq, :], writes=[wstB[a]])
                    wb = wbf[b][:, 0:kc_n * wcols].rearrange("p (k c) -> p k c", k=kc_n)
                    ceng = "dve" if wi % 2 == 0 else "act"
                    if gvec is None:
                        if ceng == "dve":
                            S.op("dve", lambda e, a=a, b=b: e.tensor_copy(out=wbf[b][:, 0:kc_n * wcols],
                                                                          in_=wst[a][:, 0:kc_n * wcols]),
                                 reads=[wstB[a]], writes=[wbfB[b]])
                        else:
                            S.op("act", lambda e, a=a, b=b: e.copy(out=wbf[b][:, 0:kc_n * wcols],
                                                                   in_=wst[a][:, 0:kc_n * wcols]),
                                 reads=[wstB[a]], writes=[wbfB[b]])
                    else:
                        def cast(e, wv=wv, wb=wb, ceng=ceng):
                            ins = None
                            for k in range(kc_n):
                                if ceng == "dve":
                                    ins = e.tensor_scalar(out=wb[:, k, :], in0=wv[:, k, :],
                                                          scalar1=gvec[:, k:k + 1], scalar2=None, op0=ALU.mult)
                                else:
                                    ins = e.activation(out=wb[:, k, :], in_=wv[:, k, :], func=AF.Copy,
                                                       scale=gvec[:, k:k + 1])
                            return ins
                        S.op(ceng, cast, reads=[wstB[a]], writes=[wbfB[b]])
                    wsl[ti] = (wb, wbfB[b])
                    wi += 1
                for bi, blk in enumerate(blks):
                    if resident is None:
                        a = ai % 2
                        ai += 1
                        S.dma("sp", at[a][:], AT_d[blk].rearrange("p k t -> p (k t)"), writes=[atB[a]])
                        av = at[a][:].rearrange("p (k t) -> p k t", k=KC)
                        aB = atB[a]
                    else:
                        av, aB = resident[bi]
                    for ti in tiles:
                        wb, wB = wsl[ti]
                        if mode == "fm":
                            for ct in range(wcols // 128):
                                pb = pbanks[pi % len(pbanks)]
                                pi += 1

                                def mm(e, wb=wb, av=av, ct=ct, pb=pb):
                                    ins = None
                                    for k in range(kc_n):
                                        ins = e.matmul(PS[pb][:], lhsT=wb[:, k, ct * 128:(ct + 1) * 128],
                                                       rhs=av[:, k, :], start=(k == 0), stop=(k == kc_n - 1))
                                    return ins
                                S.op("pe", mm, reads=[wB, aB], writes=[PSB[pb]])
                                epilogue(pb, ti, ct, blk, bi)
                        else:
                            for s in range(4):
                                pb = pbanks[pi % len(pbanks)]
                                pi += 1

                                def mm(e, wb=wb, av=av, s=s, pb=pb):
                                    ins = None
                                    for k in range(kc_n):
                                        ins = e.matmul(PS[pb][:, 0:wcols], lhsT=av[:, k, s * 128:(s + 1) * 128],
                                                       rhs=wb[:, k, :], start=(k == 0), stop=(k == kc_n - 1))
                                    return ins
                                S.op("pe", mm, reads=[wB, aB], writes=[PSB[pb]])
                                epilogue(pb, ti, s, blk, bi)

        def w_tiles(Wap, c0, wcols):
            Wv = Wap.rearrange("(k p) n -> p k n", p=128)
            return lambda ti: Wv[:, :, c0 + ti * wcols: c0 + (ti + 1) * wcols]

        if upto >= 0:
            with contextlib.ExitStack() as ls:
                pos_i = sb("pos_i", [128, 512], I32, ls)
                pos_f = sb("pos_f", [128, 512], F32, ls)
                ang = sb("ang", [128, 512], F32, ls)
                a1 = sb("a1", [128, 512], F32, ls)
                a2 = sb("a2", [128, 512], F32, ls)
                tq = sb("tq", [128, 512], F32, ls)
                tB = Buf()
                posv = posf.rearrange("(b t) -> b t", t=512)
                for blk in range(16):
                    S.dma("sp", pos_i[:], posv[blk:blk + 1, :].partition_broadcast(128), writes=[tB])
                    S.op("dve", lambda e: e.tensor_copy(out=pos_f[:], in_=pos_i[:]), reads=[tB], writes=[tB])
                    S.op("dve", lambda e: e.tensor_scalar(out=ang[:], in0=pos_f[:], scalar1=rope_c[:, 0:1],
                                                          scalar2=None, op0=ALU.mult), reads=[tB], writes=[tB])
                    def trig(dst, shift):
                        S.op("dve", lambda e: e.tensor_scalar(out=dst[:], in0=ang[:], scalar1=shift, scalar2=None, op0=ALU.add), reads=[tB], writes=[tB])
                        S.op("dve", lambda e: e.tensor_scalar(out=tq[:], in0=dst[:], scalar1=1.0 / TWO_PI, scalar2=None, op0=ALU.mult), reads=[tB], writes=[tB])
                        S.op("dve", lambda e: e.tensor_copy(out=pos_i[:], in_=tq[:]), reads=[tB], writes=[tB])
                        S.op("dve", lambda e: e.tensor_copy(out=tq[:], in_=pos_i[:]), reads=[tB], writes=[tB])
                        S.op("dve", lambda e: e.scalar_tensor_tensor(out=dst[:], in0=tq[:], scalar=-TWO_PI, in1=dst[:], op0=ALU.mult, op1=ALU.add), reads=[tB], writes=[tB])
                        S.op("dve", lambda e: e.tensor_scalar(out=tq[:], in0=dst[:], scalar1=math.pi, scalar2=-TWO_PI, op0=ALU.is_gt, op1=ALU.mult), reads=[tB], writes=[tB])
                        S.op("dve", lambda e: e.tensor_tensor(out=dst[:], in0=dst[:], in1=tq[:], op=ALU.add), reads=[tB], writes=[tB])
                        S.op("dve", lambda e: e.tensor_scalar(out=tq[:], in0=dst[:], scalar1=-math.pi, scalar2=TWO_PI, op0=ALU.is_lt, op1=ALU.mult), reads=[tB], writes=[tB])
                        S.op("dve", lambda e: e.tensor_tensor(out=dst[:], in0=dst[:], in1=tq[:], op=ALU.add), reads=[tB], writes=[tB])
                        S.op("dve", lambda e: e.tensor_scalar(out=dst[:], in0=dst[:], scalar1=-math.pi, scalar2=math.pi, op0=ALU.max, op1=ALU.min), reads=[tB], writes=[tB])
                        S.op("act", lambda e: e.activation(out=dst[:], in_=dst[:], func=AF.Sin), reads=[tB], writes=[tB])
                    trig(a1, 0.0)
                    S.op("dve", lambda e: e.tensor_scalar(out=a1[:], in0=a1[:], scalar1=rope_c[:, 1:2], scalar2=None, op0=ALU.mult), reads=[tB], writes=[tB])
                    S.dma("pool", sin_d[blk], a1[:], reads=[tB])
                    trig(a2, math.pi / 2)
                    S.dma("pool", cos_d[blk], a2[:], reads=[tB])
                S.barrier()

        if upto >= 1:
            norm_transpose(xf, 0, 16, hT_d, 1e-6)

        def rope_epilogue_factory(ls, dst_d, tok_of_blk, scale_cols0):
            cs = [sb(f"rp_c{i}", [128, 512], F32, ls) for i in range(2)]
            sn = [sb(f"rp_s{i}", [128, 512], F32, ls) for i in range(2)]
            csB = [Buf() for _ in range(2)]
            tb_ = [sb(f"rp_t{i}", [128, 512], BF16, ls) for i in range(2)]
            tbB = [Buf() for _ in range(2)]
            o1 = [sb(f"rp_o1{i}", [128, 512], F32, ls) for i in range(2)]
            o2 = [sb(f"rp_o2{i}", [128, 512], F32, ls) for i in range(2)]
            ob = [sb(f"rp_ob{i}", [128, 512], BF16, ls) for i in range(2)]
            oB = [Buf() for _ in range(2)]
            state = dict(n=0, lastblk=None, ci=0)

            def epi(pb, ti, ct, blk, bi):
                n = state["n"]
                state["n"] += 1
                i2 = n % 2
                if state["lastblk"] != blk:
                    state["ci"] += 1
                    c2 = state["ci"] % 2
                    S.dma("sp", cs[c2][:], cos_d[blk], writes=[csB[c2]])
                    S.dma("sp", sn[c2][:], sin_d[blk], writes=[csB[c2]])
                    state["lastblk"] = blk
                c2 = state["ci"] % 2
                hm = scale_cols0 + ti * 2 + ct
                pb2 = 4 + (n % 2)
                S.op("act", lambda e: e.copy(out=tb_[i2][:], in_=PS[pb][:]), reads=[PSB[pb]], writes=[tbB[i2]])
                S.op("pe", lambda e: e.matmul(PS[pb2][:], lhsT=swap_b[:], rhs=tb_[i2][:], start=True, stop=True),
                     reads=[tbB[i2]], writes=[PSB[pb2]])
                S.op("dve", lambda e: e.tensor_tensor(out=o1[i2][:], in0=PS[pb][:], in1=cs[c2][:], op=ALU.mult),
                     reads=[PSB[pb], csB[c2], tbB[i2]], writes=[oB[i2]])
                S.op("dve", lambda e: e.tensor_tensor(out=o2[i2][:], in0=PS[pb2][:], in1=sn[c2][:], op=ALU.mult),
                     reads=[PSB[pb2], csB[c2]], writes=[oB[i2]])
                S.op("dve", lambda e: e.tensor_tensor(out=ob[i2][:], in0=o1[i2][:], in1=o2[i2][:], op=ALU.add),
                     reads=[oB[i2]], writes=[oB[i2]])
                t0 = tok_of_blk(blk)
                S.dma("pool", dst_d[hm, :, t0:t0 + 512], ob[i2][:], reads=[oB[i2]])
            return epi

        ALLB = list(range(16))
        OWNB = [12, 13, 14, 15]
        if upto >= 2:
            with contextlib.ExitStack() as ls:
                ut = [sb(f"e_u{i}", [128, 512], F32, ls) for i in range(3)]
                utB = [Buf() for _ in range(3)]
                cnt = [0]

                def epi_u(pb, ti, ct, blk, bi):
                    i3 = cnt[0] % 3
                    cnt[0] += 1
                    r0 = ti * 256 + ct * 128
                    S.op("act", lambda e: e.copy(out=ut[i3][:], in_=PS[pb][:]), reads=[PSB[pb]], writes=[utB[i3]])
                    S.dma("pool", uT_d[r0:r0 + 128, blk * 512:(blk + 1) * 512], ut[i3][:], reads=[utB[i3]])
                gemm(ls, hT_d, ALLB, w_tiles(w_in, 0, 256), 8, KC, 256, "fm", gmix_s, epi_u)
                S.barrier()
        if upto >= 3:
            with contextlib.ExitStack() as ls:
                epi_k = rope_epilogue_factory(ls, KT_d, lambda blk: blk * 512, 0)
                gemm(ls, hT_d, ALLB, w_tiles(w_in, 3 * D, 256), 8, KC, 256, "fm", gmix_s, epi_k)
                S.barrier()
            with contextlib.ExitStack() as ls:
                epi_q = rope_epilogue_factory(ls, QT_d, lambda blk: (blk - 12) * 512, 0)
                gemm(ls, hT_d, OWNB, w_tiles(w_in, 2 * D, 256), 8, KC, 256, "fm", gmix_s, epi_q)
                S.barrier()
        if upto >= 4:
            with contextlib.ExitStack() as ls:
                vt = [sb(f"e_v{i}", [128, 256], BF16, ls) for i in range(3)]
                vtB = [Buf() for _ in range(3)]
                cnt = [0]

                def epi_v(pb, ti, s, blk, bi):
                    i3 = cnt[0] % 3
                    cnt[0] += 1
                    r0 = blk * 512 + s * 128
                    eng = "act" if cnt[0] % 2 else "dve"
                    if eng == "act":
                        S.op("act", lambda e: e.copy(out=vt[i3][:], in_=PS[pb][:, 0:256]), reads=[PSB[pb]], writes=[vtB[i3]])
                    else:
                        S.op("dve", lambda e: e.tensor_copy(out=vt[i3][:], in_=PS[pb][:, 0:256]), reads=[PSB[pb]], writes=[vtB[i3]])
                    S.dma("pool", V_d[r0:r0 + 128, ti * 256:(ti + 1) * 256], vt[i3][:], reads=[vtB[i3]])
                gemm(ls, hT_d, ALLB, w_tiles(w_in, 4 * D, 256), 8, KC, 256, "tm", gmix_s, epi_v)
                S.barrier()
        if upto >= 5:
            with contextlib.ExitStack() as ls:
                xs = [sb(f"e_x{i}", [128, 512], F32, ls) for i in range(2)]
                t1 = [sb(f"e_t{i}", [128, 512], F32, ls) for i in range(2)]
                ob = [sb(f"e_o{i}", [128, 512], BF16, ls) for i in range(2)]
                eB = [Buf() for _ in range(2)]
                cnt = [0]

                def epi_gelu(pb, ti, ct, blk, bi):
                    i2 = cnt[0] % 2
                    cnt[0] += 1
                    r0 = ti * 256 + ct * 128
                    t0 = (blk - 12) * 512
                    S.op("act", lambda e: e.copy(out=xs[i2][:], in_=PS[pb][:]), reads=[PSB[pb]], writes=[eB[i2]])
                    S.op("dve", lambda e: e.tensor_tensor(out=t1[i2][:], in0=xs[i2][:], in1=xs[i2][:], op=ALU.mult),
                         reads=[eB[i2]], writes=[eB[i2]])
                    S.op("dve", lambda e: e.tensor_scalar(out=t1[i2][:], in0=t1[i2][:], scalar1=0.044715, scalar2=1.0,
                                                          op0=ALU.mult, op1=ALU.add), reads=[eB[i2]], writes=[eB[i2]])
                    S.op("dve", lambda e: e.tensor_tensor(out=t1[i2][:], in0=t1[i2][:], in1=xs[i2][:], op=ALU.mult),
                         reads=[eB[i2]], writes=[eB[i2]])
                    S.op("act", lambda e: e.activation(out=t1[i2][:], in_=t1[i2][:], func=AF.Sigmoid,
                                                       scale=2.0 * math.sqrt(2.0 / math.pi)),
                         reads=[eB[i2]], writes=[eB[i2]])
                    S.op("dve", lambda e: e.tensor_tensor(out=ob[i2][:], in0=t1[i2][:], in1=xs[i2][:], op=ALU.mult),
                         reads=[eB[i2]], writes=[eB[i2]])
                    S.dma("pool", gbT_d[r0:r0 + 128, t0:t0 + 512], ob[i2][:], reads=[eB[i2]])
                gemm(ls, hT_d, OWNB, w_tiles(w_in, D, 256), 8, KC, 256, "fm", gmix_s, epi_gelu)
                S.barrier()
            with contextlib.ExitStack() as ls:
                ob = [sb(f"e_o{i}", [128, 512], BF16, ls) for i in range(3)]
                eB = [Buf() for _ in range(3)]
                cnt = [0]

                def epi_sig(pb, ti, ct, blk, bi):
                    i3 = cnt[0] % 3
                    cnt[0] += 1
                    col = ti * 256 + ct * 128
                    which, r0 = col // D, col % D
                    t0 = (blk - 12) * 512
                    S.op("act", lambda e: e.activation(out=ob[i3][:], in_=PS[pb][:], func=AF.Sigmoid),
                         reads=[PSB[pb]], writes=[eB[i3]])
                    S.dma("pool", sg_d[which, r0:r0 + 128, t0:t0 + 512], ob[i3][:], reads=[eB[i3]])
                gemm(ls, hT_d, OWNB, w_tiles(w_in, 5 * D, 256), 16, KC, 256, "fm", gmix_s, epi_sig)
                S.barrier()

        if upto >= 6:
            with contextlib.ExitStack() as ls:
                CH = 2048
                nmb = sb("l_nm", [128, T], BF16, ls)
                vb = sb("l_vb", [128, T], BF16, ls)
                tmpf = sb("l_tmpf", [128, CH], F32, ls)
                tmpi = sb("l_tmpi", [128, CH], I32, ls)
                mB = Buf()
                for q in range(4):
                    sl = slice(q * CH, (q + 1) * CH)
                    S.dma("sp", tmpf[:], valid.rearrange("(o t) -> o t", o=1)[0:1, sl].partition_broadcast(128), writes=[mB])
                    S.op("dve", lambda e: e.tensor_copy(out=vb[:, sl], in_=tmpf[:]), reads=[mB], writes=[mB])
                    S.dma("sp", tmpi[:], posf.rearrange("(o t) -> o t", o=1)[0:1, sl].partition_broadcast(128), writes=[mB])
                    S.op("dve", lambda e: e.tensor_copy(out=tmpf[:], in_=tmpi[:]), reads=[mB], writes=[mB])
                    S.op("dve", lambda e: e.tensor_scalar(out=tmpf[:], in0=tmpf[:], scalar1=0.0, scalar2=None,
                                                          op0=ALU.not_equal), reads=[mB], writes=[mB])
                    S.op("dve", lambda e: e.tensor_tensor(out=nmb[:, sl], in0=tmpf[:], in1=vb[:, sl], op=ALU.mult),
                         reads=[mB], writes=[mB])
                cw = sb("l_cw", [128, 4, KC], F32, ls)
                cbs = sb("l_cb", [128, KC], F32, ls)
                bas = sb("l_ba", [128, KC], F32, ls)
                bxs = sb("l_bx", [128, KC], F32, ls)
                lps = sb("l_lp", [128, KC], F32, ls)
                nc8 = sb("l_nc8", [128, KC], F32, ls)
                pB = Buf()
                S.dma("sp", cw[:], conv_w[:, :, :], writes=[pB])
                S.dma("sp", cbs[:], conv_b[:, :], writes=[pB])
                S.dma("sp", bas[:], b_rg_a[:, :], writes=[pB])
                S.dma("sp", bxs[:], b_rg_x[:, :], writes=[pB])
                S.dma("sp", lps[:], lru_param[:, :], writes=[pB])
                S.op("act", lambda e: e.activation(out=nc8[:], in_=lps[:], func=AF.Exp, scale=-1.0), reads=[pB], writes=[pB])
                S.op("act", lambda e: e.activation(out=nc8[:], in_=nc8[:], func=AF.Ln, bias=1.0), reads=[pB], writes=[pB])
                S.op("dve", lambda e: e.tensor_scalar(out=nc8[:], in0=nc8[:], scalar1=-8.0, scalar2=None, op0=ALU.mult),
                     reads=[pB], writes=[pB])
                waf = sb("l_waf", [128, 2, 128], F32, ls)
                wab = [sb(f"l_wab{i}", [128, 2, 128], BF16, ls) for i in range(2)]
                wB = [Buf() for _ in range(2)]
                wfB = Buf()
                u = [sb(f"l_u{i}", [128, 3 + CH], F32, ls) for i in range(2)]
                uB = [Buf() for _ in range(2)]
                uc = sb("l_uc", [128, CH], F32, ls)
                ucb = sb("l_ucb", [128, CH], BF16, ls)
                rr = sb("l_r", [128, CH], F32, ls)
                ii = sb("l_i", [128, CH], F32, ls)
                aa = sb("l_a", [128, CH], F32, ls)
                mm_ = sb("l_m", [128, CH], F32, ls)
                hh = [sb(f"l_h{i}", [128, CH], F32, ls) for i in range(2)]
                hB = [Buf() for _ in range(2)]
                gbt = sb("l_gb", [128, NOWN], BF16, ls)
                yb = sb("l_y", [128, NOWN], BF16, ls)
                wkB = Buf()
                gB = Buf()
                yB = Buf()
                n = 0
                yr_v = yrT_d.rearrange("b p k t -> p k b t")
                for c in range(16):
                    w2 = c % 2
                    S.dma("sp", waf[:, 0, :], w_rg_a[c], writes=[wfB])
                    S.dma("sp", waf[:, 1, :], w_rg_x[c], writes=[wfB])
                    S.op("dve", lambda e: e.tensor_copy(out=wab[w2][:], in_=waf[:]), reads=[wfB], writes=[wB[w2]])
                    S.dma("sp", gbt[:], gbT_d[c * 128:(c + 1) * 128, :], writes=[gB])
                    for q in range(4):
                        i2 = n % 2
                        n += 1
                        t0 = q * CH
                        if q == 0:
                            S.op("dve", lambda e: e.memset(u[i2][:, 0:3], 0.0), writes=[uB[i2]])
                            S.dma("sp", u[i2][:, 3:3 + CH], uT_d[c * 128:(c + 1) * 128, 0:CH], writes=[uB[i2]])
                        else:
                            S.dma("sp", u[i2][:], uT_d[c * 128:(c + 1) * 128, t0 - 3:t0 + CH], writes=[uB[i2]])
                        uu = u[i2]
                        S.op("dve", lambda e: e.tensor_scalar(out=uc[:], in0=uu[:, 3:3 + CH], scalar1=cw[:, 3, c:c + 1],
                                                              scalar2=cbs[:, c:c + 1], op0=ALU.mult, op1=ALU.add),
                             reads=[uB[i2], pB], writes=[wkB])
                        for j in range(3):
                            S.op("dve", lambda e, j=j: e.scalar_tensor_tensor(out=uc[:], in0=uu[:, j:j + CH],
                                                                              scalar=cw[:, j, c:c + 1], in1=uc[:],
                                                                              op0=ALU.mult, op1=ALU.add),
                                 reads=[uB[i2], pB, wkB], writes=[wkB])
                        S.op("dve", lambda e: e.tensor_tensor(out=uc[:], in0=uc[:], in1=vb[:, t0:t0 + CH], op=ALU.mult),
                             reads=[wkB, mB], writes=[wkB])
                        S.op("act", lambda e: e.copy(out=ucb[:], in_=uc[:]), reads=[wkB], writes=[wkB])
                        for sblk in range(CH // 512):
                            ssl = slice(sblk * 512, (sblk + 1) * 512)
                            pa, px = (sblk * 2) % 4, (sblk * 2 + 1) % 4
                            S.op("pe", lambda e: e.matmul(PS[pa][:], lhsT=wab[w2][:, 0, :], rhs=ucb[:, ssl], start=True, stop=True),
                                 reads=[wB[w2], wkB], writes=[PSB[pa]])
                            S.op("pe", lambda e: e.matmul(PS[px][:], lhsT=wab[w2][:, 1, :], rhs=ucb[:, ssl], start=True, stop=True),
                                 reads=[wB[w2], wkB], writes=[PSB[px]])
                            S.op("act", lambda e: e.activation(out=rr[:, ssl], in_=PS[pa][:], func=AF.Sigmoid, bias=bas[:, c:c + 1]),
                                 reads=[PSB[pa], pB], writes=[wkB])
                            S.op("act", lambda e: e.activation(out=ii[:, ssl], in_=PS[px][:], func=AF.Sigmoid, bias=bxs[:, c:c + 1]),
                                 reads=[PSB[px], pB], writes=[wkB])
                        S.op("act", lambda e: e.activation(out=aa[:], in_=rr[:], func=AF.Exp, scale=nc8[:, c:c + 1]),
                             reads=[wkB, pB], writes=[wkB])
                        S.op("dve", lambda e: e.tensor_tensor(out=aa[:], in0=aa[:], in1=nmb[:, t0:t0 + CH], op=ALU.mult),
                             reads=[wkB, mB], writes=[wkB])
                        S.op("act", lambda e: e.activation(out=mm_[:], in_=aa[:], func=AF.Square), reads=[wkB], writes=[wkB])
                        S.op("act", lambda e: e.activation(out=mm_[:], in_=mm_[:], func=AF.Sqrt, scale=-1.0, bias=1.0),
                             reads=[wkB], writes=[wkB])
                        S.op("dve", lambda e: e.tensor_tensor(out=ii[:], in0=ii[:], in1=uc[:], op=ALU.mult), reads=[wkB], writes=[wkB])
                        S.op("dve", lambda e: e.tensor_tensor(out=ii[:], in0=ii[:], in1=mm_[:], op=ALU.mult), reads=[wkB], writes=[wkB])
                        hprev = hh[(i2 + 1) % 2]
                        init = 0.0 if q == 0 else hprev[:, CH - 1:CH]
                        S.op("dve", lambda e: e.tensor_tensor_scan(out=hh[i2][:], data0=aa[:], data1=ii[:], initial=init,
                                                                   op0=ALU.mult, op1=ALU.add),
                             reads=[wkB, hB[(i2 + 1) % 2]], writes=[hB[i2]])
                        if q == 3:
                            S.op("dve", lambda e: e.tensor_tensor(out=yb[:], in0=hh[i2][:], in1=gbt[:], op=ALU.mult),
                                 reads=[hB[i2], gB], writes=[yB])
                            S.dma("pool", yr_v[:, c], yb[:].rearrange("p (b t) -> p b t", b=4), reads=[yB])
                S.barrier()

        if upto >= 7:
            with contextlib.ExitStack() as ls:
                SCALE = 128 ** -0.5
                lamv = sb("a_lamv", [128, 4, 128], F32, ls)
                lams = sb("a_lams", [128, 8], F32, ls)
                lB = Buf()
                S.dma("sp", lamv[:].rearrange("p a b -> p (a b)"),
                      lam_in.rearrange("(o a) b -> o (a b)", o=1).partition_broadcast(128), writes=[lB])
                S.op("dve", lambda e: e.tensor_tensor(out=lamv[:, 0, :], in0=lamv[:, 0, :], in1=lamv[:, 1, :], op=ALU.mult), reads=[lB], writes=[lB])
                S.op("dve", lambda e: e.tensor_tensor(out=lamv[:, 2, :], in0=lamv[:, 2, :], in1=lamv[:, 3, :], op=ALU.mult), reads=[lB], writes=[lB])
                S.op("dve", lambda e: e.reduce_sum(out=lams[:, 0:1], in_=lamv[:, 0, :], axis=AX.X), reads=[lB], writes=[lB])
                S.op("dve", lambda e: e.reduce_sum(out=lams[:, 1:2], in_=lamv[:, 2, :], axis=AX.X), reads=[lB], writes=[lB])
                S.op("act", lambda e: e.activation(out=lams[:, 2:4], in_=lams[:, 0:2], func=AF.Exp), reads=[lB], writes=[lB])
                S.op("dve", lambda e: e.tensor_tensor(out=lams[:, 4:5], in0=lams[:, 3:4], in1=lams[:, 2:3], op=ALU.subtract), reads=[lB], writes=[lB])
                S.op("dve", lambda e: e.tensor_scalar(out=lams[:, 5:6], in0=lams[:, 4:5], scalar1=-0.2, scalar2=None, op0=ALU.add), reads=[lB], writes=[lB])
                neglam = lams[:, 5:6]
                sg_t = sb("a_sg", [128, 256], F32, ls)
                S.dma("sp", sg_t[:], subln_g.rearrange("(o t) -> o t", o=1).partition_broadcast(128), writes=[lB])
                S.op("dve", lambda e: e.tensor_scalar(out=sg_t[:], in0=sg_t[:], scalar1=0.8, scalar2=None, op0=ALU.mult), reads=[lB], writes=[lB])
                kb = sb("a_kb", [128, 64], F32, ls)
                S.dma("sp", kb[:], valid_pk[:, :], writes=[lB])
                S.op("dve", lambda e: e.tensor_scalar(out=kb[:], in0=kb[:], scalar1=-1.0, scalar2=30000.0, op0=ALU.add, op1=ALU.mult), reads=[lB], writes=[lB])
                trf = sb("a_trf", [128, 4, 512], F32, ls)
                trb = sb("a_trb", [128, 4, 512], BF16, ls)
                S.dma("sp", trf[:], c_tri[:, :, :], writes=[lB])
                S.op("dve", lambda e: e.tensor_copy(out=trb[:], in_=trf[:]), reads=[lB], writes=[lB])

                Va = [sb(f"a_V{i}", [128, 64, 257], BF16, ls) for i in range(2)]
                VaB = [Buf() for _ in range(2)]
                for i in range(2):
                    S.op("dve", lambda e, i=i: e.memset(Va[i][:, :, 256:257], 1.0), writes=[VaB[i]])
                KTs = [sb(f"a_K{i}", [128, T], BF16, ls) for i in range(2)]
                KTB = [Buf() for _ in range(2)]
                QTs = [sb(f"a_Q{i}", [128, NOWN], BF16, ls) for i in range(2)]
                QTB = [Buf() for _ in range(2)]
                Es = [sb(f"a_E{i}", [128, 512], BF16, ls) for i in range(3)]
                EB = [Buf() for _ in range(3)]
                om = [sb(f"a_om{i}", [128, 4, 256], F32, ls) for i in range(2)]
                omB = [Buf() for _ in range(2)]
                rs = sb("a_rs", [128, 8], F32, ls)
                rsB = Buf()
                od = sb("a_od", [128, 4, 256], F32, ls)
                odB = Buf()
                junk = sb("a_junk", [128, 256], F32, ls)
                onb = sb("a_onb", [128, 4, 256], BF16, ls)
                yo = [sb(f"a_yo{i}", [128, 2, 512], BF16, ls) for i in range(2)]
                yoB = [Buf() for _ in range(2)]
                V_v = V_d.rearrange("(k p) c -> p k c", p=128)
                it = 0
                ei = 0
                hq = 0
                for h in range(NH):
                    vi = h % 2
                    for k4 in range(16):
                        S.dma("sp", Va[vi][:, k4 * 4:(k4 + 1) * 4, 0:256], V_v[:, k4 * 4:(k4 + 1) * 4, h * 256:(h + 1) * 256],
                              writes=[VaB[vi]])
                    for qb in range(4):
                        nkt = 48 + (qb + 1) * 4
                        for m in range(2):
                            hm = h * 2 + m
                            ki = hm % 2
                            if qb == 0:
                                S.dma("sp", KTs[ki][:], KT_d[hm], writes=[KTB[ki]])
                                S.dma("sp", QTs[ki][:], QT_d[hm], writes=[QTB[ki]])
                            for kt in range(nkt):
                                pS = 4 + (it % 2)
                                it += 1
                                e3 = ei % 3
                                ei += 1
                                S.op("pe", lambda e: e.matmul(PS[pS][:], lhsT=KTs[ki][:, kt * 128:(kt + 1) * 128],
                                                              rhs=QTs[ki][:, qb * 512:(qb + 1) * 512], start=True, stop=True),
                                     reads=[KTB[ki], QTB[ki]], writes=[PSB[pS]])
                                S.op("act", lambda e: e.activation(out=Es[e3][:], in_=PS[pS][:], func=AF.Exp,
                                                                   scale=SCALE, bias=kb[:, kt:kt + 1]),
                                     reads=[PSB[pS], lB], writes=[EB[e3]])
                                dg = kt - (48 + qb * 4)
                                if dg >= 0:
                                    S.op("dve", lambda e: e.tensor_tensor(out=Es[e3][:], in0=Es[e3][:], in1=trb[:, dg, :], op=ALU.mult),
                                         reads=[EB[e3], lB], writes=[EB[e3]])

                                def pv(e, e3=e3, kt=kt, nkt=nkt, vi=vi):
                                    ins = None
                                    for qs in range(4):
                                        ins = e.matmul(PS[qs][:, 0:257], lhsT=Es[e3][:, qs * 128:(qs + 1) * 128],
                                                       rhs=Va[vi][:, kt, :], start=(kt == 0), stop=(kt == nkt - 1))
                                    return ins
                                S.op("pe", pv, reads=[EB[e3], VaB[vi]], writes=[PSB[0], PSB[1], PSB[2], PSB[3]])
                            for qs in range(4):
                                S.op("dve", lambda e, qs=qs: e.reciprocal(out=rs[:, m * 4 + qs:m * 4 + qs + 1], in_=PS[qs][:, 256:257]),
                                     reads=[PSB[qs]], writes=[rsB])
                                S.op("dve", lambda e, qs=qs: e.tensor_scalar(out=om[m][:, qs, :], in0=PS[qs][:, 0:256],
                                                                             scalar1=rs[:, m * 4 + qs:m * 4 + qs + 1], scalar2=None,
                                                                             op0=ALU.mult),
                                     reads=[PSB[qs], rsB], writes=[omB[m]])
                        y2 = hq % 2
                        hq += 1
                        S.op("dve", lambda e: e.scalar_tensor_tensor(out=od[:].rearrange("p a b -> p (a b)"),
                                                                     in0=om[1][:].rearrange("p a b -> p (a b)"),
                                                                     scalar=neglam,
                                                                     in1=om[0][:].rearrange("p a b -> p (a b)"),
                                                                     op0=ALU.mult, op1=ALU.add),
                             reads=[omB[0], omB[1], lB], writes=[odB])
                        for qs in range(4):
                            S.op("act", lambda e, qs=qs: e.activation(out=junk[:], in_=od[:, qs, :], func=AF.Square,
                                                                      accum_out=rs[:, qs:qs + 1]),
                                 reads=[odB], writes=[rsB])
                        S.op("dve", lambda e: e.tensor_scalar(out=rs[:, 0:4], in0=rs[:, 0:4], scalar1=1.0 / 256, scalar2=1e-5,
                                                              op0=ALU.mult, op1=ALU.add), reads=[rsB], writes=[rsB])
                        S.op("act", lambda e: e.activation(out=rs[:, 4:8], in_=rs[:, 0:4], func=AF.Sqrt), reads=[rsB], writes=[rsB])
                        S.op("dve", lambda e: e.reciprocal(out=rs[:, 0:4], in_=rs[:, 4:8]), reads=[rsB], writes=[rsB])
                        for qs in range(4):
                            S.op("dve", lambda e, qs=qs: e.scalar_tensor_tensor(out=onb[:, qs, :], in0=od[:, qs, :],
                                                                                scalar=rs[:, qs:qs + 1], in1=sg_t[:],
                                                                                op0=ALU.mult, op1=ALU.mult),
                                 reads=[odB, rsB, lB], writes=[odB])
                        for eh in range(2):
                            pT = 6 + eh
                            pvw = PS[pT][:].bitcast(BF16)

                            def tr(e, eh=eh, pvw=pvw):
                                ins = None
                                for qs in range(4):
                                    ins = e.transpose(out=pvw[:, qs * 128:(qs + 1) * 128],
                                                      in_=onb[:, qs, eh * 128:(eh + 1) * 128], identity=ident_b[:])
                                return ins
                            S.op("pe", tr, reads=[odB], writes=[PSB[pT]])
                            S.op("act", lambda e, eh=eh, pvw=pvw: e.copy(out=yo[y2][:, eh, :], in_=pvw[:, 0:512]),
                                 reads=[PSB[pT]], writes=[yoB[y2]])
                        S.dma("pool", yaT_d[qb, :, h * 2:(h + 1) * 2, :], yo[y2][:], reads=[yoB[y2]])
                S.barrier()

        if upto >= 8:
            for which, AT, W in [(0, yrT_d, w_br_rnn), (1, yaT_d, w_br_attn)]:
                with contextlib.ExitStack() as ls:
                    gt = [sb(f"b_g{i}", [128, 512], BF16, ls) for i in range(2)]
                    m1 = [sb(f"b_m{i}", [128, 512], BF16, ls) for i in range(2)]
                    ot = [sb(f"b_o{i}", [128, 512], F32, ls) for i in range(2)]
                    ob = [sb(f"b_ob{i}", [128, 512], BF16, ls) for i in range(2)]
                    bB = [Buf() for _ in range(2)]
                    oB = [Buf() for _ in range(2)]
                    cnt = [0]

                    def epi_b(pb, ti, ct, blk, bi, which=which):
                        i2 = cnt[0] % 2
                        cnt[0] += 1
                        r0 = ti * 256 + ct * 128
                        t0 = blk * 512
                        S.dma("sp", gt[i2][:], sg_d[which, r0:r0 + 128, t0:t0 + 512], writes=[bB[i2]])
                        if which == 0:
                            S.op("dve", lambda e: e.tensor_tensor(out=ob[i2][:], in0=PS[pb][:], in1=gt[i2][:], op=ALU.mult),
                                 reads=[PSB[pb], bB[i2]], writes=[oB[i2]])
                            S.dma("pool", m1_d[r0:r0 + 128, t0:t0 + 512], ob[i2][:], reads=[oB[i2]])
                        else:
                            S.dma("sp", m1[i2][:], m1_d[r0:r0 + 128, t0:t0 + 512], writes=[bB[i2]])
                            S.op("dve", lambda e: e.tensor_tensor(out=ot[i2][:], in0=PS[pb][:], in1=gt[i2][:], op=ALU.mult),
                                 reads=[PSB[pb], bB[i2]], writes=[oB[i2]])
                            S.op("dve", lambda e: e.tensor_tensor(out=ob[i2][:], in0=ot[i2][:], in1=m1[i2][:], op=ALU.add),
                                 reads=[oB[i2], bB[i2]], writes=[oB[i2]])
                            S.dma("pool", mT_d[blk, :, r0 // 128, :], ob[i2][:], reads=[oB[i2]])
                    gemm(ls, AT, [0, 1, 2, 3], w_tiles(W, 0, 256), 8, KC, 256, "fm", None, epi_b)
                    S.barrier()
            with contextlib.ExitStack() as ls:
                xo = [sb(f"o_x{i}", [128, 256], F32, ls) for i in range(2)]
                xB = [Buf() for _ in range(2)]
                cnt = [0]

                def epi_o(pb, ti, s, blk, bi):
                    i2 = cnt[0] % 2
                    cnt[0] += 1
                    r0 = blk * 512 + s * 128
                    S.dma("sp", xo[i2][:], xf[OWN0 + r0:OWN0 + r0 + 128, ti * 256:(ti + 1) * 256], writes=[xB[i2]])
                    S.op("dve", lambda e: e.tensor_tensor(out=xo[i2][:], in0=PS[pb][:, 0:256], in1=xo[i2][:], op=ALU.add),
                         reads=[PSB[pb], xB[i2]], writes=[xB[i2]])
                    S.dma("pool", x2_d[r0:r0 + 128, ti * 256:(ti + 1) * 256], xo[i2][:], reads=[xB[i2]])
                gemm(ls, mT_d, [0, 1, 2, 3], w_tiles(w_out, 0, 256), 8, KC, 256, "tm", None, epi_o)
                S.barrier()

        if upto >= 9:
            norm_transpose(x2_d, 0, 4, hnT_d, 1e-6)
            with contextlib.ExitStack() as ls:
                wrf = sb("r_wf", [128, KC, 36], F32, ls)
                wrb = sb("r_wb", [128, KC, 36], BF16, ls)
                rB = Buf()
                S.dma("sp", wrf[:], w_router.rearrange("(k p) n -> p k n", p=128), writes=[rB])

                def castr(e):
                    ins = None
                    for k in range(KC):
                        ins = e.tensor_scalar(out=wrb[:, k, :], in0=wrf[:, k, :], scalar1=gffn_s[:, k:k + 1], scalar2=None, op0=ALU.mult)
                    return ins
                S.op("dve", castr, reads=[rB], writes=[rB])
                at = [sb(f"r_at{i}", [128, KC, 512], BF16, ls) for i in range(2)]
                atB = [Buf() for _ in range(2)]
                lg = sb("r_lg", [128, 36], F32, ls)
                w = {nm: sb("r_" + nm, [128, shape], F32, ls) for nm, shape in
                     [("gmax", 1), ("gex", 4), ("gsum", 1), ("gw", 1), ("gm", 4), ("pen", 32), ("el", 32), ("m1", 1),
                      ("k1", 32), ("el2", 32), ("m2", 1), ("k2", 32), ("dl", 1), ("w1", 1), ("w2", 1), ("c", 32), ("c2", 32)]}
                wkB = Buf()
                for blk in range(4):
                    a = blk % 2
                    S.dma("sp", at[a][:], hnT_d[blk], writes=[atB[a]])
                    for s in range(4):
                        pb = s % 4

                        def mm(e, a=a, s=s, pb=pb):
                            ins = None
                            for k in range(KC):
                                ins = e.matmul(PS[pb][:, 0:36], lhsT=at[a][:, k, s * 128:(s + 1) * 128], rhs=wrb[:, k, :],
                                               start=(k == 0), stop=(k == KC - 1))
                            return ins
                        S.op("pe", mm, reads=[atB[a], rB], writes=[PSB[pb]])
                        R = [wkB]

                        def D_(fn, extra=()):
                            S.op("dve", fn, reads=R + list(extra), writes=R)
                        D_(lambda e: e.tensor_copy(out=lg[:], in_=PS[pb][:, 0:36]), extra=[PSB[pb]])
                        D_(lambda e: e.reduce_max(out=w["gmax"][:], in_=lg[:, 0:4], axis=AX.X))
                        D_(lambda e: e.tensor_scalar(out=w["gex"][:], in0=lg[:, 0:4], scalar1=w["gmax"][:, 0:1], scalar2=None, op0=ALU.subtract))
                        S.op("act", lambda e: e.activation(out=w["gex"][:], in_=w["gex"][:], func=AF.Exp), reads=R, writes=R)
                        D_(lambda e: e.reduce_sum(out=w["gsum"][:], in_=w["gex"][:], axis=AX.X))
                        D_(lambda e: e.reciprocal(out=w["gw"][:], in_=w["gsum"][:]))
                        D_(lambda e: e.tensor_scalar(out=w["gm"][:], in0=lg[:, 0:4], scalar1=w["gmax"][:, 0:1], scalar2=None, op0=ALU.is_ge))
                        for g in range(4):
                            D_(lambda e, g=g: e.tensor_scalar(out=w["pen"][:, g * 8:(g + 1) * 8], in0=lg[:, 4 + g * 8:4 + (g + 1) * 8],
                                                              scalar1=0.0, scalar2=w["gm"][:, g:g + 1], op0=ALU.mult, op1=ALU.add))
                        D_(lambda e: e.tensor_scalar(out=w["pen"][:], in0=w["pen"][:], scalar1=-1.0, scalar2=1e9, op0=ALU.add, op1=ALU.mult))
                        D_(lambda e: e.tensor_tensor(out=w["el"][:], in0=lg[:, 4:36], in1=w["pen"][:], op=ALU.add))
                        D_(lambda e: e.reduce_max(out=w["m1"][:], in_=w["el"][:], axis=AX.X))
                        D_(lambda e: e.tensor_scalar(out=w["k1"][:], in0=w["el"][:], scalar1=w["m1"][:, 0:1], scalar2=None, op0=ALU.is_ge))
                        D_(lambda e: e.scalar_tensor_tensor(out=w["el2"][:], in0=w["k1"][:], scalar=-1e9, in1=w["el"][:], op0=ALU.mult, op1=ALU.add))
                        D_(lambda e: e.reduce_max(out=w["m2"][:], in_=w["el2"][:], axis=AX.X))
                        D_(lambda e: e.tensor_scalar(out=w["k2"][:], in0=w["el2"][:], scalar1=w["m2"][:, 0:1], scalar2=None, op0=ALU.is_ge))
                        D_(lambda e: e.tensor_tensor(out=w["dl"][:], in0=w["m1"][:], in1=w["m2"][:], op=ALU.subtract))
                        S.op("act", lambda e: e.activation(out=w["w1"][:], in_=w["dl"][:], func=AF.Sigmoid), reads=R, writes=R)
                        D_(lambda e: e.tensor_scalar(out=w["w2"][:], in0=w["w1"][:], scalar1=-1.0, scalar2=1.0, op0=ALU.mult, op1=ALU.add))
                        D_(lambda e: e.tensor_tensor(out=w["w1"][:], in0=w["w1"][:], in1=w["gw"][:], op=ALU.mult))
                        D_(lambda e: e.tensor_tensor(out=w["w2"][:], in0=w["w2"][:], in1=w["gw"][:], op=ALU.mult))
                        D_(lambda e: e.tensor_scalar(out=w["c"][:], in0=w["k1"][:], scalar1=w["w1"][:, 0:1], scalar2=None, op0=ALU.mult))
                        D_(lambda e: e.scalar_tensor_tensor(out=w["c2"][:], in0=w["k2"][:], scalar=w["w2"][:, 0:1], in1=w["c"][:], op0=ALU.mult, op1=ALU.add))
                        r0 = blk * 512 + s * 128
                        S.dma("pool", C_d[r0:r0 + 128, :], w["c2"][:], reads=R)
                S.barrier()

        if upto >= 10:
            with contextlib.ExitStack() as ls:
                Cs = sb("m_C", [128, 16, NE], F32, ls)
                cB = Buf()
                S.dma("sp", Cs[:], C_d.rearrange("(t p) e -> p t e", p=128), writes=[cB])
                acc = sb("m_acc", [128, 8, D], F32, ls)
                accB = [Buf() for _ in range(8)]
                hn = [sb(f"m_hn{i}", [128, KC, 512], BF16, ls) for i in range(2)]
                hnB = [Buf() for _ in range(2)]
                actT = sb("m_act", [128, 8, 1024], BF16, ls)
                actB = [Buf() for _ in range(8)]
                sgt = [sb(f"m_sg{i}", [128, 512], F32, ls) for i in range(2)]
                sgB = [Buf() for _ in range(2)]
                fst = sb("m_fst", [128, 4], F32, ls)
                fB = Buf()
                wst = [sb(f"m_wst{i}", [128, 4096], F32, ls) for i in range(2)]
                wstB = [Buf() for _ in range(2)]
                wbf = [sb(f"m_wbf{i}", [128, 4096], BF16, ls) for i in range(4)]
                wbfB = [Buf() for _ in range(4)]
                wi = [0]
                sgi = [0]
                pi = [0]

                def load_w(view, kc_n, wcols, gvec):
                    a = wi[0] % 2
                    b = wi[0] % 4
                    wi[0] += 1
                    wv = wst[a][:, 0:kc_n * wcols].rearrange("p (k c) -> p k c", k=kc_n)
                    wb = wbf[b][:, 0:kc_n * wcols].rearrange("p (k c) -> p k c", k=kc_n)
                    kq = max(1, kc_n // 4)
                    for k0 in range(0, kc_n, kq):
                        S.dma("sp", wv[:, k0:k0 + kq, :], view[:, k0:k0 + kq, :], writes=[wstB[a]])
                    ceng = "dve" if wi[0] % 2 == 0 else "act"
                    if gvec is None:
                        if ceng == "dve":
                            S.op("dve", lambda e: e.tensor_copy(out=wbf[b][:, 0:kc_n * wcols], in_=wst[a][:, 0:kc_n * wcols]),
                                 reads=[wstB[a]], writes=[wbfB[b]])
                        else:
                            S.op("act", lambda e: e.copy(out=wbf[b][:, 0:kc_n * wcols], in_=wst[a][:, 0:kc_n * wcols]),
                                 reads=[wstB[a]], writes=[wbfB[b]])
                    else:
                        def cast(e):
                            ins = None
                            for k in range(kc_n):
                                if ceng == "dve":
                                    ins = e.tensor_scalar(out=wb[:, k, :], in0=wv[:, k, :], scalar1=gvec[:, k:k + 1], scalar2=None, op0=ALU.mult)
                                else:
                                    ins = e.activation(out=wb[:, k, :], in_=wv[:, k, :], func=AF.Copy, scale=gvec[:, k:k + 1])
                            return ins
                        S.op(ceng, cast, reads=[wstB[a]], writes=[wbfB[b]])
                    return wb, wbfB[b]

                for tb in range(2):
                    for j in range(2):
                        S.dma("sp", hn[j][:], hnT_d[tb * 2 + j], writes=[hnB[j]])
                    for tt in range(8):
                        r0 = tb * 1024 + tt * 128
                        S.dma("sp", acc[:, tt, :], x2_d[r0:r0 + 128, :], writes=[accB[tt]])
                    for ex in range(NE):
                        wgv = w_gate[ex].rearrange("(k p) n -> p k n", p=128)
                        wuv = w_up[ex].rearrange("(k p) n -> p k n", p=128)
                        wdv = w_down[ex].rearrange("(k p) n -> p k n", p=128)
                        for f2 in range(4):
                            wg, wgB = load_w(wgv[:, :, f2 * 256:(f2 + 1) * 256], KC, 256, gffn_s)
                            wu, wuB = load_w(wuv[:, :, f2 * 256:(f2 + 1) * 256], KC, 256, gffn_s)
                            for j in range(2):
                                for ct in range(2):
                                    fc = f2 * 2 + ct
                                    pg = (pi[0] * 2) % 4
                                    pu = pg + 1
                                    pi[0] += 1

                                    def mmg(e, W=wg, P=pg, j=j, ct=ct):
                                        ins = None
                                        for k in range(KC):
                                            ins = e.matmul(PS[P][:], lhsT=W[:, k, ct * 128:(ct + 1) * 128], rhs=hn[j][:, k, :],
                                                           start=(k == 0), stop=(k == KC - 1))
                                        return ins
                                    S.op("pe", mmg, reads=[wgB, hnB[j]], writes=[PSB[pg]])
                                    S.op("pe", lambda e: mmg(e, W=wu, P=pu), reads=[wuB, hnB[j]], writes=[PSB[pu]])
                                    s2 = sgi[0] % 2
                                    sgi[0] += 1
                                    S.op("act", lambda e: e.activation(out=sgt[s2][:], in_=PS[pg][:], func=AF.Silu),
                                         reads=[PSB[pg]], writes=[sgB[s2]])
                                    S.op("dve", lambda e: e.tensor_tensor(out=actT[:, fc, j * 512:(j + 1) * 512], in0=PS[pu][:],
                                                                          in1=sgt[s2][:], op=ALU.mult),
                                         reads=[PSB[pu], sgB[s2]], writes=[actB[fc]])
                        for cg in range(4):
                            wd, wdB = load_w(wdv[:, :, cg * 512:(cg + 1) * 512], 8, 512, None)
                            for tt in range(8):
                                pd = 4 + (pi[0] % 4)
                                pi[0] += 1

                                def mmd(e, wd=wd, pd=pd, tt=tt):
                                    ins = None
                                    for k in range(8):
                                        ins = e.matmul(PS[pd][:], lhsT=actT[:, k, tt * 128:(tt + 1) * 128], rhs=wd[:, k, :],
                                                       start=(k == 0), stop=(k == 7))
                                    return ins
                                S.op("pe", mmd, reads=[wdB] + actB, writes=[PSB[pd]])
                                S.op("dve", lambda e: e.scalar_tensor_tensor(out=acc[:, tt, cg * 512:(cg + 1) * 512], in0=PS[pd][:],
                                                                             scalar=Cs[:, tb * 8 + tt, ex:ex + 1],
                                                                             in1=acc[:, tt, cg * 512:(cg + 1) * 512],
                                                                             op0=ALU.mult, op1=ALU.add),
                                     reads=[PSB[pd], cB, accB[tt]], writes=[accB[tt]])
                    gfin = wst[0][:, 0:D]
                    fjunk = actT[:, 0:2, :]
                    S.dma("sp", gfin, g_final.rearrange("(o t) -> o t", o=1).partition_broadcast(128), writes=[wstB[0]])
                    for tt in range(8):
                        r0 = tb * 1024 + tt * 128
                        S.op("act", lambda e: e.activation(out=fjunk, in_=acc[:, tt, :].rearrange("p (a b) -> p a b", a=2), func=AF.Square, accum_out=fst[:, 0:1]),
                             reads=[accB[tt]], writes=[fB, actB[0], actB[1]])
                        S.op("dve", lambda e: e.tensor_scalar(out=fst[:, 1:2], in0=fst[:, 0:1], scalar1=1.0 / D, scalar2=1e-6,
                                                              op0=ALU.mult, op1=ALU.add), reads=[fB], writes=[fB])
                        S.op("act", lambda e: e.activation(out=fst[:, 3:4], in_=fst[:, 1:2], func=AF.Sqrt), reads=[fB], writes=[fB])
                        S.op("dve", lambda e: e.reciprocal(out=fst[:, 2:3], in_=fst[:, 3:4]), reads=[fB], writes=[fB])
                        S.op("dve", lambda e: e.scalar_tensor_tensor(out=acc[:, tt, :], in0=acc[:, tt, :], scalar=fst[:, 2:3],
                                                                     in1=gfin, op0=ALU.mult, op1=ALU.mult),
                             reads=[accB[tt], fB, wstB[0]], writes=[accB[tt]])
                        S.dma("pool", out[r0:r0 + 128, :], acc[:, tt, :], reads=[accB[tt]])
                S.barrier()
        S.barrier()
    return nc


def _consts():
    i = np.arange(64, dtype=np.float32)
    inv = (1.0 / (10000.0 ** (np.arange(0, 128, 2, dtype=np.float32) / 128.0))).astype(np.float32)
    c_rope = np.zeros((128, 2), np.float32)
    c_rope[:, 0] = np.concatenate([inv, inv])
    c_rope[:64, 1] = -1.0
    c_rope[64:, 1] = 1.0
    c_swap = np.zeros((128, 128), np.float32)
    for m in range(128):
        c_swap[(m + 64) % 128, m] = 1.0
    c_ident = np.eye(128, dtype=np.float32)
    kl = np.arange(128)[:, None, None] + 128 * np.arange(4)[None, :, None]
    ql = np.arange(512)[None, None, :]
    c_tri = (kl <= ql).astype(np.float32)
    return dict(c_rope=c_rope, c_swap=c_swap, c_ident=c_ident, c_tri=np.ascontiguousarray(c_tri))


def make_in_maps(inputs):
    x = np.asarray(inputs["x"], np.float32)
    pos = np.asarray(inputs["positions"], np.int32)
    f = lambda k: np.ascontiguousarray(np.asarray(inputs[k], np.float32)[0])
    pk = lambda v: np.ascontiguousarray(v.reshape(-1, 128).T)
    shared = dict(
        g_mix=pk(f("g_mix")), w_in=f("w_in"), conv_w=np.ascontiguousarray(f("conv_w").reshape(4, KC, 128).transpose(2, 0, 1)), conv_b=pk(f("conv_b")),
        w_rg_a=f("w_rg_a"), b_rg_a=pk(f("b_rg_a")), w_rg_x=f("w_rg_x"), b_rg_x=pk(f("b_rg_x")),
        lru_param=pk(f("lru_param")),
        lam_in=np.ascontiguousarray(np.stack([f("lambda_q1"), f("lambda_k1"), f("lambda_q2"), f("lambda_k2")])),
        subln_g=f("subln_g"), w_br_rnn=f("w_br_rnn"), w_br_attn=f("w_br_attn"), w_out=f("w_out"),
        g_ffn=pk(f("g_ffn")),
        w_router=np.ascontiguousarray(np.concatenate([f("w_grp_router"), f("w_exp_router")], axis=1)),
        w_gate=f("w_gate"), w_up=f("w_up"), w_down=f("w_down"),
        g_final=np.ascontiguousarray(np.asarray(inputs["g_final"], np.float32)),
    )
    shared.update(_consts())
    maps = []
    for b in range(2):
        for j in range(4):
            n = (j + 1) * 2048
            xfp = np.zeros((T, D), np.float32)
            xfp[T - n:] = x[b, :n]
            pp = np.zeros((T,), np.int32)
            pp[T - n:] = pos[b, :n]
            vv = np.zeros((T,), np.float32)
            vv[T - n:] = 1.0
            m = dict(shared)
            m.update(xf=xfp, posf=pp, valid=vv, valid_pk=pk(vv))
            maps.append(m)
    return maps


def kernel(**inputs):
    nc = build()
    maps = make_in_maps(inputs)
    res = run_bass_kernel_spmd(nc, maps, core_ids=list(range(8)))
    outp = np.zeros((2, 8192, D), np.float32)
    for c in range(8):
        b, j = c // 4, c % 4
        outp[b, j * 2048:(j + 1) * 2048] = res.results[c]["out"]
    return outp
```

```python
import math
import contextlib
import numpy as np
import concourse.bass as bass
import concourse.mybir as mybir
from concourse.bass_utils import run_bass_kernel_spmd

F32 = mybir.dt.float32
BF16 = mybir.dt.bfloat16
I32 = mybir.dt.int32
ALU = mybir.AluOpType
AF = mybir.ActivationFunctionType
AX = mybir.AxisListType

D = 2048
T = 8192
OWN0 = 6144
NOWN = 2048
KC = 16
NE = 32
DE = 1024
NH = 8
TWO_PI = 2.0 * math.pi


class Buf:
    __slots__ = ("w", "r")

    def __init__(self):
        self.w = None
        self.r = {}


class Sched:
    def __init__(self, nc, es):
        self.nc = nc
        self.st = {}
        for name, h, nd in [("pe", nc.tensor, 0), ("dve", nc.vector, 0), ("act", nc.scalar, 6),
                            ("pool", nc.gpsimd, 8), ("sp", nc.sync, 10)]:
            st = dict(h=h, cnt=0, seen={}, dcnt=0, name=name)
            st["sem"] = es.enter_context(nc.semaphore("c_" + name))
            st["dsems"] = [es.enter_context(nc.semaphore(f"d_{name}{i}")) for i in range(nd)]
            self.st[name] = st

    @staticmethod
    def _add(d, ev):
        if ev is None:
            return
        sem, val, key = ev
        if key not in d or d[key][1] < val:
            d[key] = (sem, val, key)

    def _deps(self, reads, writes):
        d = {}
        for b in reads:
            self._add(d, b.w)
        for b in writes:
            self._add(d, b.w)
            for ev in b.r.values():
                self._add(d, ev)
        return d

    def _wait(self, st, d, skip=None):
        for key, (sem, val, _) in d.items():
            if key == skip:
                continue
            if st["seen"].get(key, 0) >= val:
                continue
            st["h"].wait_ge(sem, val)
            st["seen"][key] = val

    def _mark(self, reads, writes, ev):
        for b in reads:
            self._add(b.r, ev)
        for b in writes:
            b.w = ev
            b.r = {}

    def op(self, stname, fn, reads=(), writes=()):
        st = self.st[stname]
        d = self._deps(reads, writes)
        self._wait(st, d, skip=("c_pe" if stname == "pe" else None))
        ins = fn(st["h"])
        st["cnt"] += 1
        ins.then_inc(st["sem"], 1)
        self._mark(reads, writes, (st["sem"], st["cnt"], "c_" + stname))

    def dma(self, stname, out, in_, reads=(), writes=()):
        st = self.st[stname]
        d = self._deps(reads, writes)
        i = st["dcnt"]
        R = len(st["dsems"])
        k = i % R
        sem = st["dsems"][k]
        val = 16 * (i // R + 1)
        key = f"d_{stname}{k}"
        if i >= R:
            self._add(d, (sem, val - 16, key))
        self._wait(st, d)
        st["h"].dma_start(out=out, in_=in_).then_inc(sem, 16)
        st["dcnt"] += 1
        self._mark(reads, writes, (sem, val, key))

    def idma(self, out, out_off, in_, in_off, reads=(), writes=()):
        st = self.st["pool"]
        d = self._deps(reads, writes)
        i = st["dcnt"]
        R = len(st["dsems"])
        k = i % R
        sem = st["dsems"][k]
        val = 16 * (i // R + 1)
        key = f"d_pool{k}"
        if i >= R:
            self._add(d, (sem, val - 16, key))
        self._wait(st, d)
        st["h"].indirect_dma_start(out=out, out_offset=out_off, in_=in_, in_offset=in_off).then_inc(sem, 16)
        st["dcnt"] += 1
        self._mark(reads, writes, (sem, val, key))

    def all_events(self):
        d = {}
        for name, st in self.st.items():
            if st["cnt"] > 0:
                self._add(d, (st["sem"], st["cnt"], "c_" + name))
            R = len(st["dsems"])
            for k in range(R):
                n = (st["dcnt"] - k + R - 1) // R if st["dcnt"] > k else 0
                if n > 0:
                    self._add(d, (st["dsems"][k], 16 * n, f"d_{name}{k}"))
        return d

    def barrier(self, only=None):
        d = self.all_events()
        for name, st in self.st.items():
            if only is not None and name not in only:
                continue
            self._wait(st, d)


def build(upto=99, debug=(), sparse=True):
    nc = bass.Bass("TRN2", target_bir_lowering=False)
    dbg = set(debug)

    def dram_in(name, shape, dt=F32):
        return nc.dram_tensor(name, list(shape), dt, kind="ExternalInput").ap()

    def dram_tmp(name, shape, dt):
        kind = "ExternalOutput" if name in dbg else "Internal"
        return nc.dram_tensor(name, list(shape), dt, kind=kind).ap()

    xf = dram_in("xf", [T, D])
    posf = dram_in("posf", [T], I32)
    valid = dram_in("valid", [T])
    g_mix = dram_in("g_mix", [128, KC])
    w_in = dram_in("w_in", [D, 14336])
    conv_w = dram_in("conv_w", [128, 4, KC])
    conv_b = dram_in("conv_b", [128, KC])
    w_rg_a = dram_in("w_rg_a", [16, 128, 128])
    b_rg_a = dram_in("b_rg_a", [128, KC])
    w_rg_x = dram_in("w_rg_x", [16, 128, 128])
    b_rg_x = dram_in("b_rg_x", [128, KC])
    lru_param = dram_in("lru_param", [128, KC])
    lam_in = dram_in("lam_in", [4, 128])
    subln_g = dram_in("subln_g", [256])
    w_br_rnn = dram_in("w_br_rnn", [D, D])
    w_br_attn = dram_in("w_br_attn", [D, D])
    w_out = dram_in("w_out", [D, D])
    g_ffn = dram_in("g_ffn", [128, KC])
    valid_pk = dram_in("valid_pk", [128, 64])
    w_router = dram_in("w_router", [D, 36])
    w_gate = dram_in("w_gate", [NE, D, DE])
    w_up = dram_in("w_up", [NE, D, DE])
    w_down = dram_in("w_down", [NE, DE, D])
    g_final = dram_in("g_final", [D])
    c_rope = dram_in("c_rope", [128, 2])
    c_swap = dram_in("c_swap", [128, 128])
    c_ident = dram_in("c_ident", [128, 128])
    c_tri = dram_in("c_tri", [128, 4, 512])
    c_ls = dram_in("c_ls", [128, 128])
    c_iog = dram_in("c_iog", [128, 16])
    c_iod = dram_in("c_iod", [128, 8])
    out = nc.dram_tensor("out", [NOWN, D], F32, kind="ExternalOutput").ap()

    hT_d = dram_tmp("hT_d", [16, 128, KC, 512], BF16)
    uT_d = dram_tmp("uT_d", [D, T], F32)
    KT_d = dram_tmp("KT_d", [16, 128, T], BF16)
    V_d = dram_tmp("V_d", [T, D], BF16)
    gbT_d = dram_tmp("gbT_d", [D, NOWN], BF16)
    QT_d = dram_tmp("QT_d", [16, 128, NOWN], BF16)
    sg_d = dram_tmp("sg_d", [2, D, NOWN], BF16)
    cos_d = dram_tmp("cos_d", [16, 128, 512], F32)
    sin_d = dram_tmp("sin_d", [16, 128, 512], F32)
    yrT_d = dram_tmp("yrT_d", [4, 128, KC, 512], BF16)
    yaT_d = dram_tmp("yaT_d", [4, 128, KC, 512], BF16)
    m1_d = dram_tmp("m1_d", [D, NOWN], BF16)
    mT_d = dram_tmp("mT_d", [4, 128, KC, 512], BF16)
    x2_d = dram_tmp("x2_d", [NOWN, D], F32)
    hnT_d = dram_tmp("hnT_d", [4, 128, KC, 512], BF16)
    C_d = dram_tmp("C_d", [NOWN, NE], F32)
    R_d = dram_tmp("R_d", [NOWN, 66], F32)
    hn_d = dram_tmp("hn_d", [NOWN, D], BF16)
    xs_d = dram_tmp("xs_d", [8192, D], BF16)
    ys_d = dram_tmp("ys_d", [8192, D], BF16)

    with contextlib.ExitStack() as es:
        S = Sched(nc, es)

        uid = [0]

        def sb(name, shape, dt, stack=es):
            uid[0] += 1
            return stack.enter_context(nc.sbuf_tensor(f"{name}_{uid[0]}", list(shape), dt))

        PS = [es.enter_context(nc.psum_tensor(f"ps{i}", [128, 512], F32)) for i in range(8)]
        PSB = [Buf() for _ in range(8)]

        ident_f = sb("ident_f", [128, 128], F32)
        ident_b = sb("ident_b", [128, 128], BF16)
        swap_f = sb("swap_f", [128, 128], F32)
        swap_b = sb("swap_b", [128, 128], BF16)
        rope_c = sb("rope_c", [128, 2], F32)
        gmix_s = sb("gmix_s", [128, KC], F32)
        gffn_s = sb("gffn_s", [128, KC], F32)
        cst = Buf()
        S.dma("sp", ident_f[:], c_ident[:, :], writes=[cst])
        S.dma("sp", swap_f[:], c_swap[:, :], writes=[cst])
        S.dma("sp", rope_c[:], c_rope[:, :], writes=[cst])
        S.dma("sp", gmix_s[:], g_mix[:, :], writes=[cst])
        S.dma("sp", gffn_s[:], g_ffn[:, :], writes=[cst])
        S.op("dve", lambda e: e.tensor_copy(out=ident_b[:], in_=ident_f[:]), reads=[cst], writes=[cst])
        S.op("dve", lambda e: e.tensor_copy(out=swap_b[:], in_=swap_f[:]), reads=[cst], writes=[cst])
        S.barrier()

        def norm_transpose(src, row0, nblk, dst_d, eps, tm_dst=None):
            with contextlib.ExitStack() as ls:
                xt = [sb(f"nt_x{i}", [128, D], F32, ls) for i in range(2)]
                xtB = [Buf() for _ in range(2)]
                junk = sb("nt_junk", [128, D], BF16, ls)
                junkB = Buf()
                xn = [sb(f"nt_xn{i}", [128, D], BF16, ls) for i in range(2)]
                xnB = [Buf() for _ in range(2)]
                st_ = [sb(f"nt_s{i}", [128, 4], F32, ls) for i in range(2)]
                stB = [Buf() for _ in range(2)]
                hb = [sb(f"nt_h{i}", [128, KC, 512], BF16, ls) for i in range(2)]
                hbB = [Buf() for _ in range(2)]
                it = 0
                for blk in range(nblk):
                    h = hb[blk % 2]
                    hB = hbB[blk % 2]
                    for s in range(4):
                        i2 = it % 2
                        r0 = row0 + (blk * 4 + s) * 128
                        S.dma("sp", xt[i2][:], src[r0:r0 + 128, :], writes=[xtB[i2]])
                        S.op("act", lambda e: e.activation(out=junk[:], in_=xt[i2][:], func=AF.Square,
                                                           accum_out=st_[i2][:, 0:1]),
                             reads=[xtB[i2]], writes=[junkB, stB[i2]])
                        S.op("dve", lambda e: e.tensor_scalar(out=st_[i2][:, 1:2], in0=st_[i2][:, 0:1],
                                                              scalar1=1.0 / D, scalar2=eps,
                                                              op0=ALU.mult, op1=ALU.add),
                             reads=[stB[i2]], writes=[stB[i2]])
                        S.op("act", lambda e: e.activation(out=st_[i2][:, 3:4], in_=st_[i2][:, 1:2], func=AF.Sqrt),
                             reads=[stB[i2]], writes=[stB[i2]])
                        S.op("dve", lambda e: e.reciprocal(out=st_[i2][:, 2:3], in_=st_[i2][:, 3:4]),
                             reads=[stB[i2]], writes=[stB[i2]])
                        S.op("dve", lambda e: e.tensor_scalar(out=xn[i2][:], in0=xt[i2][:],
                                                              scalar1=st_[i2][:, 2:3], scalar2=None,
                                                              op0=ALU.mult),
                             reads=[xtB[i2], stB[i2]], writes=[xnB[i2]])
                        if tm_dst is not None:
                            S.dma("pool", tm_dst[r0 - row0:r0 - row0 + 128, :], xn[i2][:], reads=[xnB[i2]])
                        for g4 in range(4):
                            pb = (it * 4 + g4) % 4
                            pv = PS[pb][:].bitcast(BF16)

                            def tr(e, g4=g4, pv=pv, i2=i2):
                                ins = None
                                for q in range(4):
                                    kc = g4 * 4 + q
                                    ins = e.transpose(out=pv[:, q * 128:(q + 1) * 128],
                                                      in_=xn[i2][:, kc * 128:(kc + 1) * 128],
                                                      identity=ident_b[:])
                                return ins
                            S.op("pe", tr, reads=[xnB[i2]], writes=[PSB[pb]])
                            eng = "act" if g4 % 2 == 0 else "dve"

                            def ev(e, g4=g4, pv=pv, s=s, h=h, eng=eng):
                                o = h[:, g4 * 4:(g4 + 1) * 4, s * 128:(s + 1) * 128]
                                i_ = pv[:, 0:512].rearrange("p (k t) -> p k t", k=4)
                                if eng == "act":
                                    return e.copy(out=o, in_=i_)
                                return e.tensor_copy(out=o, in_=i_)
                            S.op(eng, ev, reads=[PSB[pb]], writes=[hB])
                        it += 1
                    S.dma("pool", dst_d[blk], h[:], reads=[hB])
                S.barrier()

        def gemm(ls, AT_d, blks, Wview_fn, ntiles, kc_n, wcols, mode, gvec, epilogue, group=2,
                 resident=None, pbanks=(0, 1, 2, 3)):
            wst = [sb(f"g_wst{i}", [128, 4096], F32, ls) for i in range(2)]
            wstB = [Buf() for _ in range(2)]
            nwb = 2 * group
            wbf = [sb(f"g_wbf{i}", [128, 4096], BF16, ls) for i in range(nwb)]
            wbfB = [Buf() for _ in range(nwb)]
            if resident is None:
                at = [sb(f"g_at{i}", [128, KC * 512], BF16, ls) for i in range(2)]
                atB = [Buf() for _ in range(2)]
            wi = 0
            ai = 0
            pi = 0
            for t0 in range(0, ntiles, group):
                tiles = list(range(t0, min(ntiles, t0 + group)))
                wsl = {}
                for ti in tiles:
                    a = wi % 2
                    b = wi % nwb
                    wv = wst[a][:, 0:kc_n * wcols].rearrange("p (k c) -> p k c", k=kc_n)
                    wsrc = Wview_fn(ti)
                    kq = max(1, kc_n // 4)
                    for k0 in range(0, kc_n, kq):
                        S.dma("sp", wv[:, k0:k0 + kq, :], wsrc[:, k0:k0 + kq, :], writes=[wstB[a]])
                    wb = wbf[b][:, 0:kc_n * wcols].rearrange("p (k c) -> p k c", k=kc_n)
                    ceng = "dve" if wi % 2 == 0 else "act"
                    if gvec is None:
                        if ceng == "dve":
                            S.op("dve", lambda e, a=a, b=b: e.tensor_copy(out=wbf[b][:, 0:kc_n * wcols],
                                                                          in_=wst[a][:, 0:kc_n * wcols]),
                                 reads=[wstB[a]], writes=[wbfB[b]])
                        else:
                            S.op("act", lambda e, a=a, b=b: e.copy(out=wbf[b][:, 0:kc_n * wcols],
                                                                   in_=wst[a][:, 0:kc_n * wcols]),
                                 reads=[wstB[a]], writes=[wbfB[b]])
                    else:
                        def cast(e, wv=wv, wb=wb, ceng=ceng):
                            ins = None
                            for k in range(kc_n):
                                if ceng == "dve":
                                    ins = e.tensor_scalar(out=wb[:, k, :], in0=wv[:, k, :],
                                                          scalar1=gvec[:, k:k + 1], scalar2=None, op0=ALU.mult)
                                else:
                                    ins = e.activation(out=wb[:, k, :], in_=wv[:, k, :], func=AF.Copy,
                                                       scale=gvec[:, k:k + 1])
                            return ins
                        S.op(ceng, cast, reads=[wstB[a]], writes=[wbfB[b]])
                    wsl[ti] = (wb, wbfB[b])
                    wi += 1
                for bi, blk in enumerate(blks):
                    if resident is None:
                        a = ai % 2
                        ai += 1
                        S.dma("sp", at[a][:], AT_d[blk].rearrange("p k t -> p (k t)"), writes=[atB[a]])
                        av = at[a][:].rearrange("p (k t) -> p k t", k=KC)
                        aB = atB[a]
                    else:
                        av, aB = resident[bi]
                    for ti in tiles:
                        wb, wB = wsl[ti]
                        if mode == "fm":
                            for ct in range(wcols // 128):
                                pb = pbanks[pi % len(pbanks)]
                                pi += 1

                                def mm(e, wb=wb, av=av, ct=ct, pb=pb):
                                    ins = None
                                    for k in range(kc_n):
                                        ins = e.matmul(PS[pb][:], lhsT=wb[:, k, ct * 128:(ct + 1) * 128],
                                                       rhs=av[:, k, :], start=(k == 0), stop=(k == kc_n - 1))
                                    return ins
                                S.op("pe", mm, reads=[wB, aB], writes=[PSB[pb]])
                                epilogue(pb, ti, ct, blk, bi)
                        else:
                            for s in range(4):
                                pb = pbanks[pi % len(pbanks)]
                                pi += 1

                                def mm(e, wb=wb, av=av, s=s, pb=pb):
                                    ins = None
                                    for k in range(kc_n):
                                        ins = e.matmul(PS[pb][:, 0:wcols], lhsT=av[:, k, s * 128:(s + 1) * 128],
                                                       rhs=wb[:, k, :], start=(k == 0), stop=(k == kc_n - 1))
                                    return ins
                                S.op("pe", mm, reads=[wB, aB], writes=[PSB[pb]])
                                epilogue(pb, ti, s, blk, bi)

        def w_tiles(Wap, c0, wcols):
            Wv = Wap.rearrange("(k p) n -> p k n", p=128)
            return lambda ti: Wv[:, :, c0 + ti * wcols: c0 + (ti + 1) * wcols]

        if upto >= 0:
            with contextlib.ExitStack() as ls:
                pos_i = sb("pos_i", [128, 512], I32, ls)
                pos_f = sb("pos_f", [128, 512], F32, ls)
                ang = sb("ang", [128, 512], F32, ls)
                a1 = sb("a1", [128, 512], F32, ls)
                a2 = sb("a2", [128, 512], F32, ls)
                tq = sb("tq", [128, 512], F32, ls)
                tB = Buf()
                posv = posf.rearrange("(b t) -> b t", t=512)
                for blk in range(16):
                    S.dma("sp", pos_i[:], posv[blk:blk + 1, :].partition_broadcast(128), writes=[tB])
                    S.op("dve", lambda e: e.tensor_copy(out=pos_f[:], in_=pos_i[:]), reads=[tB], writes=[tB])
                    S.op("dve", lambda e: e.tensor_scalar(out=ang[:], in0=pos_f[:], scalar1=rope_c[:, 0:1],
                                                          scalar2=None, op0=ALU.mult), reads=[tB], writes=[tB])
                    def trig(dst, shift):
                        S.op("dve", lambda e: e.tensor_scalar(out=dst[:], in0=ang[:], scalar1=shift, scalar2=None, op0=ALU.add), reads=[tB], writes=[tB])
                        S.op("dve", lambda e: e.tensor_scalar(out=tq[:], in0=dst[:], scalar1=1.0 / TWO_PI, scalar2=None, op0=ALU.mult), reads=[tB], writes=[tB])
                        S.op("dve", lambda e: e.tensor_copy(out=pos_i[:], in_=tq[:]), reads=[tB], writes=[tB])
                        S.op("dve", lambda e: e.tensor_copy(out=tq[:], in_=pos_i[:]), reads=[tB], writes=[tB])
                        S.op("dve", lambda e: e.scalar_tensor_tensor(out=dst[:], in0=tq[:], scalar=-TWO_PI, in1=dst[:], op0=ALU.mult, op1=ALU.add), reads=[tB], writes=[tB])
                        S.op("dve", lambda e: e.tensor_scalar(out=tq[:], in0=dst[:], scalar1=math.pi, scalar2=-TWO_PI, op0=ALU.is_gt, op1=ALU.mult), reads=[tB], writes=[tB])
                        S.op("dve", lambda e: e.tensor_tensor(out=dst[:], in0=dst[:], in1=tq[:], op=ALU.add), reads=[tB], writes=[tB])
                        S.op("dve", lambda e: e.tensor_scalar(out=tq[:], in0=dst[:], scalar1=-math.pi, scalar2=TWO_PI, op0=ALU.is_lt, op1=ALU.mult), reads=[tB], writes=[tB])
                        S.op("dve", lambda e: e.tensor_tensor(out=dst[:], in0=dst[:], in1=tq[:], op=ALU.add), reads=[tB], writes=[tB])
                        S.op("dve", lambda e: e.tensor_scalar(out=dst[:], in0=dst[:], scalar1=-math.pi, scalar2=math.pi, op0=ALU.max, op1=ALU.min), reads=[tB], writes=[tB])
                        S.op("act", lambda e: e.activation(out=dst[:], in_=dst[:], func=AF.Sin), reads=[tB], writes=[tB])
                    trig(a1, 0.0)
                    S.op("dve", lambda e: e.tensor_scalar(out=a1[:], in0=a1[:], scalar1=rope_c[:, 1:2], scalar2=None, op0=ALU.mult), reads=[tB], writes=[tB])
                    S.dma("pool", sin_d[blk], a1[:], reads=[tB])
                    trig(a2, math.pi / 2)
                    S.dma("pool", cos_d[blk], a2[:], reads=[tB])
                S.barrier()

        if upto >= 1:
            norm_transpose(xf, 0, 16, hT_d, 1e-6)

        def rope_epilogue_factory(ls, dst_d, tok_of_blk, scale_cols0):
            cs = [sb(f"rp_c{i}", [128, 512], F32, ls) for i in range(2)]
            sn = [sb(f"rp_s{i}", [128, 512], F32, ls) for i in range(2)]
            csB = [Buf() for _ in range(2)]
            tb_ = [sb(f"rp_t{i}", [128, 512], BF16, ls) for i in range(2)]
            tbB = [Buf() for _ in range(2)]
            o1 = [sb(f"rp_o1{i}", [128, 512], F32, ls) for i in range(2)]
            o2 = [sb(f"rp_o2{i}", [128, 512], F32, ls) for i in range(2)]
            ob = [sb(f"rp_ob{i}", [128, 512], BF16, ls) for i in range(2)]
            oB = [Buf() for _ in range(2)]
            state = dict(n=0, lastblk=None, ci=0)

            def epi(pb, ti, ct, blk, bi):
                n = state["n"]
                state["n"] += 1
                i2 = n % 2
                if state["lastblk"] != blk:
                    state["ci"] += 1
                    c2 = state["ci"] % 2
                    S.dma("sp", cs[c2][:], cos_d[blk], writes=[csB[c2]])
                    S.dma("sp", sn[c2][:], sin_d[blk], writes=[csB[c2]])
                    state["lastblk"] = blk
                c2 = state["ci"] % 2
                hm = scale_cols0 + ti * 2 + ct
                pb2 = 4 + (n % 2)
                S.op("act", lambda e: e.copy(out=tb_[i2][:], in_=PS[pb][:]), reads=[PSB[pb]], writes=[tbB[i2]])
                S.op("pe", lambda e: e.matmul(PS[pb2][:], lhsT=swap_b[:], rhs=tb_[i2][:], start=True, stop=True),
                     reads=[tbB[i2]], writes=[PSB[pb2]])
                S.op("dve", lambda e: e.tensor_tensor(out=o1[i2][:], in0=PS[pb][:], in1=cs[c2][:], op=ALU.mult),
                     reads=[PSB[pb], csB[c2], tbB[i2]], writes=[oB[i2]])
                S.op("dve", lambda e: e.tensor_tensor(out=o2[i2][:], in0=PS[pb2][:], in1=sn[c2][:], op=ALU.mult),
                     reads=[PSB[pb2], csB[c2]], writes=[oB[i2]])
                S.op("dve", lambda e: e.tensor_tensor(out=ob[i2][:], in0=o1[i2][:], in1=o2[i2][:], op=ALU.add),
                     reads=[oB[i2]], writes=[oB[i2]])
                t0 = tok_of_blk(blk)
                S.dma("pool", dst_d[hm, :, t0:t0 + 512], ob[i2][:], reads=[oB[i2]])
            return epi

        ALLB = list(range(16))
        OWNB = [12, 13, 14, 15]
        if upto >= 2:
            with contextlib.ExitStack() as ls:
                ut = [sb(f"e_u{i}", [128, 512], F32, ls) for i in range(3)]
                utB = [Buf() for _ in range(3)]
                cnt = [0]

                def epi_u(pb, ti, ct, blk, bi):
                    i3 = cnt[0] % 3
                    cnt[0] += 1
                    r0 = ti * 256 + ct * 128
                    S.op("act", lambda e: e.copy(out=ut[i3][:], in_=PS[pb][:]), reads=[PSB[pb]], writes=[utB[i3]])
                    S.dma("pool", uT_d[r0:r0 + 128, blk * 512:(blk + 1) * 512], ut[i3][:], reads=[utB[i3]])
                gemm(ls, hT_d, ALLB, w_tiles(w_in, 0, 256), 8, KC, 256, "fm", gmix_s, epi_u)
                S.barrier()
        if upto >= 3:
            with contextlib.ExitStack() as ls:
                epi_k = rope_epilogue_factory(ls, KT_d, lambda blk: blk * 512, 0)
                gemm(ls, hT_d, ALLB, w_tiles(w_in, 3 * D, 256), 8, KC, 256, "fm", gmix_s, epi_k)
                S.barrier()
            with contextlib.ExitStack() as ls:
                epi_q = rope_epilogue_factory(ls, QT_d, lambda blk: (blk - 12) * 512, 0)
                gemm(ls, hT_d, OWNB, w_tiles(w_in, 2 * D, 256), 8, KC, 256, "fm", gmix_s, epi_q)
                S.barrier()
        if upto >= 4:
            with contextlib.ExitStack() as ls:
                vt = [sb(f"e_v{i}", [128, 256], BF16, ls) for i in range(3)]
                vtB = [Buf() for _ in range(3)]
                cnt = [0]

                def epi_v(pb, ti, s, blk, bi):
                    i3 = cnt[0] % 3
                    cnt[0] += 1
                    r0 = blk * 512 + s * 128
                    eng = "act" if cnt[0] % 2 else "dve"
                    if eng == "act":
                        S.op("act", lambda e: e.copy(out=vt[i3][:], in_=PS[pb][:, 0:256]), reads=[PSB[pb]], writes=[vtB[i3]])
                    else:
                        S.op("dve", lambda e: e.tensor_copy(out=vt[i3][:], in_=PS[pb][:, 0:256]), reads=[PSB[pb]], writes=[vtB[i3]])
                    S.dma("pool", V_d[r0:r0 + 128, ti * 256:(ti + 1) * 256], vt[i3][:], reads=[vtB[i3]])
                gemm(ls, hT_d, ALLB, w_tiles(w_in, 4 * D, 256), 8, KC, 256, "tm", gmix_s, epi_v)
                S.barrier()
        if upto >= 5:
            with contextlib.ExitStack() as ls:
                xs = [sb(f"e_x{i}", [128, 512], F32, ls) for i in range(2)]
                t1 = [sb(f"e_t{i}", [128, 512], F32, ls) for i in range(2)]
                ob = [sb(f"e_o{i}", [128, 512], BF16, ls) for i in range(2)]
                eB = [Buf() for _ in range(2)]
                cnt = [0]

                def epi_gelu(pb, ti, ct, blk, bi):
                    i2 = cnt[0] % 2
                    cnt[0] += 1
                    r0 = ti * 256 + ct * 128
                    t0 = (blk - 12) * 512
                    S.op("act", lambda e: e.copy(out=xs[i2][:], in_=PS[pb][:]), reads=[PSB[pb]], writes=[eB[i2]])
                    S.op("dve", lambda e: e.tensor_tensor(out=t1[i2][:], in0=xs[i2][:], in1=xs[i2][:], op=ALU.mult),
                         reads=[eB[i2]], writes=[eB[i2]])
                    S.op("dve", lambda e: e.tensor_scalar(out=t1[i2][:], in0=t1[i2][:], scalar1=0.044715, scalar2=1.0,
                                                          op0=ALU.mult, op1=ALU.add), reads=[eB[i2]], writes=[eB[i2]])
                    S.op("dve", lambda e: e.tensor_tensor(out=t1[i2][:], in0=t1[i2][:], in1=xs[i2][:], op=ALU.mult),
                         reads=[eB[i2]], writes=[eB[i2]])
                    S.op("act", lambda e: e.activation(out=t1[i2][:], in_=t1[i2][:], func=AF.Sigmoid,
                                                       scale=2.0 * math.sqrt(2.0 / math.pi)),
                         reads=[eB[i2]], writes=[eB[i2]])
                    S.op("dve", lambda e: e.tensor_tensor(out=ob[i2][:], in0=t1[i2][:], in1=xs[i2][:], op=ALU.mult),
                         reads=[eB[i2]], writes=[eB[i2]])
                    S.dma("pool", gbT_d[r0:r0 + 128, t0:t0 + 512], ob[i2][:], reads=[eB[i2]])
                gemm(ls, hT_d, OWNB, w_tiles(w_in, D, 256), 8, KC, 256, "fm", gmix_s, epi_gelu)
                S.barrier()
            with contextlib.ExitStack() as ls:
                ob = [sb(f"e_o{i}", [128, 512], BF16, ls) for i in range(3)]
                eB = [Buf() for _ in range(3)]
                cnt = [0]

                def epi_sig(pb, ti, ct, blk, bi):
                    i3 = cnt[0] % 3
                    cnt[0] += 1
                    col = ti * 256 + ct * 128
                    which, r0 = col // D, col % D
                    t0 = (blk - 12) * 512
                    S.op("act", lambda e: e.activation(out=ob[i3][:], in_=PS[pb][:], func=AF.Sigmoid),
                         reads=[PSB[pb]], writes=[eB[i3]])
                    S.dma("pool", sg_d[which, r0:r0 + 128, t0:t0 + 512], ob[i3][:], reads=[eB[i3]])
                gemm(ls, hT_d, OWNB, w_tiles(w_in, 5 * D, 256), 16, KC, 256, "fm", gmix_s, epi_sig)
                S.barrier()

        if upto >= 6:
            with contextlib.ExitStack() as ls:
                CH = 2048
                nmb = sb("l_nm", [128, T], BF16, ls)
                vb = sb("l_vb", [128, T], BF16, ls)
                tmpf = sb("l_tmpf", [128, CH], F32, ls)
                tmpi = sb("l_tmpi", [128, CH], I32, ls)
                mB = Buf()
                for q in range(4):
                    sl = slice(q * CH, (q + 1) * CH)
                    S.dma("sp", tmpf[:], valid.rearrange("(o t) -> o t", o=1)[0:1, sl].partition_broadcast(128), writes=[mB])
                    S.op("dve", lambda e: e.tensor_copy(out=vb[:, sl], in_=tmpf[:]), reads=[mB], writes=[mB])
                    S.dma("sp", tmpi[:], posf.rearrange("(o t) -> o t", o=1)[0:1, sl].partition_broadcast(128), writes=[mB])
                    S.op("dve", lambda e: e.tensor_copy(out=tmpf[:], in_=tmpi[:]), reads=[mB], writes=[mB])
                    S.op("dve", lambda e: e.tensor_scalar(out=tmpf[:], in0=tmpf[:], scalar1=0.0, scalar2=None,
                                                          op0=ALU.not_equal), reads=[mB], writes=[mB])
                    S.op("dve", lambda e: e.tensor_tensor(out=nmb[:, sl], in0=tmpf[:], in1=vb[:, sl], op=ALU.mult),
                         reads=[mB], writes=[mB])
                cw = sb("l_cw", [128, 4, KC], F32, ls)
                cbs = sb("l_cb", [128, KC], F32, ls)
                bas = sb("l_ba", [128, KC], F32, ls)
                bxs = sb("l_bx", [128, KC], F32, ls)
                lps = sb("l_lp", [128, KC], F32, ls)
                nc8 = sb("l_nc8", [128, KC], F32, ls)
                pB = Buf()
                S.dma("sp", cw[:], conv_w[:, :, :], writes=[pB])
                S.dma("sp", cbs[:], conv_b[:, :], writes=[pB])
                S.dma("sp", bas[:], b_rg_a[:, :], writes=[pB])
                S.dma("sp", bxs[:], b_rg_x[:, :], writes=[pB])
                S.dma("sp", lps[:], lru_param[:, :], writes=[pB])
                S.op("act", lambda e: e.activation(out=nc8[:], in_=lps[:], func=AF.Exp, scale=-1.0), reads=[pB], writes=[pB])
                S.op("act", lambda e: e.activation(out=nc8[:], in_=nc8[:], func=AF.Ln, bias=1.0), reads=[pB], writes=[pB])
                S.op("dve", lambda e: e.tensor_scalar(out=nc8[:], in0=nc8[:], scalar1=-8.0, scalar2=None, op0=ALU.mult),
                     reads=[pB], writes=[pB])
                waf = sb("l_waf", [128, 2, 128], F32, ls)
                wab = [sb(f"l_wab{i}", [128, 2, 128], BF16, ls) for i in range(2)]
                wB = [Buf() for _ in range(2)]
                wfB = Buf()
                u = [sb(f"l_u{i}", [128, 3 + CH], F32, ls) for i in range(2)]
                uB = [Buf() for _ in range(2)]
                uc = sb("l_uc", [128, CH], F32, ls)
                ucb = sb("l_ucb", [128, CH], BF16, ls)
                rr = sb("l_r", [128, CH], F32, ls)
                ii = sb("l_i", [128, CH], F32, ls)
                aa = sb("l_a", [128, CH], F32, ls)
                mm_ = sb("l_m", [128, CH], F32, ls)
                hh = [sb(f"l_h{i}", [128, CH], F32, ls) for i in range(2)]
                hB = [Buf() for _ in range(2)]
                gbt = sb("l_gb", [128, NOWN], BF16, ls)
                yb = sb("l_y", [128, NOWN], BF16, ls)
                wkB = Buf()
                gB = Buf()
                yB = Buf()
                n = 0
                yr_v = yrT_d.rearrange("b p k t -> p k b t")
                for c in range(16):
                    w2 = c % 2
                    S.dma("sp", waf[:, 0, :], w_rg_a[c], writes=[wfB])
                    S.dma("sp", waf[:, 1, :], w_rg_x[c], writes=[wfB])
                    S.op("dve", lambda e: e.tensor_copy(out=wab[w2][:], in_=waf[:]), reads=[wfB], writes=[wB[w2]])
                    S.dma("sp", gbt[:], gbT_d[c * 128:(c + 1) * 128, :], writes=[gB])
                    for q in range(4):
                        i2 = n % 2
                        n += 1
                        t0 = q * CH
                        if q == 0:
                            S.op("dve", lambda e: e.memset(u[i2][:, 0:3], 0.0), writes=[uB[i2]])
                            S.dma("sp", u[i2][:, 3:3 + CH], uT_d[c * 128:(c + 1) * 128, 0:CH], writes=[uB[i2]])
                        else:
                            S.dma("sp", u[i2][:], uT_d[c * 128:(c + 1) * 128, t0 - 3:t0 + CH], writes=[uB[i2]])
                        uu = u[i2]
                        S.op("dve", lambda e: e.tensor_scalar(out=uc[:], in0=uu[:, 3:3 + CH], scalar1=cw[:, 3, c:c + 1],
                                                              scalar2=cbs[:, c:c + 1], op0=ALU.mult, op1=ALU.add),
                             reads=[uB[i2], pB], writes=[wkB])
                        for j in range(3):
                            S.op("dve", lambda e, j=j: e.scalar_tensor_tensor(out=uc[:], in0=uu[:, j:j + CH],
                                                                              scalar=cw[:, j, c:c + 1], in1=uc[:],
                                                                              op0=ALU.mult, op1=ALU.add),
                                 reads=[uB[i2], pB, wkB], writes=[wkB])
                        S.op("dve", lambda e: e.tensor_tensor(out=uc[:], in0=uc[:], in1=vb[:, t0:t0 + CH], op=ALU.mult),
                             reads=[wkB, mB], writes=[wkB])
                        S.op("act", lambda e: e.copy(out=ucb[:], in_=uc[:]), reads=[wkB], writes=[wkB])
                        for sblk in range(CH // 512):
                            ssl = slice(sblk * 512, (sblk + 1) * 512)
                            pa, px = (sblk * 2) % 4, (sblk * 2 + 1) % 4
                            S.op("pe", lambda e: e.matmul(PS[pa][:], lhsT=wab[w2][:, 0, :], rhs=ucb[:, ssl], start=True, stop=True),
                                 reads=[wB[w2], wkB], writes=[PSB[pa]])
                            S.op("pe", lambda e: e.matmul(PS[px][:], lhsT=wab[w2][:, 1, :], rhs=ucb[:, ssl], start=True, stop=True),
                                 reads=[wB[w2], wkB], writes=[PSB[px]])
                            S.op("act", lambda e: e.activation(out=rr[:, ssl], in_=PS[pa][:], func=AF.Sigmoid, bias=bas[:, c:c + 1]),
                                 reads=[PSB[pa], pB], writes=[wkB])
                            S.op("act", lambda e: e.activation(out=ii[:, ssl], in_=PS[px][:], func=AF.Sigmoid, bias=bxs[:, c:c + 1]),
                                 reads=[PSB[px], pB], writes=[wkB])
                        S.op("act", lambda e: e.activation(out=aa[:], in_=rr[:], func=AF.Exp, scale=nc8[:, c:c + 1]),
                             reads=[wkB, pB], writes=[wkB])
                        S.op("dve", lambda e: e.tensor_tensor(out=aa[:], in0=aa[:], in1=nmb[:, t0:t0 + CH], op=ALU.mult),
                             reads=[wkB, mB], writes=[wkB])
                        S.op("act", lambda e: e.activation(out=mm_[:], in_=aa[:], func=AF.Square), reads=[wkB], writes=[wkB])
                        S.op("act", lambda e: e.activation(out=mm_[:], in_=mm_[:], func=AF.Sqrt, scale=-1.0, bias=1.0),
                             reads=[wkB], writes=[wkB])
                        S.op("dve", lambda e: e.tensor_tensor(out=ii[:], in0=ii[:], in1=uc[:], op=ALU.mult), reads=[wkB], writes=[wkB])
                        S.op("dve", lambda e: e.tensor_tensor(out=ii[:], in0=ii[:], in1=mm_[:], op=ALU.mult), reads=[wkB], writes=[wkB])
                        hprev = hh[(i2 + 1) % 2]
                        init = 0.0 if q == 0 else hprev[:, CH - 1:CH]
                        S.op("dve", lambda e: e.tensor_tensor_scan(out=hh[i2][:], data0=aa[:], data1=ii[:], initial=init,
                                                                   op0=ALU.mult, op1=ALU.add),
                             reads=[wkB, hB[(i2 + 1) % 2]], writes=[hB[i2]])
                        if q == 3:
                            S.op("dve", lambda e: e.tensor_tensor(out=yb[:], in0=hh[i2][:], in1=gbt[:], op=ALU.mult),
                                 reads=[hB[i2], gB], writes=[yB])
                            S.dma("pool", yr_v[:, c], yb[:].rearrange("p (b t) -> p b t", b=4), reads=[yB])
                S.barrier()

        if upto >= 7:
            with contextlib.ExitStack() as ls:
                SCALE = 128 ** -0.5
                lamv = sb("a_lamv", [128, 4, 128], F32, ls)
                lams = sb("a_lams", [128, 8], F32, ls)
                lB = Buf()
                S.dma("sp", lamv[:].rearrange("p a b -> p (a b)"),
                      lam_in.rearrange("(o a) b -> o (a b)", o=1).partition_broadcast(128), writes=[lB])
                S.op("dve", lambda e: e.tensor_tensor(out=lamv[:, 0, :], in0=lamv[:, 0, :], in1=lamv[:, 1, :], op=ALU.mult), reads=[lB], writes=[lB])
                S.op("dve", lambda e: e.tensor_tensor(out=lamv[:, 2, :], in0=lamv[:, 2, :], in1=lamv[:, 3, :], op=ALU.mult), reads=[lB], writes=[lB])
                S.op("dve", lambda e: e.reduce_sum(out=lams[:, 0:1], in_=lamv[:, 0, :], axis=AX.X), reads=[lB], writes=[lB])
                S.op("dve", lambda e: e.reduce_sum(out=lams[:, 1:2], in_=lamv[:, 2, :], axis=AX.X), reads=[lB], writes=[lB])
                S.op("act", lambda e: e.activation(out=lams[:, 2:4], in_=lams[:, 0:2], func=AF.Exp), reads=[lB], writes=[lB])
                S.op("dve", lambda e: e.tensor_tensor(out=lams[:, 4:5], in0=lams[:, 3:4], in1=lams[:, 2:3], op=ALU.subtract), reads=[lB], writes=[lB])
                S.op("dve", lambda e: e.tensor_scalar(out=lams[:, 5:6], in0=lams[:, 4:5], scalar1=-0.2, scalar2=None, op0=ALU.add), reads=[lB], writes=[lB])
                neglam = lams[:, 5:6]
                sg_t = sb("a_sg", [128, 256], F32, ls)
                S.dma("sp", sg_t[:], subln_g.rearrange("(o t) -> o t", o=1).partition_broadcast(128), writes=[lB])
                S.op("dve", lambda e: e.tensor_scalar(out=sg_t[:], in0=sg_t[:], scalar1=0.8, scalar2=None, op0=ALU.mult), reads=[lB], writes=[lB])
                kb = sb("a_kb", [128, 64], F32, ls)
                S.dma("sp", kb[:], valid_pk[:, :], writes=[lB])
                S.op("dve", lambda e: e.tensor_scalar(out=kb[:], in0=kb[:], scalar1=-1.0, scalar2=30000.0, op0=ALU.add, op1=ALU.mult), reads=[lB], writes=[lB])
                trf = sb("a_trf", [128, 4, 512], F32, ls)
                trb = sb("a_trb", [128, 4, 512], BF16, ls)
                S.dma("sp", trf[:], c_tri[:, :, :], writes=[lB])
                S.op("dve", lambda e: e.tensor_copy(out=trb[:], in_=trf[:]), reads=[lB], writes=[lB])

                Va = [sb(f"a_V{i}", [128, 64, 257], BF16, ls) for i in range(2)]
                VaB = [Buf() for _ in range(2)]
                for i in range(2):
                    S.op("dve", lambda e, i=i: e.memset(Va[i][:, :, 256:257], 1.0), writes=[VaB[i]])
                KTs = [sb(f"a_K{i}", [128, T], BF16, ls) for i in range(2)]
                KTB = [Buf() for _ in range(2)]
                QTs = [sb(f"a_Q{i}", [128, NOWN], BF16, ls) for i in range(2)]
                QTB = [Buf() for _ in range(2)]
                Es = [sb(f"a_E{i}", [128, 512], BF16, ls) for i in range(3)]
                EB = [Buf() for _ in range(3)]
                om = [sb(f"a_om{i}", [128, 4, 256], F32, ls) for i in range(2)]
                omB = [Buf() for _ in range(2)]
                rs = sb("a_rs", [128, 8], F32, ls)
                rsB = Buf()
                od = sb("a_od", [128, 4, 256], F32, ls)
                odB = Buf()
                junk = sb("a_junk", [128, 256], F32, ls)
                onb = sb("a_onb", [128, 4, 256], BF16, ls)
                yo = [sb(f"a_yo{i}", [128, 2, 512], BF16, ls) for i in range(2)]
                yoB = [Buf() for _ in range(2)]
                V_v = V_d.rearrange("(k p) c -> p k c", p=128)
                it = 0
                ei = 0
                hq = 0
                for h in range(NH):
                    vi = h % 2
                    for k4 in range(16):
                        S.dma("sp", Va[vi][:, k4 * 4:(k4 + 1) * 4, 0:256], V_v[:, k4 * 4:(k4 + 1) * 4, h * 256:(h + 1) * 256],
                              writes=[VaB[vi]])
                    for qb in range(4):
                        nkt = 48 + (qb + 1) * 4
                        for m in range(2):
                            hm = h * 2 + m
                            ki = hm % 2
                            if qb == 0:
                                S.dma("sp", KTs[ki][:], KT_d[hm], writes=[KTB[ki]])
                                S.dma("sp", QTs[ki][:], QT_d[hm], writes=[QTB[ki]])
                            for kt in range(nkt):
                                pS = 4 + (it % 2)
                                it += 1
                                e3 = ei % 3
                                ei += 1
                                S.op("pe", lambda e: e.matmul(PS[pS][:], lhsT=KTs[ki][:, kt * 128:(kt + 1) * 128],
                                                              rhs=QTs[ki][:, qb * 512:(qb + 1) * 512], start=True, stop=True),
                                     reads=[KTB[ki], QTB[ki]], writes=[PSB[pS]])
                                S.op("act", lambda e: e.activation(out=Es[e3][:], in_=PS[pS][:], func=AF.Exp,
                                                                   scale=SCALE, bias=kb[:, kt:kt + 1]),
                                     reads=[PSB[pS], lB], writes=[EB[e3]])
                                dg = kt - (48 + qb * 4)
                                if dg >= 0:
                                    S.op("dve", lambda e: e.tensor_tensor(out=Es[e3][:], in0=Es[e3][:], in1=trb[:, dg, :], op=ALU.mult),
                                         reads=[EB[e3], lB], writes=[EB[e3]])

                                def pv(e, e3=e3, kt=kt, nkt=nkt, vi=vi):
                                    ins = None
                                    for qs in range(4):
                                        ins = e.matmul(PS[qs][:, 0:257], lhsT=Es[e3][:, qs * 128:(qs + 1) * 128],
                                                       rhs=Va[vi][:, kt, :], start=(kt == 0), stop=(kt == nkt - 1))
                                    return ins
                                S.op("pe", pv, reads=[EB[e3], VaB[vi]], writes=[PSB[0], PSB[1], PSB[2], PSB[3]])
                            for qs in range(4):
                                S.op("dve", lambda e, qs=qs: e.reciprocal(out=rs[:, m * 4 + qs:m * 4 + qs + 1], in_=PS[qs][:, 256:257]),
                                     reads=[PSB[qs]], writes=[rsB])
                                S.op("dve", lambda e, qs=qs: e.tensor_scalar(out=om[m][:, qs, :], in0=PS[qs][:, 0:256],
                                                                             scalar1=rs[:, m * 4 + qs:m * 4 + qs + 1], scalar2=None,
                                                                             op0=ALU.mult),
                                     reads=[PSB[qs], rsB], writes=[omB[m]])
                        y2 = hq % 2
                        hq += 1
                        S.op("dve", lambda e: e.scalar_tensor_tensor(out=od[:].rearrange("p a b -> p (a b)"),
                                                                     in0=om[1][:].rearrange("p a b -> p (a b)"),
                                                                     scalar=neglam,
                                                                     in1=om[0][:].rearrange("p a b -> p (a b)"),
                                                                     op0=ALU.mult, op1=ALU.add),
                             reads=[omB[0], omB[1], lB], writes=[odB])
                        for qs in range(4):
                            S.op("act", lambda e, qs=qs: e.activation(out=junk[:], in_=od[:, qs, :], func=AF.Square,
                                                                      accum_out=rs[:, qs:qs + 1]),
                                 reads=[odB], writes=[rsB])
                        S.op("dve", lambda e: e.tensor_scalar(out=rs[:, 0:4], in0=rs[:, 0:4], scalar1=1.0 / 256, scalar2=1e-5,
                                                              op0=ALU.mult, op1=ALU.add), reads=[rsB], writes=[rsB])
                        S.op("act", lambda e: e.activation(out=rs[:, 4:8], in_=rs[:, 0:4], func=AF.Sqrt), reads=[rsB], writes=[rsB])
                        S.op("dve", lambda e: e.reciprocal(out=rs[:, 0:4], in_=rs[:, 4:8]), reads=[rsB], writes=[rsB])
                        for qs in range(4):
                            S.op("dve", lambda e, qs=qs: e.scalar_tensor_tensor(out=onb[:, qs, :], in0=od[:, qs, :],
                                                                                scalar=rs[:, qs:qs + 1], in1=sg_t[:],
                                                                                op0=ALU.mult, op1=ALU.mult),
                                 reads=[odB, rsB, lB], writes=[odB])
                        for eh in range(2):
                            pT = 6 + eh
                            pvw = PS[pT][:].bitcast(BF16)

                            def tr(e, eh=eh, pvw=pvw):
                                ins = None
                                for qs in range(4):
                                    ins = e.transpose(out=pvw[:, qs * 128:(qs + 1) * 128],
                                                      in_=onb[:, qs, eh * 128:(eh + 1) * 128], identity=ident_b[:])
                                return ins
                            S.op("pe", tr, reads=[odB], writes=[PSB[pT]])
                            S.op("act", lambda e, eh=eh, pvw=pvw: e.copy(out=yo[y2][:, eh, :], in_=pvw[:, 0:512]),
                                 reads=[PSB[pT]], writes=[yoB[y2]])
                        S.dma("pool", yaT_d[qb, :, h * 2:(h + 1) * 2, :], yo[y2][:], reads=[yoB[y2]])
                S.barrier()

        if upto >= 8:
            for which, AT, W in [(0, yrT_d, w_br_rnn), (1, yaT_d, w_br_attn)]:
                with contextlib.ExitStack() as ls:
                    gt = [sb(f"b_g{i}", [128, 512], BF16, ls) for i in range(2)]
                    m1 = [sb(f"b_m{i}", [128, 512], BF16, ls) for i in range(2)]
                    ot = [sb(f"b_o{i}", [128, 512], F32, ls) for i in range(2)]
                    ob = [sb(f"b_ob{i}", [128, 512], BF16, ls) for i in range(2)]
                    bB = [Buf() for _ in range(2)]
                    oB = [Buf() for _ in range(2)]
                    cnt = [0]

                    def epi_b(pb, ti, ct, blk, bi, which=which):
                        i2 = cnt[0] % 2
                        cnt[0] += 1
                        r0 = ti * 256 + ct * 128
                        t0 = blk * 512
                        S.dma("sp", gt[i2][:], sg_d[which, r0:r0 + 128, t0:t0 + 512], writes=[bB[i2]])
                        if which == 0:
                            S.op("dve", lambda e: e.tensor_tensor(out=ob[i2][:], in0=PS[pb][:], in1=gt[i2][:], op=ALU.mult),
                                 reads=[PSB[pb], bB[i2]], writes=[oB[i2]])
                            S.dma("pool", m1_d[r0:r0 + 128, t0:t0 + 512], ob[i2][:], reads=[oB[i2]])
                        else:
                            S.dma("sp", m1[i2][:], m1_d[r0:r0 + 128, t0:t0 + 512], writes=[bB[i2]])
                            S.op("dve", lambda e: e.tensor_tensor(out=ot[i2][:], in0=PS[pb][:], in1=gt[i2][:], op=ALU.mult),
                                 reads=[PSB[pb], bB[i2]], writes=[oB[i2]])
                            S.op("dve", lambda e: e.tensor_tensor(out=ob[i2][:], in0=ot[i2][:], in1=m1[i2][:], op=ALU.add),
                                 reads=[oB[i2], bB[i2]], writes=[oB[i2]])
                            S.dma("pool", mT_d[blk, :, r0 // 128, :], ob[i2][:], reads=[oB[i2]])
                    gemm(ls, AT, [0, 1, 2, 3], w_tiles(W, 0, 256), 8, KC, 256, "fm", None, epi_b)
                    S.barrier()
            with contextlib.ExitStack() as ls:
                xo = [sb(f"o_x{i}", [128, 256], F32, ls) for i in range(2)]
                xB = [Buf() for _ in range(2)]
                cnt = [0]

                def epi_o(pb, ti, s, blk, bi):
                    i2 = cnt[0] % 2
                    cnt[0] += 1
                    r0 = blk * 512 + s * 128
                    S.dma("sp", xo[i2][:], xf[OWN0 + r0:OWN0 + r0 + 128, ti * 256:(ti + 1) * 256], writes=[xB[i2]])
                    S.op("dve", lambda e: e.tensor_tensor(out=xo[i2][:], in0=PS[pb][:, 0:256], in1=xo[i2][:], op=ALU.add),
                         reads=[PSB[pb], xB[i2]], writes=[xB[i2]])
                    S.dma("pool", x2_d[r0:r0 + 128, ti * 256:(ti + 1) * 256], xo[i2][:], reads=[xB[i2]])
                gemm(ls, mT_d, [0, 1, 2, 3], w_tiles(w_out, 0, 256), 8, KC, 256, "tm", None, epi_o)
                S.barrier()

        if upto >= 9:
            norm_transpose(x2_d, 0, 4, hnT_d, 1e-6, tm_dst=hn_d)
            with contextlib.ExitStack() as ls:
                wrf = sb("r_wf", [128, KC, 36], F32, ls)
                wrb = sb("r_wb", [128, KC, 36], BF16, ls)
                rB = Buf()
                S.dma("sp", wrf[:], w_router.rearrange("(k p) n -> p k n", p=128), writes=[rB])

                def castr(e):
                    ins = None
                    for k in range(KC):
                        ins = e.tensor_scalar(out=wrb[:, k, :], in0=wrf[:, k, :], scalar1=gffn_s[:, k:k + 1], scalar2=None, op0=ALU.mult)
                    return ins
                S.op("dve", castr, reads=[rB], writes=[rB])
                at = [sb(f"r_at{i}", [128, KC, 512], BF16, ls) for i in range(2)]
                atB = [Buf() for _ in range(2)]
                lg = sb("r_lg", [128, 36], F32, ls)
                w = {nm: sb("r_" + nm, [128, shape], F32, ls) for nm, shape in
                     [("gmax", 1), ("gex", 4), ("gsum", 1), ("gw", 1), ("gm", 4), ("pen", 32), ("el", 32), ("m1", 1),
                      ("k1", 32), ("el2", 32), ("m2", 1), ("k2", 32), ("dl", 1), ("w1", 1), ("w2", 1), ("c", 32), ("c2", 32), ("rt", 66)]}
                wkB = Buf()
                for blk in range(4):
                    a = blk % 2
                    S.dma("sp", at[a][:], hnT_d[blk], writes=[atB[a]])
                    for s in range(4):
                        pb = s % 4

                        def mm(e, a=a, s=s, pb=pb):
                            ins = None
                            for k in range(KC):
                                ins = e.matmul(PS[pb][:, 0:36], lhsT=at[a][:, k, s * 128:(s + 1) * 128], rhs=wrb[:, k, :],
                                               start=(k == 0), stop=(k == KC - 1))
                            return ins
                        S.op("pe", mm, reads=[atB[a], rB], writes=[PSB[pb]])
                        R = [wkB]

                        def D_(fn, extra=()):
                            S.op("dve", fn, reads=R + list(extra), writes=R)
                        D_(lambda e: e.tensor_copy(out=lg[:], in_=PS[pb][:, 0:36]), extra=[PSB[pb]])
                        D_(lambda e: e.reduce_max(out=w["gmax"][:], in_=lg[:, 0:4], axis=AX.X))
                        D_(lambda e: e.tensor_scalar(out=w["gex"][:], in0=lg[:, 0:4], scalar1=w["gmax"][:, 0:1], scalar2=None, op0=ALU.subtract))
                        S.op("act", lambda e: e.activation(out=w["gex"][:], in_=w["gex"][:], func=AF.Exp), reads=R, writes=R)
                        D_(lambda e: e.reduce_sum(out=w["gsum"][:], in_=w["gex"][:], axis=AX.X))
                        D_(lambda e: e.reciprocal(out=w["gw"][:], in_=w["gsum"][:]))
                        D_(lambda e: e.tensor_scalar(out=w["gm"][:], in0=lg[:, 0:4], scalar1=w["gmax"][:, 0:1], scalar2=None, op0=ALU.is_ge))
                        for g in range(4):
                            D_(lambda e, g=g: e.tensor_scalar(out=w["pen"][:, g * 8:(g + 1) * 8], in0=lg[:, 4 + g * 8:4 + (g + 1) * 8],
                                                              scalar1=0.0, scalar2=w["gm"][:, g:g + 1], op0=ALU.mult, op1=ALU.add))
                        D_(lambda e: e.tensor_scalar(out=w["pen"][:], in0=w["pen"][:], scalar1=-1.0, scalar2=1e9, op0=ALU.add, op1=ALU.mult))
                        D_(lambda e: e.tensor_tensor(out=w["el"][:], in0=lg[:, 4:36], in1=w["pen"][:], op=ALU.add))
                        D_(lambda e: e.reduce_max(out=w["m1"][:], in_=w["el"][:], axis=AX.X))
                        D_(lambda e: e.tensor_scalar(out=w["k1"][:], in0=w["el"][:], scalar1=w["m1"][:, 0:1], scalar2=None, op0=ALU.is_ge))
                        D_(lambda e: e.scalar_tensor_tensor(out=w["el2"][:], in0=w["k1"][:], scalar=-1e9, in1=w["el"][:], op0=ALU.mult, op1=ALU.add))
                        D_(lambda e: e.reduce_max(out=w["m2"][:], in_=w["el2"][:], axis=AX.X))
                        D_(lambda e: e.tensor_scalar(out=w["k2"][:], in0=w["el2"][:], scalar1=w["m2"][:, 0:1], scalar2=None, op0=ALU.is_ge))
                        D_(lambda e: e.tensor_tensor(out=w["dl"][:], in0=w["m1"][:], in1=w["m2"][:], op=ALU.subtract))
                        S.op("act", lambda e: e.activation(out=w["w1"][:], in_=w["dl"][:], func=AF.Sigmoid), reads=R, writes=R)
                        D_(lambda e: e.tensor_scalar(out=w["w2"][:], in0=w["w1"][:], scalar1=-1.0, scalar2=1.0, op0=ALU.mult, op1=ALU.add))
                        D_(lambda e: e.tensor_tensor(out=w["w1"][:], in0=w["w1"][:], in1=w["gw"][:], op=ALU.mult))
                        D_(lambda e: e.tensor_tensor(out=w["w2"][:], in0=w["w2"][:], in1=w["gw"][:], op=ALU.mult))
                        D_(lambda e: e.tensor_scalar(out=w["c"][:], in0=w["k1"][:], scalar1=w["w1"][:, 0:1], scalar2=None, op0=ALU.mult))
                        D_(lambda e: e.scalar_tensor_tensor(out=w["c2"][:], in0=w["k2"][:], scalar=w["w2"][:, 0:1], in1=w["c"][:], op0=ALU.mult, op1=ALU.add))
                        r0 = blk * 512 + s * 128
                        S.dma("pool", C_d[r0:r0 + 128, :], w["c2"][:], reads=R)
                        D_(lambda e: e.tensor_copy(out=w["rt"][:, 0:32], in_=w["k1"][:]))
                        D_(lambda e: e.tensor_copy(out=w["rt"][:, 32:64], in_=w["k2"][:]))
                        D_(lambda e: e.tensor_copy(out=w["rt"][:, 64:65], in_=w["w1"][:]))
                        D_(lambda e: e.tensor_copy(out=w["rt"][:, 65:66], in_=w["w2"][:]))
                        S.dma("pool", R_d[r0:r0 + 128, :], w["rt"][:], reads=R)
                S.barrier()

        if upto >= 10 and not sparse:
            with contextlib.ExitStack() as ls:
                Cs = sb("m_C", [128, 16, NE], F32, ls)
                cB = Buf()
                S.dma("sp", Cs[:], C_d.rearrange("(t p) e -> p t e", p=128), writes=[cB])
                acc = sb("m_acc", [128, 8, D], F32, ls)
                accB = [Buf() for _ in range(8)]
                hn = [sb(f"m_hn{i}", [128, KC, 512], BF16, ls) for i in range(2)]
                hnB = [Buf() for _ in range(2)]
                actT = sb("m_act", [128, 8, 1024], BF16, ls)
                actB = [Buf() for _ in range(8)]
                sgt = [sb(f"m_sg{i}", [128, 512], F32, ls) for i in range(2)]
                sgB = [Buf() for _ in range(2)]
                fst = sb("m_fst", [128, 4], F32, ls)
                fB = Buf()
                wst = [sb(f"m_wst{i}", [128, 4096], F32, ls) for i in range(2)]
                wstB = [Buf() for _ in range(2)]
                wbf = [sb(f"m_wbf{i}", [128, 4096], BF16, ls) for i in range(4)]
                wbfB = [Buf() for _ in range(4)]
                wi = [0]
                sgi = [0]
                pi = [0]

                def load_w(view, kc_n, wcols, gvec):
                    a = wi[0] % 2
                    b = wi[0] % 4
                    wi[0] += 1
                    wv = wst[a][:, 0:kc_n * wcols].rearrange("p (k c) -> p k c", k=kc_n)
                    wb = wbf[b][:, 0:kc_n * wcols].rearrange("p (k c) -> p k c", k=kc_n)
                    kq = max(1, kc_n // 4)
                    for k0 in range(0, kc_n, kq):
                        S.dma("sp", wv[:, k0:k0 + kq, :], view[:, k0:k0 + kq, :], writes=[wstB[a]])
                    ceng = "dve" if wi[0] % 2 == 0 else "act"
                    if gvec is None:
                        if ceng == "dve":
                            S.op("dve", lambda e: e.tensor_copy(out=wbf[b][:, 0:kc_n * wcols], in_=wst[a][:, 0:kc_n * wcols]),
                                 reads=[wstB[a]], writes=[wbfB[b]])
                        else:
                            S.op("act", lambda e: e.copy(out=wbf[b][:, 0:kc_n * wcols], in_=wst[a][:, 0:kc_n * wcols]),
                                 reads=[wstB[a]], writes=[wbfB[b]])
                    else:
                        def cast(e):
                            ins = None
                            for k in range(kc_n):
                                if ceng == "dve":
                                    ins = e.tensor_scalar(out=wb[:, k, :], in0=wv[:, k, :], scalar1=gvec[:, k:k + 1], scalar2=None, op0=ALU.mult)
                                else:
                                    ins = e.activation(out=wb[:, k, :], in_=wv[:, k, :], func=AF.Copy, scale=gvec[:, k:k + 1])
                            return ins
                        S.op(ceng, cast, reads=[wstB[a]], writes=[wbfB[b]])
                    return wb, wbfB[b]

                for tb in range(2):
                    for j in range(2):
                        S.dma("sp", hn[j][:], hnT_d[tb * 2 + j], writes=[hnB[j]])
                    for tt in range(8):
                        r0 = tb * 1024 + tt * 128
                        S.dma("sp", acc[:, tt, :], x2_d[r0:r0 + 128, :], writes=[accB[tt]])
                    for ex in range(NE):
                        wgv = w_gate[ex].rearrange("(k p) n -> p k n", p=128)
                        wuv = w_up[ex].rearrange("(k p) n -> p k n", p=128)
                        wdv = w_down[ex].rearrange("(k p) n -> p k n", p=128)
                        for f2 in range(4):
                            wg, wgB = load_w(wgv[:, :, f2 * 256:(f2 + 1) * 256], KC, 256, gffn_s)
                            wu, wuB = load_w(wuv[:, :, f2 * 256:(f2 + 1) * 256], KC, 256, gffn_s)
                            for j in range(2):
                                for ct in range(2):
                                    fc = f2 * 2 + ct
                                    pg = (pi[0] * 2) % 4
                                    pu = pg + 1
                                    pi[0] += 1

                                    def mmg(e, W=wg, P=pg, j=j, ct=ct):
                                        ins = None
                                        for k in range(KC):
                                            ins = e.matmul(PS[P][:], lhsT=W[:, k, ct * 128:(ct + 1) * 128], rhs=hn[j][:, k, :],
                                                           start=(k == 0), stop=(k == KC - 1))
                                        return ins
                                    S.op("pe", mmg, reads=[wgB, hnB[j]], writes=[PSB[pg]])
                                    S.op("pe", lambda e: mmg(e, W=wu, P=pu), reads=[wuB, hnB[j]], writes=[PSB[pu]])
                                    s2 = sgi[0] % 2
                                    sgi[0] += 1
                                    S.op("act", lambda e: e.activation(out=sgt[s2][:], in_=PS[pg][:], func=AF.Silu),
                                         reads=[PSB[pg]], writes=[sgB[s2]])
                                    S.op("dve", lambda e: e.tensor_tensor(out=actT[:, fc, j * 512:(j + 1) * 512], in0=PS[pu][:],
                                                                          in1=sgt[s2][:], op=ALU.mult),
                                         reads=[PSB[pu], sgB[s2]], writes=[actB[fc]])
                        for cg in range(4):
                            wd, wdB = load_w(wdv[:, :, cg * 512:(cg + 1) * 512], 8, 512, None)
                            for tt in range(8):
                                pd = 4 + (pi[0] % 4)
                                pi[0] += 1

                                def mmd(e, wd=wd, pd=pd, tt=tt):
                                    ins = None
                                    for k in range(8):
                                        ins = e.matmul(PS[pd][:], lhsT=actT[:, k, tt * 128:(tt + 1) * 128], rhs=wd[:, k, :],
                                                       start=(k == 0), stop=(k == 7))
                                    return ins
                                S.op("pe", mmd, reads=[wdB] + actB, writes=[PSB[pd]])
                                S.op("dve", lambda e: e.scalar_tensor_tensor(out=acc[:, tt, cg * 512:(cg + 1) * 512], in0=PS[pd][:],
                                                                             scalar=Cs[:, tb * 8 + tt, ex:ex + 1],
                                                                             in1=acc[:, tt, cg * 512:(cg + 1) * 512],
                                                                             op0=ALU.mult, op1=ALU.add),
                                     reads=[PSB[pd], cB, accB[tt]], writes=[accB[tt]])
                    gfin = wst[0][:, 0:D]
                    fjunk = actT[:, 0:2, :]
                    S.dma("sp", gfin, g_final.rearrange("(o t) -> o t", o=1).partition_broadcast(128), writes=[wstB[0]])
                    for tt in range(8):
                        r0 = tb * 1024 + tt * 128
                        S.op("act", lambda e: e.activation(out=fjunk, in_=acc[:, tt, :].rearrange("p (a b) -> p a b", a=2), func=AF.Square, accum_out=fst[:, 0:1]),
                             reads=[accB[tt]], writes=[fB, actB[0], actB[1]])
                        S.op("dve", lambda e: e.tensor_scalar(out=fst[:, 1:2], in0=fst[:, 0:1], scalar1=1.0 / D, scalar2=1e-6,
                                                              op0=ALU.mult, op1=ALU.add), reads=[fB], writes=[fB])
                        S.op("act", lambda e: e.activation(out=fst[:, 3:4], in_=fst[:, 1:2], func=AF.Sqrt), reads=[fB], writes=[fB])
                        S.op("dve", lambda e: e.reciprocal(out=fst[:, 2:3], in_=fst[:, 3:4]), reads=[fB], writes=[fB])
                        S.op("dve", lambda e: e.scalar_tensor_tensor(out=acc[:, tt, :], in0=acc[:, tt, :], scalar=fst[:, 2:3],
                                                                     in1=gfin, op0=ALU.mult, op1=ALU.mult),
                             reads=[accB[tt], fB, wstB[0]], writes=[accB[tt]])
                        S.dma("pool", out[r0:r0 + 128, :], acc[:, tt, :], reads=[accB[tt]])
                S.barrier()
        if upto >= 10 and sparse:
            with contextlib.ExitStack() as ls:
                Rs = sb("s_R", [128, 16, 66], F32, ls)
                rB = Buf()
                S.dma("sp", Rs[:], R_d.rearrange("(t p) e -> p t e", p=128), writes=[rB])
                lsf = sb("s_lsf", [128, 128], F32, ls)
                lsb = sb("s_lsb", [128, 128], BF16, ls)
                onb = sb("s_onb", [128, 128], BF16, ls)
                iog = sb("s_iog", [128, 16], F32, ls)
                iod = sb("s_iod", [128, 8], F32, ls)
                S.dma("sp", lsf[:], c_ls[:, :], writes=[rB])
                S.dma("sp", iog[:], c_iog[:, :], writes=[rB])
                S.dma("sp", iod[:], c_iod[:, :], writes=[rB])
                S.op("dve", lambda e: e.tensor_copy(out=lsb[:], in_=lsf[:]), reads=[rB], writes=[rB])
                S.op("dve", lambda e: e.memset(onb[:], 1.0), writes=[rB])
                Ab = sb("s_Ab", [128, 16, 32], BF16, ls)
                S.op("dve", lambda e: e.tensor_tensor(out=Ab[:], in0=Rs[:, :, 0:32], in1=Rs[:, :, 32:64], op=ALU.add), reads=[rB], writes=[rB])
                def mmc(e):
                    ins = None
                    for i in range(16):
                        ins = e.matmul(PS[0][:, 0:32], lhsT=onb[:], rhs=Ab[:, i, :], start=(i == 0), stop=(i == 15))
                    return ins
                S.op("pe", mmc, reads=[rB], writes=[PSB[0]])
                cnt = sb("s_cnt", [128, 32], F32, ls)
                nb = sb("s_nb", [128, 32], F32, ls)
                pend = sb("s_pend", [128, 32], F32, ls)
                pst = sb("s_pst", [128, 32], F32, ls)
                one32 = sb("s_one32", [128, 32], F32, ls)
                tmp32 = sb("s_tmp32", [128, 32], F32, ls)
                eb = sb("s_eb", [128, 64], F32, ls)
                cB = Buf()
                S.op("dve", lambda e: e.tensor_copy(out=cnt[:], in_=PS[0][:, 0:32]), reads=[PSB[0]], writes=[cB])
                S.op("dve", lambda e: e.memset(nb[:], 0.0), writes=[cB])
                S.op("dve", lambda e: e.memset(one32[:], 1.0), writes=[cB])
                for j in range(16):
                    S.op("dve", lambda e, j=j: e.scalar_tensor_tensor(out=nb[:], in0=cnt[:], scalar=128.0 * j, in1=nb[:],
                                                                      op0=ALU.is_gt, op1=ALU.add), reads=[cB], writes=[cB])
                S.op("dve", lambda e: e.tensor_scalar(out=nb[:], in0=nb[:], scalar1=128.0, scalar2=None, op0=ALU.mult), reads=[cB], writes=[cB])
                S.op("dve", lambda e: e.tensor_tensor_scan(out=pend[:], data0=one32[:], data1=nb[:], initial=0.0,
                                                           op0=ALU.mult, op1=ALU.add), reads=[cB], writes=[cB])
                S.op("dve", lambda e: e.tensor_tensor(out=pst[:], in0=pend[:], in1=nb[:], op=ALU.subtract), reads=[cB], writes=[cB])
                for b_ in range(64):
                    S.op("dve", lambda e, b_=b_: e.tensor_scalar(out=tmp32[:], in0=pend[:], scalar1=128.0 * b_, scalar2=0.0,
                                                                 op0=ALU.is_le, op1=ALU.add, accum_out=eb[:, b_:b_ + 1]),
                         reads=[cB], writes=[cB])
                S.op("dve", lambda e: e.tensor_scalar(out=eb[:], in0=eb[:], scalar1=31.0, scalar2=None, op0=ALU.min), reads=[cB], writes=[cB])
                ebg = sb("s_ebg", [128, 64], F32, ls)
                ebd = sb("s_ebd", [128, 64], F32, ls)
                S.op("dve", lambda e: e.tensor_scalar(out=ebg[:], in0=eb[:], scalar1=2048.0, scalar2=None, op0=ALU.mult), reads=[cB], writes=[cB])
                S.op("dve", lambda e: e.tensor_scalar(out=ebd[:], in0=eb[:], scalar1=1024.0, scalar2=None, op0=ALU.mult), reads=[cB], writes=[cB])
                igf = sb("s_igf", [128, 64, 16], F32, ls)
                idf = sb("s_idf", [128, 64, 8], F32, ls)
                igi = sb("s_igi", [128, 64, 16], I32, ls)
                idi = sb("s_idi", [128, 64, 8], I32, ls)
                for b_ in range(64):
                    S.op("dve", lambda e, b_=b_: e.tensor_scalar(out=igf[:, b_, :], in0=iog[:], scalar1=ebg[:, b_:b_ + 1], scalar2=None, op0=ALU.add),
                         reads=[cB, rB], writes=[cB])
                    S.op("dve", lambda e, b_=b_: e.tensor_scalar(out=idf[:, b_, :], in0=iod[:], scalar1=ebd[:, b_:b_ + 1], scalar2=None, op0=ALU.add),
                         reads=[cB, rB], writes=[cB])
                S.op("dve", lambda e: e.tensor_copy(out=igi[:], in_=igf[:]), reads=[cB], writes=[cB])
                S.op("dve", lambda e: e.tensor_copy(out=idi[:], in_=idf[:]), reads=[cB], writes=[cB])
                dsf = sb("s_dsf", [128, 16, 2], F32, ls)
                dsi = sb("s_dsi", [128, 16, 2], I32, ls)
                pos = sb("s_pos", [128, 32], F32, ls)
                dB = Buf()
                for i in range(16):
                    pb = 1 + (i % 3)

                    def mmr(e, i=i, pb=pb):
                        for i2 in range(i):
                            e.matmul(PS[pb][:, 0:32], lhsT=onb[:], rhs=Ab[:, i2, :], start=(i2 == 0), stop=False)
                        return e.matmul(PS[pb][:, 0:32], lhsT=lsb[:], rhs=Ab[:, i, :], start=(i == 0), stop=True)
                    S.op("pe", mmr, reads=[rB], writes=[PSB[pb]])
                    S.op("dve", lambda e: e.tensor_tensor(out=pos[:], in0=PS[pb][:, 0:32], in1=pst[:], op=ALU.add),
                         reads=[PSB[pb], cB, dB], writes=[dB])
                    for k_ in range(2):
                        S.op("dve", lambda e, k_=k_: e.tensor_tensor(out=tmp32[:], in0=pos[:], in1=Rs[:, i, k_ * 32:(k_ + 1) * 32], op=ALU.mult),
                             reads=[dB, rB, cB], writes=[cB])
                        S.op("dve", lambda e, k_=k_: e.reduce_sum(out=dsf[:, i, k_:k_ + 1], in_=tmp32[:], axis=AX.X), reads=[cB, dB], writes=[dB])
                S.op("dve", lambda e: e.tensor_copy(out=dsi[:], in_=dsf[:]), reads=[dB], writes=[dB])
                ht = [sb(f"s_ht{i}", [128, D], BF16, ls) for i in range(2)]
                htB = [Buf() for _ in range(2)]
                xsB = Buf()
                for i in range(16):
                    a = i % 2
                    S.dma("sp", ht[a][:], hn_d[i * 128:(i + 1) * 128, :], writes=[htB[a]])
                    for k_ in range(2):
                        S.idma(xs_d[:, :], bass.IndirectOffsetOnAxis(ap=dsi[:, i, k_:k_ + 1], axis=0), ht[a][:], None,
                               reads=[htB[a], dB], writes=[xsB])
                S.barrier()
                wg_rows = w_gate.rearrange("e k n -> (e k) n")
                wu_rows = w_up.rearrange("e k n -> (e k) n")
                wd_rows = w_down.rearrange("e k n -> (e k) n")
                xb = [sb(f"s_xb{i}", [128, D], BF16, ls) for i in range(2)]
                xbB = [Buf() for _ in range(2)]
                xT = [sb(f"s_xT{i}", [128, KC, 128], BF16, ls) for i in range(2)]
                xTB = [Buf() for _ in range(2)]
                gst = [sb(f"s_gst{i}", [128, 1024], F32, ls) for i in range(4)]
                gstB = [Buf() for _ in range(4)]
                gbf = [sb(f"s_gbf{i}", [128, 1024], BF16, ls) for i in range(6)]
                gbfB = [Buf() for _ in range(6)]
                dst_ = [sb(f"s_dst{i}", [128, D], F32, ls) for i in range(2)]
                dstB = [Buf() for _ in range(2)]
                dbf = [sb(f"s_dbf{i}", [128, D], BF16, ls) for i in range(3)]
                dbfB = [Buf() for _ in range(3)]
                sgl = sb("s_sgl", [128, 1024], F32, ls)
                sglB = Buf()
                actb = sb("s_act", [128, 1024], BF16, ls)
                actB_ = Buf()
                aT = sb("s_aT", [128, 8, 128], BF16, ls)
                aTB = Buf()
                yb = [sb(f"s_yb{i}", [128, D], BF16, ls) for i in range(2)]
                ybB = [Buf() for _ in range(2)]
                ysB = Buf()
                gi = 0
                gbi = 0
                di = 0
                dbi = 0
                ce = 0
                for b_ in range(64):
                    x2i = b_ % 2
                    S.dma("sp", xb[x2i][:], xs_d[b_ * 128:(b_ + 1) * 128, :], writes=[xbB[x2i]])
                    for g4 in range(4):
                        pb = 4 + g4
                        pvw = PS[pb][:].bitcast(BF16)

                        def tr(e, g4=g4, pvw=pvw, x2i=x2i):
                            ins = None
                            for q in range(4):
                                kc = g4 * 4 + q
                                ins = e.transpose(out=pvw[:, q * 128:(q + 1) * 128], in_=xb[x2i][:, kc * 128:(kc + 1) * 128], identity=ident_b[:])
                            return ins
                        S.op("pe", tr, reads=[xbB[x2i]], writes=[PSB[pb]])
                        eng = "act" if g4 % 2 == 0 else "dve"
                        o_ = xT[x2i][:, g4 * 4:(g4 + 1) * 4, :]
                        i_ = pvw[:, 0:512].rearrange("p (k t) -> p k t", k=4)
                        if eng == "act":
                            S.op("act", lambda e: e.copy(out=o_, in_=i_), reads=[PSB[pb]], writes=[xTB[x2i]])
                        else:
                            S.op("dve", lambda e: e.tensor_copy(out=o_, in_=i_), reads=[PSB[pb]], writes=[xTB[x2i]])
                    for kc in range(KC):
                        wpair = []
                        for rows in (wg_rows, wu_rows):
                            a = gi % 4
                            gi += 1
                            bq = gbi % 6
                            gbi += 1
                            S.idma(gst[a][:], None, rows[:, :], bass.IndirectOffsetOnAxis(ap=igi[:, b_, kc:kc + 1], axis=0),
                                   reads=[cB], writes=[gstB[a]])
                            ceng = "dve" if ce % 2 == 0 else "act"
                            ce += 1
                            if ceng == "dve":
                                S.op("dve", lambda e: e.tensor_scalar(out=gbf[bq][:], in0=gst[a][:], scalar1=gffn_s[:, kc:kc + 1], scalar2=None, op0=ALU.mult),
                                     reads=[gstB[a]], writes=[gbfB[bq]])
                            else:
                                S.op("act", lambda e: e.activation(out=gbf[bq][:], in_=gst[a][:], func=AF.Copy, scale=gffn_s[:, kc:kc + 1]),
                                     reads=[gstB[a]], writes=[gbfB[bq]])
                            wpair.append(bq)

                        def mmgu(e, kc=kc, wpair=wpair, x2i=x2i):
                            ins = None
                            for wi_, bq in enumerate(wpair):
                                for hf in range(2):
                                    ins = e.matmul(PS[wi_ * 2 + hf][:], lhsT=xT[x2i][:, kc, :], rhs=gbf[bq][:, hf * 512:(hf + 1) * 512],
                                                   start=(kc == 0), stop=(kc == KC - 1))
                            return ins
                        S.op("pe", mmgu, reads=[xTB[x2i], gbfB[wpair[0]], gbfB[wpair[1]]], writes=[PSB[0], PSB[1], PSB[2], PSB[3]])
                    for hf in range(2):
                        S.op("act", lambda e, hf=hf: e.activation(out=sgl[:, hf * 512:(hf + 1) * 512], in_=PS[hf][:], func=AF.Silu),
                             reads=[PSB[hf]], writes=[sglB])
                        S.op("dve", lambda e, hf=hf: e.tensor_tensor(out=actb[:, hf * 512:(hf + 1) * 512], in0=PS[2 + hf][:],
                                                                     in1=sgl[:, hf * 512:(hf + 1) * 512], op=ALU.mult),
                             reads=[PSB[2 + hf], sglB], writes=[actB_])
                    for g2 in range(2):
                        pb = g2
                        pvw = PS[pb][:].bitcast(BF16)

                        def tr2(e, g2=g2, pvw=pvw):
                            ins = None
                            for q in range(4):
                                fc = g2 * 4 + q
                                ins = e.transpose(out=pvw[:, q * 128:(q + 1) * 128], in_=actb[:, fc * 128:(fc + 1) * 128], identity=ident_b[:])
                            return ins
                        S.op("pe", tr2, reads=[actB_], writes=[PSB[pb]])
                        o_ = aT[:, g2 * 4:(g2 + 1) * 4, :]
                        i_ = pvw[:, 0:512].rearrange("p (k t) -> p k t", k=4)
                        if g2 == 0:
                            S.op("act", lambda e: e.copy(out=o_, in_=i_), reads=[PSB[pb]], writes=[aTB])
                        else:
                            S.op("dve", lambda e: e.tensor_copy(out=o_, in_=i_), reads=[PSB[pb]], writes=[aTB])
                    for fc in range(8):
                        a = di % 2
                        di += 1
                        bq = dbi % 3
                        dbi += 1
                        S.idma(dst_[a][:], None, wd_rows[:, :], bass.IndirectOffsetOnAxis(ap=idi[:, b_, fc:fc + 1], axis=0),
                               reads=[cB], writes=[dstB[a]])
                        if fc % 2 == 0:
                            S.op("dve", lambda e: e.tensor_copy(out=dbf[bq][:], in_=dst_[a][:]), reads=[dstB[a]], writes=[dbfB[bq]])
                        else:
                            S.op("act", lambda e: e.copy(out=dbf[bq][:], in_=dst_[a][:]), reads=[dstB[a]], writes=[dbfB[bq]])

                        def mmd(e, fc=fc, bq=bq):
                            ins = None
                            for cg in range(4):
                                ins = e.matmul(PS[4 + cg][:], lhsT=aT[:, fc, :], rhs=dbf[bq][:, cg * 512:(cg + 1) * 512],
                                               start=(fc == 0), stop=(fc == 7))
                            return ins
                        S.op("pe", mmd, reads=[aTB, dbfB[bq]], writes=[PSB[4], PSB[5], PSB[6], PSB[7]])
                    for cg in range(4):
                        if cg % 2 == 0:
                            S.op("act", lambda e, cg=cg: e.copy(out=yb[x2i][:, cg * 512:(cg + 1) * 512], in_=PS[4 + cg][:]),
                                 reads=[PSB[4 + cg]], writes=[ybB[x2i]])
                        else:
                            S.op("dve", lambda e, cg=cg: e.tensor_copy(out=yb[x2i][:, cg * 512:(cg + 1) * 512], in_=PS[4 + cg][:]),
                                 reads=[PSB[4 + cg]], writes=[ybB[x2i]])
                    S.dma("sp", ys_d[b_ * 128:(b_ + 1) * 128, :], yb[x2i][:], reads=[ybB[x2i]], writes=[ysB])
                S.barrier()
                gfin = sb("s_gf", [128, D], F32, ls)
                fB0 = Buf()
                S.dma("sp", gfin[:], g_final.rearrange("(o t) -> o t", o=1).partition_broadcast(128), writes=[fB0])
                g1 = [sb(f"s_g1{i}", [128, D], BF16, ls) for i in range(2)]
                g2_ = [sb(f"s_g2{i}", [128, D], BF16, ls) for i in range(2)]
                xr = [sb(f"s_xr{i}", [128, D], F32, ls) for i in range(2)]
                gB_ = [Buf() for _ in range(2)]
                xrB = [Buf() for _ in range(2)]
                fst = [sb(f"s_fst{i}", [128, 4], F32, ls) for i in range(2)]
                fj = sb("s_fj", [128, D], BF16, ls)
                fjB = Buf()
                for i in range(16):
                    a = i % 2
                    S.dma("sp", xr[a][:], x2_d[i * 128:(i + 1) * 128, :], writes=[xrB[a]])
                    S.idma(g1[a][:], None, ys_d[:, :], bass.IndirectOffsetOnAxis(ap=dsi[:, i, 0:1], axis=0), reads=[dB, ysB], writes=[gB_[a]])
                    S.idma(g2_[a][:], None, ys_d[:, :], bass.IndirectOffsetOnAxis(ap=dsi[:, i, 1:2], axis=0), reads=[dB, ysB], writes=[gB_[a]])
                    S.op("dve", lambda e: e.scalar_tensor_tensor(out=xr[a][:], in0=g1[a][:], scalar=Rs[:, i, 64:65], in1=xr[a][:],
                                                                 op0=ALU.mult, op1=ALU.add), reads=[gB_[a], rB, xrB[a]], writes=[xrB[a]])
                    S.op("dve", lambda e: e.scalar_tensor_tensor(out=xr[a][:], in0=g2_[a][:], scalar=Rs[:, i, 65:66], in1=xr[a][:],
                                                                 op0=ALU.mult, op1=ALU.add), reads=[gB_[a], rB, xrB[a]], writes=[xrB[a]])
                    S.op("act", lambda e: e.activation(out=fj[:], in_=xr[a][:], func=AF.Square, accum_out=fst[a][:, 0:1]),
                         reads=[xrB[a]], writes=[fjB, xrB[a]])
                    S.op("dve", lambda e: e.tensor_scalar(out=fst[a][:, 1:2], in0=fst[a][:, 0:1], scalar1=1.0 / D, scalar2=1e-6,
                                                          op0=ALU.mult, op1=ALU.add), reads=[xrB[a]], writes=[xrB[a]])
                    S.op("act", lambda e: e.activation(out=fst[a][:, 3:4], in_=fst[a][:, 1:2], func=AF.Sqrt), reads=[xrB[a]], writes=[xrB[a]])
                    S.op("dve", lambda e: e.reciprocal(out=fst[a][:, 2:3], in_=fst[a][:, 3:4]), reads=[xrB[a]], writes=[xrB[a]])
                    S.op("dve", lambda e: e.scalar_tensor_tensor(out=xr[a][:], in0=xr[a][:], scalar=fst[a][:, 2:3], in1=gfin[:],
                                                                 op0=ALU.mult, op1=ALU.mult), reads=[xrB[a], fB0], writes=[xrB[a]])
                    S.dma("sp", out[i * 128:(i + 1) * 128, :], xr[a][:], reads=[xrB[a]])
                S.barrier()
        S.barrier()
    return nc


def _consts():
    i = np.arange(64, dtype=np.float32)
    inv = (1.0 / (10000.0 ** (np.arange(0, 128, 2, dtype=np.float32) / 128.0))).astype(np.float32)
    c_rope = np.zeros((128, 2), np.float32)
    c_rope[:, 0] = np.concatenate([inv, inv])
    c_rope[:64, 1] = -1.0
    c_rope[64:, 1] = 1.0
    c_swap = np.zeros((128, 128), np.float32)
    for m in range(128):
        c_swap[(m + 64) % 128, m] = 1.0
    c_ident = np.eye(128, dtype=np.float32)
    kl = np.arange(128)[:, None, None] + 128 * np.arange(4)[None, :, None]
    ql = np.arange(512)[None, None, :]
    c_tri = (kl <= ql).astype(np.float32)
    c_ls = (np.arange(128)[:, None] < np.arange(128)[None, :]).astype(np.float32)
    c_iog = (np.arange(16)[None, :] * 128 + np.arange(128)[:, None]).astype(np.float32)
    c_iod = (np.arange(8)[None, :] * 128 + np.arange(128)[:, None]).astype(np.float32)
    return dict(c_rope=c_rope, c_swap=c_swap, c_ident=c_ident, c_tri=np.ascontiguousarray(c_tri),
                c_ls=c_ls, c_iog=np.ascontiguousarray(c_iog), c_iod=np.ascontiguousarray(c_iod))


def make_in_maps(inputs):
    x = np.asarray(inputs["x"], np.float32)
    pos = np.asarray(inputs["positions"], np.int32)
    f = lambda k: np.ascontiguousarray(np.asarray(inputs[k], np.float32)[0])
    pk = lambda v: np.ascontiguousarray(v.reshape(-1, 128).T)
    shared = dict(
        g_mix=pk(f("g_mix")), w_in=f("w_in"), conv_w=np.ascontiguousarray(f("conv_w").reshape(4, KC, 128).transpose(2, 0, 1)), conv_b=pk(f("conv_b")),
        w_rg_a=f("w_rg_a"), b_rg_a=pk(f("b_rg_a")), w_rg_x=f("w_rg_x"), b_rg_x=pk(f("b_rg_x")),
        lru_param=pk(f("lru_param")),
        lam_in=np.ascontiguousarray(np.stack([f("lambda_q1"), f("lambda_k1"), f("lambda_q2"), f("lambda_k2")])),
        subln_g=f("subln_g"), w_br_rnn=f("w_br_rnn"), w_br_attn=f("w_br_attn"), w_out=f("w_out"),
        g_ffn=pk(f("g_ffn")),
        w_router=np.ascontiguousarray(np.concatenate([f("w_grp_router"), f("w_exp_router")], axis=1)),
        w_gate=f("w_gate"), w_up=f("w_up"), w_down=f("w_down"),
        g_final=np.ascontiguousarray(np.asarray(inputs["g_final"], np.float32)),
    )
    shared.update(_consts())
    maps = []
    for b in range(2):
        for j in range(4):
            n = (j + 1) * 2048
            xfp = np.zeros((T, D), np.float32)
            xfp[T - n:] = x[b, :n]
            pp = np.zeros((T,), np.int32)
            pp[T - n:] = pos[b, :n]
            vv = np.zeros((T,), np.float32)
            vv[T - n:] = 1.0
            m = dict(shared)
            m.update(xf=xfp, posf=pp, valid=vv, valid_pk=pk(vv))
            maps.append(m)
    return maps


def kernel(**inputs):
    nc = build()
    maps = make_in_maps(inputs)
    res = run_bass_kernel_spmd(nc, maps, core_ids=list(range(8)))
    outp = np.zeros((2, 8192, D), np.float32)
    for c in range(8):
        b, j = c // 4, c % 4
        outp[b, j * 2048:(j + 1) * 2048] = res.results[c]["out"]
    return outp
```

```python
import math
import contextlib
import numpy as np
import concourse.bass as bass
import concourse.mybir as mybir
from concourse.bass_utils import run_bass_kernel_spmd

F32 = mybir.dt.float32
BF16 = mybir.dt.bfloat16
I32 = mybir.dt.int32
ALU = mybir.AluOpType
AF = mybir.ActivationFunctionType
AX = mybir.AxisListType

D = 2048
T = 8192
OWN0 = 6144
NOWN = 2048
KC = 16
NE = 32
DE = 1024
NH = 8
TWO_PI = 2.0 * math.pi


class Buf:
    __slots__ = ("w", "r")

    def __init__(self):
        self.w = None
        self.r = {}


class Sched:
    def __init__(self, nc, es):
        self.nc = nc
        self.st = {}
        for name, h, nd in [("pe", nc.tensor, 0), ("dve", nc.vector, 0), ("act", nc.scalar, 6),
                            ("pool", nc.gpsimd, 14), ("sp", nc.sync, 10)]:
            st = dict(h=h, cnt=0, seen={}, dcnt=0, name=name)
            st["sem"] = es.enter_context(nc.semaphore("c_" + name))
            st["dsems"] = [es.enter_context(nc.semaphore(f"d_{name}{i}")) for i in range(nd)]
            self.st[name] = st

    @staticmethod
    def _add(d, ev):
        if ev is None:
            return
        sem, val, key = ev
        if key not in d or d[key][1] < val:
            d[key] = (sem, val, key)

    def _deps(self, reads, writes):
        d = {}
        for b in reads:
            self._add(d, b.w)
        for b in writes:
            self._add(d, b.w)
            for ev in b.r.values():
                self._add(d, ev)
        return d

    def _wait(self, st, d, skip=None):
        for key, (sem, val, _) in d.items():
            if key == skip:
                continue
            if st["seen"].get(key, 0) >= val:
                continue
            st["h"].wait_ge(sem, val)
            st["seen"][key] = val

    def _mark(self, reads, writes, ev):
        for b in reads:
            self._add(b.r, ev)
        for b in writes:
            b.w = ev
            b.r = {}

    def op(self, stname, fn, reads=(), writes=()):
        st = self.st[stname]
        d = self._deps(reads, writes)
        self._wait(st, d, skip=("c_pe" if stname == "pe" else None))
        ins = fn(st["h"])
        st["cnt"] += 1
        ins.then_inc(st["sem"], 1)
        self._mark(reads, writes, (st["sem"], st["cnt"], "c_" + stname))

    def dma(self, stname, out, in_, reads=(), writes=()):
        st = self.st[stname]
        d = self._deps(reads, writes)
        i = st["dcnt"]
        R = len(st["dsems"])
        k = i % R
        sem = st["dsems"][k]
        val = 16 * (i // R + 1)
        key = f"d_{stname}{k}"
        if i >= R:
            self._add(d, (sem, val - 16, key))
        self._wait(st, d)
        st["h"].dma_start(out=out, in_=in_).then_inc(sem, 16)
        st["dcnt"] += 1
        self._mark(reads, writes, (sem, val, key))

    def idma(self, out, out_off, in_, in_off, reads=(), writes=()):
        st = self.st["pool"]
        d = self._deps(reads, writes)
        i = st["dcnt"]
        R = len(st["dsems"])
        k = i % R
        sem = st["dsems"][k]
        val = 16 * (i // R + 1)
        key = f"d_pool{k}"
        if i >= R:
            self._add(d, (sem, val - 16, key))
        self._wait(st, d)
        st["h"].indirect_dma_start(out=out, out_offset=out_off, in_=in_, in_offset=in_off).then_inc(sem, 16)
        st["dcnt"] += 1
        self._mark(reads, writes, (sem, val, key))

    def all_events(self):
        d = {}
        for name, st in self.st.items():
            if st["cnt"] > 0:
                self._add(d, (st["sem"], st["cnt"], "c_" + name))
            R = len(st["dsems"])
            for k in range(R):
                n = (st["dcnt"] - k + R - 1) // R if st["dcnt"] > k else 0
                if n > 0:
                    self._add(d, (st["dsems"][k], 16 * n, f"d_{name}{k}"))
        return d

    def barrier(self, only=None):
        d = self.all_events()
        for name, st in self.st.items():
            if only is not None and name not in only:
                continue
            self._wait(st, d)


def build(upto=99, debug=(), sparse=True):
    nc = bass.Bass("TRN2", target_bir_lowering=False)
    dbg = set(debug)

    def dram_in(name, shape, dt=F32):
        return nc.dram_tensor(name, list(shape), dt, kind="ExternalInput").ap()

    def dram_tmp(name, shape, dt):
        kind = "ExternalOutput" if name in dbg else "Internal"
        return nc.dram_tensor(name, list(shape), dt, kind=kind).ap()

    xf = dram_in("xf", [T, D])
    posf = dram_in("posf", [T], I32)
    valid = dram_in("valid", [T])
    g_mix = dram_in("g_mix", [128, KC])
    w_in = dram_in("w_in", [D, 14336])
    conv_w = dram_in("conv_w", [128, 4, KC])
    conv_b = dram_in("conv_b", [128, KC])
    w_rg_a = dram_in("w_rg_a", [16, 128, 128])
    b_rg_a = dram_in("b_rg_a", [128, KC])
    w_rg_x = dram_in("w_rg_x", [16, 128, 128])
    b_rg_x = dram_in("b_rg_x", [128, KC])
    lru_param = dram_in("lru_param", [128, KC])
    lam_in = dram_in("lam_in", [4, 128])
    subln_g = dram_in("subln_g", [256])
    w_br_rnn = dram_in("w_br_rnn", [D, D])
    w_br_attn = dram_in("w_br_attn", [D, D])
    w_out = dram_in("w_out", [D, D])
    g_ffn = dram_in("g_ffn", [128, KC])
    valid_pk = dram_in("valid_pk", [128, 64])
    w_router = dram_in("w_router", [D, 36])
    w_gate = dram_in("w_gate", [NE, D, DE])
    w_up = dram_in("w_up", [NE, D, DE])
    w_down = dram_in("w_down", [NE, DE, D])
    g_final = dram_in("g_final", [D])
    c_rope = dram_in("c_rope", [128, 2])
    c_swap = dram_in("c_swap", [128, 128])
    c_ident = dram_in("c_ident", [128, 128])
    c_tri = dram_in("c_tri", [128, 4, 512])
    c_ls = dram_in("c_ls", [128, 128])
    c_iog = dram_in("c_iog", [128, 16])
    c_iod = dram_in("c_iod", [128, 8])
    out = nc.dram_tensor("out", [NOWN, D], F32, kind="ExternalOutput").ap()

    hT_d = dram_tmp("hT_d", [16, 128, KC, 512], BF16)
    uT_d = dram_tmp("uT_d", [D, T], F32)
    KT_d = dram_tmp("KT_d", [16, 128, T], BF16)
    V_d = dram_tmp("V_d", [T, D], BF16)
    gbT_d = dram_tmp("gbT_d", [D, NOWN], BF16)
    QT_d = dram_tmp("QT_d", [16, 128, NOWN], BF16)
    sg_d = dram_tmp("sg_d", [2, D, NOWN], BF16)
    cos_d = dram_tmp("cos_d", [16, 128, 512], F32)
    sin_d = dram_tmp("sin_d", [16, 128, 512], F32)
    yrT_d = dram_tmp("yrT_d", [4, 128, KC, 512], BF16)
    yaT_d = dram_tmp("yaT_d", [4, 128, KC, 512], BF16)
    m1_d = dram_tmp("m1_d", [D, NOWN], BF16)
    mT_d = dram_tmp("mT_d", [4, 128, KC, 512], BF16)
    x2_d = dram_tmp("x2_d", [NOWN, D], F32)
    hnT_d = dram_tmp("hnT_d", [4, 128, KC, 512], BF16)
    C_d = dram_tmp("C_d", [NOWN, NE], F32)
    R_d = dram_tmp("R_d", [NOWN, 66], F32)
    hn_d = dram_tmp("hn_d", [NOWN, D], BF16)
    xs_d = dram_tmp("xs_d", [8192, D], BF16)
    ys_d = dram_tmp("ys_d", [8192, D], BF16)

    with contextlib.ExitStack() as es:
        S = Sched(nc, es)

        uid = [0]

        def sb(name, shape, dt, stack=es):
            uid[0] += 1
            return stack.enter_context(nc.sbuf_tensor(f"{name}_{uid[0]}", list(shape), dt))

        PS = [es.enter_context(nc.psum_tensor(f"ps{i}", [128, 512], F32)) for i in range(8)]
        PSB = [Buf() for _ in range(8)]

        ident_f = sb("ident_f", [128, 128], F32)
        ident_b = sb("ident_b", [128, 128], BF16)
        swap_f = sb("swap_f", [128, 128], F32)
        swap_b = sb("swap_b", [128, 128], BF16)
        rope_c = sb("rope_c", [128, 2], F32)
        gmix_s = sb("gmix_s", [128, KC], F32)
        gffn_s = sb("gffn_s", [128, KC], F32)
        cst = Buf()
        S.dma("sp", ident_f[:], c_ident[:, :], writes=[cst])
        S.dma("sp", swap_f[:], c_swap[:, :], writes=[cst])
        S.dma("sp", rope_c[:], c_rope[:, :], writes=[cst])
        S.dma("sp", gmix_s[:], g_mix[:, :], writes=[cst])
        S.dma("sp", gffn_s[:], g_ffn[:, :], writes=[cst])
        S.op("dve", lambda e: e.tensor_copy(out=ident_b[:], in_=ident_f[:]), reads=[cst], writes=[cst])
        S.op("dve", lambda e: e.tensor_copy(out=swap_b[:], in_=swap_f[:]), reads=[cst], writes=[cst])
        S.barrier()

        def norm_transpose(src, row0, nblk, dst_d, eps, tm_dst=None):
            with contextlib.ExitStack() as ls:
                xt = [sb(f"nt_x{i}", [128, D], F32, ls) for i in range(2)]
                xtB = [Buf() for _ in range(2)]
                junk = sb("nt_junk", [128, D], BF16, ls)
                junkB = Buf()
                xn = [sb(f"nt_xn{i}", [128, D], BF16, ls) for i in range(2)]
                xnB = [Buf() for _ in range(2)]
                st_ = [sb(f"nt_s{i}", [128, 4], F32, ls) for i in range(2)]
                stB = [Buf() for _ in range(2)]
                hb = [sb(f"nt_h{i}", [128, KC, 512], BF16, ls) for i in range(2)]
                hbB = [Buf() for _ in range(2)]
                it = 0
                for blk in range(nblk):
                    h = hb[blk % 2]
                    hB = hbB[blk % 2]
                    for s in range(4):
                        i2 = it % 2
                        r0 = row0 + (blk * 4 + s) * 128
                        S.dma("sp", xt[i2][:], src[r0:r0 + 128, :], writes=[xtB[i2]])
                        S.op("act", lambda e: e.activation(out=junk[:], in_=xt[i2][:], func=AF.Square,
                                                           accum_out=st_[i2][:, 0:1]),
                             reads=[xtB[i2]], writes=[junkB, stB[i2]])
                        S.op("dve", lambda e: e.tensor_scalar(out=st_[i2][:, 1:2], in0=st_[i2][:, 0:1],
                                                              scalar1=1.0 / D, scalar2=eps,
                                                              op0=ALU.mult, op1=ALU.add),
                             reads=[stB[i2]], writes=[stB[i2]])
                        S.op("act", lambda e: e.activation(out=st_[i2][:, 3:4], in_=st_[i2][:, 1:2], func=AF.Sqrt),
                             reads=[stB[i2]], writes=[stB[i2]])
                        S.op("dve", lambda e: e.reciprocal(out=st_[i2][:, 2:3], in_=st_[i2][:, 3:4]),
                             reads=[stB[i2]], writes=[stB[i2]])
                        S.op("dve", lambda e: e.tensor_scalar(out=xn[i2][:], in0=xt[i2][:],
                                                              scalar1=st_[i2][:, 2:3], scalar2=None,
                                                              op0=ALU.mult),
                             reads=[xtB[i2], stB[i2]], writes=[xnB[i2]])
                        if tm_dst is not None:
                            S.dma("pool", tm_dst[r0 - row0:r0 - row0 + 128, :], xn[i2][:], reads=[xnB[i2]])
                        for g4 in range(4):
                            pb = (it * 4 + g4) % 4
                            pv = PS[pb][:].bitcast(BF16)

                            def tr(e, g4=g4, pv=pv, i2=i2):
                                ins = None
                                for q in range(4):
                                    kc = g4 * 4 + q
                                    ins = e.transpose(out=pv[:, q * 128:(q + 1) * 128],
                                                      in_=xn[i2][:, kc * 128:(kc + 1) * 128],
                                                      identity=ident_b[:])
                                return ins
                            S.op("pe", tr, reads=[xnB[i2]], writes=[PSB[pb]])
                            eng = "act" if g4 % 2 == 0 else "dve"

                            def ev(e, g4=g4, pv=pv, s=s, h=h, eng=eng):
                                o = h[:, g4 * 4:(g4 + 1) * 4, s * 128:(s + 1) * 128]
                                i_ = pv[:, 0:512].rearrange("p (k t) -> p k t", k=4)
                                if eng == "act":
                                    return e.copy(out=o, in_=i_)
                                return e.tensor_copy(out=o, in_=i_)
                            S.op(eng, ev, reads=[PSB[pb]], writes=[hB])
                        it += 1
                    S.dma("pool", dst_d[blk], h[:], reads=[hB])
                S.barrier()

        def gemm(ls, AT_d, blks, Wview_fn, ntiles, kc_n, wcols, mode, gvec, epilogue, group=2,
                 resident=None, pbanks=(0, 1, 2, 3)):
            wst = [sb(f"g_wst{i}", [128, 4096], F32, ls) for i in range(2)]
            wstB = [Buf() for _ in range(2)]
            nwb = 2 * group
            wbf = [sb(f"g_wbf{i}", [128, 4096], BF16, ls) for i in range(nwb)]
            wbfB = [Buf() for _ in range(nwb)]
            if resident is None:
                at = [sb(f"g_at{i}", [128, KC * 512], BF16, ls) for i in range(2)]
                atB = [Buf() for _ in range(2)]
            wi = 0
            ai = 0
            pi = 0
            for t0 in range(0, ntiles, group):
                tiles = list(range(t0, min(ntiles, t0 + group)))
                wsl = {}
                for ti in tiles:
                    a = wi % 2
                    b = wi % nwb
                    wv = wst[a][:, 0:kc_n * wcols].rearrange("p (k c) -> p k c", k=kc_n)
                    wsrc = Wview_fn(ti)
                    kq = max(1, kc_n // 4)
                    for k0 in range(0, kc_n, kq):
                        S.dma("sp", wv[:, k0:k0 + kq, :], wsrc[:, k0:k0 + kq, :], writes=[wstB[a]])
                    wb = wbf[b][:, 0:kc_n * wcols].rearrange("p (k c) -> p k c", k=kc_n)
                    ceng = "dve" if wi % 2 == 0 else "act"
                    if gvec is None:
                        if ceng == "dve":
                            S.op("dve", lambda e, a=a, b=b: e.tensor_copy(out=wbf[b][:, 0:kc_n * wcols],
                                                                          in_=wst[a][:, 0:kc_n * wcols]),
                                 reads=[wstB[a]], writes=[wbfB[b]])
                        else:
                            S.op("act", lambda e, a=a, b=b: e.copy(out=wbf[b][:, 0:kc_n * wcols],
                                                                   in_=wst[a][:, 0:kc_n * wcols]),
                                 reads=[wstB[a]], writes=[wbfB[b]])
                    else:
                        def cast(e, wv=wv, wb=wb, ceng=ceng):
                            ins = None
                            for k in range(kc_n):
                                if ceng == "dve":
                                    ins = e.tensor_scalar(out=wb[:, k, :], in0=wv[:, k, :],
                                                          scalar1=gvec[:, k:k + 1], scalar2=None, op0=ALU.mult)
                                else:
                                    ins = e.activation(out=wb[:, k, :], in_=wv[:, k, :], func=AF.Copy,
                                                       scale=gvec[:, k:k + 1])
                            return ins
                        S.op(ceng, cast, reads=[wstB[a]], writes=[wbfB[b]])
                    wsl[ti] = (wb, wbfB[b])
                    wi += 1
                for bi, blk in enumerate(blks):
                    if resident is None:
                        a = ai % 2
                        ai += 1
                        S.dma("sp", at[a][:], AT_d[blk].rearrange("p k t -> p (k t)"), writes=[atB[a]])
                        av = at[a][:].rearrange("p (k t) -> p k t", k=KC)
                        aB = atB[a]
                    else:
                        av, aB = resident[bi]
                    for ti in tiles:
                        wb, wB = wsl[ti]
                        if mode == "fm":
                            for ct in range(wcols // 128):
                                pb = pbanks[pi % len(pbanks)]
                                pi += 1

                                def mm(e, wb=wb, av=av, ct=ct, pb=pb):
                                    ins = None
                                    for k in range(kc_n):
                                        ins = e.matmul(PS[pb][:], lhsT=wb[:, k, ct * 128:(ct + 1) * 128],
                                                       rhs=av[:, k, :], start=(k == 0), stop=(k == kc_n - 1))
                                    return ins
                                S.op("pe", mm, reads=[wB, aB], writes=[PSB[pb]])
                                epilogue(pb, ti, ct, blk, bi)
                        else:
                            for s in range(4):
                                pb = pbanks[pi % len(pbanks)]
                                pi += 1

                                def mm(e, wb=wb, av=av, s=s, pb=pb):
                                    ins = None
                                    for k in range(kc_n):
                                        ins = e.matmul(PS[pb][:, 0:wcols], lhsT=av[:, k, s * 128:(s + 1) * 128],
                                                       rhs=wb[:, k, :], start=(k == 0), stop=(k == kc_n - 1))
                                    return ins
                                S.op("pe", mm, reads=[wB, aB], writes=[PSB[pb]])
                                epilogue(pb, ti, s, blk, bi)

        def w_tiles(Wap, c0, wcols):
            Wv = Wap.rearrange("(k p) n -> p k n", p=128)
            return lambda ti: Wv[:, :, c0 + ti * wcols: c0 + (ti + 1) * wcols]

        if upto >= 0:
            with contextlib.ExitStack() as ls:
                pos_i = sb("pos_i", [128, 512], I32, ls)
                pos_f = sb("pos_f", [128, 512], F32, ls)
                ang = sb("ang", [128, 512], F32, ls)
                a1 = sb("a1", [128, 512], F32, ls)
                a2 = sb("a2", [128, 512], F32, ls)
                tq = sb("tq", [128, 512], F32, ls)
                tB = Buf()
                posv = posf.rearrange("(b t) -> b t", t=512)
                for blk in range(16):
                    S.dma("sp", pos_i[:], posv[blk:blk + 1, :].partition_broadcast(128), writes=[tB])
                    S.op("dve", lambda e: e.tensor_copy(out=pos_f[:], in_=pos_i[:]), reads=[tB], writes=[tB])
                    S.op("dve", lambda e: e.tensor_scalar(out=ang[:], in0=pos_f[:], scalar1=rope_c[:, 0:1],
                                                          scalar2=None, op0=ALU.mult), reads=[tB], writes=[tB])
                    def trig(dst, shift):
                        S.op("dve", lambda e: e.tensor_scalar(out=dst[:], in0=ang[:], scalar1=shift, scalar2=None, op0=ALU.add), reads=[tB], writes=[tB])
                        S.op("dve", lambda e: e.tensor_scalar(out=tq[:], in0=dst[:], scalar1=1.0 / TWO_PI, scalar2=None, op0=ALU.mult), reads=[tB], writes=[tB])
                        S.op("dve", lambda e: e.tensor_copy(out=pos_i[:], in_=tq[:]), reads=[tB], writes=[tB])
                        S.op("dve", lambda e: e.tensor_copy(out=tq[:], in_=pos_i[:]), reads=[tB], writes=[tB])
                        S.op("dve", lambda e: e.scalar_tensor_tensor(out=dst[:], in0=tq[:], scalar=-TWO_PI, in1=dst[:], op0=ALU.mult, op1=ALU.add), reads=[tB], writes=[tB])
                        S.op("dve", lambda e: e.tensor_scalar(out=tq[:], in0=dst[:], scalar1=math.pi, scalar2=-TWO_PI, op0=ALU.is_gt, op1=ALU.mult), reads=[tB], writes=[tB])
                        S.op("dve", lambda e: e.tensor_tensor(out=dst[:], in0=dst[:], in1=tq[:], op=ALU.add), reads=[tB], writes=[tB])
                        S.op("dve", lambda e: e.tensor_scalar(out=tq[:], in0=dst[:], scalar1=-math.pi, scalar2=TWO_PI, op0=ALU.is_lt, op1=ALU.mult), reads=[tB], writes=[tB])
                        S.op("dve", lambda e: e.tensor_tensor(out=dst[:], in0=dst[:], in1=tq[:], op=ALU.add), reads=[tB], writes=[tB])
                        S.op("dve", lambda e: e.tensor_scalar(out=dst[:], in0=dst[:], scalar1=-math.pi, scalar2=math.pi, op0=ALU.max, op1=ALU.min), reads=[tB], writes=[tB])
                        S.op("act", lambda e: e.activation(out=dst[:], in_=dst[:], func=AF.Sin), reads=[tB], writes=[tB])
                    trig(a1, 0.0)
                    S.op("dve", lambda e: e.tensor_scalar(out=a1[:], in0=a1[:], scalar1=rope_c[:, 1:2], scalar2=None, op0=ALU.mult), reads=[tB], writes=[tB])
                    S.dma("pool", sin_d[blk], a1[:], reads=[tB])
                    trig(a2, math.pi / 2)
                    S.dma("pool", cos_d[blk], a2[:], reads=[tB])
                S.barrier()

        if upto >= 1:
            norm_transpose(xf, 0, 16, hT_d, 1e-6)

        def rope_epilogue_factory(ls, dst_d, tok_of_blk, scale_cols0):
            cs = [sb(f"rp_c{i}", [128, 512], F32, ls) for i in range(2)]
            sn = [sb(f"rp_s{i}", [128, 512], F32, ls) for i in range(2)]
            csB = [Buf() for _ in range(2)]
            tb_ = [sb(f"rp_t{i}", [128, 512], BF16, ls) for i in range(2)]
            tbB = [Buf() for _ in range(2)]
            o1 = [sb(f"rp_o1{i}", [128, 512], F32, ls) for i in range(2)]
            o2 = [sb(f"rp_o2{i}", [128, 512], F32, ls) for i in range(2)]
            ob = [sb(f"rp_ob{i}", [128, 512], BF16, ls) for i in range(2)]
            oB = [Buf() for _ in range(2)]
            state = dict(n=0, lastblk=None, ci=0)

            def epi(pb, ti, ct, blk, bi):
                n = state["n"]
                state["n"] += 1
                i2 = n % 2
                if state["lastblk"] != blk:
                    state["ci"] += 1
                    c2 = state["ci"] % 2
                    S.dma("sp", cs[c2][:], cos_d[blk], writes=[csB[c2]])
                    S.dma("sp", sn[c2][:], sin_d[blk], writes=[csB[c2]])
                    state["lastblk"] = blk
                c2 = state["ci"] % 2
                hm = scale_cols0 + ti * 2 + ct
                pb2 = 4 + (n % 2)
                S.op("act", lambda e: e.copy(out=tb_[i2][:], in_=PS[pb][:]), reads=[PSB[pb]], writes=[tbB[i2]])
                S.op("pe", lambda e: e.matmul(PS[pb2][:], lhsT=swap_b[:], rhs=tb_[i2][:], start=True, stop=True),
                     reads=[tbB[i2]], writes=[PSB[pb2]])
                S.op("dve", lambda e: e.tensor_tensor(out=o1[i2][:], in0=PS[pb][:], in1=cs[c2][:], op=ALU.mult),
                     reads=[PSB[pb], csB[c2], tbB[i2]], writes=[oB[i2]])
                S.op("dve", lambda e: e.tensor_tensor(out=o2[i2][:], in0=PS[pb2][:], in1=sn[c2][:], op=ALU.mult),
                     reads=[PSB[pb2], csB[c2]], writes=[oB[i2]])
                S.op("dve", lambda e: e.tensor_tensor(out=ob[i2][:], in0=o1[i2][:], in1=o2[i2][:], op=ALU.add),
                     reads=[oB[i2]], writes=[oB[i2]])
                t0 = tok_of_blk(blk)
                S.dma("pool", dst_d[hm, :, t0:t0 + 512], ob[i2][:], reads=[oB[i2]])
            return epi

        ALLB = list(range(16))
        OWNB = [12, 13, 14, 15]
        if upto >= 2:
            with contextlib.ExitStack() as ls:
                ut = [sb(f"e_u{i}", [128, 512], F32, ls) for i in range(3)]
                utB = [Buf() for _ in range(3)]
                cnt = [0]

                def epi_u(pb, ti, ct, blk, bi):
                    i3 = cnt[0] % 3
                    cnt[0] += 1
                    r0 = ti * 256 + ct * 128
                    S.op("act", lambda e: e.copy(out=ut[i3][:], in_=PS[pb][:]), reads=[PSB[pb]], writes=[utB[i3]])
                    S.dma("pool", uT_d[r0:r0 + 128, blk * 512:(blk + 1) * 512], ut[i3][:], reads=[utB[i3]])
                gemm(ls, hT_d, ALLB, w_tiles(w_in, 0, 256), 8, KC, 256, "fm", gmix_s, epi_u)
                S.barrier()
        if upto >= 3:
            with contextlib.ExitStack() as ls:
                epi_k = rope_epilogue_factory(ls, KT_d, lambda blk: blk * 512, 0)
                gemm(ls, hT_d, ALLB, w_tiles(w_in, 3 * D, 256), 8, KC, 256, "fm", gmix_s, epi_k)
                S.barrier()
            with contextlib.ExitStack() as ls:
                epi_q = rope_epilogue_factory(ls, QT_d, lambda blk: (blk - 12) * 512, 0)
                gemm(ls, hT_d, OWNB, w_tiles(w_in, 2 * D, 256), 8, KC, 256, "fm", gmix_s, epi_q)
                S.barrier()
        if upto >= 4:
            with contextlib.ExitStack() as ls:
                vt = [sb(f"e_v{i}", [128, 256], BF16, ls) for i in range(3)]
                vtB = [Buf() for _ in range(3)]
                cnt = [0]

                def epi_v(pb, ti, s, blk, bi):
                    i3 = cnt[0] % 3
                    cnt[0] += 1
                    r0 = blk * 512 + s * 128
                    eng = "act" if cnt[0] % 2 else "dve"
                    if eng == "act":
                        S.op("act", lambda e: e.copy(out=vt[i3][:], in_=PS[pb][:, 0:256]), reads=[PSB[pb]], writes=[vtB[i3]])
                    else:
                        S.op("dve", lambda e: e.tensor_copy(out=vt[i3][:], in_=PS[pb][:, 0:256]), reads=[PSB[pb]], writes=[vtB[i3]])
                    S.dma("pool", V_d[r0:r0 + 128, ti * 256:(ti + 1) * 256], vt[i3][:], reads=[vtB[i3]])
                gemm(ls, hT_d, ALLB, w_tiles(w_in, 4 * D, 256), 8, KC, 256, "tm", gmix_s, epi_v)
                S.barrier()
        if upto >= 5:
            with contextlib.ExitStack() as ls:
                xs = [sb(f"e_x{i}", [128, 512], F32, ls) for i in range(2)]
                t1 = [sb(f"e_t{i}", [128, 512], F32, ls) for i in range(2)]
                ob = [sb(f"e_o{i}", [128, 512], BF16, ls) for i in range(2)]
                eB = [Buf() for _ in range(2)]
                cnt = [0]

                def epi_gelu(pb, ti, ct, blk, bi):
                    i2 = cnt[0] % 2
                    cnt[0] += 1
                    r0 = ti * 256 + ct * 128
                    t0 = (blk - 12) * 512
                    S.op("act", lambda e: e.copy(out=xs[i2][:], in_=PS[pb][:]), reads=[PSB[pb]], writes=[eB[i2]])
                    S.op("dve", lambda e: e.tensor_tensor(out=t1[i2][:], in0=xs[i2][:], in1=xs[i2][:], op=ALU.mult),
                         reads=[eB[i2]], writes=[eB[i2]])
                    S.op("dve", lambda e: e.tensor_scalar(out=t1[i2][:], in0=t1[i2][:], scalar1=0.044715, scalar2=1.0,
                                                          op0=ALU.mult, op1=ALU.add), reads=[eB[i2]], writes=[eB[i2]])
                    S.op("dve", lambda e: e.tensor_tensor(out=t1[i2][:], in0=t1[i2][:], in1=xs[i2][:], op=ALU.mult),
                         reads=[eB[i2]], writes=[eB[i2]])
                    S.op("act", lambda e: e.activation(out=t1[i2][:], in_=t1[i2][:], func=AF.Sigmoid,
                                                       scale=2.0 * math.sqrt(2.0 / math.pi)),
                         reads=[eB[i2]], writes=[eB[i2]])
                    S.op("dve", lambda e: e.tensor_tensor(out=ob[i2][:], in0=t1[i2][:], in1=xs[i2][:], op=ALU.mult),
                         reads=[eB[i2]], writes=[eB[i2]])
                    S.dma("pool", gbT_d[r0:r0 + 128, t0:t0 + 512], ob[i2][:], reads=[eB[i2]])
                gemm(ls, hT_d, OWNB, w_tiles(w_in, D, 256), 8, KC, 256, "fm", gmix_s, epi_gelu)
                S.barrier()
            with contextlib.ExitStack() as ls:
                ob = [sb(f"e_o{i}", [128, 512], BF16, ls) for i in range(3)]
                eB = [Buf() for _ in range(3)]
                cnt = [0]

                def epi_sig(pb, ti, ct, blk, bi):
                    i3 = cnt[0] % 3
                    cnt[0] += 1
                    col = ti * 256 + ct * 128
                    which, r0 = col // D, col % D
                    t0 = (blk - 12) * 512
                    S.op("act", lambda e: e.activation(out=ob[i3][:], in_=PS[pb][:], func=AF.Sigmoid),
                         reads=[PSB[pb]], writes=[eB[i3]])
                    S.dma("pool", sg_d[which, r0:r0 + 128, t0:t0 + 512], ob[i3][:], reads=[eB[i3]])
                gemm(ls, hT_d, OWNB, w_tiles(w_in, 5 * D, 256), 16, KC, 256, "fm", gmix_s, epi_sig)
                S.barrier()

        if upto >= 6:
            with contextlib.ExitStack() as ls:
                CH = 2048
                nmb = sb("l_nm", [128, T], BF16, ls)
                vb = sb("l_vb", [128, T], BF16, ls)
                tmpf = sb("l_tmpf", [128, CH], F32, ls)
                tmpi = sb("l_tmpi", [128, CH], I32, ls)
                mB = Buf()
                for q in range(4):
                    sl = slice(q * CH, (q + 1) * CH)
                    S.dma("sp", tmpf[:], valid.rearrange("(o t) -> o t", o=1)[0:1, sl].partition_broadcast(128), writes=[mB])
                    S.op("dve", lambda e: e.tensor_copy(out=vb[:, sl], in_=tmpf[:]), reads=[mB], writes=[mB])
                    S.dma("sp", tmpi[:], posf.rearrange("(o t) -> o t", o=1)[0:1, sl].partition_broadcast(128), writes=[mB])
                    S.op("dve", lambda e: e.tensor_copy(out=tmpf[:], in_=tmpi[:]), reads=[mB], writes=[mB])
                    S.op("dve", lambda e: e.tensor_scalar(out=tmpf[:], in0=tmpf[:], scalar1=0.0, scalar2=None,
                                                          op0=ALU.not_equal), reads=[mB], writes=[mB])
                    S.op("dve", lambda e: e.tensor_tensor(out=nmb[:, sl], in0=tmpf[:], in1=vb[:, sl], op=ALU.mult),
                         reads=[mB], writes=[mB])
                cw = sb("l_cw", [128, 4, KC], F32, ls)
                cbs = sb("l_cb", [128, KC], F32, ls)
                bas = sb("l_ba", [128, KC], F32, ls)
                bxs = sb("l_bx", [128, KC], F32, ls)
                lps = sb("l_lp", [128, KC], F32, ls)
                nc8 = sb("l_nc8", [128, KC], F32, ls)
                pB = Buf()
                S.dma("sp", cw[:], conv_w[:, :, :], writes=[pB])
                S.dma("sp", cbs[:], conv_b[:, :], writes=[pB])
                S.dma("sp", bas[:], b_rg_a[:, :], writes=[pB])
                S.dma("sp", bxs[:], b_rg_x[:, :], writes=[pB])
                S.dma("sp", lps[:], lru_param[:, :], writes=[pB])
                S.op("act", lambda e: e.activation(out=nc8[:], in_=lps[:], func=AF.Exp, scale=-1.0), reads=[pB], writes=[pB])
                S.op("act", lambda e: e.activation(out=nc8[:], in_=nc8[:], func=AF.Ln, bias=1.0), reads=[pB], writes=[pB])
                S.op("dve", lambda e: e.tensor_scalar(out=nc8[:], in0=nc8[:], scalar1=-8.0, scalar2=None, op0=ALU.mult),
                     reads=[pB], writes=[pB])
                waf = sb("l_waf", [128, 2, 128], F32, ls)
                wab = [sb(f"l_wab{i}", [128, 2, 128], BF16, ls) for i in range(2)]
                wB = [Buf() for _ in range(2)]
                wfB = Buf()
                u = [sb(f"l_u{i}", [128, 3 + CH], F32, ls) for i in range(2)]
                uB = [Buf() for _ in range(2)]
                uc_ = [sb(f"l_uc{i}", [128, CH], F32, ls) for i in range(2)]
                ucb_ = [sb(f"l_ucb{i}", [128, CH], BF16, ls) for i in range(2)]
                rr_ = [sb(f"l_r{i}", [128, CH], F32, ls) for i in range(2)]
                ii_ = [sb(f"l_i{i}", [128, CH], F32, ls) for i in range(2)]
                aa_ = [sb(f"l_a{i}", [128, CH], F32, ls) for i in range(2)]
                mm__ = [sb(f"l_m{i}", [128, CH], F32, ls) for i in range(2)]
                wkB_ = [Buf() for _ in range(2)]
                hh = [sb(f"l_h{i}", [128, CH], F32, ls) for i in range(2)]
                hB = [Buf() for _ in range(2)]
                gbt = sb("l_gb", [128, NOWN], BF16, ls)
                yb = sb("l_y", [128, NOWN], BF16, ls)
                gB = Buf()
                yB = Buf()
                n = 0
                yr_v = yrT_d.rearrange("b p k t -> p k b t")
                for c in range(16):
                    w2 = c % 2
                    S.dma("sp", waf[:, 0, :], w_rg_a[c], writes=[wfB])
                    S.dma("sp", waf[:, 1, :], w_rg_x[c], writes=[wfB])
                    S.op("dve", lambda e: e.tensor_copy(out=wab[w2][:], in_=waf[:]), reads=[wfB], writes=[wB[w2]])
                    S.dma("sp", gbt[:], gbT_d[c * 128:(c + 1) * 128, :], writes=[gB])
                    for q in range(4):
                        i2 = n % 2
                        n += 1
                        t0 = q * CH
                        if q == 0:
                            S.op("dve", lambda e: e.memset(u[i2][:, 0:3], 0.0), writes=[uB[i2]])
                            S.dma("sp", u[i2][:, 3:3 + CH], uT_d[c * 128:(c + 1) * 128, 0:CH], writes=[uB[i2]])
                        else:
                            S.dma("sp", u[i2][:], uT_d[c * 128:(c + 1) * 128, t0 - 3:t0 + CH], writes=[uB[i2]])
                        uu = u[i2]
                        uc, ucb, rr, ii, aa, mm_, wkB = uc_[i2], ucb_[i2], rr_[i2], ii_[i2], aa_[i2], mm__[i2], wkB_[i2]
                        S.op("dve", lambda e: e.tensor_scalar(out=uc[:], in0=uu[:, 3:3 + CH], scalar1=cw[:, 3, c:c + 1],
                                                              scalar2=cbs[:, c:c + 1], op0=ALU.mult, op1=ALU.add),
                             reads=[uB[i2], pB], writes=[wkB])
                        for j in range(3):
                            S.op("dve", lambda e, j=j: e.scalar_tensor_tensor(out=uc[:], in0=uu[:, j:j + CH],
                                                                              scalar=cw[:, j, c:c + 1], in1=uc[:],
                                                                              op0=ALU.mult, op1=ALU.add),
                                 reads=[uB[i2], pB, wkB], writes=[wkB])
                        S.op("dve", lambda e: e.tensor_tensor(out=uc[:], in0=uc[:], in1=vb[:, t0:t0 + CH], op=ALU.mult),
                             reads=[wkB, mB], writes=[wkB])
                        S.op("act", lambda e: e.copy(out=ucb[:], in_=uc[:]), reads=[wkB], writes=[wkB])
                        for sblk in range(CH // 512):
                            ssl = slice(sblk * 512, (sblk + 1) * 512)
                            pa, px = (sblk * 2) % 4 + 4 * i2, (sblk * 2 + 1) % 4 + 4 * i2
                            S.op("pe", lambda e: e.matmul(PS[pa][:], lhsT=wab[w2][:, 0, :], rhs=ucb[:, ssl], start=True, stop=True),
                                 reads=[wB[w2], wkB], writes=[PSB[pa]])
                            S.op("pe", lambda e: e.matmul(PS[px][:], lhsT=wab[w2][:, 1, :], rhs=ucb[:, ssl], start=True, stop=True),
                                 reads=[wB[w2], wkB], writes=[PSB[px]])
                            S.op("act", lambda e: e.activation(out=rr[:, ssl], in_=PS[pa][:], func=AF.Sigmoid, bias=bas[:, c:c + 1]),
                                 reads=[PSB[pa], pB], writes=[wkB])
                            S.op("act", lambda e: e.activation(out=ii[:, ssl], in_=PS[px][:], func=AF.Sigmoid, bias=bxs[:, c:c + 1]),
                                 reads=[PSB[px], pB], writes=[wkB])
                        S.op("act", lambda e: e.activation(out=aa[:], in_=rr[:], func=AF.Exp, scale=nc8[:, c:c + 1]),
                             reads=[wkB, pB], writes=[wkB])
                        S.op("dve", lambda e: e.tensor_tensor(out=aa[:], in0=aa[:], in1=nmb[:, t0:t0 + CH], op=ALU.mult),
                             reads=[wkB, mB], writes=[wkB])
                        S.op("act", lambda e: e.activation(out=mm_[:], in_=aa[:], func=AF.Square), reads=[wkB], writes=[wkB])
                        S.op("act", lambda e: e.activation(out=mm_[:], in_=mm_[:], func=AF.Sqrt, scale=-1.0, bias=1.0),
                             reads=[wkB], writes=[wkB])
                        S.op("dve", lambda e: e.tensor_tensor(out=ii[:], in0=ii[:], in1=uc[:], op=ALU.mult), reads=[wkB], writes=[wkB])
                        S.op("dve", lambda e: e.tensor_tensor(out=ii[:], in0=ii[:], in1=mm_[:], op=ALU.mult), reads=[wkB], writes=[wkB])
                        hprev = hh[(i2 + 1) % 2]
                        init = 0.0 if q == 0 else hprev[:, CH - 1:CH]
                        S.op("dve", lambda e: e.tensor_tensor_scan(out=hh[i2][:], data0=aa[:], data1=ii[:], initial=init,
                                                                   op0=ALU.mult, op1=ALU.add),
                             reads=[wkB, hB[(i2 + 1) % 2]], writes=[hB[i2]])
                        if q == 3:
                            S.op("dve", lambda e: e.tensor_tensor(out=yb[:], in0=hh[i2][:], in1=gbt[:], op=ALU.mult),
                                 reads=[hB[i2], gB], writes=[yB])
                            S.dma("pool", yr_v[:, c], yb[:].rearrange("p (b t) -> p b t", b=4), reads=[yB])
                S.barrier()

        if upto >= 7:
            with contextlib.ExitStack() as ls:
                SCALE = 128 ** -0.5
                lamv = sb("a_lamv", [128, 4, 128], F32, ls)
                lams = sb("a_lams", [128, 8], F32, ls)
                lB = Buf()
                S.dma("sp", lamv[:].rearrange("p a b -> p (a b)"),
                      lam_in.rearrange("(o a) b -> o (a b)", o=1).partition_broadcast(128), writes=[lB])
                S.op("dve", lambda e: e.tensor_tensor(out=lamv[:, 0, :], in0=lamv[:, 0, :], in1=lamv[:, 1, :], op=ALU.mult), reads=[lB], writes=[lB])
                S.op("dve", lambda e: e.tensor_tensor(out=lamv[:, 2, :], in0=lamv[:, 2, :], in1=lamv[:, 3, :], op=ALU.mult), reads=[lB], writes=[lB])
                S.op("dve", lambda e: e.reduce_sum(out=lams[:, 0:1], in_=lamv[:, 0, :], axis=AX.X), reads=[lB], writes=[lB])
                S.op("dve", lambda e: e.reduce_sum(out=lams[:, 1:2], in_=lamv[:, 2, :], axis=AX.X), reads=[lB], writes=[lB])
                S.op("act", lambda e: e.activation(out=lams[:, 2:4], in_=lams[:, 0:2], func=AF.Exp), reads=[lB], writes=[lB])
                S.op("dve", lambda e: e.tensor_tensor(out=lams[:, 4:5], in0=lams[:, 3:4], in1=lams[:, 2:3], op=ALU.subtract), reads=[lB], writes=[lB])
                S.op("dve", lambda e: e.tensor_scalar(out=lams[:, 5:6], in0=lams[:, 4:5], scalar1=-0.2, scalar2=None, op0=ALU.add), reads=[lB], writes=[lB])
                neglam = lams[:, 5:6]
                sg_t = sb("a_sg", [128, 256], F32, ls)
                S.dma("sp", sg_t[:], subln_g.rearrange("(o t) -> o t", o=1).partition_broadcast(128), writes=[lB])
                S.op("dve", lambda e: e.tensor_scalar(out=sg_t[:], in0=sg_t[:], scalar1=0.8, scalar2=None, op0=ALU.mult), reads=[lB], writes=[lB])
                kb = sb("a_kb", [128, 64], F32, ls)
                S.dma("sp", kb[:], valid_pk[:, :], writes=[lB])
                S.op("dve", lambda e: e.tensor_scalar(out=kb[:], in0=kb[:], scalar1=-1.0, scalar2=30000.0, op0=ALU.add, op1=ALU.mult), reads=[lB], writes=[lB])
                trf = sb("a_trf", [128, 4, 512], F32, ls)
                trb = sb("a_trb", [128, 4, 512], BF16, ls)
                S.dma("sp", trf[:], c_tri[:, :, :], writes=[lB])
                S.op("dve", lambda e: e.tensor_copy(out=trb[:], in_=trf[:]), reads=[lB], writes=[lB])

                Va = [sb(f"a_V{i}", [128, 64, 257], BF16, ls) for i in range(2)]
                VaB = [Buf() for _ in range(2)]
                for i in range(2):
                    S.op("dve", lambda e, i=i: e.memset(Va[i][:, :, 256:257], 1.0), writes=[VaB[i]])
                KTs = [sb(f"a_K{i}", [128, T], BF16, ls) for i in range(2)]
                KTB = [Buf() for _ in range(2)]
                QTs = [sb(f"a_Q{i}", [128, NOWN], BF16, ls) for i in range(2)]
                QTB = [Buf() for _ in range(2)]
                Es = [sb(f"a_E{i}", [128, 512], BF16, ls) for i in range(3)]
                EB = [Buf() for _ in range(3)]
                om = [sb(f"a_om{i}", [128, 4, 256], F32, ls) for i in range(2)]
                omB = [Buf() for _ in range(2)]
                rs = sb("a_rs", [128, 8], F32, ls)
                rsB = Buf()
                od = sb("a_od", [128, 4, 256], F32, ls)
                odB = Buf()
                junk = sb("a_junk", [128, 256], F32, ls)
                onb = sb("a_onb", [128, 4, 256], BF16, ls)
                yo = [sb(f"a_yo{i}", [128, 2, 512], BF16, ls) for i in range(2)]
                yoB = [Buf() for _ in range(2)]
                V_v = V_d.rearrange("(k p) c -> p k c", p=128)
                iters = []
                for h in range(NH):
                    for qb in range(4):
                        nkt = 48 + (qb + 1) * 4
                        for m in range(2):
                            for kt in range(nkt):
                                iters.append((h, qb, m, kt, nkt))
                hqc = [0]

                def front(idx):
                    h, qb, m, kt, nkt = iters[idx]
                    vi = h % 2
                    hm = h * 2 + m
                    ki = hm % 2
                    pS = 4 + (idx % 2)
                    e3 = idx % 3
                    if qb == 0 and m == 0 and kt == 0:
                        for k4 in range(16):
                            S.dma("sp", Va[vi][:, k4 * 4:(k4 + 1) * 4, 0:256], V_v[:, k4 * 4:(k4 + 1) * 4, h * 256:(h + 1) * 256],
                                  writes=[VaB[vi]])
                    if qb == 0 and kt == 0:
                        S.dma("sp", KTs[ki][:], KT_d[hm], writes=[KTB[ki]])
                        S.dma("sp", QTs[ki][:], QT_d[hm], writes=[QTB[ki]])
                    S.op("pe", lambda e: e.matmul(PS[pS][:], lhsT=KTs[ki][:, kt * 128:(kt + 1) * 128],
                                                  rhs=QTs[ki][:, qb * 512:(qb + 1) * 512], start=True, stop=True),
                         reads=[KTB[ki], QTB[ki]], writes=[PSB[pS]])
                    S.op("act", lambda e: e.activation(out=Es[e3][:], in_=PS[pS][:], func=AF.Exp,
                                                       scale=SCALE, bias=kb[:, kt:kt + 1]),
                         reads=[PSB[pS], lB], writes=[EB[e3]])
                    dg = kt - (48 + qb * 4)
                    if dg >= 0:
                        S.op("dve", lambda e: e.tensor_tensor(out=Es[e3][:], in0=Es[e3][:], in1=trb[:, dg, :], op=ALU.mult),
                             reads=[EB[e3], lB], writes=[EB[e3]])

                def back(idx):
                    h, qb, m, kt, nkt = iters[idx]
                    vi = h % 2
                    e3 = idx % 3
                    dg = kt - (48 + qb * 4)

                    def pv(e):
                        ins = None
                        for qs in range(4):
                            ins = e.matmul(PS[qs][:, 0:257], lhsT=Es[e3][:, qs * 128:(qs + 1) * 128],
                                           rhs=Va[vi][:, kt, :], start=(kt == 0), stop=(kt == nkt - 1))
                        return ins
                    S.op("pe", pv, reads=[EB[e3], VaB[vi]], writes=[PSB[0], PSB[1], PSB[2], PSB[3]])
                    if kt != nkt - 1:
                        return
                    for qs in range(4):
                        S.op("dve", lambda e, qs=qs: e.reciprocal(out=rs[:, m * 4 + qs:m * 4 + qs + 1], in_=PS[qs][:, 256:257]),
                             reads=[PSB[qs]], writes=[rsB])
                        S.op("dve", lambda e, qs=qs: e.tensor_scalar(out=om[m][:, qs, :], in0=PS[qs][:, 0:256],
                                                                     scalar1=rs[:, m * 4 + qs:m * 4 + qs + 1], scalar2=None,
                                                                     op0=ALU.mult),
                             reads=[PSB[qs], rsB], writes=[omB[m]])
                    if m != 1:
                        return
                    y2 = hqc[0] % 2
                    hqc[0] += 1
                    S.op("dve", lambda e: e.scalar_tensor_tensor(out=od[:].rearrange("p a b -> p (a b)"),
                                                                 in0=om[1][:].rearrange("p a b -> p (a b)"),
                                                                 scalar=neglam,
                                                                 in1=om[0][:].rearrange("p a b -> p (a b)"),
                                                                 op0=ALU.mult, op1=ALU.add),
                         reads=[omB[0], omB[1], lB], writes=[odB])
                    for qs in range(4):
                        S.op("act", lambda e, qs=qs: e.activation(out=junk[:], in_=od[:, qs, :], func=AF.Square,
                                                                  accum_out=rs[:, qs:qs + 1]),
                             reads=[odB], writes=[rsB])
                    S.op("dve", lambda e: e.tensor_scalar(out=rs[:, 0:4], in0=rs[:, 0:4], scalar1=1.0 / 256, scalar2=1e-5,
                                                          op0=ALU.mult, op1=ALU.add), reads=[rsB], writes=[rsB])
                    S.op("act", lambda e: e.activation(out=rs[:, 4:8], in_=rs[:, 0:4], func=AF.Sqrt), reads=[rsB], writes=[rsB])
                    S.op("dve", lambda e: e.reciprocal(out=rs[:, 0:4], in_=rs[:, 4:8]), reads=[rsB], writes=[rsB])
                    for qs in range(4):
                        S.op("dve", lambda e, qs=qs: e.scalar_tensor_tensor(out=onb[:, qs, :], in0=od[:, qs, :],
                                                                            scalar=rs[:, qs:qs + 1], in1=sg_t[:],
                                                                            op0=ALU.mult, op1=ALU.mult),
                             reads=[odB, rsB, lB], writes=[odB])
                    for eh in range(2):
                        pT = 6 + eh
                        pvw = PS[pT][:].bitcast(BF16)

                        def tr(e, eh=eh, pvw=pvw):
                            ins = None
                            for qs in range(4):
                                ins = e.transpose(out=pvw[:, qs * 128:(qs + 1) * 128],
                                                  in_=onb[:, qs, eh * 128:(eh + 1) * 128], identity=ident_b[:])
                            return ins
                        S.op("pe", tr, reads=[odB], writes=[PSB[pT]])
                        S.op("act", lambda e, eh=eh, pvw=pvw: e.copy(out=yo[y2][:, eh, :], in_=pvw[:, 0:512]),
                             reads=[PSB[pT]], writes=[yoB[y2]])
                    S.dma("pool", yaT_d[qb, :, h * 2:(h + 1) * 2, :], yo[y2][:], reads=[yoB[y2]])

                front(0)
                for idx in range(len(iters)):
                    if idx + 1 < len(iters):
                        front(idx + 1)
                    back(idx)
                S.barrier()

        if upto >= 8:
            for which, AT, W in [(0, yrT_d, w_br_rnn), (1, yaT_d, w_br_attn)]:
                with contextlib.ExitStack() as ls:
                    gt = [sb(f"b_g{i}", [128, 512], BF16, ls) for i in range(2)]
                    m1 = [sb(f"b_m{i}", [128, 512], BF16, ls) for i in range(2)]
                    ot = [sb(f"b_o{i}", [128, 512], F32, ls) for i in range(2)]
                    ob = [sb(f"b_ob{i}", [128, 512], BF16, ls) for i in range(2)]
                    bB = [Buf() for _ in range(2)]
                    oB = [Buf() for _ in range(2)]
                    cnt = [0]

                    def epi_b(pb, ti, ct, blk, bi, which=which):
                        i2 = cnt[0] % 2
                        cnt[0] += 1
                        r0 = ti * 256 + ct * 128
                        t0 = blk * 512
                        S.dma("sp", gt[i2][:], sg_d[which, r0:r0 + 128, t0:t0 + 512], writes=[bB[i2]])
                        if which == 0:
                            S.op("dve", lambda e: e.tensor_tensor(out=ob[i2][:], in0=PS[pb][:], in1=gt[i2][:], op=ALU.mult),
                                 reads=[PSB[pb], bB[i2]], writes=[oB[i2]])
                            S.dma("pool", m1_d[r0:r0 + 128, t0:t0 + 512], ob[i2][:], reads=[oB[i2]])
                        else:
                            S.dma("sp", m1[i2][:], m1_d[r0:r0 + 128, t0:t0 + 512], writes=[bB[i2]])
                            S.op("dve", lambda e: e.tensor_tensor(out=ot[i2][:], in0=PS[pb][:], in1=gt[i2][:], op=ALU.mult),
                                 reads=[PSB[pb], bB[i2]], writes=[oB[i2]])
                            S.op("dve", lambda e: e.tensor_tensor(out=ob[i2][:], in0=ot[i2][:], in1=m1[i2][:], op=ALU.add),
                                 reads=[oB[i2], bB[i2]], writes=[oB[i2]])
                            S.dma("pool", mT_d[blk, :, r0 // 128, :], ob[i2][:], reads=[oB[i2]])
                    gemm(ls, AT, [0, 1, 2, 3], w_tiles(W, 0, 256), 8, KC, 256, "fm", None, epi_b)
                    S.barrier()
            with contextlib.ExitStack() as ls:
                xo = [sb(f"o_x{i}", [128, 256], F32, ls) for i in range(2)]
                xB = [Buf() for _ in range(2)]
                cnt = [0]

                def epi_o(pb, ti, s, blk, bi):
                    i2 = cnt[0] % 2
                    cnt[0] += 1
                    r0 = blk * 512 + s * 128
                    S.dma("sp", xo[i2][:], xf[OWN0 + r0:OWN0 + r0 + 128, ti * 256:(ti + 1) * 256], writes=[xB[i2]])
                    S.op("dve", lambda e: e.tensor_tensor(out=xo[i2][:], in0=PS[pb][:, 0:256], in1=xo[i2][:], op=ALU.add),
                         reads=[PSB[pb], xB[i2]], writes=[xB[i2]])
                    S.dma("pool", x2_d[r0:r0 + 128, ti * 256:(ti + 1) * 256], xo[i2][:], reads=[xB[i2]])
                gemm(ls, mT_d, [0, 1, 2, 3], w_tiles(w_out, 0, 256), 8, KC, 256, "tm", None, epi_o)
                S.barrier()

        if upto >= 9:
            norm_transpose(x2_d, 0, 4, hnT_d, 1e-6, tm_dst=hn_d)
            with contextlib.ExitStack() as ls:
                wrf = sb("r_wf", [128, KC, 36], F32, ls)
                wrb = sb("r_wb", [128, KC, 36], BF16, ls)
                rB = Buf()
                S.dma("sp", wrf[:], w_router.rearrange("(k p) n -> p k n", p=128), writes=[rB])

                def castr(e):
                    ins = None
                    for k in range(KC):
                        ins = e.tensor_scalar(out=wrb[:, k, :], in0=wrf[:, k, :], scalar1=gffn_s[:, k:k + 1], scalar2=None, op0=ALU.mult)
                    return ins
                S.op("dve", castr, reads=[rB], writes=[rB])
                at = [sb(f"r_at{i}", [128, KC, 512], BF16, ls) for i in range(2)]
                atB = [Buf() for _ in range(2)]
                lg = sb("r_lg", [128, 36], F32, ls)
                w = {nm: sb("r_" + nm, [128, shape], F32, ls) for nm, shape in
                     [("gmax", 1), ("gex", 4), ("gsum", 1), ("gw", 1), ("gm", 4), ("pen", 32), ("el", 32), ("m1", 1),
                      ("k1", 32), ("el2", 32), ("m2", 1), ("k2", 32), ("dl", 1), ("w1", 1), ("w2", 1), ("c", 32), ("c2", 32), ("rt", 66)]}
                wkB = Buf()
                for blk in range(4):
                    a = blk % 2
                    S.dma("sp", at[a][:], hnT_d[blk], writes=[atB[a]])
                    for s in range(4):
                        pb = s % 4

                        def mm(e, a=a, s=s, pb=pb):
                            ins = None
                            for k in range(KC):
                                ins = e.matmul(PS[pb][:, 0:36], lhsT=at[a][:, k, s * 128:(s + 1) * 128], rhs=wrb[:, k, :],
                                               start=(k == 0), stop=(k == KC - 1))
                            return ins
                        S.op("pe", mm, reads=[atB[a], rB], writes=[PSB[pb]])
                        R = [wkB]

                        def D_(fn, extra=()):
                            S.op("dve", fn, reads=R + list(extra), writes=R)
                        D_(lambda e: e.tensor_copy(out=lg[:], in_=PS[pb][:, 0:36]), extra=[PSB[pb]])
                        D_(lambda e: e.reduce_max(out=w["gmax"][:], in_=lg[:, 0:4], axis=AX.X))
                        D_(lambda e: e.tensor_scalar(out=w["gex"][:], in0=lg[:, 0:4], scalar1=w["gmax"][:, 0:1], scalar2=None, op0=ALU.subtract))
                        S.op("act", lambda e: e.activation(out=w["gex"][:], in_=w["gex"][:], func=AF.Exp), reads=R, writes=R)
                        D_(lambda e: e.reduce_sum(out=w["gsum"][:], in_=w["gex"][:], axis=AX.X))
                        D_(lambda e: e.reciprocal(out=w["gw"][:], in_=w["gsum"][:]))
                        D_(lambda e: e.tensor_scalar(out=w["gm"][:], in0=lg[:, 0:4], scalar1=w["gmax"][:, 0:1], scalar2=None, op0=ALU.is_ge))
                        for g in range(4):
                            D_(lambda e, g=g: e.tensor_scalar(out=w["pen"][:, g * 8:(g + 1) * 8], in0=lg[:, 4 + g * 8:4 + (g + 1) * 8],
                                                              scalar1=0.0, scalar2=w["gm"][:, g:g + 1], op0=ALU.mult, op1=ALU.add))
                        D_(lambda e: e.tensor_scalar(out=w["pen"][:], in0=w["pen"][:], scalar1=-1.0, scalar2=1e9, op0=ALU.add, op1=ALU.mult))
                        D_(lambda e: e.tensor_tensor(out=w["el"][:], in0=lg[:, 4:36], in1=w["pen"][:], op=ALU.add))
                        D_(lambda e: e.reduce_max(out=w["m1"][:], in_=w["el"][:], axis=AX.X))
                        D_(lambda e: e.tensor_scalar(out=w["k1"][:], in0=w["el"][:], scalar1=w["m1"][:, 0:1], scalar2=None, op0=ALU.is_ge))
                        D_(lambda e: e.scalar_tensor_tensor(out=w["el2"][:], in0=w["k1"][:], scalar=-1e9, in1=w["el"][:], op0=ALU.mult, op1=ALU.add))
                        D_(lambda e: e.reduce_max(out=w["m2"][:], in_=w["el2"][:], axis=AX.X))
                        D_(lambda e: e.tensor_scalar(out=w["k2"][:], in0=w["el2"][:], scalar1=w["m2"][:, 0:1], scalar2=None, op0=ALU.is_ge))
                        D_(lambda e: e.tensor_tensor(out=w["dl"][:], in0=w["m1"][:], in1=w["m2"][:], op=ALU.subtract))
                        S.op("act", lambda e: e.activation(out=w["w1"][:], in_=w["dl"][:], func=AF.Sigmoid), reads=R, writes=R)
                        D_(lambda e: e.tensor_scalar(out=w["w2"][:], in0=w["w1"][:], scalar1=-1.0, scalar2=1.0, op0=ALU.mult, op1=ALU.add))
                        D_(lambda e: e.tensor_tensor(out=w["w1"][:], in0=w["w1"][:], in1=w["gw"][:], op=ALU.mult))
                        D_(lambda e: e.tensor_tensor(out=w["w2"][:], in0=w["w2"][:], in1=w["gw"][:], op=ALU.mult))
                        D_(lambda e: e.tensor_scalar(out=w["c"][:], in0=w["k1"][:], scalar1=w["w1"][:, 0:1], scalar2=None, op0=ALU.mult))
                        D_(lambda e: e.scalar_tensor_tensor(out=w["c2"][:], in0=w["k2"][:], scalar=w["w2"][:, 0:1], in1=w["c"][:], op0=ALU.mult, op1=ALU.add))
                        r0 = blk * 512 + s * 128
                        S.dma("pool", C_d[r0:r0 + 128, :], w["c2"][:], reads=R)
                        D_(lambda e: e.tensor_copy(out=w["rt"][:, 0:32], in_=w["k1"][:]))
                        D_(lambda e: e.tensor_copy(out=w["rt"][:, 32:64], in_=w["k2"][:]))
                        D_(lambda e: e.tensor_copy(out=w["rt"][:, 64:65], in_=w["w1"][:]))
                        D_(lambda e: e.tensor_copy(out=w["rt"][:, 65:66], in_=w["w2"][:]))
                        S.dma("pool", R_d[r0:r0 + 128, :], w["rt"][:], reads=R)
                S.barrier()

        if upto >= 10 and not sparse:
            with contextlib.ExitStack() as ls:
                Cs = sb("m_C", [128, 16, NE], F32, ls)
                cB = Buf()
                S.dma("sp", Cs[:], C_d.rearrange("(t p) e -> p t e", p=128), writes=[cB])
                acc = sb("m_acc", [128, 8, D], F32, ls)
                accB = [Buf() for _ in range(8)]
                hn = [sb(f"m_hn{i}", [128, KC, 512], BF16, ls) for i in range(2)]
                hnB = [Buf() for _ in range(2)]
                actT = sb("m_act", [128, 8, 1024], BF16, ls)
                actB = [Buf() for _ in range(8)]
                sgt = [sb(f"m_sg{i}", [128, 512], F32, ls) for i in range(2)]
                sgB = [Buf() for _ in range(2)]
                fst = sb("m_fst", [128, 4], F32, ls)
                fB = Buf()
                wst = [sb(f"m_wst{i}", [128, 4096], F32, ls) for i in range(2)]
                wstB = [Buf() for _ in range(2)]
                wbf = [sb(f"m_wbf{i}", [128, 4096], BF16, ls) for i in range(4)]
                wbfB = [Buf() for _ in range(4)]
                wi = [0]
                sgi = [0]
                pi = [0]

                def load_w(view, kc_n, wcols, gvec):
                    a = wi[0] % 2
                    b = wi[0] % 4
                    wi[0] += 1
                    wv = wst[a][:, 0:kc_n * wcols].rearrange("p (k c) -> p k c", k=kc_n)
                    wb = wbf[b][:, 0:kc_n * wcols].rearrange("p (k c) -> p k c", k=kc_n)
                    kq = max(1, kc_n // 4)
                    for k0 in range(0, kc_n, kq):
                        S.dma("sp", wv[:, k0:k0 + kq, :], view[:, k0:k0 + kq, :], writes=[wstB[a]])
                    ceng = "dve" if wi[0] % 2 == 0 else "act"
                    if gvec is None:
                        if ceng == "dve":
                            S.op("dve", lambda e: e.tensor_copy(out=wbf[b][:, 0:kc_n * wcols], in_=wst[a][:, 0:kc_n * wcols]),
                                 reads=[wstB[a]], writes=[wbfB[b]])
                        else:
                            S.op("act", lambda e: e.copy(out=wbf[b][:, 0:kc_n * wcols], in_=wst[a][:, 0:kc_n * wcols]),
                                 reads=[wstB[a]], writes=[wbfB[b]])
                    else:
                        def cast(e):
                            ins = None
                            for k in range(kc_n):
                                if ceng == "dve":
                                    ins = e.tensor_scalar(out=wb[:, k, :], in0=wv[:, k, :], scalar1=gvec[:, k:k + 1], scalar2=None, op0=ALU.mult)
                                else:
                                    ins = e.activation(out=wb[:, k, :], in_=wv[:, k, :], func=AF.Copy, scale=gvec[:, k:k + 1])
                            return ins
                        S.op(ceng, cast, reads=[wstB[a]], writes=[wbfB[b]])
                    return wb, wbfB[b]

                for tb in range(2):
                    for j in range(2):
                        S.dma("sp", hn[j][:], hnT_d[tb * 2 + j], writes=[hnB[j]])
                    for tt in range(8):
                        r0 = tb * 1024 + tt * 128
                        S.dma("sp", acc[:, tt, :], x2_d[r0:r0 + 128, :], writes=[accB[tt]])
                    for ex in range(NE):
                        wgv = w_gate[ex].rearrange("(k p) n -> p k n", p=128)
                        wuv = w_up[ex].rearrange("(k p) n -> p k n", p=128)
                        wdv = w_down[ex].rearrange("(k p) n -> p k n", p=128)
                        for f2 in range(4):
                            wg, wgB = load_w(wgv[:, :, f2 * 256:(f2 + 1) * 256], KC, 256, gffn_s)
                            wu, wuB = load_w(wuv[:, :, f2 * 256:(f2 + 1) * 256], KC, 256, gffn_s)
                            for j in range(2):
                                for ct in range(2):
                                    fc = f2 * 2 + ct
                                    pg = (pi[0] * 2) % 4
                                    pu = pg + 1
                                    pi[0] += 1

                                    def mmg(e, W=wg, P=pg, j=j, ct=ct):
                                        ins = None
                                        for k in range(KC):
                                            ins = e.matmul(PS[P][:], lhsT=W[:, k, ct * 128:(ct + 1) * 128], rhs=hn[j][:, k, :],
                                                           start=(k == 0), stop=(k == KC - 1))
                                        return ins
                                    S.op("pe", mmg, reads=[wgB, hnB[j]], writes=[PSB[pg]])
                                    S.op("pe", lambda e: mmg(e, W=wu, P=pu), reads=[wuB, hnB[j]], writes=[PSB[pu]])
                                    s2 = sgi[0] % 2
                                    sgi[0] += 1
                                    S.op("act", lambda e: e.activation(out=sgt[s2][:], in_=PS[pg][:], func=AF.Silu),
                                         reads=[PSB[pg]], writes=[sgB[s2]])
                                    S.op("dve", lambda e: e.tensor_tensor(out=actT[:, fc, j * 512:(j + 1) * 512], in0=PS[pu][:],
                                                                          in1=sgt[s2][:], op=ALU.mult),
                                         reads=[PSB[pu], sgB[s2]], writes=[actB[fc]])
                        for cg in range(4):
                            wd, wdB = load_w(wdv[:, :, cg * 512:(cg + 1) * 512], 8, 512, None)
                            for tt in range(8):
                                pd = 4 + (pi[0] % 4)
                                pi[0] += 1

                                def mmd(e, wd=wd, pd=pd, tt=tt):
                                    ins = None
                                    for k in range(8):
                                        ins = e.matmul(PS[pd][:], lhsT=actT[:, k, tt * 128:(tt + 1) * 128], rhs=wd[:, k, :],
                                                       start=(k == 0), stop=(k == 7))
                                    return ins
                                S.op("pe", mmd, reads=[wdB] + actB, writes=[PSB[pd]])
                                S.op("dve", lambda e: e.scalar_tensor_tensor(out=acc[:, tt, cg * 512:(cg + 1) * 512], in0=PS[pd][:],
                                                                             scalar=Cs[:, tb * 8 + tt, ex:ex + 1],
                                                                             in1=acc[:, tt, cg * 512:(cg + 1) * 512],
                                                                             op0=ALU.mult, op1=ALU.add),
                                     reads=[PSB[pd], cB, accB[tt]], writes=[accB[tt]])
                    gfin = wst[0][:, 0:D]
                    fjunk = actT[:, 0:2, :]
                    S.dma("sp", gfin, g_final.rearrange("(o t) -> o t", o=1).partition_broadcast(128), writes=[wstB[0]])
                    for tt in range(8):
                        r0 = tb * 1024 + tt * 128
                        S.op("act", lambda e: e.activation(out=fjunk, in_=acc[:, tt, :].rearrange("p (a b) -> p a b", a=2), func=AF.Square, accum_out=fst[:, 0:1]),
                             reads=[accB[tt]], writes=[fB, actB[0], actB[1]])
                        S.op("dve", lambda e: e.tensor_scalar(out=fst[:, 1:2], in0=fst[:, 0:1], scalar1=1.0 / D, scalar2=1e-6,
                                                              op0=ALU.mult, op1=ALU.add), reads=[fB], writes=[fB])
                        S.op("act", lambda e: e.activation(out=fst[:, 3:4], in_=fst[:, 1:2], func=AF.Sqrt), reads=[fB], writes=[fB])
                        S.op("dve", lambda e: e.reciprocal(out=fst[:, 2:3], in_=fst[:, 3:4]), reads=[fB], writes=[fB])
                        S.op("dve", lambda e: e.scalar_tensor_tensor(out=acc[:, tt, :], in0=acc[:, tt, :], scalar=fst[:, 2:3],
                                                                     in1=gfin, op0=ALU.mult, op1=ALU.mult),
                             reads=[accB[tt], fB, wstB[0]], writes=[accB[tt]])
                        S.dma("pool", out[r0:r0 + 128, :], acc[:, tt, :], reads=[accB[tt]])
                S.barrier()
        if upto >= 10 and sparse:
            with contextlib.ExitStack() as ls:
                Rs = sb("s_R", [128, 16, 66], F32, ls)
                rB = Buf()
                S.dma("sp", Rs[:], R_d.rearrange("(t p) e -> p t e", p=128), writes=[rB])
                lsf = sb("s_lsf", [128, 128], F32, ls)
                lsb = sb("s_lsb", [128, 128], BF16, ls)
                onb = sb("s_onb", [128, 128], BF16, ls)
                iog = sb("s_iog", [128, 16], F32, ls)
                iod = sb("s_iod", [128, 8], F32, ls)
                S.dma("sp", lsf[:], c_ls[:, :], writes=[rB])
                S.dma("sp", iog[:], c_iog[:, :], writes=[rB])
                S.dma("sp", iod[:], c_iod[:, :], writes=[rB])
                S.op("dve", lambda e: e.tensor_copy(out=lsb[:], in_=lsf[:]), reads=[rB], writes=[rB])
                S.op("dve", lambda e: e.memset(onb[:], 1.0), writes=[rB])
                Ab = sb("s_Ab", [128, 16, 32], BF16, ls)
                S.op("dve", lambda e: e.tensor_tensor(out=Ab[:], in0=Rs[:, :, 0:32], in1=Rs[:, :, 32:64], op=ALU.add), reads=[rB], writes=[rB])
                def mmc(e):
                    ins = None
                    for i in range(16):
                        ins = e.matmul(PS[0][:, 0:32], lhsT=onb[:], rhs=Ab[:, i, :], start=(i == 0), stop=(i == 15))
                    return ins
                S.op("pe", mmc, reads=[rB], writes=[PSB[0]])
                cnt = sb("s_cnt", [128, 32], F32, ls)
                nb = sb("s_nb", [128, 32], F32, ls)
                pend = sb("s_pend", [128, 32], F32, ls)
                pst = sb("s_pst", [128, 32], F32, ls)
                one32 = sb("s_one32", [128, 32], F32, ls)
                tmp32 = sb("s_tmp32", [128, 32], F32, ls)
                eb = sb("s_eb", [128, 64], F32, ls)
                cB = Buf()
                S.op("dve", lambda e: e.tensor_copy(out=cnt[:], in_=PS[0][:, 0:32]), reads=[PSB[0]], writes=[cB])
                S.op("dve", lambda e: e.memset(nb[:], 0.0), writes=[cB])
                S.op("dve", lambda e: e.memset(one32[:], 1.0), writes=[cB])
                for j in range(16):
                    S.op("dve", lambda e, j=j: e.scalar_tensor_tensor(out=nb[:], in0=cnt[:], scalar=128.0 * j, in1=nb[:],
                                                                      op0=ALU.is_gt, op1=ALU.add), reads=[cB], writes=[cB])
                S.op("dve", lambda e: e.tensor_scalar(out=nb[:], in0=nb[:], scalar1=128.0, scalar2=None, op0=ALU.mult), reads=[cB], writes=[cB])
                S.op("dve", lambda e: e.tensor_tensor_scan(out=pend[:], data0=one32[:], data1=nb[:], initial=0.0,
                                                           op0=ALU.mult, op1=ALU.add), reads=[cB], writes=[cB])
                S.op("dve", lambda e: e.tensor_tensor(out=pst[:], in0=pend[:], in1=nb[:], op=ALU.subtract), reads=[cB], writes=[cB])
                for b_ in range(64):
                    S.op("dve", lambda e, b_=b_: e.tensor_scalar(out=tmp32[:], in0=pend[:], scalar1=128.0 * b_, scalar2=0.0,
                                                                 op0=ALU.is_le, op1=ALU.add, accum_out=eb[:, b_:b_ + 1]),
                         reads=[cB], writes=[cB])
                S.op("dve", lambda e: e.tensor_scalar(out=eb[:], in0=eb[:], scalar1=31.0, scalar2=None, op0=ALU.min), reads=[cB], writes=[cB])
                ebg = sb("s_ebg", [128, 64], F32, ls)
                ebd = sb("s_ebd", [128, 64], F32, ls)
                S.op("dve", lambda e: e.tensor_scalar(out=ebg[:], in0=eb[:], scalar1=2048.0, scalar2=None, op0=ALU.mult), reads=[cB], writes=[cB])
                S.op("dve", lambda e: e.tensor_scalar(out=ebd[:], in0=eb[:], scalar1=1024.0, scalar2=None, op0=ALU.mult), reads=[cB], writes=[cB])
                igf = sb("s_igf", [128, 64, 16], F32, ls)
                idf = sb("s_idf", [128, 64, 8], F32, ls)
                igi = sb("s_igi", [128, 64, 16], I32, ls)
                idi = sb("s_idi", [128, 64, 8], I32, ls)
                for b_ in range(64):
                    S.op("dve", lambda e, b_=b_: e.tensor_scalar(out=igf[:, b_, :], in0=iog[:], scalar1=ebg[:, b_:b_ + 1], scalar2=None, op0=ALU.add),
                         reads=[cB, rB], writes=[cB])
                    S.op("dve", lambda e, b_=b_: e.tensor_scalar(out=idf[:, b_, :], in0=iod[:], scalar1=ebd[:, b_:b_ + 1], scalar2=None, op0=ALU.add),
                         reads=[cB, rB], writes=[cB])
                S.op("dve", lambda e: e.tensor_copy(out=igi[:], in_=igf[:]), reads=[cB], writes=[cB])
                S.op("dve", lambda e: e.tensor_copy(out=idi[:], in_=idf[:]), reads=[cB], writes=[cB])
                dsf = sb("s_dsf", [128, 16, 2], F32, ls)
                dsi = sb("s_dsi", [128, 16, 2], I32, ls)
                pos = sb("s_pos", [128, 32], F32, ls)
                dB = Buf()
                for i in range(16):
                    pb = 1 + (i % 3)

                    def mmr(e, i=i, pb=pb):
                        for i2 in range(i):
                            e.matmul(PS[pb][:, 0:32], lhsT=onb[:], rhs=Ab[:, i2, :], start=(i2 == 0), stop=False)
                        return e.matmul(PS[pb][:, 0:32], lhsT=lsb[:], rhs=Ab[:, i, :], start=(i == 0), stop=True)
                    S.op("pe", mmr, reads=[rB], writes=[PSB[pb]])
                    S.op("dve", lambda e: e.tensor_tensor(out=pos[:], in0=PS[pb][:, 0:32], in1=pst[:], op=ALU.add),
                         reads=[PSB[pb], cB, dB], writes=[dB])
                    for k_ in range(2):
                        S.op("dve", lambda e, k_=k_: e.tensor_tensor(out=tmp32[:], in0=pos[:], in1=Rs[:, i, k_ * 32:(k_ + 1) * 32], op=ALU.mult),
                             reads=[dB, rB, cB], writes=[cB])
                        S.op("dve", lambda e, k_=k_: e.reduce_sum(out=dsf[:, i, k_:k_ + 1], in_=tmp32[:], axis=AX.X), reads=[cB, dB], writes=[dB])
                S.op("dve", lambda e: e.tensor_copy(out=dsi[:], in_=dsf[:]), reads=[dB], writes=[dB])
                ht = [sb(f"s_ht{i}", [128, D], BF16, ls) for i in range(2)]
                htB = [Buf() for _ in range(2)]
                xsB = Buf()
                for i in range(16):
                    a = i % 2
                    S.dma("sp", ht[a][:], hn_d[i * 128:(i + 1) * 128, :], writes=[htB[a]])
                    for k_ in range(2):
                        S.idma(xs_d[:, :], bass.IndirectOffsetOnAxis(ap=dsi[:, i, k_:k_ + 1], axis=0), ht[a][:], None,
                               reads=[htB[a], dB], writes=[xsB])
                S.barrier()
                wg_rows = w_gate.rearrange("e k n -> (e k) n")
                wu_rows = w_up.rearrange("e k n -> (e k) n")
                wd_rows = w_down.rearrange("e k n -> (e k) n")
                xb = [sb(f"s_xb{i}", [128, D], BF16, ls) for i in range(2)]
                xbB = [Buf() for _ in range(2)]
                xT = [sb(f"s_xT{i}", [128, KC, 128], BF16, ls) for i in range(2)]
                xTB = [Buf() for _ in range(2)]
                gst = [sb(f"s_gst{i}", [128, 1024], F32, ls) for i in range(8)]
                gstB = [Buf() for _ in range(8)]
                gbf = [sb(f"s_gbf{i}", [128, 1024], BF16, ls) for i in range(8)]
                gbfB = [Buf() for _ in range(8)]
                dst_ = [sb(f"s_dst{i}", [128, D], F32, ls) for i in range(4)]
                dstB = [Buf() for _ in range(4)]
                dbf = [sb(f"s_dbf{i}", [128, D], BF16, ls) for i in range(4)]
                dbfB = [Buf() for _ in range(4)]
                sgl = sb("s_sgl", [128, 1024], F32, ls)
                sglB = Buf()
                actb = sb("s_act", [128, 1024], BF16, ls)
                actB_ = Buf()
                aT = sb("s_aT", [128, 8, 128], BF16, ls)
                aTB = Buf()
                yb = [sb(f"s_yb{i}", [128, D], BF16, ls) for i in range(2)]
                ybB = [Buf() for _ in range(2)]
                ysB = Buf()
                gi = 0
                gbi = 0
                di = 0
                dbi = 0
                ce = 0
                for b_ in range(64):
                    x2i = b_ % 2
                    S.dma("sp", xb[x2i][:], xs_d[b_ * 128:(b_ + 1) * 128, :], writes=[xbB[x2i]])
                    for g4 in range(4):
                        pb = 4 + g4
                        pvw = PS[pb][:].bitcast(BF16)

                        def tr(e, g4=g4, pvw=pvw, x2i=x2i):
                            ins = None
                            for q in range(4):
                                kc = g4 * 4 + q
                                ins = e.transpose(out=pvw[:, q * 128:(q + 1) * 128], in_=xb[x2i][:, kc * 128:(kc + 1) * 128], identity=ident_b[:])
                            return ins
                        S.op("pe", tr, reads=[xbB[x2i]], writes=[PSB[pb]])
                        eng = "act" if g4 % 2 == 0 else "dve"
                        o_ = xT[x2i][:, g4 * 4:(g4 + 1) * 4, :]
                        i_ = pvw[:, 0:512].rearrange("p (k t) -> p k t", k=4)
                        if eng == "act":
                            S.op("act", lambda e: e.copy(out=o_, in_=i_), reads=[PSB[pb]], writes=[xTB[x2i]])
                        else:
                            S.op("dve", lambda e: e.tensor_copy(out=o_, in_=i_), reads=[PSB[pb]], writes=[xTB[x2i]])
                    for kc in range(KC):
                        wpair = []
                        for rows in (wg_rows, wu_rows):
                            a = gi % 8
                            gi += 1
                            bq = gbi % 8
                            gbi += 1
                            S.idma(gst[a][:], None, rows[:, :], bass.IndirectOffsetOnAxis(ap=igi[:, b_, kc:kc + 1], axis=0),
                                   reads=[cB], writes=[gstB[a]])
                            ceng = "dve" if ce % 2 == 0 else "act"
                            ce += 1
                            if ceng == "dve":
                                S.op("dve", lambda e: e.tensor_scalar(out=gbf[bq][:], in0=gst[a][:], scalar1=gffn_s[:, kc:kc + 1], scalar2=None, op0=ALU.mult),
                                     reads=[gstB[a]], writes=[gbfB[bq]])
                            else:
                                S.op("act", lambda e: e.activation(out=gbf[bq][:], in_=gst[a][:], func=AF.Copy, scale=gffn_s[:, kc:kc + 1]),
                                     reads=[gstB[a]], writes=[gbfB[bq]])
                            wpair.append(bq)

                        def mmgu(e, kc=kc, wpair=wpair, x2i=x2i):
                            ins = None
                            for wi_, bq in enumerate(wpair):
                                for hf in range(2):
                                    ins = e.matmul(PS[wi_ * 2 + hf][:], lhsT=xT[x2i][:, kc, :], rhs=gbf[bq][:, hf * 512:(hf + 1) * 512],
                                                   start=(kc == 0), stop=(kc == KC - 1))
                            return ins
                        S.op("pe", mmgu, reads=[xTB[x2i], gbfB[wpair[0]], gbfB[wpair[1]]], writes=[PSB[0], PSB[1], PSB[2], PSB[3]])
                    for hf in range(2):
                        S.op("act", lambda e, hf=hf: e.activation(out=sgl[:, hf * 512:(hf + 1) * 512], in_=PS[hf][:], func=AF.Silu),
                             reads=[PSB[hf]], writes=[sglB])
                        S.op("dve", lambda e, hf=hf: e.tensor_tensor(out=actb[:, hf * 512:(hf + 1) * 512], in0=PS[2 + hf][:],
                                                                     in1=sgl[:, hf * 512:(hf + 1) * 512], op=ALU.mult),
                             reads=[PSB[2 + hf], sglB], writes=[actB_])
                    for g2 in range(2):
                        pb = g2
                        pvw = PS[pb][:].bitcast(BF16)

                        def tr2(e, g2=g2, pvw=pvw):
                            ins = None
                            for q in range(4):
                                fc = g2 * 4 + q
                                ins = e.transpose(out=pvw[:, q * 128:(q + 1) * 128], in_=actb[:, fc * 128:(fc + 1) * 128], identity=ident_b[:])
                            return ins
                        S.op("pe", tr2, reads=[actB_], writes=[PSB[pb]])
                        o_ = aT[:, g2 * 4:(g2 + 1) * 4, :]
                        i_ = pvw[:, 0:512].rearrange("p (k t) -> p k t", k=4)
                        if g2 == 0:
                            S.op("act", lambda e: e.copy(out=o_, in_=i_), reads=[PSB[pb]], writes=[aTB])
                        else:
                            S.op("dve", lambda e: e.tensor_copy(out=o_, in_=i_), reads=[PSB[pb]], writes=[aTB])
                    for fc in range(8):
                        a = di % 4
                        di += 1
                        bq = dbi % 4
                        dbi += 1
                        S.idma(dst_[a][:], None, wd_rows[:, :], bass.IndirectOffsetOnAxis(ap=idi[:, b_, fc:fc + 1], axis=0),
                               reads=[cB], writes=[dstB[a]])
                        if fc % 2 == 0:
                            S.op("dve", lambda e: e.tensor_copy(out=dbf[bq][:], in_=dst_[a][:]), reads=[dstB[a]], writes=[dbfB[bq]])
                        else:
                            S.op("act", lambda e: e.copy(out=dbf[bq][:], in_=dst_[a][:]), reads=[dstB[a]], writes=[dbfB[bq]])

                        def mmd(e, fc=fc, bq=bq):
                            ins = None
                            for cg in range(4):
                                ins = e.matmul(PS[4 + cg][:], lhsT=aT[:, fc, :], rhs=dbf[bq][:, cg * 512:(cg + 1) * 512],
                                               start=(fc == 0), stop=(fc == 7))
                            return ins
                        S.op("pe", mmd, reads=[aTB, dbfB[bq]], writes=[PSB[4], PSB[5], PSB[6], PSB[7]])
                    for cg in range(4):
                        if cg % 2 == 0:
                            S.op("act", lambda e, cg=cg: e.copy(out=yb[x2i][:, cg * 512:(cg + 1) * 512], in_=PS[4 + cg][:]),
                                 reads=[PSB[4 + cg]], writes=[ybB[x2i]])
                        else:
                            S.op("dve", lambda e, cg=cg: e.tensor_copy(out=yb[x2i][:, cg * 512:(cg + 1) * 512], in_=PS[4 + cg][:]),
                                 reads=[PSB[4 + cg]], writes=[ybB[x2i]])
                    S.dma("sp", ys_d[b_ * 128:(b_ + 1) * 128, :], yb[x2i][:], reads=[ybB[x2i]], writes=[ysB])
                S.barrier()
                gfin = sb("s_gf", [128, D], F32, ls)
                fB0 = Buf()
                S.dma("sp", gfin[:], g_final.rearrange("(o t) -> o t", o=1).partition_broadcast(128), writes=[fB0])
                g1 = [sb(f"s_g1{i}", [128, D], BF16, ls) for i in range(2)]
                g2_ = [sb(f"s_g2{i}", [128, D], BF16, ls) for i in range(2)]
                xr = [sb(f"s_xr{i}", [128, D], F32, ls) for i in range(2)]
                gB_ = [Buf() for _ in range(2)]
                xrB = [Buf() for _ in range(2)]
                fst = [sb(f"s_fst{i}", [128, 4], F32, ls) for i in range(2)]
                fj = sb("s_fj", [128, D], BF16, ls)
                fjB = Buf()
                for i in range(16):
                    a = i % 2
                    S.dma("sp", xr[a][:], x2_d[i * 128:(i + 1) * 128, :], writes=[xrB[a]])
                    S.idma(g1[a][:], None, ys_d[:, :], bass.IndirectOffsetOnAxis(ap=dsi[:, i, 0:1], axis=0), reads=[dB, ysB], writes=[gB_[a]])
                    S.idma(g2_[a][:], None, ys_d[:, :], bass.IndirectOffsetOnAxis(ap=dsi[:, i, 1:2], axis=0), reads=[dB, ysB], writes=[gB_[a]])
                    S.op("dve", lambda e: e.scalar_tensor_tensor(out=xr[a][:], in0=g1[a][:], scalar=Rs[:, i, 64:65], in1=xr[a][:],
                                                                 op0=ALU.mult, op1=ALU.add), reads=[gB_[a], rB, xrB[a]], writes=[xrB[a]])
                    S.op("dve", lambda e: e.scalar_tensor_tensor(out=xr[a][:], in0=g2_[a][:], scalar=Rs[:, i, 65:66], in1=xr[a][:],
                                                                 op0=ALU.mult, op1=ALU.add), reads=[gB_[a], rB, xrB[a]], writes=[xrB[a]])
                    S.op("act", lambda e: e.activation(out=fj[:], in_=xr[a][:], func=AF.Square, accum_out=fst[a][:, 0:1]),
                         reads=[xrB[a]], writes=[fjB, xrB[a]])
                    S.op("dve", lambda e: e.tensor_scalar(out=fst[a][:, 1:2], in0=fst[a][:, 0:1], scalar1=1.0 / D, scalar2=1e-6,
                                                          op0=ALU.mult, op1=ALU.add), reads=[xrB[a]], writes=[xrB[a]])
                    S.op("act", lambda e: e.activation(out=fst[a][:, 3:4], in_=fst[a][:, 1:2], func=AF.Sqrt), reads=[xrB[a]], writes=[xrB[a]])
                    S.op("dve", lambda e: e.reciprocal(out=fst[a][:, 2:3], in_=fst[a][:, 3:4]), reads=[xrB[a]], writes=[xrB[a]])
                    S.op("dve", lambda e: e.scalar_tensor_tensor(out=xr[a][:], in0=xr[a][:], scalar=fst[a][:, 2:3], in1=gfin[:],
                                                                 op0=ALU.mult, op1=ALU.mult), reads=[xrB[a], fB0], writes=[xrB[a]])
                    S.dma("sp", out[i * 128:(i + 1) * 128, :], xr[a][:], reads=[xrB[a]])
                S.barrier()
        S.barrier()
    return nc


def _consts():
    i = np.arange(64, dtype=np.float32)
    inv = (1.0 / (10000.0 ** (np.arange(0, 128, 2, dtype=np.float32) / 128.0))).astype(np.float32)
    c_rope = np.zeros((128, 2), np.float32)
    c_rope[:, 0] = np.concatenate([inv, inv])
    c_rope[:64, 1] = -1.0
    c_rope[64:, 1] = 1.0
    c_swap = np.zeros((128, 128), np.float32)
    for m in range(128):
        c_swap[(m + 64) % 128, m] = 1.0
    c_ident = np.eye(128, dtype=np.float32)
    kl = np.arange(128)[:, None, None] + 128 * np.arange(4)[None, :, None]
    ql = np.arange(512)[None, None, :]
    c_tri = (kl <= ql).astype(np.float32)
    c_ls = (np.arange(128)[:, None] < np.arange(128)[None, :]).astype(np.float32)
    c_iog = (np.arange(16)[None, :] * 128 + np.arange(128)[:, None]).astype(np.float32)
    c_iod = (np.arange(8)[None, :] * 128 + np.arange(128)[:, None]).astype(np.float32)
    return dict(c_rope=c_rope, c_swap=c_swap, c_ident=c_ident, c_tri=np.ascontiguousarray(c_tri),
                c_ls=c_ls, c_iog=np.ascontiguousarray(c_iog), c_iod=np.ascontiguousarray(c_iod))


def make_in_maps(inputs):
    x = np.asarray(inputs["x"], np.float32)
    pos = np.asarray(inputs["positions"], np.int32)
    f = lambda k: np.ascontiguousarray(np.asarray(inputs[k], np.float32)[0])
    pk = lambda v: np.ascontiguousarray(v.reshape(-1, 128).T)
    shared = dict(
        g_mix=pk(f("g_mix")), w_in=f("w_in"), conv_w=np.ascontiguousarray(f("conv_w").reshape(4, KC, 128).transpose(2, 0, 1)), conv_b=pk(f("conv_b")),
        w_rg_a=f("w_rg_a"), b_rg_a=pk(f("b_rg_a")), w_rg_x=f("w_rg_x"), b_rg_x=pk(f("b_rg_x")),
        lru_param=pk(f("lru_param")),
        lam_in=np.ascontiguousarray(np.stack([f("lambda_q1"), f("lambda_k1"), f("lambda_q2"), f("lambda_k2")])),
        subln_g=f("subln_g"), w_br_rnn=f("w_br_rnn"), w_br_attn=f("w_br_attn"), w_out=f("w_out"),
        g_ffn=pk(f("g_ffn")),
        w_router=np.ascontiguousarray(np.concatenate([f("w_grp_router"), f("w_exp_router")], axis=1)),
        w_gate=f("w_gate"), w_up=f("w_up"), w_down=f("w_down"),
        g_final=np.ascontiguousarray(np.asarray(inputs["g_final"], np.float32)),
    )
    shared.update(_consts())
    maps = []
    for b in range(2):
        for j in range(4):
            n = (j + 1) * 2048
            xfp = np.zeros((T, D), np.float32)
            xfp[T - n:] = x[b, :n]
            pp = np.zeros((T,), np.int32)
            pp[T - n:] = pos[b, :n]
            vv = np.zeros((T,), np.float32)
            vv[T - n:] = 1.0
            m = dict(shared)
            m.update(xf=xfp, posf=pp, valid=vv, valid_pk=pk(vv))
            maps.append(m)
    return maps


def kernel(**inputs):
    nc = build()
    maps = make_in_maps(inputs)
    res = run_bass_kernel_spmd(nc, maps, core_ids=list(range(8)))
    outp = np.zeros((2, 8192, D), np.float32)
    for c in range(8):
        b, j = c // 4, c % 4
        outp[b, j * 2048:(j + 1) * 2048] = res.results[c]["out"]
    return outp
```

```python
import math
import contextlib
import numpy as np
import concourse.bass as bass
import concourse.mybir as mybir
from concourse.bass_utils import run_bass_kernel_spmd

F32 = mybir.dt.float32
BF16 = mybir.dt.bfloat16
I32 = mybir.dt.int32
ALU = mybir.AluOpType
AF = mybir.ActivationFunctionType
AX = mybir.AxisListType

D = 2048
T = 8192
OWN0 = 6144
NOWN = 2048
KC = 16
NE = 32
DE = 1024
NH = 8
TWO_PI = 2.0 * math.pi


class Buf:
    __slots__ = ("w", "r")

    def __init__(self):
        self.w = None
        self.r = {}


class Sched:
    def __init__(self, nc, es):
        self.nc = nc
        self.st = {}
        for name, h, nd in [("pe", nc.tensor, 0), ("dve", nc.vector, 0), ("act", nc.scalar, 6),
                            ("pool", nc.gpsimd, 14), ("sp", nc.sync, 10)]:
            st = dict(h=h, cnt=0, seen={}, dcnt=0, name=name)
            st["sem"] = es.enter_context(nc.semaphore("c_" + name))
            st["dsems"] = [es.enter_context(nc.semaphore(f"d_{name}{i}")) for i in range(nd)]
            self.st[name] = st

    @staticmethod
    def _add(d, ev):
        if ev is None:
            return
        sem, val, key = ev
        if key not in d or d[key][1] < val:
            d[key] = (sem, val, key)

    def _deps(self, reads, writes):
        d = {}
        for b in reads:
            self._add(d, b.w)
        for b in writes:
            self._add(d, b.w)
            for ev in b.r.values():
                self._add(d, ev)
        return d

    def _wait(self, st, d, skip=None):
        for key, (sem, val, _) in d.items():
            if key == skip:
                continue
            if st["seen"].get(key, 0) >= val:
                continue
            st["h"].wait_ge(sem, val)
            st["seen"][key] = val

    def _mark(self, reads, writes, ev):
        for b in reads:
            self._add(b.r, ev)
        for b in writes:
            b.w = ev
            b.r = {}

    def op(self, stname, fn, reads=(), writes=()):
        st = self.st[stname]
        d = self._deps(reads, writes)
        self._wait(st, d, skip=("c_pe" if stname == "pe" else None))
        ins = fn(st["h"])
        st["cnt"] += 1
        ins.then_inc(st["sem"], 1)
        self._mark(reads, writes, (st["sem"], st["cnt"], "c_" + stname))

    def dma(self, stname, out, in_, reads=(), writes=()):
        st = self.st[stname]
        d = self._deps(reads, writes)
        i = st["dcnt"]
        R = len(st["dsems"])
        k = i % R
        sem = st["dsems"][k]
        val = 16 * (i // R + 1)
        key = f"d_{stname}{k}"
        if i >= R:
            self._add(d, (sem, val - 16, key))
        self._wait(st, d)
        st["h"].dma_start(out=out, in_=in_).then_inc(sem, 16)
        st["dcnt"] += 1
        self._mark(reads, writes, (sem, val, key))

    def idma(self, out, out_off, in_, in_off, reads=(), writes=()):
        st = self.st["pool"]
        d = self._deps(reads, writes)
        i = st["dcnt"]
        R = len(st["dsems"])
        k = i % R
        sem = st["dsems"][k]
        val = 16 * (i // R + 1)
        key = f"d_pool{k}"
        if i >= R:
            self._add(d, (sem, val - 16, key))
        self._wait(st, d)
        st["h"].indirect_dma_start(out=out, out_offset=out_off, in_=in_, in_offset=in_off).then_inc(sem, 16)
        st["dcnt"] += 1
        self._mark(reads, writes, (sem, val, key))

    def all_events(self):
        d = {}
        for name, st in self.st.items():
            if st["cnt"] > 0:
                self._add(d, (st["sem"], st["cnt"], "c_" + name))
            R = len(st["dsems"])
            for k in range(R):
                n = (st["dcnt"] - k + R - 1) // R if st["dcnt"] > k else 0
                if n > 0:
                    self._add(d, (st["dsems"][k], 16 * n, f"d_{name}{k}"))
        return d

    def barrier(self, only=None):
        d = self.all_events()
        for name, st in self.st.items():
            if only is not None and name not in only:
                continue
            self._wait(st, d)


def build(upto=99, debug=(), sparse=True):
    nc = bass.Bass("TRN2", target_bir_lowering=False)
    dbg = set(debug)

    def dram_in(name, shape, dt=F32):
        return nc.dram_tensor(name, list(shape), dt, kind="ExternalInput").ap()

    def dram_tmp(name, shape, dt):
        kind = "ExternalOutput" if name in dbg else "Internal"
        return nc.dram_tensor(name, list(shape), dt, kind=kind).ap()

    xf = dram_in("xf", [T, D])
    posf = dram_in("posf", [T], I32)
    valid = dram_in("valid", [T])
    validp = dram_in("validp", [T])
    g_mix = dram_in("g_mix", [128, KC])
    w_in = dram_in("w_in", [D, 14336])
    conv_w = dram_in("conv_w", [128, 4, KC])
    conv_b = dram_in("conv_b", [128, KC])
    w_rg_a = dram_in("w_rg_a", [16, 128, 128])
    b_rg_a = dram_in("b_rg_a", [128, KC])
    w_rg_x = dram_in("w_rg_x", [16, 128, 128])
    b_rg_x = dram_in("b_rg_x", [128, KC])
    lru_param = dram_in("lru_param", [128, KC])
    lam_in = dram_in("lam_in", [4, 128])
    subln_g = dram_in("subln_g", [256])
    w_br_rnn = dram_in("w_br_rnn", [D, D])
    w_br_attn = dram_in("w_br_attn", [D, D])
    w_out = dram_in("w_out", [D, D])
    g_ffn = dram_in("g_ffn", [128, KC])
    valid_pk = dram_in("valid_pk", [128, 64])
    w_router = dram_in("w_router", [D, 36])
    w_gate = dram_in("w_gate", [NE, D, DE])
    w_up = dram_in("w_up", [NE, D, DE])
    w_down = dram_in("w_down", [NE, DE, D])
    g_final = dram_in("g_final", [D])
    c_rope = dram_in("c_rope", [128, 2])
    c_swap = dram_in("c_swap", [128, 128])
    c_ident = dram_in("c_ident", [128, 128])
    c_tri = dram_in("c_tri", [128, 4, 512])
    c_ls = dram_in("c_ls", [128, 128])
    c_iog = dram_in("c_iog", [128, 16])
    c_iod = dram_in("c_iod", [128, 8])
    out = nc.dram_tensor("out", [NOWN, D], F32, kind="ExternalOutput").ap()

    hT_d = dram_tmp("hT_d", [16, 128, KC, 512], BF16)
    uT_d = dram_tmp("uT_d", [D, T], BF16)
    KT_d = dram_tmp("KT_d", [16, 128, T], BF16)
    V_d = dram_tmp("V_d", [T, D], BF16)
    gbT_d = dram_tmp("gbT_d", [D, NOWN], BF16)
    QT_d = dram_tmp("QT_d", [16, 128, NOWN], BF16)
    sg_d = dram_tmp("sg_d", [2, D, NOWN], BF16)
    cos_d = dram_tmp("cos_d", [16, 128, 512], F32)
    sin_d = dram_tmp("sin_d", [16, 128, 512], F32)
    yrT_d = dram_tmp("yrT_d", [4, 128, KC, 512], BF16)
    yaT_d = dram_tmp("yaT_d", [4, 128, KC, 512], BF16)
    m1_d = dram_tmp("m1_d", [D, NOWN], BF16)
    mT_d = dram_tmp("mT_d", [4, 128, KC, 512], BF16)
    x2_d = dram_tmp("x2_d", [NOWN, D], F32)
    hnT_d = dram_tmp("hnT_d", [4, 128, KC, 512], BF16)
    C_d = dram_tmp("C_d", [NOWN, NE], F32)
    R_d = dram_tmp("R_d", [NOWN, 66], F32)
    hn_d = dram_tmp("hn_d", [NOWN, D], BF16)
    xs_d = dram_tmp("xs_d", [8192, D], BF16)
    ys_d = dram_tmp("ys_d", [8192, D], BF16)

    with contextlib.ExitStack() as es:
        S = Sched(nc, es)

        uid = [0]

        def sb(name, shape, dt, stack=es):
            uid[0] += 1
            return stack.enter_context(nc.sbuf_tensor(f"{name}_{uid[0]}", list(shape), dt))

        PS = [es.enter_context(nc.psum_tensor(f"ps{i}", [128, 512], F32)) for i in range(8)]
        PSB = [Buf() for _ in range(8)]

        ident_f = sb("ident_f", [128, 128], F32)
        ident_b = sb("ident_b", [128, 128], BF16)
        swap_f = sb("swap_f", [128, 128], F32)
        swap_b = sb("swap_b", [128, 128], BF16)
        rope_c = sb("rope_c", [128, 2], F32)
        gmix_s = sb("gmix_s", [128, KC], F32)
        gffn_s = sb("gffn_s", [128, KC], F32)
        cst = Buf()
        S.dma("sp", ident_f[:], c_ident[:, :], writes=[cst])
        S.dma("sp", swap_f[:], c_swap[:, :], writes=[cst])
        S.dma("sp", rope_c[:], c_rope[:, :], writes=[cst])
        S.dma("sp", gmix_s[:], g_mix[:, :], writes=[cst])
        S.dma("sp", gffn_s[:], g_ffn[:, :], writes=[cst])
        S.op("dve", lambda e: e.tensor_copy(out=ident_b[:], in_=ident_f[:]), reads=[cst], writes=[cst])
        S.op("dve", lambda e: e.tensor_copy(out=swap_b[:], in_=swap_f[:]), reads=[cst], writes=[cst])
        S.barrier()

        def norm_transpose(src, row0, nblk, dst_d, eps, tm_dst=None):
            with contextlib.ExitStack() as ls:
                xt = [sb(f"nt_x{i}", [128, D], F32, ls) for i in range(2)]
                xtB = [Buf() for _ in range(2)]
                junk = sb("nt_junk", [128, D], BF16, ls)
                junkB = Buf()
                xn = [sb(f"nt_xn{i}", [128, D], BF16, ls) for i in range(2)]
                xnB = [Buf() for _ in range(2)]
                st_ = [sb(f"nt_s{i}", [128, 4], F32, ls) for i in range(2)]
                stB = [Buf() for _ in range(2)]
                hb = [sb(f"nt_h{i}", [128, KC, 512], BF16, ls) for i in range(2)]
                hbB = [Buf() for _ in range(2)]
                it = 0
                for blk in range(nblk):
                    h = hb[blk % 2]
                    hB = hbB[blk % 2]
                    for s in range(4):
                        i2 = it % 2
                        r0 = row0 + (blk * 4 + s) * 128
                        S.dma("sp", xt[i2][:], src[r0:r0 + 128, :], writes=[xtB[i2]])
                        S.op("act", lambda e: e.activation(out=junk[:], in_=xt[i2][:], func=AF.Square,
                                                           accum_out=st_[i2][:, 0:1]),
                             reads=[xtB[i2]], writes=[junkB, stB[i2]])
                        S.op("dve", lambda e: e.tensor_scalar(out=st_[i2][:, 1:2], in0=st_[i2][:, 0:1],
                                                              scalar1=1.0 / D, scalar2=eps,
                                                              op0=ALU.mult, op1=ALU.add),
                             reads=[stB[i2]], writes=[stB[i2]])
                        S.op("act", lambda e: e.activation(out=st_[i2][:, 3:4], in_=st_[i2][:, 1:2], func=AF.Sqrt),
                             reads=[stB[i2]], writes=[stB[i2]])
                        S.op("dve", lambda e: e.reciprocal(out=st_[i2][:, 2:3], in_=st_[i2][:, 3:4]),
                             reads=[stB[i2]], writes=[stB[i2]])
                        S.op("dve", lambda e: e.tensor_scalar(out=xn[i2][:], in0=xt[i2][:],
                                                              scalar1=st_[i2][:, 2:3], scalar2=None,
                                                              op0=ALU.mult),
                             reads=[xtB[i2], stB[i2]], writes=[xnB[i2]])
                        if tm_dst is not None:
                            S.dma("pool", tm_dst[r0 - row0:r0 - row0 + 128, :], xn[i2][:], reads=[xnB[i2]])
                        for g4 in range(4):
                            pb = (it * 4 + g4) % 4
                            pv = PS[pb][:].bitcast(BF16)

                            def tr(e, g4=g4, pv=pv, i2=i2):
                                ins = None
                                for q in range(4):
                                    kc = g4 * 4 + q
                                    ins = e.transpose(out=pv[:, q * 128:(q + 1) * 128],
                                                      in_=xn[i2][:, kc * 128:(kc + 1) * 128],
                                                      identity=ident_b[:])
                                return ins
                            S.op("pe", tr, reads=[xnB[i2]], writes=[PSB[pb]])
                            eng = "act" if g4 % 2 == 0 else "dve"

                            def ev(e, g4=g4, pv=pv, s=s, h=h, eng=eng):
                                o = h[:, g4 * 4:(g4 + 1) * 4, s * 128:(s + 1) * 128]
                                i_ = pv[:, 0:512].rearrange("p (k t) -> p k t", k=4)
                                if eng == "act":
                                    return e.copy(out=o, in_=i_)
                                return e.tensor_copy(out=o, in_=i_)
                            S.op(eng, ev, reads=[PSB[pb]], writes=[hB])
                        it += 1
                    S.dma("pool", dst_d[blk], h[:], reads=[hB])
                S.barrier()

        def gemm(ls, AT_d, blks, Wview_fn, ntiles, kc_n, wcols, mode, gvec, epilogue, group=2,
                 resident=None, pbanks=(0, 1, 2, 3)):
            wst = [sb(f"g_wst{i}", [128, 4096], F32, ls) for i in range(2)]
            wstB = [Buf() for _ in range(2)]
            nwb = 2 * group
            wbf = [sb(f"g_wbf{i}", [128, 4096], BF16, ls) for i in range(nwb)]
            wbfB = [Buf() for _ in range(nwb)]
            if resident is None:
                at = [sb(f"g_at{i}", [128, KC * 512], BF16, ls) for i in range(2)]
                atB = [Buf() for _ in range(2)]
            wi = 0
            ai = 0
            pi = 0
            for t0 in range(0, ntiles, group):
                tiles = list(range(t0, min(ntiles, t0 + group)))
                wsl = {}
                for ti in tiles:
                    a = wi % 2
                    b = wi % nwb
                    wv = wst[a][:, 0:kc_n * wcols].rearrange("p (k c) -> p k c", k=kc_n)
                    wsrc = Wview_fn(ti)
                    kq = max(1, kc_n // 4)
                    for k0 in range(0, kc_n, kq):
                        S.dma("sp", wv[:, k0:k0 + kq, :], wsrc[:, k0:k0 + kq, :], writes=[wstB[a]])
                    wb = wbf[b][:, 0:kc_n * wcols].rearrange("p (k c) -> p k c", k=kc_n)
                    ceng = "dve" if wi % 2 == 0 else "act"
                    if gvec is None:
                        if ceng == "dve":
                            S.op("dve", lambda e, a=a, b=b: e.tensor_copy(out=wbf[b][:, 0:kc_n * wcols],
                                                                          in_=wst[a][:, 0:kc_n * wcols]),
                                 reads=[wstB[a]], writes=[wbfB[b]])
                        else:
                            S.op("act", lambda e, a=a, b=b: e.copy(out=wbf[b][:, 0:kc_n * wcols],
                                                                   in_=wst[a][:, 0:kc_n * wcols]),
                                 reads=[wstB[a]], writes=[wbfB[b]])
                    else:
                        def cast(e, wv=wv, wb=wb, ceng=ceng):
                            ins = None
                            for k in range(kc_n):
                                if ceng == "dve":
                                    ins = e.tensor_scalar(out=wb[:, k, :], in0=wv[:, k, :],
                                                          scalar1=gvec[:, k:k + 1], scalar2=None, op0=ALU.mult)
                                else:
                                    ins = e.activation(out=wb[:, k, :], in_=wv[:, k, :], func=AF.Copy,
                                                       scale=gvec[:, k:k + 1])
                            return ins
                        S.op(ceng, cast, reads=[wstB[a]], writes=[wbfB[b]])
                    wsl[ti] = (wb, wbfB[b])
                    wi += 1
                for bi, blk in enumerate(blks):
                    if resident is None:
                        a = ai % 2
                        ai += 1
                        S.dma("sp", at[a][:], AT_d[blk].rearrange("p k t -> p (k t)"), writes=[atB[a]])
                        av = at[a][:].rearrange("p (k t) -> p k t", k=KC)
                        aB = atB[a]
                    else:
                        av, aB = resident[bi]
                    for ti in tiles:
                        wb, wB = wsl[ti]
                        if mode == "fm":
                            for ct in range(wcols // 128):
                                pb = pbanks[pi % len(pbanks)]
                                pi += 1

                                def mm(e, wb=wb, av=av, ct=ct, pb=pb):
                                    ins = None
                                    for k in range(kc_n):
                                        ins = e.matmul(PS[pb][:], lhsT=wb[:, k, ct * 128:(ct + 1) * 128],
                                                       rhs=av[:, k, :], start=(k == 0), stop=(k == kc_n - 1))
                                    return ins
                                S.op("pe", mm, reads=[wB, aB], writes=[PSB[pb]])
                                epilogue(pb, ti, ct, blk, bi)
                        else:
                            for s in range(4):
                                pb = pbanks[pi % len(pbanks)]
                                pi += 1

                                def mm(e, wb=wb, av=av, s=s, pb=pb):
                                    ins = None
                                    for k in range(kc_n):
                                        ins = e.matmul(PS[pb][:, 0:wcols], lhsT=av[:, k, s * 128:(s + 1) * 128],
                                                       rhs=wb[:, k, :], start=(k == 0), stop=(k == kc_n - 1))
                                    return ins
                                S.op("pe", mm, reads=[wB, aB], writes=[PSB[pb]])
                                epilogue(pb, ti, s, blk, bi)

        def w_tiles(Wap, c0, wcols):
            Wv = Wap.rearrange("(k p) n -> p k n", p=128)
            return lambda ti: Wv[:, :, c0 + ti * wcols: c0 + (ti + 1) * wcols]

        if upto >= 0:
            with contextlib.ExitStack() as ls:
                pos_i = sb("pos_i", [128, 512], I32, ls)
                pos_f = sb("pos_f", [128, 512], F32, ls)
                ang = sb("ang", [128, 512], F32, ls)
                a1 = sb("a1", [128, 512], F32, ls)
                a2 = sb("a2", [128, 512], F32, ls)
                tq = sb("tq", [128, 512], F32, ls)
                tB = Buf()
                posv = posf.rearrange("(b t) -> b t", t=512)
                for blk in range(16):
                    S.dma("sp", pos_i[:], posv[blk:blk + 1, :].partition_broadcast(128), writes=[tB])
                    S.op("dve", lambda e: e.tensor_copy(out=pos_f[:], in_=pos_i[:]), reads=[tB], writes=[tB])
                    S.op("dve", lambda e: e.tensor_scalar(out=ang[:], in0=pos_f[:], scalar1=rope_c[:, 0:1],
                                                          scalar2=None, op0=ALU.mult), reads=[tB], writes=[tB])
                    def trig(dst, shift):
                        S.op("dve", lambda e: e.tensor_scalar(out=dst[:], in0=ang[:], scalar1=shift, scalar2=None, op0=ALU.add), reads=[tB], writes=[tB])
                        S.op("dve", lambda e: e.tensor_scalar(out=tq[:], in0=dst[:], scalar1=1.0 / TWO_PI, scalar2=None, op0=ALU.mult), reads=[tB], writes=[tB])
                        S.op("dve", lambda e: e.tensor_copy(out=pos_i[:], in_=tq[:]), reads=[tB], writes=[tB])
                        S.op("dve", lambda e: e.tensor_copy(out=tq[:], in_=pos_i[:]), reads=[tB], writes=[tB])
                        S.op("dve", lambda e: e.scalar_tensor_tensor(out=dst[:], in0=tq[:], scalar=-TWO_PI, in1=dst[:], op0=ALU.mult, op1=ALU.add), reads=[tB], writes=[tB])
                        S.op("dve", lambda e: e.tensor_scalar(out=tq[:], in0=dst[:], scalar1=math.pi, scalar2=-TWO_PI, op0=ALU.is_gt, op1=ALU.mult), reads=[tB], writes=[tB])
                        S.op("dve", lambda e: e.tensor_tensor(out=dst[:], in0=dst[:], in1=tq[:], op=ALU.add), reads=[tB], writes=[tB])
                        S.op("dve", lambda e: e.tensor_scalar(out=tq[:], in0=dst[:], scalar1=-math.pi, scalar2=TWO_PI, op0=ALU.is_lt, op1=ALU.mult), reads=[tB], writes=[tB])
                        S.op("dve", lambda e: e.tensor_tensor(out=dst[:], in0=dst[:], in1=tq[:], op=ALU.add), reads=[tB], writes=[tB])
                        S.op("dve", lambda e: e.tensor_scalar(out=dst[:], in0=dst[:], scalar1=-math.pi, scalar2=math.pi, op0=ALU.max, op1=ALU.min), reads=[tB], writes=[tB])
                        S.op("act", lambda e: e.activation(out=dst[:], in_=dst[:], func=AF.Sin), reads=[tB], writes=[tB])
                    trig(a1, 0.0)
                    S.op("dve", lambda e: e.tensor_scalar(out=a1[:], in0=a1[:], scalar1=rope_c[:, 1:2], scalar2=None, op0=ALU.mult), reads=[tB], writes=[tB])
                    S.dma("pool", sin_d[blk], a1[:], reads=[tB])
                    trig(a2, math.pi / 2)
                    S.dma("pool", cos_d[blk], a2[:], reads=[tB])
                S.barrier()

        if upto >= 1:
            norm_transpose(xf, 0, 16, hT_d, 1e-6)

        def rope_epilogue_factory(ls, dst_d, tok_of_blk, scale_cols0):
            cs = [sb(f"rp_c{i}", [128, 512], F32, ls) for i in range(2)]
            sn = [sb(f"rp_s{i}", [128, 512], F32, ls) for i in range(2)]
            csB = [Buf() for _ in range(2)]
            tb_ = [sb(f"rp_t{i}", [128, 512], BF16, ls) for i in range(2)]
            tbB = [Buf() for _ in range(2)]
            o1 = [sb(f"rp_o1{i}", [128, 512], F32, ls) for i in range(2)]
            o2 = [sb(f"rp_o2{i}", [128, 512], F32, ls) for i in range(2)]
            ob = [sb(f"rp_ob{i}", [128, 512], BF16, ls) for i in range(2)]
            oB = [Buf() for _ in range(2)]
            state = dict(n=0, lastblk=None, ci=0)

            def epi(pb, ti, ct, blk, bi):
                n = state["n"]
                state["n"] += 1
                i2 = n % 2
                if state["lastblk"] != blk:
                    state["ci"] += 1
                    c2 = state["ci"] % 2
                    S.dma("sp", cs[c2][:], cos_d[blk], writes=[csB[c2]])
                    S.dma("sp", sn[c2][:], sin_d[blk], writes=[csB[c2]])
                    state["lastblk"] = blk
                c2 = state["ci"] % 2
                hm = scale_cols0 + ti * 2 + ct
                pb2 = 4 + (n % 2)
                S.op("act", lambda e: e.copy(out=tb_[i2][:], in_=PS[pb][:]), reads=[PSB[pb]], writes=[tbB[i2]])
                S.op("pe", lambda e: e.matmul(PS[pb2][:], lhsT=swap_b[:], rhs=tb_[i2][:], start=True, stop=True),
                     reads=[tbB[i2]], writes=[PSB[pb2]])
                S.op("dve", lambda e: e.tensor_tensor(out=o1[i2][:], in0=PS[pb][:], in1=cs[c2][:], op=ALU.mult),
                     reads=[PSB[pb], csB[c2], tbB[i2]], writes=[oB[i2]])
                S.op("dve", lambda e: e.tensor_tensor(out=o2[i2][:], in0=PS[pb2][:], in1=sn[c2][:], op=ALU.mult),
                     reads=[PSB[pb2], csB[c2]], writes=[oB[i2]])
                S.op("dve", lambda e: e.tensor_tensor(out=ob[i2][:], in0=o1[i2][:], in1=o2[i2][:], op=ALU.add),
                     reads=[oB[i2]], writes=[oB[i2]])
                t0 = tok_of_blk(blk)
                S.dma("pool", dst_d[hm, :, t0:t0 + 512], ob[i2][:], reads=[oB[i2]])
            return epi

        ALLB = list(range(16))
        OWNB = [12, 13, 14, 15]
        if upto >= 2:
            with contextlib.ExitStack() as ls:
                ut = [sb(f"e_u{i}", [128, 512], BF16, ls) for i in range(3)]
                utB = [Buf() for _ in range(3)]
                cnt = [0]

                def epi_u(pb, ti, ct, blk, bi):
                    i3 = cnt[0] % 3
                    cnt[0] += 1
                    r0 = ti * 256 + ct * 128
                    S.op("act", lambda e: e.copy(out=ut[i3][:], in_=PS[pb][:]), reads=[PSB[pb]], writes=[utB[i3]])
                    S.dma("pool", uT_d[r0:r0 + 128, blk * 512:(blk + 1) * 512], ut[i3][:], reads=[utB[i3]])
                gemm(ls, hT_d, ALLB, w_tiles(w_in, 0, 256), 8, KC, 256, "fm", gmix_s, epi_u)
                S.barrier()
        if upto >= 3:
            with contextlib.ExitStack() as ls:
                epi_k = rope_epilogue_factory(ls, KT_d, lambda blk: blk * 512, 0)
                gemm(ls, hT_d, ALLB, w_tiles(w_in, 3 * D, 256), 8, KC, 256, "fm", gmix_s, epi_k)
                S.barrier()
            with contextlib.ExitStack() as ls:
                epi_q = rope_epilogue_factory(ls, QT_d, lambda blk: (blk - 12) * 512, 0)
                gemm(ls, hT_d, OWNB, w_tiles(w_in, 2 * D, 256), 8, KC, 256, "fm", gmix_s, epi_q)
                S.barrier()
        if upto >= 4:
            with contextlib.ExitStack() as ls:
                vt = [sb(f"e_v{i}", [128, 256], BF16, ls) for i in range(3)]
                vtB = [Buf() for _ in range(3)]
                cnt = [0]

                def epi_v(pb, ti, s, blk, bi):
                    i3 = cnt[0] % 3
                    cnt[0] += 1
                    r0 = blk * 512 + s * 128
                    eng = "act" if cnt[0] % 2 else "dve"
                    if eng == "act":
                        S.op("act", lambda e: e.copy(out=vt[i3][:], in_=PS[pb][:, 0:256]), reads=[PSB[pb]], writes=[vtB[i3]])
                    else:
                        S.op("dve", lambda e: e.tensor_copy(out=vt[i3][:], in_=PS[pb][:, 0:256]), reads=[PSB[pb]], writes=[vtB[i3]])
                    S.dma("pool", V_d[r0:r0 + 128, ti * 256:(ti + 1) * 256], vt[i3][:], reads=[vtB[i3]])
                gemm(ls, hT_d, ALLB, w_tiles(w_in, 4 * D, 256), 8, KC, 256, "tm", gmix_s, epi_v)
                S.barrier()
        if upto >= 5:
            with contextlib.ExitStack() as ls:
                xs = [sb(f"e_x{i}", [128, 512], F32, ls) for i in range(2)]
                t1 = [sb(f"e_t{i}", [128, 512], F32, ls) for i in range(2)]
                ob = [sb(f"e_o{i}", [128, 512], BF16, ls) for i in range(2)]
                eB = [Buf() for _ in range(2)]
                cnt = [0]

                def epi_gelu(pb, ti, ct, blk, bi):
                    i2 = cnt[0] % 2
                    cnt[0] += 1
                    r0 = ti * 256 + ct * 128
                    t0 = (blk - 12) * 512
                    S.op("act", lambda e: e.copy(out=xs[i2][:], in_=PS[pb][:]), reads=[PSB[pb]], writes=[eB[i2]])
                    S.op("dve", lambda e: e.tensor_tensor(out=t1[i2][:], in0=xs[i2][:], in1=xs[i2][:], op=ALU.mult),
                         reads=[eB[i2]], writes=[eB[i2]])
                    S.op("dve", lambda e: e.tensor_scalar(out=t1[i2][:], in0=t1[i2][:], scalar1=0.044715, scalar2=1.0,
                                                          op0=ALU.mult, op1=ALU.add), reads=[eB[i2]], writes=[eB[i2]])
                    S.op("dve", lambda e: e.tensor_tensor(out=t1[i2][:], in0=t1[i2][:], in1=xs[i2][:], op=ALU.mult),
                         reads=[eB[i2]], writes=[eB[i2]])
                    S.op("act", lambda e: e.activation(out=t1[i2][:], in_=t1[i2][:], func=AF.Sigmoid,
                                                       scale=2.0 * math.sqrt(2.0 / math.pi)),
                         reads=[eB[i2]], writes=[eB[i2]])
                    S.op("dve", lambda e: e.tensor_tensor(out=ob[i2][:], in0=t1[i2][:], in1=xs[i2][:], op=ALU.mult),
                         reads=[eB[i2]], writes=[eB[i2]])
                    S.dma("pool", gbT_d[r0:r0 + 128, t0:t0 + 512], ob[i2][:], reads=[eB[i2]])
                gemm(ls, hT_d, OWNB, w_tiles(w_in, D, 256), 8, KC, 256, "fm", gmix_s, epi_gelu)
                S.barrier()
            with contextlib.ExitStack() as ls:
                ob = [sb(f"e_o{i}", [128, 512], BF16, ls) for i in range(3)]
                eB = [Buf() for _ in range(3)]
                cnt = [0]

                def epi_sig(pb, ti, ct, blk, bi):
                    i3 = cnt[0] % 3
                    cnt[0] += 1
                    col = ti * 256 + ct * 128
                    which, r0 = col // D, col % D
                    t0 = (blk - 12) * 512
                    S.op("act", lambda e: e.activation(out=ob[i3][:], in_=PS[pb][:], func=AF.Sigmoid),
                         reads=[PSB[pb]], writes=[eB[i3]])
                    S.dma("pool", sg_d[which, r0:r0 + 128, t0:t0 + 512], ob[i3][:], reads=[eB[i3]])
                gemm(ls, hT_d, OWNB, w_tiles(w_in, 5 * D, 256), 16, KC, 256, "fm", gmix_s, epi_sig)
                S.barrier()

        if upto >= 6:
            with contextlib.ExitStack() as ls:
                CH = 2048
                nmb = sb("l_nm", [128, T], BF16, ls)
                tmpf = sb("l_tmpf", [128, CH], F32, ls)
                tmpf2 = sb("l_tmpf2", [128, CH], F32, ls)
                tmpi = sb("l_tmpi", [128, CH], I32, ls)
                mB = Buf()
                for q in range(4):
                    sl = slice(q * CH, (q + 1) * CH)
                    S.dma("sp", tmpf2[:], validp.rearrange("(o t) -> o t", o=1)[0:1, sl].partition_broadcast(128), writes=[mB])
                    S.dma("sp", tmpi[:], posf.rearrange("(o t) -> o t", o=1)[0:1, sl].partition_broadcast(128), writes=[mB])
                    S.op("dve", lambda e: e.tensor_copy(out=tmpf[:], in_=tmpi[:]), reads=[mB], writes=[mB])
                    S.op("dve", lambda e: e.tensor_scalar(out=tmpf[:], in0=tmpf[:], scalar1=0.0, scalar2=None,
                                                          op0=ALU.not_equal), reads=[mB], writes=[mB])
                    S.op("dve", lambda e: e.tensor_tensor(out=nmb[:, sl], in0=tmpf[:], in1=tmpf2[:], op=ALU.mult),
                         reads=[mB], writes=[mB])
                cw = sb("l_cw", [128, 4, KC], F32, ls)
                cbs = sb("l_cb", [128, KC], F32, ls)
                bas = sb("l_ba", [128, KC], F32, ls)
                bxs = sb("l_bx", [128, KC], F32, ls)
                lps = sb("l_lp", [128, KC], F32, ls)
                nc8 = sb("l_nc8", [128, KC], F32, ls)
                pB = Buf()
                S.dma("sp", cw[:], conv_w[:, :, :], writes=[pB])
                S.dma("sp", cbs[:], conv_b[:, :], writes=[pB])
                S.dma("sp", bas[:], b_rg_a[:, :], writes=[pB])
                S.dma("sp", bxs[:], b_rg_x[:, :], writes=[pB])
                S.dma("sp", lps[:], lru_param[:, :], writes=[pB])
                S.op("act", lambda e: e.activation(out=nc8[:], in_=lps[:], func=AF.Exp, scale=-1.0), reads=[pB], writes=[pB])
                S.op("act", lambda e: e.activation(out=nc8[:], in_=nc8[:], func=AF.Ln, bias=1.0), reads=[pB], writes=[pB])
                S.op("dve", lambda e: e.tensor_scalar(out=nc8[:], in0=nc8[:], scalar1=-8.0, scalar2=None, op0=ALU.mult),
                     reads=[pB], writes=[pB])
                waf = sb("l_waf", [128, 2, 128], F32, ls)
                wab = [sb(f"l_wab{i}", [128, 2, 128], BF16, ls) for i in range(2)]
                wB = [Buf() for _ in range(2)]
                wfB = Buf()
                u = [sb(f"l_u{i}", [128, 3 + CH], BF16, ls) for i in range(2)]
                dgw = [sb(f"l_dg{i}", [128, 4, 128], BF16, ls) for i in range(2)]
                dgB = [Buf() for _ in range(2)]
                uB = [Buf() for _ in range(2)]
                uc_ = [sb(f"l_uc{i}", [128, CH], F32, ls) for i in range(2)]
                ucb_ = [sb(f"l_ucb{i}", [128, CH], BF16, ls) for i in range(2)]
                rr_ = [sb(f"l_r{i}", [128, CH], F32, ls) for i in range(2)]
                ii_ = [sb(f"l_i{i}", [128, CH], F32, ls) for i in range(2)]
                aa_ = [sb(f"l_a{i}", [128, CH], F32, ls) for i in range(2)]
                mm__ = [sb(f"l_m{i}", [128, CH], F32, ls) for i in range(2)]
                wkB_ = [Buf() for _ in range(2)]
                hh = [sb(f"l_h{i}", [128, CH], F32, ls) for i in range(2)]
                hB = [Buf() for _ in range(2)]
                gbt = sb("l_gb", [128, NOWN], BF16, ls)
                yb = sb("l_y", [128, NOWN], BF16, ls)
                gB = Buf()
                yB = Buf()
                n = 0
                yr_v = yrT_d.rearrange("b p k t -> p k b t")
                for c in range(16):
                    w2 = c % 2
                    S.dma("sp", waf[:, 0, :], w_rg_a[c], writes=[wfB])
                    S.dma("sp", waf[:, 1, :], w_rg_x[c], writes=[wfB])
                    S.op("dve", lambda e: e.tensor_copy(out=wab[w2][:], in_=waf[:]), reads=[wfB], writes=[wB[w2]])
                    for j in range(4):
                        S.op("dve", lambda e, j=j: e.tensor_scalar(out=dgw[w2][:, j, :], in0=ident_f[:], scalar1=cw[:, j, c:c + 1],
                                                                   scalar2=None, op0=ALU.mult), reads=[pB], writes=[dgB[w2]])
                    S.dma("sp", gbt[:], gbT_d[c * 128:(c + 1) * 128, :], writes=[gB])
                    for q in range(4):
                        i2 = n % 2
                        n += 1
                        t0 = q * CH
                        if q == 0:
                            S.op("dve", lambda e: e.memset(u[i2][:, 0:3], 0.0), writes=[uB[i2]])
                            S.dma("sp", u[i2][:, 3:3 + CH], uT_d[c * 128:(c + 1) * 128, 0:CH], writes=[uB[i2]])
                        else:
                            S.dma("sp", u[i2][:], uT_d[c * 128:(c + 1) * 128, t0 - 3:t0 + CH], writes=[uB[i2]])
                        uu = u[i2]
                        uc, ucb, rr, ii, aa, mm_, wkB = uc_[i2], ucb_[i2], rr_[i2], ii_[i2], aa_[i2], mm__[i2], wkB_[i2]
                        for sblk in range(CH // 512):
                            ssl = slice(sblk * 512, (sblk + 1) * 512)
                            pc = 4 * i2 + (sblk % 2)

                            def convmm(e, sblk=sblk, pc=pc):
                                ins = None
                                for j in range(4):
                                    ins = e.matmul(PS[pc][:], lhsT=dgw[w2][:, j, :], rhs=uu[:, sblk * 512 + j:sblk * 512 + j + 512],
                                                   start=(j == 0), stop=(j == 3))
                                return ins
                            S.op("pe", convmm, reads=[dgB[w2], uB[i2]], writes=[PSB[pc]])
                            S.op("act", lambda e: e.activation(out=uc[:, ssl], in_=PS[pc][:], func=AF.Identity, bias=cbs[:, c:c + 1]),
                                 reads=[PSB[pc], pB], writes=[wkB])
                            S.op("dve", lambda e: e.tensor_copy(out=ucb[:, ssl], in_=uc[:, ssl]), reads=[wkB], writes=[wkB])
                        for sblk in range(CH // 512):
                            ssl = slice(sblk * 512, (sblk + 1) * 512)
                            pa, px = 4 * i2 + 2, 4 * i2 + 3
                            S.op("pe", lambda e: e.matmul(PS[pa][:], lhsT=wab[w2][:, 0, :], rhs=ucb[:, ssl], start=True, stop=True),
                                 reads=[wB[w2], wkB], writes=[PSB[pa]])
                            S.op("pe", lambda e: e.matmul(PS[px][:], lhsT=wab[w2][:, 1, :], rhs=ucb[:, ssl], start=True, stop=True),
                                 reads=[wB[w2], wkB], writes=[PSB[px]])
                            S.op("act", lambda e: e.activation(out=rr[:, ssl], in_=PS[pa][:], func=AF.Sigmoid, bias=bas[:, c:c + 1]),
                                 reads=[PSB[pa], pB], writes=[wkB])
                            S.op("act", lambda e: e.activation(out=ii[:, ssl], in_=PS[px][:], func=AF.Sigmoid, bias=bxs[:, c:c + 1]),
                                 reads=[PSB[px], pB], writes=[wkB])
                        S.op("act", lambda e: e.activation(out=aa[:], in_=rr[:], func=AF.Exp, scale=nc8[:, c:c + 1]),
                             reads=[wkB, pB], writes=[wkB])
                        S.op("dve", lambda e: e.tensor_tensor(out=aa[:], in0=aa[:], in1=nmb[:, t0:t0 + CH], op=ALU.mult),
                             reads=[wkB, mB], writes=[wkB])
                        S.op("act", lambda e: e.activation(out=mm_[:], in_=aa[:], func=AF.Square), reads=[wkB], writes=[wkB])
                        S.op("act", lambda e: e.activation(out=mm_[:], in_=mm_[:], func=AF.Sqrt, scale=-1.0, bias=1.0),
                             reads=[wkB], writes=[wkB])
                        S.op("dve", lambda e: e.tensor_tensor(out=ii[:], in0=ii[:], in1=uc[:], op=ALU.mult), reads=[wkB], writes=[wkB])
                        S.op("dve", lambda e: e.tensor_tensor(out=ii[:], in0=ii[:], in1=mm_[:], op=ALU.mult), reads=[wkB], writes=[wkB])
                        hprev = hh[(i2 + 1) % 2]
                        init = 0.0 if q == 0 else hprev[:, CH - 1:CH]
                        S.op("dve", lambda e: e.tensor_tensor_scan(out=hh[i2][:], data0=aa[:], data1=ii[:], initial=init,
                                                                   op0=ALU.mult, op1=ALU.add),
                             reads=[wkB, hB[(i2 + 1) % 2]], writes=[hB[i2]])
                        if q == 3:
                            S.op("dve", lambda e: e.tensor_tensor(out=yb[:], in0=hh[i2][:], in1=gbt[:], op=ALU.mult),
                                 reads=[hB[i2], gB], writes=[yB])
                            S.dma("pool", yr_v[:, c], yb[:].rearrange("p (b t) -> p b t", b=4), reads=[yB])
                S.barrier()

        if upto >= 7:
            with contextlib.ExitStack() as ls:
                SCALE = 128 ** -0.5
                lamv = sb("a_lamv", [128, 4, 128], F32, ls)
                lams = sb("a_lams", [128, 8], F32, ls)
                lB = Buf()
                S.dma("sp", lamv[:].rearrange("p a b -> p (a b)"),
                      lam_in.rearrange("(o a) b -> o (a b)", o=1).partition_broadcast(128), writes=[lB])
                S.op("dve", lambda e: e.tensor_tensor(out=lamv[:, 0, :], in0=lamv[:, 0, :], in1=lamv[:, 1, :], op=ALU.mult), reads=[lB], writes=[lB])
                S.op("dve", lambda e: e.tensor_tensor(out=lamv[:, 2, :], in0=lamv[:, 2, :], in1=lamv[:, 3, :], op=ALU.mult), reads=[lB], writes=[lB])
                S.op("dve", lambda e: e.reduce_sum(out=lams[:, 0:1], in_=lamv[:, 0, :], axis=AX.X), reads=[lB], writes=[lB])
                S.op("dve", lambda e: e.reduce_sum(out=lams[:, 1:2], in_=lamv[:, 2, :], axis=AX.X), reads=[lB], writes=[lB])
                S.op("act", lambda e: e.activation(out=lams[:, 2:4], in_=lams[:, 0:2], func=AF.Exp), reads=[lB], writes=[lB])
                S.op("dve", lambda e: e.tensor_tensor(out=lams[:, 4:5], in0=lams[:, 3:4], in1=lams[:, 2:3], op=ALU.subtract), reads=[lB], writes=[lB])
                S.op("dve", lambda e: e.tensor_scalar(out=lams[:, 5:6], in0=lams[:, 4:5], scalar1=-0.2, scalar2=None, op0=ALU.add), reads=[lB], writes=[lB])
                neglam = lams[:, 5:6]
                sg_t = sb("a_sg", [128, 256], F32, ls)
                S.dma("sp", sg_t[:], subln_g.rearrange("(o t) -> o t", o=1).partition_broadcast(128), writes=[lB])
                S.op("dve", lambda e: e.tensor_scalar(out=sg_t[:], in0=sg_t[:], scalar1=0.8, scalar2=None, op0=ALU.mult), reads=[lB], writes=[lB])
                kb = sb("a_kb", [128, 64], F32, ls)
                S.dma("sp", kb[:], valid_pk[:, :], writes=[lB])
                S.op("dve", lambda e: e.tensor_scalar(out=kb[:], in0=kb[:], scalar1=-1.0, scalar2=30000.0, op0=ALU.add, op1=ALU.mult), reads=[lB], writes=[lB])
                trf = sb("a_trf", [128, 4, 512], F32, ls)
                trb = sb("a_trb", [128, 4, 512], BF16, ls)
                S.dma("sp", trf[:], c_tri[:, :, :], writes=[lB])
                S.op("dve", lambda e: e.tensor_copy(out=trb[:], in_=trf[:]), reads=[lB], writes=[lB])

                Va = [sb(f"a_V{i}", [128, 64, 257], BF16, ls) for i in range(2)]
                VaB = [Buf() for _ in range(2)]
                for i in range(2):
                    S.op("dve", lambda e, i=i: e.memset(Va[i][:, :, 256:257], 1.0), writes=[VaB[i]])
                KTs = [sb(f"a_K{i}", [128, T], BF16, ls) for i in range(2)]
                KTB = [Buf() for _ in range(2)]
                QTs = [sb(f"a_Q{i}", [128, NOWN], BF16, ls) for i in range(2)]
                QTB = [Buf() for _ in range(2)]
                Es = [sb(f"a_E{i}", [128, 512], BF16, ls) for i in range(3)]
                EB = [Buf() for _ in range(3)]
                om = [sb(f"a_om{i}", [128, 4, 256], F32, ls) for i in range(2)]
                omB = [Buf() for _ in range(2)]
                rs = sb("a_rs", [128, 8], F32, ls)
                rsB = Buf()
                od = sb("a_od", [128, 4, 256], F32, ls)
                odB = Buf()
                junk = sb("a_junk", [128, 256], F32, ls)
                onb = sb("a_onb", [128, 4, 256], BF16, ls)
                yo = [sb(f"a_yo{i}", [128, 2, 512], BF16, ls) for i in range(2)]
                yoB = [Buf() for _ in range(2)]
                V_v = V_d.rearrange("(k p) c -> p k c", p=128)
                iters = []
                for h in range(NH):
                    for qb in range(4):
                        nkt = 48 + (qb + 1) * 4
                        for m in range(2):
                            for kt in range(nkt):
                                iters.append((h, qb, m, kt, nkt))
                hqc = [0]

                def front(idx):
                    h, qb, m, kt, nkt = iters[idx]
                    vi = h % 2
                    hm = h * 2 + m
                    ki = hm % 2
                    pS = 4 + (idx % 2)
                    e3 = idx % 3
                    if qb == 0 and m == 0 and kt == 0:
                        for k4 in range(16):
                            S.dma("sp", Va[vi][:, k4 * 4:(k4 + 1) * 4, 0:256], V_v[:, k4 * 4:(k4 + 1) * 4, h * 256:(h + 1) * 256],
                                  writes=[VaB[vi]])
                    if qb == 0 and kt == 0:
                        S.dma("sp", KTs[ki][:], KT_d[hm], writes=[KTB[ki]])
                        S.dma("sp", QTs[ki][:], QT_d[hm], writes=[QTB[ki]])
                    S.op("pe", lambda e: e.matmul(PS[pS][:], lhsT=KTs[ki][:, kt * 128:(kt + 1) * 128],
                                                  rhs=QTs[ki][:, qb * 512:(qb + 1) * 512], start=True, stop=True),
                         reads=[KTB[ki], QTB[ki]], writes=[PSB[pS]])
                    S.op("act", lambda e: e.activation(out=Es[e3][:], in_=PS[pS][:], func=AF.Exp,
                                                       scale=SCALE, bias=kb[:, kt:kt + 1]),
                         reads=[PSB[pS], lB], writes=[EB[e3]])
                    dg = kt - (48 + qb * 4)
                    if dg >= 0:
                        S.op("dve", lambda e: e.tensor_tensor(out=Es[e3][:], in0=Es[e3][:], in1=trb[:, dg, :], op=ALU.mult),
                             reads=[EB[e3], lB], writes=[EB[e3]])

                def back(idx):
                    h, qb, m, kt, nkt = iters[idx]
                    vi = h % 2
                    e3 = idx % 3
                    dg = kt - (48 + qb * 4)

                    def pv(e):
                        ins = None
                        for qs in range(4):
                            ins = e.matmul(PS[qs][:, 0:257], lhsT=Es[e3][:, qs * 128:(qs + 1) * 128],
                                           rhs=Va[vi][:, kt, :], start=(kt == 0), stop=(kt == nkt - 1))
                        return ins
                    S.op("pe", pv, reads=[EB[e3], VaB[vi]], writes=[PSB[0], PSB[1], PSB[2], PSB[3]])
                    if kt != nkt - 1:
                        return
                    for qs in range(4):
                        S.op("dve", lambda e, qs=qs: e.reciprocal(out=rs[:, m * 4 + qs:m * 4 + qs + 1], in_=PS[qs][:, 256:257]),
                             reads=[PSB[qs]], writes=[rsB])
                        S.op("dve", lambda e, qs=qs: e.tensor_scalar(out=om[m][:, qs, :], in0=PS[qs][:, 0:256],
                                                                     scalar1=rs[:, m * 4 + qs:m * 4 + qs + 1], scalar2=None,
                                                                     op0=ALU.mult),
                             reads=[PSB[qs], rsB], writes=[omB[m]])
                    if m != 1:
                        return
                    y2 = hqc[0] % 2
                    hqc[0] += 1
                    S.op("dve", lambda e: e.scalar_tensor_tensor(out=od[:].rearrange("p a b -> p (a b)"),
                                                                 in0=om[1][:].rearrange("p a b -> p (a b)"),
                                                                 scalar=neglam,
                                                                 in1=om[0][:].rearrange("p a b -> p (a b)"),
                                                                 op0=ALU.mult, op1=ALU.add),
                         reads=[omB[0], omB[1], lB], writes=[odB])
                    for qs in range(4):
                        S.op("act", lambda e, qs=qs: e.activation(out=junk[:], in_=od[:, qs, :], func=AF.Square,
                                                                  accum_out=rs[:, qs:qs + 1]),
                             reads=[odB], writes=[rsB])
                    S.op("dve", lambda e: e.tensor_scalar(out=rs[:, 0:4], in0=rs[:, 0:4], scalar1=1.0 / 256, scalar2=1e-5,
                                                          op0=ALU.mult, op1=ALU.add), reads=[rsB], writes=[rsB])
                    S.op("act", lambda e: e.activation(out=rs[:, 4:8], in_=rs[:, 0:4], func=AF.Sqrt), reads=[rsB], writes=[rsB])
                    S.op("dve", lambda e: e.reciprocal(out=rs[:, 0:4], in_=rs[:, 4:8]), reads=[rsB], writes=[rsB])
                    for qs in range(4):
                        S.op("dve", lambda e, qs=qs: e.scalar_tensor_tensor(out=onb[:, qs, :], in0=od[:, qs, :],
                                                                            scalar=rs[:, qs:qs + 1], in1=sg_t[:],
                                                                            op0=ALU.mult, op1=ALU.mult),
                             reads=[odB, rsB, lB], writes=[odB])
                    for eh in range(2):
                        pT = 6 + eh
                        pvw = PS[pT][:].bitcast(BF16)

                        def tr(e, eh=eh, pvw=pvw):
                            ins = None
                            for qs in range(4):
                                ins = e.transpose(out=pvw[:, qs * 128:(qs + 1) * 128],
                                                  in_=onb[:, qs, eh * 128:(eh + 1) * 128], identity=ident_b[:])
                            return ins
                        S.op("pe", tr, reads=[odB], writes=[PSB[pT]])
                        S.op("act", lambda e, eh=eh, pvw=pvw: e.copy(out=yo[y2][:, eh, :], in_=pvw[:, 0:512]),
                             reads=[PSB[pT]], writes=[yoB[y2]])
                    S.dma("pool", yaT_d[qb, :, h * 2:(h + 1) * 2, :], yo[y2][:], reads=[yoB[y2]])

                front(0)
                for idx in range(len(iters)):
                    if idx + 1 < len(iters):
                        front(idx + 1)
                    back(idx)
                S.barrier()

        if upto >= 8:
            for which, AT, W in [(0, yrT_d, w_br_rnn), (1, yaT_d, w_br_attn)]:
                with contextlib.ExitStack() as ls:
                    gt = [sb(f"b_g{i}", [128, 512], BF16, ls) for i in range(4)]
                    m1 = [sb(f"b_m{i}", [128, 512], BF16, ls) for i in range(4)]
                    ot = [sb(f"b_o{i}", [128, 512], F32, ls) for i in range(4)]
                    ob = [sb(f"b_ob{i}", [128, 512], BF16, ls) for i in range(4)]
                    bB = [Buf() for _ in range(4)]
                    oB = [Buf() for _ in range(4)]
                    cnt = [0]

                    def epi_b(pb, ti, ct, blk, bi, which=which):
                        i2 = cnt[0] % 4
                        cnt[0] += 1
                        r0 = ti * 256 + ct * 128
                        t0 = blk * 512
                        S.dma("sp", gt[i2][:], sg_d[which, r0:r0 + 128, t0:t0 + 512], writes=[bB[i2]])
                        if which == 0:
                            S.op("dve", lambda e: e.tensor_tensor(out=ob[i2][:], in0=PS[pb][:], in1=gt[i2][:], op=ALU.mult),
                                 reads=[PSB[pb], bB[i2]], writes=[oB[i2]])
                            S.dma("pool", m1_d[r0:r0 + 128, t0:t0 + 512], ob[i2][:], reads=[oB[i2]])
                        else:
                            S.dma("sp", m1[i2][:], m1_d[r0:r0 + 128, t0:t0 + 512], writes=[bB[i2]])
                            S.op("dve", lambda e: e.tensor_tensor(out=ot[i2][:], in0=PS[pb][:], in1=gt[i2][:], op=ALU.mult),
                                 reads=[PSB[pb], bB[i2]], writes=[oB[i2]])
                            S.op("dve", lambda e: e.tensor_tensor(out=ob[i2][:], in0=ot[i2][:], in1=m1[i2][:], op=ALU.add),
                                 reads=[oB[i2], bB[i2]], writes=[oB[i2]])
                            S.dma("pool", mT_d[blk, :, r0 // 128, :], ob[i2][:], reads=[oB[i2]])
                    gemm(ls, AT, [0, 1, 2, 3], w_tiles(W, 0, 256), 8, KC, 256, "fm", None, epi_b)
                    S.barrier()
            with contextlib.ExitStack() as ls:
                xo = [sb(f"o_x{i}", [128, 256], F32, ls) for i in range(4)]
                xB = [Buf() for _ in range(4)]
                cnt = [0]

                def epi_o(pb, ti, s, blk, bi):
                    i2 = cnt[0] % 4
                    cnt[0] += 1
                    r0 = blk * 512 + s * 128
                    S.dma("sp", xo[i2][:], xf[OWN0 + r0:OWN0 + r0 + 128, ti * 256:(ti + 1) * 256], writes=[xB[i2]])
                    S.op("dve", lambda e: e.tensor_tensor(out=xo[i2][:], in0=PS[pb][:, 0:256], in1=xo[i2][:], op=ALU.add),
                         reads=[PSB[pb], xB[i2]], writes=[xB[i2]])
                    S.dma("pool", x2_d[r0:r0 + 128, ti * 256:(ti + 1) * 256], xo[i2][:], reads=[xB[i2]])
                gemm(ls, mT_d, [0, 1, 2, 3], w_tiles(w_out, 0, 256), 8, KC, 256, "tm", None, epi_o)
                S.barrier()

        if upto >= 9:
            norm_transpose(x2_d, 0, 4, hnT_d, 1e-6, tm_dst=hn_d)
            with contextlib.ExitStack() as ls:
                wrf = sb("r_wf", [128, KC, 36], F32, ls)
                wrb = sb("r_wb", [128, KC, 36], BF16, ls)
                rB = Buf()
                S.dma("sp", wrf[:], w_router.rearrange("(k p) n -> p k n", p=128), writes=[rB])

                def castr(e):
                    ins = None
                    for k in range(KC):
                        ins = e.tensor_scalar(out=wrb[:, k, :], in0=wrf[:, k, :], scalar1=gffn_s[:, k:k + 1], scalar2=None, op0=ALU.mult)
                    return ins
                S.op("dve", castr, reads=[rB], writes=[rB])
                at = [sb(f"r_at{i}", [128, KC, 512], BF16, ls) for i in range(2)]
                atB = [Buf() for _ in range(2)]
                lg = sb("r_lg", [128, 36], F32, ls)
                w = {nm: sb("r_" + nm, [128, shape], F32, ls) for nm, shape in
                     [("gmax", 1), ("gex", 4), ("gsum", 1), ("gw", 1), ("gm", 4), ("pen", 32), ("el", 32), ("m1", 1),
                      ("k1", 32), ("el2", 32), ("m2", 1), ("k2", 32), ("dl", 1), ("w1", 1), ("w2", 1), ("c", 32), ("c2", 32), ("rt", 66)]}
                wkB = Buf()
                for blk in range(4):
                    a = blk % 2
                    S.dma("sp", at[a][:], hnT_d[blk], writes=[atB[a]])
                    for s in range(4):
                        pb = s % 4

                        def mm(e, a=a, s=s, pb=pb):
                            ins = None
                            for k in range(KC):
                                ins = e.matmul(PS[pb][:, 0:36], lhsT=at[a][:, k, s * 128:(s + 1) * 128], rhs=wrb[:, k, :],
                                               start=(k == 0), stop=(k == KC - 1))
                            return ins
                        S.op("pe", mm, reads=[atB[a], rB], writes=[PSB[pb]])
                        R = [wkB]

                        def D_(fn, extra=()):
                            S.op("dve", fn, reads=R + list(extra), writes=R)
                        D_(lambda e: e.tensor_copy(out=lg[:], in_=PS[pb][:, 0:36]), extra=[PSB[pb]])
                        D_(lambda e: e.reduce_max(out=w["gmax"][:], in_=lg[:, 0:4], axis=AX.X))
                        D_(lambda e: e.tensor_scalar(out=w["gex"][:], in0=lg[:, 0:4], scalar1=w["gmax"][:, 0:1], scalar2=None, op0=ALU.subtract))
                        S.op("act", lambda e: e.activation(out=w["gex"][:], in_=w["gex"][:], func=AF.Exp), reads=R, writes=R)
                        D_(lambda e: e.reduce_sum(out=w["gsum"][:], in_=w["gex"][:], axis=AX.X))
                        D_(lambda e: e.reciprocal(out=w["gw"][:], in_=w["gsum"][:]))
                        D_(lambda e: e.tensor_scalar(out=w["gm"][:], in0=lg[:, 0:4], scalar1=w["gmax"][:, 0:1], scalar2=None, op0=ALU.is_ge))
                        for g in range(4):
                            D_(lambda e, g=g: e.tensor_scalar(out=w["pen"][:, g * 8:(g + 1) * 8], in0=lg[:, 4 + g * 8:4 + (g + 1) * 8],
                                                              scalar1=0.0, scalar2=w["gm"][:, g:g + 1], op0=ALU.mult, op1=ALU.add))
                        D_(lambda e: e.tensor_scalar(out=w["pen"][:], in0=w["pen"][:], scalar1=-1.0, scalar2=1e9, op0=ALU.add, op1=ALU.mult))
                        D_(lambda e: e.tensor_tensor(out=w["el"][:], in0=lg[:, 4:36], in1=w["pen"][:], op=ALU.add))
                        D_(lambda e: e.reduce_max(out=w["m1"][:], in_=w["el"][:], axis=AX.X))
                        D_(lambda e: e.tensor_scalar(out=w["k1"][:], in0=w["el"][:], scalar1=w["m1"][:, 0:1], scalar2=None, op0=ALU.is_ge))
                        D_(lambda e: e.scalar_tensor_tensor(out=w["el2"][:], in0=w["k1"][:], scalar=-1e9, in1=w["el"][:], op0=ALU.mult, op1=ALU.add))
                        D_(lambda e: e.reduce_max(out=w["m2"][:], in_=w["el2"][:], axis=AX.X))
                        D_(lambda e: e.tensor_scalar(out=w["k2"][:], in0=w["el2"][:], scalar1=w["m2"][:, 0:1], scalar2=None, op0=ALU.is_ge))
                        D_(lambda e: e.tensor_tensor(out=w["dl"][:], in0=w["m1"][:], in1=w["m2"][:], op=ALU.subtract))
                        S.op("act", lambda e: e.activation(out=w["w1"][:], in_=w["dl"][:], func=AF.Sigmoid), reads=R, writes=R)
                        D_(lambda e: e.tensor_scalar(out=w["w2"][:], in0=w["w1"][:], scalar1=-1.0, scalar2=1.0, op0=ALU.mult, op1=ALU.add))
                        D_(lambda e: e.tensor_tensor(out=w["w1"][:], in0=w["w1"][:], in1=w["gw"][:], op=ALU.mult))
                        D_(lambda e: e.tensor_tensor(out=w["w2"][:], in0=w["w2"][:], in1=w["gw"][:], op=ALU.mult))
                        D_(lambda e: e.tensor_scalar(out=w["c"][:], in0=w["k1"][:], scalar1=w["w1"][:, 0:1], scalar2=None, op0=ALU.mult))
                        D_(lambda e: e.scalar_tensor_tensor(out=w["c2"][:], in0=w["k2"][:], scalar=w["w2"][:, 0:1], in1=w["c"][:], op0=ALU.mult, op1=ALU.add))
                        r0 = blk * 512 + s * 128
                        S.dma("pool", C_d[r0:r0 + 128, :], w["c2"][:], reads=R)
                        D_(lambda e: e.tensor_copy(out=w["rt"][:, 0:32], in_=w["k1"][:]))
                        D_(lambda e: e.tensor_copy(out=w["rt"][:, 32:64], in_=w["k2"][:]))
                        D_(lambda e: e.tensor_copy(out=w["rt"][:, 64:65], in_=w["w1"][:]))
                        D_(lambda e: e.tensor_copy(out=w["rt"][:, 65:66], in_=w["w2"][:]))
                        S.dma("pool", R_d[r0:r0 + 128, :], w["rt"][:], reads=R)
                S.barrier()

        if upto >= 10 and not sparse:
            with contextlib.ExitStack() as ls:
                Cs = sb("m_C", [128, 16, NE], F32, ls)
                cB = Buf()
                S.dma("sp", Cs[:], C_d.rearrange("(t p) e -> p t e", p=128), writes=[cB])
                acc = sb("m_acc", [128, 8, D], F32, ls)
                accB = [Buf() for _ in range(8)]
                hn = [sb(f"m_hn{i}", [128, KC, 512], BF16, ls) for i in range(2)]
                hnB = [Buf() for _ in range(2)]
                actT = sb("m_act", [128, 8, 1024], BF16, ls)
                actB = [Buf() for _ in range(8)]
                sgt = [sb(f"m_sg{i}", [128, 512], F32, ls) for i in range(2)]
                sgB = [Buf() for _ in range(2)]
                fst = sb("m_fst", [128, 4], F32, ls)
                fB = Buf()
                wst = [sb(f"m_wst{i}", [128, 4096], F32, ls) for i in range(2)]
                wstB = [Buf() for _ in range(2)]
                wbf = [sb(f"m_wbf{i}", [128, 4096], BF16, ls) for i in range(4)]
                wbfB = [Buf() for _ in range(4)]
                wi = [0]
                sgi = [0]
                pi = [0]

                def load_w(view, kc_n, wcols, gvec):
                    a = wi[0] % 2
                    b = wi[0] % 4
                    wi[0] += 1
                    wv = wst[a][:, 0:kc_n * wcols].rearrange("p (k c) -> p k c", k=kc_n)
                    wb = wbf[b][:, 0:kc_n * wcols].rearrange("p (k c) -> p k c", k=kc_n)
                    kq = max(1, kc_n // 4)
                    for k0 in range(0, kc_n, kq):
                        S.dma("sp", wv[:, k0:k0 + kq, :], view[:, k0:k0 + kq, :], writes=[wstB[a]])
                    ceng = "dve" if wi[0] % 2 == 0 else "act"
                    if gvec is None:
                        if ceng == "dve":
                            S.op("dve", lambda e: e.tensor_copy(out=wbf[b][:, 0:kc_n * wcols], in_=wst[a][:, 0:kc_n * wcols]),
                                 reads=[wstB[a]], writes=[wbfB[b]])
                        else:
                            S.op("act", lambda e: e.copy(out=wbf[b][:, 0:kc_n * wcols], in_=wst[a][:, 0:kc_n * wcols]),
                                 reads=[wstB[a]], writes=[wbfB[b]])
                    else:
                        def cast(e):
                            ins = None
                            for k in range(kc_n):
                                if ceng == "dve":
                                    ins = e.tensor_scalar(out=wb[:, k, :], in0=wv[:, k, :], scalar1=gvec[:, k:k + 1], scalar2=None, op0=ALU.mult)
                                else:
                                    ins = e.activation(out=wb[:, k, :], in_=wv[:, k, :], func=AF.Copy, scale=gvec[:, k:k + 1])
                            return ins
                        S.op(ceng, cast, reads=[wstB[a]], writes=[wbfB[b]])
                    return wb, wbfB[b]

                for tb in range(2):
                    for j in range(2):
                        S.dma("sp", hn[j][:], hnT_d[tb * 2 + j], writes=[hnB[j]])
                    for tt in range(8):
                        r0 = tb * 1024 + tt * 128
                        S.dma("sp", acc[:, tt, :], x2_d[r0:r0 + 128, :], writes=[accB[tt]])
                    for ex in range(NE):
                        wgv = w_gate[ex].rearrange("(k p) n -> p k n", p=128)
                        wuv = w_up[ex].rearrange("(k p) n -> p k n", p=128)
                        wdv = w_down[ex].rearrange("(k p) n -> p k n", p=128)
                        for f2 in range(4):
                            wg, wgB = load_w(wgv[:, :, f2 * 256:(f2 + 1) * 256], KC, 256, gffn_s)
                            wu, wuB = load_w(wuv[:, :, f2 * 256:(f2 + 1) * 256], KC, 256, gffn_s)
                            for j in range(2):
                                for ct in range(2):
                                    fc = f2 * 2 + ct
                                    pg = (pi[0] * 2) % 4
                                    pu = pg + 1
                                    pi[0] += 1

                                    def mmg(e, W=wg, P=pg, j=j, ct=ct):
                                        ins = None
                                        for k in range(KC):
                                            ins = e.matmul(PS[P][:], lhsT=W[:, k, ct * 128:(ct + 1) * 128], rhs=hn[j][:, k, :],
                                                           start=(k == 0), stop=(k == KC - 1))
                                        return ins
                                    S.op("pe", mmg, reads=[wgB, hnB[j]], writes=[PSB[pg]])
                                    S.op("pe", lambda e: mmg(e, W=wu, P=pu), reads=[wuB, hnB[j]], writes=[PSB[pu]])
                                    s2 = sgi[0] % 2
                                    sgi[0] += 1
                                    S.op("act", lambda e: e.activation(out=sgt[s2][:], in_=PS[pg][:], func=AF.Silu),
                                         reads=[PSB[pg]], writes=[sgB[s2]])
                                    S.op("dve", lambda e: e.tensor_tensor(out=actT[:, fc, j * 512:(j + 1) * 512], in0=PS[pu][:],
                                                                          in1=sgt[s2][:], op=ALU.mult),
                                         reads=[PSB[pu], sgB[s2]], writes=[actB[fc]])
                        for cg in range(4):
                            wd, wdB = load_w(wdv[:, :, cg * 512:(cg + 1) * 512], 8, 512, None)
                            for tt in range(8):
                                pd = 4 + (pi[0] % 4)
                                pi[0] += 1

                                def mmd(e, wd=wd, pd=pd, tt=tt):
                                    ins = None
                                    for k in range(8):
                                        ins = e.matmul(PS[pd][:], lhsT=actT[:, k, tt * 128:(tt + 1) * 128], rhs=wd[:, k, :],
                                                       start=(k == 0), stop=(k == 7))
                                    return ins
                                S.op("pe", mmd, reads=[wdB] + actB, writes=[PSB[pd]])
                                S.op("dve", lambda e: e.scalar_tensor_tensor(out=acc[:, tt, cg * 512:(cg + 1) * 512], in0=PS[pd][:],
                                                                             scalar=Cs[:, tb * 8 + tt, ex:ex + 1],
                                                                             in1=acc[:, tt, cg * 512:(cg + 1) * 512],
                                                                             op0=ALU.mult, op1=ALU.add),
                                     reads=[PSB[pd], cB, accB[tt]], writes=[accB[tt]])
                    gfin = wst[0][:, 0:D]
                    fjunk = actT[:, 0:2, :]
                    S.dma("sp", gfin, g_final.rearrange("(o t) -> o t", o=1).partition_broadcast(128), writes=[wstB[0]])
                    for tt in range(8):
                        r0 = tb * 1024 + tt * 128
                        S.op("act", lambda e: e.activation(out=fjunk, in_=acc[:, tt, :].rearrange("p (a b) -> p a b", a=2), func=AF.Square, accum_out=fst[:, 0:1]),
                             reads=[accB[tt]], writes=[fB, actB[0], actB[1]])
                        S.op("dve", lambda e: e.tensor_scalar(out=fst[:, 1:2], in0=fst[:, 0:1], scalar1=1.0 / D, scalar2=1e-6,
                                                              op0=ALU.mult, op1=ALU.add), reads=[fB], writes=[fB])
                        S.op("act", lambda e: e.activation(out=fst[:, 3:4], in_=fst[:, 1:2], func=AF.Sqrt), reads=[fB], writes=[fB])
                        S.op("dve", lambda e: e.reciprocal(out=fst[:, 2:3], in_=fst[:, 3:4]), reads=[fB], writes=[fB])
                        S.op("dve", lambda e: e.scalar_tensor_tensor(out=acc[:, tt, :], in0=acc[:, tt, :], scalar=fst[:, 2:3],
                                                                     in1=gfin, op0=ALU.mult, op1=ALU.mult),
                             reads=[accB[tt], fB, wstB[0]], writes=[accB[tt]])
                        S.dma("pool", out[r0:r0 + 128, :], acc[:, tt, :], reads=[accB[tt]])
                S.barrier()
        if upto >= 10 and sparse:
            with contextlib.ExitStack() as ls:
                Rs = sb("s_R", [128, 16, 66], F32, ls)
                rB = Buf()
                S.dma("sp", Rs[:], R_d.rearrange("(t p) e -> p t e", p=128), writes=[rB])
                lsf = sb("s_lsf", [128, 128], F32, ls)
                lsb = sb("s_lsb", [128, 128], BF16, ls)
                onb = sb("s_onb", [128, 128], BF16, ls)
                iog = sb("s_iog", [128, 16], F32, ls)
                iod = sb("s_iod", [128, 8], F32, ls)
                S.dma("sp", lsf[:], c_ls[:, :], writes=[rB])
                S.dma("sp", iog[:], c_iog[:, :], writes=[rB])
                S.dma("sp", iod[:], c_iod[:, :], writes=[rB])
                S.op("dve", lambda e: e.tensor_copy(out=lsb[:], in_=lsf[:]), reads=[rB], writes=[rB])
                S.op("dve", lambda e: e.memset(onb[:], 1.0), writes=[rB])
                Ab = sb("s_Ab", [128, 16, 32], BF16, ls)
                S.op("dve", lambda e: e.tensor_tensor(out=Ab[:], in0=Rs[:, :, 0:32], in1=Rs[:, :, 32:64], op=ALU.add), reads=[rB], writes=[rB])
                def mmc(e):
                    ins = None
                    for i in range(16):
                        ins = e.matmul(PS[0][:, 0:32], lhsT=onb[:], rhs=Ab[:, i, :], start=(i == 0), stop=(i == 15))
                    return ins
                S.op("pe", mmc, reads=[rB], writes=[PSB[0]])
                cnt = sb("s_cnt", [128, 32], F32, ls)
                nb = sb("s_nb", [128, 32], F32, ls)
                pend = sb("s_pend", [128, 32], F32, ls)
                pst = sb("s_pst", [128, 32], F32, ls)
                one32 = sb("s_one32", [128, 32], F32, ls)
                tmp32 = sb("s_tmp32", [128, 32], F32, ls)
                eb = sb("s_eb", [128, 64], F32, ls)
                cB = Buf()
                S.op("dve", lambda e: e.tensor_copy(out=cnt[:], in_=PS[0][:, 0:32]), reads=[PSB[0]], writes=[cB])
                S.op("dve", lambda e: e.memset(nb[:], 0.0), writes=[cB])
                S.op("dve", lambda e: e.memset(one32[:], 1.0), writes=[cB])
                for j in range(16):
                    S.op("dve", lambda e, j=j: e.scalar_tensor_tensor(out=nb[:], in0=cnt[:], scalar=128.0 * j, in1=nb[:],
                                                                      op0=ALU.is_gt, op1=ALU.add), reads=[cB], writes=[cB])
                S.op("dve", lambda e: e.tensor_scalar(out=nb[:], in0=nb[:], scalar1=128.0, scalar2=None, op0=ALU.mult), reads=[cB], writes=[cB])
                S.op("dve", lambda e: e.tensor_tensor_scan(out=pend[:], data0=one32[:], data1=nb[:], initial=0.0,
                                                           op0=ALU.mult, op1=ALU.add), reads=[cB], writes=[cB])
                S.op("dve", lambda e: e.tensor_tensor(out=pst[:], in0=pend[:], in1=nb[:], op=ALU.subtract), reads=[cB], writes=[cB])
                for b_ in range(64):
                    S.op("dve", lambda e, b_=b_: e.tensor_scalar(out=tmp32[:], in0=pend[:], scalar1=128.0 * b_, scalar2=0.0,
                                                                 op0=ALU.is_le, op1=ALU.add, accum_out=eb[:, b_:b_ + 1]),
                         reads=[cB], writes=[cB])
                S.op("dve", lambda e: e.tensor_scalar(out=eb[:], in0=eb[:], scalar1=31.0, scalar2=None, op0=ALU.min), reads=[cB], writes=[cB])
                ebg = sb("s_ebg", [128, 64], F32, ls)
                ebd = sb("s_ebd", [128, 64], F32, ls)
                S.op("dve", lambda e: e.tensor_scalar(out=ebg[:], in0=eb[:], scalar1=2048.0, scalar2=None, op0=ALU.mult), reads=[cB], writes=[cB])
                S.op("dve", lambda e: e.tensor_scalar(out=ebd[:], in0=eb[:], scalar1=1024.0, scalar2=None, op0=ALU.mult), reads=[cB], writes=[cB])
                igf = sb("s_igf", [128, 64, 16], F32, ls)
                idf = sb("s_idf", [128, 64, 8], F32, ls)
                igi = sb("s_igi", [128, 64, 16], I32, ls)
                idi = sb("s_idi", [128, 64, 8], I32, ls)
                for b_ in range(64):
                    S.op("dve", lambda e, b_=b_: e.tensor_scalar(out=igf[:, b_, :], in0=iog[:], scalar1=ebg[:, b_:b_ + 1], scalar2=None, op0=ALU.add),
                         reads=[cB, rB], writes=[cB])
                    S.op("dve", lambda e, b_=b_: e.tensor_scalar(out=idf[:, b_, :], in0=iod[:], scalar1=ebd[:, b_:b_ + 1], scalar2=None, op0=ALU.add),
                         reads=[cB, rB], writes=[cB])
                S.op("dve", lambda e: e.tensor_copy(out=igi[:], in_=igf[:]), reads=[cB], writes=[cB])
                S.op("dve", lambda e: e.tensor_copy(out=idi[:], in_=idf[:]), reads=[cB], writes=[cB])
                dsf = sb("s_dsf", [128, 16, 2], F32, ls)
                dsi = sb("s_dsi", [128, 16, 2], I32, ls)
                pos = sb("s_pos", [128, 32], F32, ls)
                dB = Buf()
                for i in range(16):
                    pb = 1 + (i % 3)

                    def mmr(e, i=i, pb=pb):
                        for i2 in range(i):
                            e.matmul(PS[pb][:, 0:32], lhsT=onb[:], rhs=Ab[:, i2, :], start=(i2 == 0), stop=False)
                        return e.matmul(PS[pb][:, 0:32], lhsT=lsb[:], rhs=Ab[:, i, :], start=(i == 0), stop=True)
                    S.op("pe", mmr, reads=[rB], writes=[PSB[pb]])
                    S.op("dve", lambda e: e.tensor_tensor(out=pos[:], in0=PS[pb][:, 0:32], in1=pst[:], op=ALU.add),
                         reads=[PSB[pb], cB, dB], writes=[dB])
                    for k_ in range(2):
                        S.op("dve", lambda e, k_=k_: e.tensor_tensor(out=tmp32[:], in0=pos[:], in1=Rs[:, i, k_ * 32:(k_ + 1) * 32], op=ALU.mult),
                             reads=[dB, rB, cB], writes=[cB])
                        S.op("dve", lambda e, k_=k_: e.reduce_sum(out=dsf[:, i, k_:k_ + 1], in_=tmp32[:], axis=AX.X), reads=[cB, dB], writes=[dB])
                S.op("dve", lambda e: e.tensor_copy(out=dsi[:], in_=dsf[:]), reads=[dB], writes=[dB])
                ht = [sb(f"s_ht{i}", [128, D], BF16, ls) for i in range(2)]
                htB = [Buf() for _ in range(2)]
                xsB = Buf()
                for i in range(16):
                    a = i % 2
                    S.dma("sp", ht[a][:], hn_d[i * 128:(i + 1) * 128, :], writes=[htB[a]])
                    for k_ in range(2):
                        S.idma(xs_d[:, :], bass.IndirectOffsetOnAxis(ap=dsi[:, i, k_:k_ + 1], axis=0), ht[a][:], None,
                               reads=[htB[a], dB], writes=[xsB])
                S.barrier()
                wg_rows = w_gate.rearrange("e k n -> (e k) n")
                wu_rows = w_up.rearrange("e k n -> (e k) n")
                wd_rows = w_down.rearrange("e k n -> (e k) n")
                xb = [sb(f"s_xb{i}", [128, D], BF16, ls) for i in range(2)]
                xbB = [Buf() for _ in range(2)]
                xT = [sb(f"s_xT{i}", [128, KC, 128], BF16, ls) for i in range(2)]
                xTB = [Buf() for _ in range(2)]
                gst = [sb(f"s_gst{i}", [128, 1024], F32, ls) for i in range(8)]
                gstB = [Buf() for _ in range(8)]
                gbf = [sb(f"s_gbf{i}", [128, 1024], BF16, ls) for i in range(8)]
                gbfB = [Buf() for _ in range(8)]
                dst_ = [sb(f"s_dst{i}", [128, D], F32, ls) for i in range(4)]
                dstB = [Buf() for _ in range(4)]
                dbf = [sb(f"s_dbf{i}", [128, D], BF16, ls) for i in range(4)]
                dbfB = [Buf() for _ in range(4)]
                sgl = sb("s_sgl", [128, 1024], F32, ls)
                sglB = Buf()
                actb = sb("s_act", [128, 1024], BF16, ls)
                actB_ = Buf()
                aT = sb("s_aT", [128, 8, 128], BF16, ls)
                aTB = Buf()
                yb = [sb(f"s_yb{i}", [128, D], BF16, ls) for i in range(2)]
                ybB = [Buf() for _ in range(2)]
                ysB = Buf()
                gi = 0
                gbi = 0
                di = 0
                dbi = 0
                ce = 0
                for b_ in range(64):
                    x2i = b_ % 2
                    S.dma("sp", xb[x2i][:], xs_d[b_ * 128:(b_ + 1) * 128, :], writes=[xbB[x2i]])
                    for g4 in range(4):
                        pb = 4 + g4
                        pvw = PS[pb][:].bitcast(BF16)

                        def tr(e, g4=g4, pvw=pvw, x2i=x2i):
                            ins = None
                            for q in range(4):
                                kc = g4 * 4 + q
                                ins = e.transpose(out=pvw[:, q * 128:(q + 1) * 128], in_=xb[x2i][:, kc * 128:(kc + 1) * 128], identity=ident_b[:])
                            return ins
                        S.op("pe", tr, reads=[xbB[x2i]], writes=[PSB[pb]])
                        eng = "act" if g4 % 2 == 0 else "dve"
                        o_ = xT[x2i][:, g4 * 4:(g4 + 1) * 4, :]
                        i_ = pvw[:, 0:512].rearrange("p (k t) -> p k t", k=4)
                        if eng == "act":
                            S.op("act", lambda e: e.copy(out=o_, in_=i_), reads=[PSB[pb]], writes=[xTB[x2i]])
                        else:
                            S.op("dve", lambda e: e.tensor_copy(out=o_, in_=i_), reads=[PSB[pb]], writes=[xTB[x2i]])
                    for kc in range(KC):
                        wpair = []
                        for rows in (wg_rows, wu_rows):
                            a = gi % 8
                            gi += 1
                            bq = gbi % 8
                            gbi += 1
                            S.idma(gst[a][:], None, rows[:, :], bass.IndirectOffsetOnAxis(ap=igi[:, b_, kc:kc + 1], axis=0),
                                   reads=[cB], writes=[gstB[a]])
                            ceng = "dve" if ce % 2 == 0 else "act"
                            ce += 1
                            if ceng == "dve":
                                S.op("dve", lambda e: e.tensor_scalar(out=gbf[bq][:], in0=gst[a][:], scalar1=gffn_s[:, kc:kc + 1], scalar2=None, op0=ALU.mult),
                                     reads=[gstB[a]], writes=[gbfB[bq]])
                            else:
                                S.op("act", lambda e: e.activation(out=gbf[bq][:], in_=gst[a][:], func=AF.Copy, scale=gffn_s[:, kc:kc + 1]),
                                     reads=[gstB[a]], writes=[gbfB[bq]])
                            wpair.append(bq)

                        def mmgu(e, kc=kc, wpair=wpair, x2i=x2i):
                            ins = None
                            for wi_, bq in enumerate(wpair):
                                for hf in range(2):
                                    ins = e.matmul(PS[wi_ * 2 + hf][:], lhsT=xT[x2i][:, kc, :], rhs=gbf[bq][:, hf * 512:(hf + 1) * 512],
                                                   start=(kc == 0), stop=(kc == KC - 1))
                            return ins
                        S.op("pe", mmgu, reads=[xTB[x2i], gbfB[wpair[0]], gbfB[wpair[1]]], writes=[PSB[0], PSB[1], PSB[2], PSB[3]])
                    for hf in range(2):
                        S.op("act", lambda e, hf=hf: e.activation(out=sgl[:, hf * 512:(hf + 1) * 512], in_=PS[hf][:], func=AF.Silu),
                             reads=[PSB[hf]], writes=[sglB])
                        S.op("dve", lambda e, hf=hf: e.tensor_tensor(out=actb[:, hf * 512:(hf + 1) * 512], in0=PS[2 + hf][:],
                                                                     in1=sgl[:, hf * 512:(hf + 1) * 512], op=ALU.mult),
                             reads=[PSB[2 + hf], sglB], writes=[actB_])
                    for g2 in range(2):
                        pb = g2
                        pvw = PS[pb][:].bitcast(BF16)

                        def tr2(e, g2=g2, pvw=pvw):
                            ins = None
                            for q in range(4):
                                fc = g2 * 4 + q
                                ins = e.transpose(out=pvw[:, q * 128:(q + 1) * 128], in_=actb[:, fc * 128:(fc + 1) * 128], identity=ident_b[:])
                            return ins
                        S.op("pe", tr2, reads=[actB_], writes=[PSB[pb]])
                        o_ = aT[:, g2 * 4:(g2 + 1) * 4, :]
                        i_ = pvw[:, 0:512].rearrange("p (k t) -> p k t", k=4)
                        if g2 == 0:
                            S.op("act", lambda e: e.copy(out=o_, in_=i_), reads=[PSB[pb]], writes=[aTB])
                        else:
                            S.op("dve", lambda e: e.tensor_copy(out=o_, in_=i_), reads=[PSB[pb]], writes=[aTB])
                    for fc in range(8):
                        a = di % 4
                        di += 1
                        bq = dbi % 4
                        dbi += 1
                        S.idma(dst_[a][:], None, wd_rows[:, :], bass.IndirectOffsetOnAxis(ap=idi[:, b_, fc:fc + 1], axis=0),
                               reads=[cB], writes=[dstB[a]])
                        if fc % 2 == 0:
                            S.op("dve", lambda e: e.tensor_copy(out=dbf[bq][:], in_=dst_[a][:]), reads=[dstB[a]], writes=[dbfB[bq]])
                        else:
                            S.op("act", lambda e: e.copy(out=dbf[bq][:], in_=dst_[a][:]), reads=[dstB[a]], writes=[dbfB[bq]])

                        def mmd(e, fc=fc, bq=bq):
                            ins = None
                            for cg in range(4):
                                ins = e.matmul(PS[4 + cg][:], lhsT=aT[:, fc, :], rhs=dbf[bq][:, cg * 512:(cg + 1) * 512],
                                               start=(fc == 0), stop=(fc == 7))
                            return ins
                        S.op("pe", mmd, reads=[aTB, dbfB[bq]], writes=[PSB[4], PSB[5], PSB[6], PSB[7]])
                    for cg in range(4):
                        if cg % 2 == 0:
                            S.op("act", lambda e, cg=cg: e.copy(out=yb[x2i][:, cg * 512:(cg + 1) * 512], in_=PS[4 + cg][:]),
                                 reads=[PSB[4 + cg]], writes=[ybB[x2i]])
                        else:
                            S.op("dve", lambda e, cg=cg: e.tensor_copy(out=yb[x2i][:, cg * 512:(cg + 1) * 512], in_=PS[4 + cg][:]),
                                 reads=[PSB[4 + cg]], writes=[ybB[x2i]])
                    S.dma("sp", ys_d[b_ * 128:(b_ + 1) * 128, :], yb[x2i][:], reads=[ybB[x2i]], writes=[ysB])
                S.barrier()
                gfin = sb("s_gf", [128, D], F32, ls)
                fB0 = Buf()
                S.dma("sp", gfin[:], g_final.rearrange("(o t) -> o t", o=1).partition_broadcast(128), writes=[fB0])
                g1 = [sb(f"s_g1{i}", [128, D], BF16, ls) for i in range(2)]
                g2_ = [sb(f"s_g2{i}", [128, D], BF16, ls) for i in range(2)]
                xr = [sb(f"s_xr{i}", [128, D], F32, ls) for i in range(2)]
                gB_ = [Buf() for _ in range(2)]
                xrB = [Buf() for _ in range(2)]
                fst = [sb(f"s_fst{i}", [128, 4], F32, ls) for i in range(2)]
                fj = sb("s_fj", [128, D], BF16, ls)
                fjB = Buf()
                for i in range(16):
                    a = i % 2
                    S.dma("sp", xr[a][:], x2_d[i * 128:(i + 1) * 128, :], writes=[xrB[a]])
                    S.idma(g1[a][:], None, ys_d[:, :], bass.IndirectOffsetOnAxis(ap=dsi[:, i, 0:1], axis=0), reads=[dB, ysB], writes=[gB_[a]])
                    S.idma(g2_[a][:], None, ys_d[:, :], bass.IndirectOffsetOnAxis(ap=dsi[:, i, 1:2], axis=0), reads=[dB, ysB], writes=[gB_[a]])
                    S.op("dve", lambda e: e.scalar_tensor_tensor(out=xr[a][:], in0=g1[a][:], scalar=Rs[:, i, 64:65], in1=xr[a][:],
                                                                 op0=ALU.mult, op1=ALU.add), reads=[gB_[a], rB, xrB[a]], writes=[xrB[a]])
                    S.op("dve", lambda e: e.scalar_tensor_tensor(out=xr[a][:], in0=g2_[a][:], scalar=Rs[:, i, 65:66], in1=xr[a][:],
                                                                 op0=ALU.mult, op1=ALU.add), reads=[gB_[a], rB, xrB[a]], writes=[xrB[a]])
                    S.op("act", lambda e: e.activation(out=fj[:], in_=xr[a][:], func=AF.Square, accum_out=fst[a][:, 0:1]),
                         reads=[xrB[a]], writes=[fjB, xrB[a]])
                    S.op("dve", lambda e: e.tensor_scalar(out=fst[a][:, 1:2], in0=fst[a][:, 0:1], scalar1=1.0 / D, scalar2=1e-6,
                                                          op0=ALU.mult, op1=ALU.add), reads=[xrB[a]], writes=[xrB[a]])
                    S.op("act", lambda e: e.activation(out=fst[a][:, 3:4], in_=fst[a][:, 1:2], func=AF.Sqrt), reads=[xrB[a]], writes=[xrB[a]])
                    S.op("dve", lambda e: e.reciprocal(out=fst[a][:, 2:3], in_=fst[a][:, 3:4]), reads=[xrB[a]], writes=[xrB[a]])
                    S.op("dve", lambda e: e.scalar_tensor_tensor(out=xr[a][:], in0=xr[a][:], scalar=fst[a][:, 2:3], in1=gfin[:],
                                                                 op0=ALU.mult, op1=ALU.mult), reads=[xrB[a], fB0], writes=[xrB[a]])
                    S.dma("sp", out[i * 128:(i + 1) * 128, :], xr[a][:], reads=[xrB[a]])
                S.barrier()
        S.barrier()
    return nc


def _consts():
    i = np.arange(64, dtype=np.float32)
    inv = (1.0 / (10000.0 ** (np.arange(0, 128, 2, dtype=np.float32) / 128.0))).astype(np.float32)
    c_rope = np.zeros((128, 2), np.float32)
    c_rope[:, 0] = np.concatenate([inv, inv])
    c_rope[:64, 1] = -1.0
    c_rope[64:, 1] = 1.0
    c_swap = np.zeros((128, 128), np.float32)
    for m in range(128):
        c_swap[(m + 64) % 128, m] = 1.0
    c_ident = np.eye(128, dtype=np.float32)
    kl = np.arange(128)[:, None, None] + 128 * np.arange(4)[None, :, None]
    ql = np.arange(512)[None, None, :]
    c_tri = (kl <= ql).astype(np.float32)
    c_ls = (np.arange(128)[:, None] < np.arange(128)[None, :]).astype(np.float32)
    c_iog = (np.arange(16)[None, :] * 128 + np.arange(128)[:, None]).astype(np.float32)
    c_iod = (np.arange(8)[None, :] * 128 + np.arange(128)[:, None]).astype(np.float32)
    return dict(c_rope=c_rope, c_swap=c_swap, c_ident=c_ident, c_tri=np.ascontiguousarray(c_tri),
                c_ls=c_ls, c_iog=np.ascontiguousarray(c_iog), c_iod=np.ascontiguousarray(c_iod))


def make_in_maps(inputs):
    x = np.asarray(inputs["x"], np.float32)
    pos = np.asarray(inputs["positions"], np.int32)
    f = lambda k: np.ascontiguousarray(np.asarray(inputs[k], np.float32)[0])
    pk = lambda v: np.ascontiguousarray(v.reshape(-1, 128).T)
    shared = dict(
        g_mix=pk(f("g_mix")), w_in=f("w_in"), conv_w=np.ascontiguousarray(f("conv_w").reshape(4, KC, 128).transpose(2, 0, 1)), conv_b=pk(f("conv_b")),
        w_rg_a=f("w_rg_a"), b_rg_a=pk(f("b_rg_a")), w_rg_x=f("w_rg_x"), b_rg_x=pk(f("b_rg_x")),
        lru_param=pk(f("lru_param")),
        lam_in=np.ascontiguousarray(np.stack([f("lambda_q1"), f("lambda_k1"), f("lambda_q2"), f("lambda_k2")])),
        subln_g=f("subln_g"), w_br_rnn=f("w_br_rnn"), w_br_attn=f("w_br_attn"), w_out=f("w_out"),
        g_ffn=pk(f("g_ffn")),
        w_router=np.ascontiguousarray(np.concatenate([f("w_grp_router"), f("w_exp_router")], axis=1)),
        w_gate=f("w_gate"), w_up=f("w_up"), w_down=f("w_down"),
        g_final=np.ascontiguousarray(np.asarray(inputs["g_final"], np.float32)),
    )
    shared.update(_consts())
    maps = []
    for b in range(2):
        for j in range(4):
            n = (j + 1) * 2048
            xfp = np.zeros((T, D), np.float32)
            xfp[T - n:] = x[b, :n]
            pp = np.zeros((T,), np.int32)
            pp[T - n:] = pos[b, :n]
            vv = np.zeros((T,), np.float32)
            vv[T - n:] = 1.0
            m = dict(shared)
            vp = np.zeros((T,), np.float32)
            vp[1:] = vv[:-1]
            m.update(xf=xfp, posf=pp, valid=vv, validp=vp, valid_pk=pk(vv))
            maps.append(m)
    return maps


def kernel(**inputs):
    nc = build()
    maps = make_in_maps(inputs)
    res = run_bass_kernel_spmd(nc, maps, core_ids=list(range(8)))
    outp = np.zeros((2, 8192, D), np.float32)
    for c in range(8):
        b, j = c // 4, c % 4
        outp[b, j * 2048:(j + 1) * 2048] = res.results[c]["out"]
    return outp
```

```python
import math
import contextlib
import numpy as np
import concourse.bass as bass
import concourse.mybir as mybir
from concourse.bass_utils import run_bass_kernel_spmd

F32 = mybir.dt.float32
BF16 = mybir.dt.bfloat16
I32 = mybir.dt.int32
ALU = mybir.AluOpType
AF = mybir.ActivationFunctionType
AX = mybir.AxisListType

D = 2048
T = 8192
OWN0 = 6144
NOWN = 2048
KC = 16
NE = 32
DE = 1024
NH = 8
TWO_PI = 2.0 * math.pi


class Buf:
    __slots__ = ("w", "r")

    def __init__(self):
        self.w = None
        self.r = {}


class Sched:
    def __init__(self, nc, es):
        self.nc = nc
        self.st = {}
        for name, h, nd in [("pe", nc.tensor, 0), ("dve", nc.vector, 0), ("act", nc.scalar, 6),
                            ("pool", nc.gpsimd, 20), ("sp", nc.sync, 10)]:
            st = dict(h=h, cnt=0, seen={}, dcnt=0, name=name)
            st["sem"] = es.enter_context(nc.semaphore("c_" + name))
            st["dsems"] = [es.enter_context(nc.semaphore(f"d_{name}{i}")) for i in range(nd)]
            self.st[name] = st

    @staticmethod
    def _add(d, ev):
        if ev is None:
            return
        sem, val, key = ev
        if key not in d or d[key][1] < val:
            d[key] = (sem, val, key)

    def _deps(self, reads, writes):
        d = {}
        for b in reads:
            self._add(d, b.w)
        for b in writes:
            self._add(d, b.w)
            for ev in b.r.values():
                self._add(d, ev)
        return d

    def _wait(self, st, d, skip=None):
        for key, (sem, val, _) in d.items():
            if key == skip:
                continue
            if st["seen"].get(key, 0) >= val:
                continue
            st["h"].wait_ge(sem, val)
            st["seen"][key] = val

    def _mark(self, reads, writes, ev):
        for b in reads:
            self._add(b.r, ev)
        for b in writes:
            b.w = ev
            b.r = {}

    def op(self, stname, fn, reads=(), writes=()):
        st = self.st[stname]
        d = self._deps(reads, writes)
        self._wait(st, d, skip=("c_pe" if stname == "pe" else None))
        ins = fn(st["h"])
        st["cnt"] += 1
        ins.then_inc(st["sem"], 1)
        self._mark(reads, writes, (st["sem"], st["cnt"], "c_" + stname))

    def dma(self, stname, out, in_, reads=(), writes=()):
        st = self.st[stname]
        d = self._deps(reads, writes)
        i = st["dcnt"]
        R = len(st["dsems"])
        k = i % R
        sem = st["dsems"][k]
        val = 16 * (i // R + 1)
        key = f"d_{stname}{k}"
        if i >= R:
            self._add(d, (sem, val - 16, key))
        self._wait(st, d)
        st["h"].dma_start(out=out, in_=in_).then_inc(sem, 16)
        st["dcnt"] += 1
        self._mark(reads, writes, (sem, val, key))

    def idma(self, out, out_off, in_, in_off, reads=(), writes=()):
        st = self.st["pool"]
        d = self._deps(reads, writes)
        i = st["dcnt"]
        R = len(st["dsems"])
        k = i % R
        sem = st["dsems"][k]
        val = 16 * (i // R + 1)
        key = f"d_pool{k}"
        if i >= R:
            self._add(d, (sem, val - 16, key))
        self._wait(st, d)
        st["h"].indirect_dma_start(out=out, out_offset=out_off, in_=in_, in_offset=in_off).then_inc(sem, 16)
        st["dcnt"] += 1
        self._mark(reads, writes, (sem, val, key))

    def all_events(self):
        d = {}
        for name, st in self.st.items():
            if st["cnt"] > 0:
                self._add(d, (st["sem"], st["cnt"], "c_" + name))
            R = len(st["dsems"])
            for k in range(R):
                n = (st["dcnt"] - k + R - 1) // R if st["dcnt"] > k else 0
                if n > 0:
                    self._add(d, (st["dsems"][k], 16 * n, f"d_{name}{k}"))
        return d

    def barrier(self, only=None):
        d = self.all_events()
        for name, st in self.st.items():
            if only is not None and name not in only:
                continue
            self._wait(st, d)


def build(upto=99, debug=(), sparse=True):
    nc = bass.Bass("TRN2", target_bir_lowering=False)
    dbg = set(debug)

    def dram_in(name, shape, dt=F32):
        return nc.dram_tensor(name, list(shape), dt, kind="ExternalInput").ap()

    def dram_tmp(name, shape, dt):
        kind = "ExternalOutput" if name in dbg else "Internal"
        return nc.dram_tensor(name, list(shape), dt, kind=kind).ap()

    xf = dram_in("xf", [T, D])
    posf = dram_in("posf", [T], I32)
    valid = dram_in("valid", [T])
    validp = dram_in("validp", [T])
    g_mix = dram_in("g_mix", [128, KC])
    w_in = dram_in("w_in", [D, 14336])
    conv_w = dram_in("conv_w", [128, 4, KC])
    conv_b = dram_in("conv_b", [128, KC])
    w_rg_a = dram_in("w_rg_a", [16, 128, 128])
    b_rg_a = dram_in("b_rg_a", [128, KC])
    w_rg_x = dram_in("w_rg_x", [16, 128, 128])
    b_rg_x = dram_in("b_rg_x", [128, KC])
    lru_param = dram_in("lru_param", [128, KC])
    lam_in = dram_in("lam_in", [4, 128])
    subln_g = dram_in("subln_g", [256])
    w_br_rnn = dram_in("w_br_rnn", [D, D])
    w_br_attn = dram_in("w_br_attn", [D, D])
    w_out = dram_in("w_out", [D, D])
    g_ffn = dram_in("g_ffn", [128, KC])
    valid_pk = dram_in("valid_pk", [128, 64])
    w_router = dram_in("w_router", [D, 36])
    w_gate = dram_in("w_gate", [NE, D, DE])
    w_up = dram_in("w_up", [NE, D, DE])
    w_down = dram_in("w_down", [NE, DE, D])
    g_final = dram_in("g_final", [D])
    c_rope = dram_in("c_rope", [128, 2])
    c_swap = dram_in("c_swap", [128, 128])
    c_ident = dram_in("c_ident", [128, 128])
    c_tri = dram_in("c_tri", [128, 4, 512])
    c_ls = dram_in("c_ls", [128, 128])
    c_iog = dram_in("c_iog", [128, 16])
    c_iod = dram_in("c_iod", [128, 8])
    out = nc.dram_tensor("out", [NOWN, D], F32, kind="ExternalOutput").ap()

    hT_d = dram_tmp("hT_d", [16, 128, KC, 512], BF16)
    uT_d = dram_tmp("uT_d", [D, T], BF16)
    KT_d = dram_tmp("KT_d", [16, 128, T], BF16)
    V_d = dram_tmp("V_d", [T, D], BF16)
    gbT_d = dram_tmp("gbT_d", [D, NOWN], BF16)
    QT_d = dram_tmp("QT_d", [16, 128, NOWN], BF16)
    sg_d = dram_tmp("sg_d", [2, D, NOWN], BF16)
    cos_d = dram_tmp("cos_d", [16, 128, 512], F32)
    sin_d = dram_tmp("sin_d", [16, 128, 512], F32)
    yrT_d = dram_tmp("yrT_d", [4, 128, KC, 512], BF16)
    yaT_d = dram_tmp("yaT_d", [4, 128, KC, 512], BF16)
    m1_d = dram_tmp("m1_d", [D, NOWN], BF16)
    mT_d = dram_tmp("mT_d", [4, 128, KC, 512], BF16)
    x2_d = dram_tmp("x2_d", [NOWN, D], F32)
    hnT_d = dram_tmp("hnT_d", [4, 128, KC, 512], BF16)
    C_d = dram_tmp("C_d", [NOWN, NE], F32)
    R_d = dram_tmp("R_d", [NOWN, 66], F32)
    hn_d = dram_tmp("hn_d", [NOWN, D], BF16)
    xs_d = dram_tmp("xs_d", [8192, D], BF16)
    ys_d = dram_tmp("ys_d", [8192, D], BF16)

    with contextlib.ExitStack() as es:
        S = Sched(nc, es)

        uid = [0]

        def sb(name, shape, dt, stack=es):
            uid[0] += 1
            return stack.enter_context(nc.sbuf_tensor(f"{name}_{uid[0]}", list(shape), dt))

        PS = [es.enter_context(nc.psum_tensor(f"ps{i}", [128, 512], F32)) for i in range(8)]
        PSB = [Buf() for _ in range(8)]

        ident_f = sb("ident_f", [128, 128], F32)
        ident_b = sb("ident_b", [128, 128], BF16)
        swap_f = sb("swap_f", [128, 128], F32)
        swap_b = sb("swap_b", [128, 128], BF16)
        rope_c = sb("rope_c", [128, 2], F32)
        gmix_s = sb("gmix_s", [128, KC], F32)
        gffn_s = sb("gffn_s", [128, KC], F32)
        cst = Buf()
        S.dma("sp", ident_f[:], c_ident[:, :], writes=[cst])
        S.dma("sp", swap_f[:], c_swap[:, :], writes=[cst])
        S.dma("sp", rope_c[:], c_rope[:, :], writes=[cst])
        S.dma("sp", gmix_s[:], g_mix[:, :], writes=[cst])
        S.dma("sp", gffn_s[:], g_ffn[:, :], writes=[cst])
        S.op("dve", lambda e: e.tensor_copy(out=ident_b[:], in_=ident_f[:]), reads=[cst], writes=[cst])
        S.op("dve", lambda e: e.tensor_copy(out=swap_b[:], in_=swap_f[:]), reads=[cst], writes=[cst])
        S.barrier()

        def norm_transpose(src, row0, nblk, dst_d, eps, tm_dst=None):
            with contextlib.ExitStack() as ls:
                xt = [sb(f"nt_x{i}", [128, D], F32, ls) for i in range(2)]
                xtB = [Buf() for _ in range(2)]
                junk = sb("nt_junk", [128, D], BF16, ls)
                junkB = Buf()
                xn = [sb(f"nt_xn{i}", [128, D], BF16, ls) for i in range(2)]
                xnB = [Buf() for _ in range(2)]
                st_ = [sb(f"nt_s{i}", [128, 4], F32, ls) for i in range(2)]
                stB = [Buf() for _ in range(2)]
                hb = [sb(f"nt_h{i}", [128, KC, 512], BF16, ls) for i in range(2)]
                hbB = [Buf() for _ in range(2)]
                it = 0
                for blk in range(nblk):
                    h = hb[blk % 2]
                    hB = hbB[blk % 2]
                    for s in range(4):
                        i2 = it % 2
                        r0 = row0 + (blk * 4 + s) * 128
                        S.dma("sp", xt[i2][:], src[r0:r0 + 128, :], writes=[xtB[i2]])
                        S.op("act", lambda e: e.activation(out=junk[:], in_=xt[i2][:], func=AF.Square,
                                                           accum_out=st_[i2][:, 0:1]),
                             reads=[xtB[i2]], writes=[junkB, stB[i2]])
                        S.op("dve", lambda e: e.tensor_scalar(out=st_[i2][:, 1:2], in0=st_[i2][:, 0:1],
                                                              scalar1=1.0 / D, scalar2=eps,
                                                              op0=ALU.mult, op1=ALU.add),
                             reads=[stB[i2]], writes=[stB[i2]])
                        S.op("act", lambda e: e.activation(out=st_[i2][:, 3:4], in_=st_[i2][:, 1:2], func=AF.Sqrt),
                             reads=[stB[i2]], writes=[stB[i2]])
                        S.op("dve", lambda e: e.reciprocal(out=st_[i2][:, 2:3], in_=st_[i2][:, 3:4]),
                             reads=[stB[i2]], writes=[stB[i2]])
                        S.op("dve", lambda e: e.tensor_scalar(out=xn[i2][:], in0=xt[i2][:],
                                                              scalar1=st_[i2][:, 2:3], scalar2=None,
                                                              op0=ALU.mult),
                             reads=[xtB[i2], stB[i2]], writes=[xnB[i2]])
                        if tm_dst is not None:
                            S.dma("pool", tm_dst[r0 - row0:r0 - row0 + 128, :], xn[i2][:], reads=[xnB[i2]])
                        for g4 in range(4):
                            pb = (it * 4 + g4) % 4
                            pv = PS[pb][:].bitcast(BF16)

                            def tr(e, g4=g4, pv=pv, i2=i2):
                                ins = None
                                for q in range(4):
                                    kc = g4 * 4 + q
                                    ins = e.transpose(out=pv[:, q * 128:(q + 1) * 128],
                                                      in_=xn[i2][:, kc * 128:(kc + 1) * 128],
                                                      identity=ident_b[:])
                                return ins
                            S.op("pe", tr, reads=[xnB[i2]], writes=[PSB[pb]])
                            eng = "act" if g4 % 2 == 0 else "dve"

                            def ev(e, g4=g4, pv=pv, s=s, h=h, eng=eng):
                                o = h[:, g4 * 4:(g4 + 1) * 4, s * 128:(s + 1) * 128]
                                i_ = pv[:, 0:512].rearrange("p (k t) -> p k t", k=4)
                                if eng == "act":
                                    return e.copy(out=o, in_=i_)
                                return e.tensor_copy(out=o, in_=i_)
                            S.op(eng, ev, reads=[PSB[pb]], writes=[hB])
                        it += 1
                    S.dma("pool", dst_d[blk], h[:], reads=[hB])
                S.barrier()

        def gemm(ls, AT_d, blks, Wview_fn, ntiles, kc_n, wcols, mode, gvec, epilogue, group=2,
                 resident=None, pbanks=(0, 1, 2, 3)):
            wst = [sb(f"g_wst{i}", [128, 4096], F32, ls) for i in range(2)]
            wstB = [Buf() for _ in range(2)]
            nwb = 2 * group
            wbf = [sb(f"g_wbf{i}", [128, 4096], BF16, ls) for i in range(nwb)]
            wbfB = [Buf() for _ in range(nwb)]
            if resident is None:
                at = [sb(f"g_at{i}", [128, KC * 512], BF16, ls) for i in range(2)]
                atB = [Buf() for _ in range(2)]
            wi = 0
            ai = 0
            pi = 0
            for t0 in range(0, ntiles, group):
                tiles = list(range(t0, min(ntiles, t0 + group)))
                wsl = {}
                for ti in tiles:
                    a = wi % 2
                    b = wi % nwb
                    wv = wst[a][:, 0:kc_n * wcols].rearrange("p (k c) -> p k c", k=kc_n)
                    wsrc = Wview_fn(ti)
                    kq = max(1, kc_n // 4)
                    for k0 in range(0, kc_n, kq):
                        S.dma("sp", wv[:, k0:k0 + kq, :], wsrc[:, k0:k0 + kq, :], writes=[wstB[a]])
                    wb = wbf[b][:, 0:kc_n * wcols].rearrange("p (k c) -> p k c", k=kc_n)
                    ceng = "dve" if wi % 2 == 0 else "act"
                    if gvec is None:
                        if ceng == "dve":
                            S.op("dve", lambda e, a=a, b=b: e.tensor_copy(out=wbf[b][:, 0:kc_n * wcols],
                                                                          in_=wst[a][:, 0:kc_n * wcols]),
                                 reads=[wstB[a]], writes=[wbfB[b]])
                        else:
                            S.op("act", lambda e, a=a, b=b: e.copy(out=wbf[b][:, 0:kc_n * wcols],
                                                                   in_=wst[a][:, 0:kc_n * wcols]),
                                 reads=[wstB[a]], writes=[wbfB[b]])
                    else:
                        def cast(e, wv=wv, wb=wb, ceng=ceng):
                            ins = None
                            for k in range(kc_n):
                                if ceng == "dve":
                                    ins = e.tensor_scalar(out=wb[:, k, :], in0=wv[:, k, :],
                                                          scalar1=gvec[:, k:k + 1], scalar2=None, op0=ALU.mult)
                                else:
                                    ins = e.activation(out=wb[:, k, :], in_=wv[:, k, :], func=AF.Copy,
                                                       scale=gvec[:, k:k + 1])
                            return ins
                        S.op(ceng, cast, reads=[wstB[a]], writes=[wbfB[b]])
                    wsl[ti] = (wb, wbfB[b])
                    wi += 1
                for bi, blk in enumerate(blks):
                    if resident is None:
                        a = ai % 2
                        ai += 1
                        S.dma("sp", at[a][:], AT_d[blk].rearrange("p k t -> p (k t)"), writes=[atB[a]])
                        av = at[a][:].rearrange("p (k t) -> p k t", k=KC)
                        aB = atB[a]
                    else:
                        av, aB = resident[bi]
                    for ti in tiles:
                        wb, wB = wsl[ti]
                        if mode == "fm":
                            for ct in range(wcols // 128):
                                pb = pbanks[pi % len(pbanks)]
                                pi += 1

                                def mm(e, wb=wb, av=av, ct=ct, pb=pb):
                                    ins = None
                                    for k in range(kc_n):
                                        ins = e.matmul(PS[pb][:], lhsT=wb[:, k, ct * 128:(ct + 1) * 128],
                                                       rhs=av[:, k, :], start=(k == 0), stop=(k == kc_n - 1))
                                    return ins
                                S.op("pe", mm, reads=[wB, aB], writes=[PSB[pb]])
                                epilogue(pb, ti, ct, blk, bi)
                        else:
                            for s in range(4):
                                pb = pbanks[pi % len(pbanks)]
                                pi += 1

                                def mm(e, wb=wb, av=av, s=s, pb=pb):
                                    ins = None
                                    for k in range(kc_n):
                                        ins = e.matmul(PS[pb][:, 0:wcols], lhsT=av[:, k, s * 128:(s + 1) * 128],
                                                       rhs=wb[:, k, :], start=(k == 0), stop=(k == kc_n - 1))
                                    return ins
                                S.op("pe", mm, reads=[wB, aB], writes=[PSB[pb]])
                                epilogue(pb, ti, s, blk, bi)

        def w_tiles(Wap, c0, wcols):
            Wv = Wap.rearrange("(k p) n -> p k n", p=128)
            return lambda ti: Wv[:, :, c0 + ti * wcols: c0 + (ti + 1) * wcols]

        if upto >= 0:
            with contextlib.ExitStack() as ls:
                pos_i = sb("pos_i", [128, 512], I32, ls)
                pos_f = sb("pos_f", [128, 512], F32, ls)
                ang = sb("ang", [128, 512], F32, ls)
                a1 = sb("a1", [128, 512], F32, ls)
                a2 = sb("a2", [128, 512], F32, ls)
                tq = sb("tq", [128, 512], F32, ls)
                tB = Buf()
                posv = posf.rearrange("(b t) -> b t", t=512)
                for blk in range(16):
                    S.dma("sp", pos_i[:], posv[blk:blk + 1, :].partition_broadcast(128), writes=[tB])
                    S.op("dve", lambda e: e.tensor_copy(out=pos_f[:], in_=pos_i[:]), reads=[tB], writes=[tB])
                    S.op("dve", lambda e: e.tensor_scalar(out=ang[:], in0=pos_f[:], scalar1=rope_c[:, 0:1],
                                                          scalar2=None, op0=ALU.mult), reads=[tB], writes=[tB])
                    def trig(dst, shift):
                        S.op("dve", lambda e: e.tensor_scalar(out=dst[:], in0=ang[:], scalar1=shift, scalar2=None, op0=ALU.add), reads=[tB], writes=[tB])
                        S.op("dve", lambda e: e.tensor_scalar(out=tq[:], in0=dst[:], scalar1=1.0 / TWO_PI, scalar2=None, op0=ALU.mult), reads=[tB], writes=[tB])
                        S.op("dve", lambda e: e.tensor_copy(out=pos_i[:], in_=tq[:]), reads=[tB], writes=[tB])
                        S.op("dve", lambda e: e.tensor_copy(out=tq[:], in_=pos_i[:]), reads=[tB], writes=[tB])
                        S.op("dve", lambda e: e.scalar_tensor_tensor(out=dst[:], in0=tq[:], scalar=-TWO_PI, in1=dst[:], op0=ALU.mult, op1=ALU.add), reads=[tB], writes=[tB])
                        S.op("dve", lambda e: e.tensor_scalar(out=tq[:], in0=dst[:], scalar1=math.pi, scalar2=-TWO_PI, op0=ALU.is_gt, op1=ALU.mult), reads=[tB], writes=[tB])
                        S.op("dve", lambda e: e.tensor_tensor(out=dst[:], in0=dst[:], in1=tq[:], op=ALU.add), reads=[tB], writes=[tB])
                        S.op("dve", lambda e: e.tensor_scalar(out=tq[:], in0=dst[:], scalar1=-math.pi, scalar2=TWO_PI, op0=ALU.is_lt, op1=ALU.mult), reads=[tB], writes=[tB])
                        S.op("dve", lambda e: e.tensor_tensor(out=dst[:], in0=dst[:], in1=tq[:], op=ALU.add), reads=[tB], writes=[tB])
                        S.op("dve", lambda e: e.tensor_scalar(out=dst[:], in0=dst[:], scalar1=-math.pi, scalar2=math.pi, op0=ALU.max, op1=ALU.min), reads=[tB], writes=[tB])
                        S.op("act", lambda e: e.activation(out=dst[:], in_=dst[:], func=AF.Sin), reads=[tB], writes=[tB])
                    trig(a1, 0.0)
                    S.op("dve", lambda e: e.tensor_scalar(out=a1[:], in0=a1[:], scalar1=rope_c[:, 1:2], scalar2=None, op0=ALU.mult), reads=[tB], writes=[tB])
                    S.dma("pool", sin_d[blk], a1[:], reads=[tB])
                    trig(a2, math.pi / 2)
                    S.dma("pool", cos_d[blk], a2[:], reads=[tB])
                S.barrier()

        if upto >= 1:
            norm_transpose(xf, 0, 16, hT_d, 1e-6)

        def rope_epilogue_factory(ls, dst_d, tok_of_blk, scale_cols0):
            cs = [sb(f"rp_c{i}", [128, 512], F32, ls) for i in range(2)]
            sn = [sb(f"rp_s{i}", [128, 512], F32, ls) for i in range(2)]
            csB = [Buf() for _ in range(2)]
            tb_ = [sb(f"rp_t{i}", [128, 512], BF16, ls) for i in range(2)]
            tbB = [Buf() for _ in range(2)]
            o1 = [sb(f"rp_o1{i}", [128, 512], F32, ls) for i in range(2)]
            o2 = [sb(f"rp_o2{i}", [128, 512], F32, ls) for i in range(2)]
            ob = [sb(f"rp_ob{i}", [128, 512], BF16, ls) for i in range(2)]
            oB = [Buf() for _ in range(2)]
            state = dict(n=0, lastblk=None, ci=0)

            def epi(pb, ti, ct, blk, bi):
                n = state["n"]
                state["n"] += 1
                i2 = n % 2
                if state["lastblk"] != blk:
                    state["ci"] += 1
                    c2 = state["ci"] % 2
                    S.dma("sp", cs[c2][:], cos_d[blk], writes=[csB[c2]])
                    S.dma("sp", sn[c2][:], sin_d[blk], writes=[csB[c2]])
                    state["lastblk"] = blk
                c2 = state["ci"] % 2
                hm = scale_cols0 + ti * 2 + ct
                pb2 = 4 + (n % 2)
                S.op("act", lambda e: e.copy(out=tb_[i2][:], in_=PS[pb][:]), reads=[PSB[pb]], writes=[tbB[i2]])
                S.op("pe", lambda e: e.matmul(PS[pb2][:], lhsT=swap_b[:], rhs=tb_[i2][:], start=True, stop=True),
                     reads=[tbB[i2]], writes=[PSB[pb2]])
                S.op("dve", lambda e: e.tensor_tensor(out=o1[i2][:], in0=PS[pb][:], in1=cs[c2][:], op=ALU.mult),
                     reads=[PSB[pb], csB[c2], tbB[i2]], writes=[oB[i2]])
                S.op("dve", lambda e: e.tensor_tensor(out=o2[i2][:], in0=PS[pb2][:], in1=sn[c2][:], op=ALU.mult),
                     reads=[PSB[pb2], csB[c2]], writes=[oB[i2]])
                S.op("dve", lambda e: e.tensor_tensor(out=ob[i2][:], in0=o1[i2][:], in1=o2[i2][:], op=ALU.add),
                     reads=[oB[i2]], writes=[oB[i2]])
                t0 = tok_of_blk(blk)
                S.dma("pool", dst_d[hm, :, t0:t0 + 512], ob[i2][:], reads=[oB[i2]])
            return epi

        ALLB = list(range(16))
        OWNB = [12, 13, 14, 15]
        if upto >= 2:
            with contextlib.ExitStack() as ls:
                ut = [sb(f"e_u{i}", [128, 512], BF16, ls) for i in range(3)]
                utB = [Buf() for _ in range(3)]
                cnt = [0]

                def epi_u(pb, ti, ct, blk, bi):
                    i3 = cnt[0] % 3
                    cnt[0] += 1
                    r0 = ti * 256 + ct * 128
                    S.op("act", lambda e: e.copy(out=ut[i3][:], in_=PS[pb][:]), reads=[PSB[pb]], writes=[utB[i3]])
                    S.dma("pool", uT_d[r0:r0 + 128, blk * 512:(blk + 1) * 512], ut[i3][:], reads=[utB[i3]])
                gemm(ls, hT_d, ALLB, w_tiles(w_in, 0, 256), 8, KC, 256, "fm", gmix_s, epi_u)
                S.barrier()
        if upto >= 3:
            with contextlib.ExitStack() as ls:
                epi_k = rope_epilogue_factory(ls, KT_d, lambda blk: blk * 512, 0)
                gemm(ls, hT_d, ALLB, w_tiles(w_in, 3 * D, 256), 8, KC, 256, "fm", gmix_s, epi_k)
                S.barrier()
            with contextlib.ExitStack() as ls:
                epi_q = rope_epilogue_factory(ls, QT_d, lambda blk: (blk - 12) * 512, 0)
                gemm(ls, hT_d, OWNB, w_tiles(w_in, 2 * D, 256), 8, KC, 256, "fm", gmix_s, epi_q)
                S.barrier()
        if upto >= 4:
            with contextlib.ExitStack() as ls:
                vt = [sb(f"e_v{i}", [128, 256], BF16, ls) for i in range(3)]
                vtB = [Buf() for _ in range(3)]
                cnt = [0]

                def epi_v(pb, ti, s, blk, bi):
                    i3 = cnt[0] % 3
                    cnt[0] += 1
                    r0 = blk * 512 + s * 128
                    eng = "act" if cnt[0] % 2 else "dve"
                    if eng == "act":
                        S.op("act", lambda e: e.copy(out=vt[i3][:], in_=PS[pb][:, 0:256]), reads=[PSB[pb]], writes=[vtB[i3]])
                    else:
                        S.op("dve", lambda e: e.tensor_copy(out=vt[i3][:], in_=PS[pb][:, 0:256]), reads=[PSB[pb]], writes=[vtB[i3]])
                    S.dma("pool", V_d[r0:r0 + 128, ti * 256:(ti + 1) * 256], vt[i3][:], reads=[vtB[i3]])
                gemm(ls, hT_d, ALLB, w_tiles(w_in, 4 * D, 256), 8, KC, 256, "tm", gmix_s, epi_v)
                S.barrier()
        if upto >= 5:
            with contextlib.ExitStack() as ls:
                xs = [sb(f"e_x{i}", [128, 512], F32, ls) for i in range(2)]
                t1 = [sb(f"e_t{i}", [128, 512], F32, ls) for i in range(2)]
                ob = [sb(f"e_o{i}", [128, 512], BF16, ls) for i in range(2)]
                eB = [Buf() for _ in range(2)]
                cnt = [0]

                def epi_gelu(pb, ti, ct, blk, bi):
                    i2 = cnt[0] % 2
                    cnt[0] += 1
                    r0 = ti * 256 + ct * 128
                    t0 = (blk - 12) * 512
                    S.op("act", lambda e: e.copy(out=xs[i2][:], in_=PS[pb][:]), reads=[PSB[pb]], writes=[eB[i2]])
                    S.op("dve", lambda e: e.tensor_tensor(out=t1[i2][:], in0=xs[i2][:], in1=xs[i2][:], op=ALU.mult),
                         reads=[eB[i2]], writes=[eB[i2]])
                    S.op("dve", lambda e: e.tensor_scalar(out=t1[i2][:], in0=t1[i2][:], scalar1=0.044715, scalar2=1.0,
                                                          op0=ALU.mult, op1=ALU.add), reads=[eB[i2]], writes=[eB[i2]])
                    S.op("dve", lambda e: e.tensor_tensor(out=t1[i2][:], in0=t1[i2][:], in1=xs[i2][:], op=ALU.mult),
                         reads=[eB[i2]], writes=[eB[i2]])
                    S.op("act", lambda e: e.activation(out=t1[i2][:], in_=t1[i2][:], func=AF.Sigmoid,
                                                       scale=2.0 * math.sqrt(2.0 / math.pi)),
                         reads=[eB[i2]], writes=[eB[i2]])
                    S.op("dve", lambda e: e.tensor_tensor(out=ob[i2][:], in0=t1[i2][:], in1=xs[i2][:], op=ALU.mult),
                         reads=[eB[i2]], writes=[eB[i2]])
                    S.dma("pool", gbT_d[r0:r0 + 128, t0:t0 + 512], ob[i2][:], reads=[eB[i2]])
                gemm(ls, hT_d, OWNB, w_tiles(w_in, D, 256), 8, KC, 256, "fm", gmix_s, epi_gelu)
                S.barrier()
            with contextlib.ExitStack() as ls:
                ob = [sb(f"e_o{i}", [128, 512], BF16, ls) for i in range(3)]
                eB = [Buf() for _ in range(3)]
                cnt = [0]

                def epi_sig(pb, ti, ct, blk, bi):
                    i3 = cnt[0] % 3
                    cnt[0] += 1
                    col = ti * 256 + ct * 128
                    which, r0 = col // D, col % D
                    t0 = (blk - 12) * 512
                    S.op("act", lambda e: e.activation(out=ob[i3][:], in_=PS[pb][:], func=AF.Sigmoid),
                         reads=[PSB[pb]], writes=[eB[i3]])
                    S.dma("pool", sg_d[which, r0:r0 + 128, t0:t0 + 512], ob[i3][:], reads=[eB[i3]])
                gemm(ls, hT_d, OWNB, w_tiles(w_in, 5 * D, 256), 16, KC, 256, "fm", gmix_s, epi_sig)
                S.barrier()

        if upto >= 6:
            with contextlib.ExitStack() as ls:
                CH = 2048
                nmb = sb("l_nm", [128, T], BF16, ls)
                tmpf = sb("l_tmpf", [128, CH], F32, ls)
                tmpf2 = sb("l_tmpf2", [128, CH], F32, ls)
                tmpi = sb("l_tmpi", [128, CH], I32, ls)
                mB = Buf()
                for q in range(4):
                    sl = slice(q * CH, (q + 1) * CH)
                    S.dma("sp", tmpf2[:], validp.rearrange("(o t) -> o t", o=1)[0:1, sl].partition_broadcast(128), writes=[mB])
                    S.dma("sp", tmpi[:], posf.rearrange("(o t) -> o t", o=1)[0:1, sl].partition_broadcast(128), writes=[mB])
                    S.op("dve", lambda e: e.tensor_copy(out=tmpf[:], in_=tmpi[:]), reads=[mB], writes=[mB])
                    S.op("dve", lambda e: e.tensor_scalar(out=tmpf[:], in0=tmpf[:], scalar1=0.0, scalar2=None,
                                                          op0=ALU.not_equal), reads=[mB], writes=[mB])
                    S.op("dve", lambda e: e.tensor_tensor(out=nmb[:, sl], in0=tmpf[:], in1=tmpf2[:], op=ALU.mult),
                         reads=[mB], writes=[mB])
                cw = sb("l_cw", [128, 4, KC], F32, ls)
                cbs = sb("l_cb", [128, KC], F32, ls)
                bas = sb("l_ba", [128, KC], F32, ls)
                bxs = sb("l_bx", [128, KC], F32, ls)
                lps = sb("l_lp", [128, KC], F32, ls)
                nc8 = sb("l_nc8", [128, KC], F32, ls)
                pB = Buf()
                S.dma("sp", cw[:], conv_w[:, :, :], writes=[pB])
                S.dma("sp", cbs[:], conv_b[:, :], writes=[pB])
                S.dma("sp", bas[:], b_rg_a[:, :], writes=[pB])
                S.dma("sp", bxs[:], b_rg_x[:, :], writes=[pB])
                S.dma("sp", lps[:], lru_param[:, :], writes=[pB])
                S.op("act", lambda e: e.activation(out=nc8[:], in_=lps[:], func=AF.Exp, scale=-1.0), reads=[pB], writes=[pB])
                S.op("act", lambda e: e.activation(out=nc8[:], in_=nc8[:], func=AF.Ln, bias=1.0), reads=[pB], writes=[pB])
                S.op("dve", lambda e: e.tensor_scalar(out=nc8[:], in0=nc8[:], scalar1=-8.0, scalar2=None, op0=ALU.mult),
                     reads=[pB], writes=[pB])
                waf = sb("l_waf", [128, 2, 128], F32, ls)
                wab = [sb(f"l_wab{i}", [128, 2, 128], BF16, ls) for i in range(2)]
                wB = [Buf() for _ in range(2)]
                wfB = Buf()
                u = [sb(f"l_u{i}", [128, 3 + CH], BF16, ls) for i in range(2)]
                dgw = [sb(f"l_dg{i}", [128, 4, 128], BF16, ls) for i in range(2)]
                dgB = [Buf() for _ in range(2)]
                uB = [Buf() for _ in range(2)]
                uc_ = [sb(f"l_uc{i}", [128, CH], F32, ls) for i in range(2)]
                ucb_ = [sb(f"l_ucb{i}", [128, CH], BF16, ls) for i in range(2)]
                rr_ = [sb(f"l_r{i}", [128, CH], F32, ls) for i in range(2)]
                ii_ = [sb(f"l_i{i}", [128, CH], F32, ls) for i in range(2)]
                aa_ = [sb(f"l_a{i}", [128, CH], F32, ls) for i in range(2)]
                mm__ = [sb(f"l_m{i}", [128, CH], F32, ls) for i in range(2)]
                wkB_ = [Buf() for _ in range(2)]
                hh = [sb(f"l_h{i}", [128, CH], F32, ls) for i in range(2)]
                hB = [Buf() for _ in range(2)]
                gbt_ = [sb(f"l_gb{i}", [128, NOWN], BF16, ls) for i in range(2)]
                yb_ = [sb(f"l_y{i}", [128, NOWN], BF16, ls) for i in range(2)]
                gB_ = [Buf() for _ in range(2)]
                yB_ = [Buf() for _ in range(2)]
                ucB_ = [Buf() for _ in range(2)]
                riB_ = [Buf() for _ in range(2)]
                yr_v = yrT_d.rearrange("b p k t -> p k b t")
                chunks = [(c, q) for c in range(16) for q in range(4)]

                def phaseA(n):
                    c, q = chunks[n]
                    w2 = c % 2
                    i2 = n % 2
                    t0 = q * CH
                    if q == 0:
                        S.dma("sp", waf[:, 0, :], w_rg_a[c], writes=[wfB])
                        S.dma("sp", waf[:, 1, :], w_rg_x[c], writes=[wfB])
                        S.op("dve", lambda e: e.tensor_copy(out=wab[w2][:], in_=waf[:]), reads=[wfB], writes=[wB[w2]])
                        for j in range(4):
                            S.op("dve", lambda e, j=j: e.tensor_scalar(out=dgw[w2][:, j, :], in0=ident_f[:], scalar1=cw[:, j, c:c + 1],
                                                                       scalar2=None, op0=ALU.mult), reads=[pB], writes=[dgB[w2]])
                        S.dma("sp", gbt_[w2][:], gbT_d[c * 128:(c + 1) * 128, :], writes=[gB_[w2]])
                        S.op("dve", lambda e: e.memset(u[i2][:, 0:3], 0.0), writes=[uB[i2]])
                        S.dma("sp", u[i2][:, 3:3 + CH], uT_d[c * 128:(c + 1) * 128, 0:CH], writes=[uB[i2]])
                    else:
                        S.dma("sp", u[i2][:], uT_d[c * 128:(c + 1) * 128, t0 - 3:t0 + CH], writes=[uB[i2]])
                    uu = u[i2]
                    uc, ucb, rr, ii = uc_[i2], ucb_[i2], rr_[i2], ii_[i2]
                    ucB, riB = ucB_[i2], riB_[i2]
                    for sblk in range(CH // 512):
                        ssl = slice(sblk * 512, (sblk + 1) * 512)
                        pc = 4 * i2 + (sblk % 2)

                        def convmm(e, sblk=sblk, pc=pc):
                            ins = None
                            for j in range(4):
                                ins = e.matmul(PS[pc][:], lhsT=dgw[w2][:, j, :], rhs=uu[:, sblk * 512 + j:sblk * 512 + j + 512],
                                               start=(j == 0), stop=(j == 3))
                            return ins
                        S.op("pe", convmm, reads=[dgB[w2], uB[i2]], writes=[PSB[pc]])
                        S.op("act", lambda e: e.activation(out=uc[:, ssl], in_=PS[pc][:], func=AF.Identity, bias=cbs[:, c:c + 1]),
                             reads=[PSB[pc], pB], writes=[ucB])
                        S.op("dve", lambda e: e.tensor_copy(out=ucb[:, ssl], in_=uc[:, ssl]), reads=[ucB], writes=[ucB])
                    for sblk in range(CH // 512):
                        ssl = slice(sblk * 512, (sblk + 1) * 512)
                        pa, px = 4 * i2 + 2, 4 * i2 + 3
                        S.op("pe", lambda e: e.matmul(PS[pa][:], lhsT=wab[w2][:, 0, :], rhs=ucb[:, ssl], start=True, stop=True),
                             reads=[wB[w2], ucB], writes=[PSB[pa]])
                        S.op("pe", lambda e: e.matmul(PS[px][:], lhsT=wab[w2][:, 1, :], rhs=ucb[:, ssl], start=True, stop=True),
                             reads=[wB[w2], ucB], writes=[PSB[px]])
                        S.op("act", lambda e: e.activation(out=rr[:, ssl], in_=PS[pa][:], func=AF.Sigmoid, bias=bas[:, c:c + 1]),
                             reads=[PSB[pa], pB], writes=[riB])
                        S.op("act", lambda e: e.activation(out=ii[:, ssl], in_=PS[px][:], func=AF.Sigmoid, bias=bxs[:, c:c + 1]),
                             reads=[PSB[px], pB], writes=[riB])

                def phaseB(n):
                    c, q = chunks[n]
                    w2 = c % 2
                    i2 = n % 2
                    t0 = q * CH
                    uc, rr, ii, aa, mm_ = uc_[i2], rr_[i2], ii_[i2], aa_[i2], mm__[i2]
                    ucB, riB, wkB = ucB_[i2], riB_[i2], wkB_[i2]
                    S.op("act", lambda e: e.activation(out=aa[:], in_=rr[:], func=AF.Exp, scale=nc8[:, c:c + 1]),
                         reads=[riB, pB], writes=[wkB])
                    S.op("dve", lambda e: e.tensor_tensor(out=aa[:], in0=aa[:], in1=nmb[:, t0:t0 + CH], op=ALU.mult),
                         reads=[wkB, mB], writes=[wkB])
                    S.op("act", lambda e: e.activation(out=mm_[:], in_=aa[:], func=AF.Square), reads=[wkB], writes=[wkB])
                    S.op("act", lambda e: e.activation(out=mm_[:], in_=mm_[:], func=AF.Sqrt, scale=-1.0, bias=1.0),
                         reads=[wkB], writes=[wkB])
                    S.op("dve", lambda e: e.tensor_tensor(out=ii[:], in0=ii[:], in1=uc[:], op=ALU.mult), reads=[riB, ucB], writes=[riB])
                    S.op("dve", lambda e: e.tensor_tensor(out=ii[:], in0=ii[:], in1=mm_[:], op=ALU.mult), reads=[riB, wkB], writes=[riB])
                    hprev = hh[(i2 + 1) % 2]
                    init = 0.0 if q == 0 else hprev[:, CH - 1:CH]
                    S.op("dve", lambda e: e.tensor_tensor_scan(out=hh[i2][:], data0=aa[:], data1=ii[:], initial=init,
                                                               op0=ALU.mult, op1=ALU.add),
                         reads=[wkB, riB, hB[(i2 + 1) % 2]], writes=[hB[i2]])
                    if q == 3:
                        S.op("dve", lambda e: e.tensor_tensor(out=yb_[w2][:], in0=hh[i2][:], in1=gbt_[w2][:], op=ALU.mult),
                             reads=[hB[i2], gB_[w2]], writes=[yB_[w2]])
                        S.dma("pool", yr_v[:, c], yb_[w2][:].rearrange("p (b t) -> p b t", b=4), reads=[yB_[w2]])

                phaseA(0)
                for n in range(len(chunks)):
                    if n + 1 < len(chunks):
                        phaseA(n + 1)
                    phaseB(n)
                S.barrier()

        if upto >= 7:
            with contextlib.ExitStack() as ls:
                SCALE = 128 ** -0.5
                lamv = sb("a_lamv", [128, 4, 128], F32, ls)
                lams = sb("a_lams", [128, 8], F32, ls)
                lB = Buf()
                S.dma("sp", lamv[:].rearrange("p a b -> p (a b)"),
                      lam_in.rearrange("(o a) b -> o (a b)", o=1).partition_broadcast(128), writes=[lB])
                S.op("dve", lambda e: e.tensor_tensor(out=lamv[:, 0, :], in0=lamv[:, 0, :], in1=lamv[:, 1, :], op=ALU.mult), reads=[lB], writes=[lB])
                S.op("dve", lambda e: e.tensor_tensor(out=lamv[:, 2, :], in0=lamv[:, 2, :], in1=lamv[:, 3, :], op=ALU.mult), reads=[lB], writes=[lB])
                S.op("dve", lambda e: e.reduce_sum(out=lams[:, 0:1], in_=lamv[:, 0, :], axis=AX.X), reads=[lB], writes=[lB])
                S.op("dve", lambda e: e.reduce_sum(out=lams[:, 1:2], in_=lamv[:, 2, :], axis=AX.X), reads=[lB], writes=[lB])
                S.op("act", lambda e: e.activation(out=lams[:, 2:4], in_=lams[:, 0:2], func=AF.Exp), reads=[lB], writes=[lB])
                S.op("dve", lambda e: e.tensor_tensor(out=lams[:, 4:5], in0=lams[:, 3:4], in1=lams[:, 2:3], op=ALU.subtract), reads=[lB], writes=[lB])
                S.op("dve", lambda e: e.tensor_scalar(out=lams[:, 5:6], in0=lams[:, 4:5], scalar1=-0.2, scalar2=None, op0=ALU.add), reads=[lB], writes=[lB])
                neglam = lams[:, 5:6]
                sg_t = sb("a_sg", [128, 256], F32, ls)
                S.dma("sp", sg_t[:], subln_g.rearrange("(o t) -> o t", o=1).partition_broadcast(128), writes=[lB])
                S.op("dve", lambda e: e.tensor_scalar(out=sg_t[:], in0=sg_t[:], scalar1=0.8, scalar2=None, op0=ALU.mult), reads=[lB], writes=[lB])
                kb = sb("a_kb", [128, 64], F32, ls)
                S.dma("sp", kb[:], valid_pk[:, :], writes=[lB])
                S.op("dve", lambda e: e.tensor_scalar(out=kb[:], in0=kb[:], scalar1=-1.0, scalar2=30000.0, op0=ALU.add, op1=ALU.mult), reads=[lB], writes=[lB])
                trf = sb("a_trf", [128, 4, 512], F32, ls)
                trb = sb("a_trb", [128, 4, 512], BF16, ls)
                S.dma("sp", trf[:], c_tri[:, :, :], writes=[lB])
                S.op("dve", lambda e: e.tensor_copy(out=trb[:], in_=trf[:]), reads=[lB], writes=[lB])

                Va = [sb(f"a_V{i}", [128, 64, 257], BF16, ls) for i in range(2)]
                VaB = [Buf() for _ in range(2)]
                for i in range(2):
                    S.op("dve", lambda e, i=i: e.memset(Va[i][:, :, 256:257], 1.0), writes=[VaB[i]])
                KTs = [sb(f"a_K{i}", [128, T], BF16, ls) for i in range(2)]
                KTB = [Buf() for _ in range(2)]
                QTs = [sb(f"a_Q{i}", [128, NOWN], BF16, ls) for i in range(2)]
                QTB = [Buf() for _ in range(2)]
                Es = [sb(f"a_E{i}", [128, 512], BF16, ls) for i in range(3)]
                EB = [Buf() for _ in range(3)]
                om = [sb(f"a_om{i}", [128, 4, 256], F32, ls) for i in range(2)]
                omB = [Buf() for _ in range(2)]
                rs = sb("a_rs", [128, 8], F32, ls)
                rsB = Buf()
                od = sb("a_od", [128, 4, 256], F32, ls)
                odB = Buf()
                junk = sb("a_junk", [128, 256], F32, ls)
                onb = sb("a_onb", [128, 4, 256], BF16, ls)
                yo = [sb(f"a_yo{i}", [128, 2, 512], BF16, ls) for i in range(2)]
                yoB = [Buf() for _ in range(2)]
                V_v = V_d.rearrange("(k p) c -> p k c", p=128)
                iters = []
                for h in range(NH):
                    for qb in range(4):
                        nkt = 48 + (qb + 1) * 4
                        for m in range(2):
                            for kt in range(nkt):
                                iters.append((h, qb, m, kt, nkt))
                hqc = [0]

                def front(idx):
                    h, qb, m, kt, nkt = iters[idx]
                    vi = h % 2
                    hm = h * 2 + m
                    ki = hm % 2
                    pS = 4 + (idx % 2)
                    e3 = idx % 3
                    if qb == 0 and m == 0 and kt == 0:
                        for k4 in range(16):
                            S.dma("sp", Va[vi][:, k4 * 4:(k4 + 1) * 4, 0:256], V_v[:, k4 * 4:(k4 + 1) * 4, h * 256:(h + 1) * 256],
                                  writes=[VaB[vi]])
                    if qb == 0 and kt == 0:
                        S.dma("sp", KTs[ki][:], KT_d[hm], writes=[KTB[ki]])
                        S.dma("sp", QTs[ki][:], QT_d[hm], writes=[QTB[ki]])
                    S.op("pe", lambda e: e.matmul(PS[pS][:], lhsT=KTs[ki][:, kt * 128:(kt + 1) * 128],
                                                  rhs=QTs[ki][:, qb * 512:(qb + 1) * 512], start=True, stop=True),
                         reads=[KTB[ki], QTB[ki]], writes=[PSB[pS]])
                    S.op("act", lambda e: e.activation(out=Es[e3][:], in_=PS[pS][:], func=AF.Exp,
                                                       scale=SCALE, bias=kb[:, kt:kt + 1]),
                         reads=[PSB[pS], lB], writes=[EB[e3]])
                    dg = kt - (48 + qb * 4)
                    if dg >= 0:
                        S.op("dve", lambda e: e.tensor_tensor(out=Es[e3][:], in0=Es[e3][:], in1=trb[:, dg, :], op=ALU.mult),
                             reads=[EB[e3], lB], writes=[EB[e3]])

                def back(idx):
                    h, qb, m, kt, nkt = iters[idx]
                    vi = h % 2
                    e3 = idx % 3
                    dg = kt - (48 + qb * 4)

                    def pv(e):
                        ins = None
                        for qs in range(4):
                            ins = e.matmul(PS[qs][:, 0:257], lhsT=Es[e3][:, qs * 128:(qs + 1) * 128],
                                           rhs=Va[vi][:, kt, :], start=(kt == 0), stop=(kt == nkt - 1))
                        return ins
                    S.op("pe", pv, reads=[EB[e3], VaB[vi]], writes=[PSB[0], PSB[1], PSB[2], PSB[3]])
                    if kt != nkt - 1:
                        return
                    for qs in range(4):
                        S.op("dve", lambda e, qs=qs: e.reciprocal(out=rs[:, m * 4 + qs:m * 4 + qs + 1], in_=PS[qs][:, 256:257]),
                             reads=[PSB[qs]], writes=[rsB])
                        S.op("dve", lambda e, qs=qs: e.tensor_scalar(out=om[m][:, qs, :], in0=PS[qs][:, 0:256],
                                                                     scalar1=rs[:, m * 4 + qs:m * 4 + qs + 1], scalar2=None,
                                                                     op0=ALU.mult),
                             reads=[PSB[qs], rsB], writes=[omB[m]])
                    if m != 1:
                        return
                    y2 = hqc[0] % 2
                    hqc[0] += 1
                    S.op("dve", lambda e: e.scalar_tensor_tensor(out=od[:].rearrange("p a b -> p (a b)"),
                                                                 in0=om[1][:].rearrange("p a b -> p (a b)"),
                                                                 scalar=neglam,
                                                                 in1=om[0][:].rearrange("p a b -> p (a b)"),
                                                                 op0=ALU.mult, op1=ALU.add),
                         reads=[omB[0], omB[1], lB], writes=[odB])
                    for qs in range(4):
                        S.op("act", lambda e, qs=qs: e.activation(out=junk[:], in_=od[:, qs, :], func=AF.Square,
                                                                  accum_out=rs[:, qs:qs + 1]),
                             reads=[odB], writes=[rsB])
                    S.op("dve", lambda e: e.tensor_scalar(out=rs[:, 0:4], in0=rs[:, 0:4], scalar1=1.0 / 256, scalar2=1e-5,
                                                          op0=ALU.mult, op1=ALU.add), reads=[rsB], writes=[rsB])
                    S.op("act", lambda e: e.activation(out=rs[:, 4:8], in_=rs[:, 0:4], func=AF.Sqrt), reads=[rsB], writes=[rsB])
                    S.op("dve", lambda e: e.reciprocal(out=rs[:, 0:4], in_=rs[:, 4:8]), reads=[rsB], writes=[rsB])
                    for qs in range(4):
                        S.op("dve", lambda e, qs=qs: e.scalar_tensor_tensor(out=onb[:, qs, :], in0=od[:, qs, :],
                                                                            scalar=rs[:, qs:qs + 1], in1=sg_t[:],
                                                                            op0=ALU.mult, op1=ALU.mult),
                             reads=[odB, rsB, lB], writes=[odB])
                    for eh in range(2):
                        pT = 6 + eh
                        pvw = PS[pT][:].bitcast(BF16)

                        def tr(e, eh=eh, pvw=pvw):
                            ins = None
                            for qs in range(4):
                                ins = e.transpose(out=pvw[:, qs * 128:(qs + 1) * 128],
                                                  in_=onb[:, qs, eh * 128:(eh + 1) * 128], identity=ident_b[:])
                            return ins
                        S.op("pe", tr, reads=[odB], writes=[PSB[pT]])
                        S.op("act", lambda e, eh=eh, pvw=pvw: e.copy(out=yo[y2][:, eh, :], in_=pvw[:, 0:512]),
                             reads=[PSB[pT]], writes=[yoB[y2]])
                    S.dma("pool", yaT_d[qb, :, h * 2:(h + 1) * 2, :], yo[y2][:], reads=[yoB[y2]])

                front(0)
                for idx in range(len(iters)):
                    if idx + 1 < len(iters):
                        front(idx + 1)
                    back(idx)
                S.barrier()

        if upto >= 8:
            for which, AT, W in [(0, yrT_d, w_br_rnn), (1, yaT_d, w_br_attn)]:
                with contextlib.ExitStack() as ls:
                    gt = [sb(f"b_g{i}", [128, 512], BF16, ls) for i in range(4)]
                    m1 = [sb(f"b_m{i}", [128, 512], BF16, ls) for i in range(4)]
                    ot = [sb(f"b_o{i}", [128, 512], F32, ls) for i in range(4)]
                    ob = [sb(f"b_ob{i}", [128, 512], BF16, ls) for i in range(4)]
                    bB = [Buf() for _ in range(4)]
                    oB = [Buf() for _ in range(4)]
                    cnt = [0]

                    def epi_b(pb, ti, ct, blk, bi, which=which):
                        i2 = cnt[0] % 4
                        cnt[0] += 1
                        r0 = ti * 256 + ct * 128
                        t0 = blk * 512
                        S.dma("sp", gt[i2][:], sg_d[which, r0:r0 + 128, t0:t0 + 512], writes=[bB[i2]])
                        if which == 0:
                            S.op("dve", lambda e: e.tensor_tensor(out=ob[i2][:], in0=PS[pb][:], in1=gt[i2][:], op=ALU.mult),
                                 reads=[PSB[pb], bB[i2]], writes=[oB[i2]])
                            S.dma("pool", m1_d[r0:r0 + 128, t0:t0 + 512], ob[i2][:], reads=[oB[i2]])
                        else:
                            S.dma("sp", m1[i2][:], m1_d[r0:r0 + 128, t0:t0 + 512], writes=[bB[i2]])
                            S.op("dve", lambda e: e.tensor_tensor(out=ot[i2][:], in0=PS[pb][:], in1=gt[i2][:], op=ALU.mult),
                                 reads=[PSB[pb], bB[i2]], writes=[oB[i2]])
                            S.op("dve", lambda e: e.tensor_tensor(out=ob[i2][:], in0=ot[i2][:], in1=m1[i2][:], op=ALU.add),
                                 reads=[oB[i2], bB[i2]], writes=[oB[i2]])
                            S.dma("pool", mT_d[blk, :, r0 // 128, :], ob[i2][:], reads=[oB[i2]])
                    gemm(ls, AT, [0, 1, 2, 3], w_tiles(W, 0, 256), 8, KC, 256, "fm", None, epi_b)
                    S.barrier()
            with contextlib.ExitStack() as ls:
                xo = [sb(f"o_x{i}", [128, 256], F32, ls) for i in range(4)]
                xB = [Buf() for _ in range(4)]
                cnt = [0]

                def epi_o(pb, ti, s, blk, bi):
                    i2 = cnt[0] % 4
                    cnt[0] += 1
                    r0 = blk * 512 + s * 128
                    S.dma("sp", xo[i2][:], xf[OWN0 + r0:OWN0 + r0 + 128, ti * 256:(ti + 1) * 256], writes=[xB[i2]])
                    S.op("dve", lambda e: e.tensor_tensor(out=xo[i2][:], in0=PS[pb][:, 0:256], in1=xo[i2][:], op=ALU.add),
                         reads=[PSB[pb], xB[i2]], writes=[xB[i2]])
                    S.dma("pool", x2_d[r0:r0 + 128, ti * 256:(ti + 1) * 256], xo[i2][:], reads=[xB[i2]])
                gemm(ls, mT_d, [0, 1, 2, 3], w_tiles(w_out, 0, 256), 8, KC, 256, "tm", None, epi_o)
                S.barrier()

        if upto >= 9:
            norm_transpose(x2_d, 0, 4, hnT_d, 1e-6, tm_dst=hn_d)
            with contextlib.ExitStack() as ls:
                wrf = sb("r_wf", [128, KC, 36], F32, ls)
                wrb = sb("r_wb", [128, KC, 36], BF16, ls)
                rB = Buf()
                S.dma("sp", wrf[:], w_router.rearrange("(k p) n -> p k n", p=128), writes=[rB])

                def castr(e):
                    ins = None
                    for k in range(KC):
                        ins = e.tensor_scalar(out=wrb[:, k, :], in0=wrf[:, k, :], scalar1=gffn_s[:, k:k + 1], scalar2=None, op0=ALU.mult)
                    return ins
                S.op("dve", castr, reads=[rB], writes=[rB])
                at = [sb(f"r_at{i}", [128, KC, 512], BF16, ls) for i in range(2)]
                atB = [Buf() for _ in range(2)]
                lg = sb("r_lg", [128, 36], F32, ls)
                w = {nm: sb("r_" + nm, [128, shape], F32, ls) for nm, shape in
                     [("gmax", 1), ("gex", 4), ("gsum", 1), ("gw", 1), ("gm", 4), ("pen", 32), ("el", 32), ("m1", 1),
                      ("k1", 32), ("el2", 32), ("m2", 1), ("k2", 32), ("dl", 1), ("w1", 1), ("w2", 1), ("c", 32), ("c2", 32), ("rt", 66)]}
                wkB = Buf()
                for blk in range(4):
                    a = blk % 2
                    S.dma("sp", at[a][:], hnT_d[blk], writes=[atB[a]])
                    for s in range(4):
                        pb = s % 4

                        def mm(e, a=a, s=s, pb=pb):
                            ins = None
                            for k in range(KC):
                                ins = e.matmul(PS[pb][:, 0:36], lhsT=at[a][:, k, s * 128:(s + 1) * 128], rhs=wrb[:, k, :],
                                               start=(k == 0), stop=(k == KC - 1))
                            return ins
                        S.op("pe", mm, reads=[atB[a], rB], writes=[PSB[pb]])
                        R = [wkB]

                        def D_(fn, extra=()):
                            S.op("dve", fn, reads=R + list(extra), writes=R)
                        D_(lambda e: e.tensor_copy(out=lg[:], in_=PS[pb][:, 0:36]), extra=[PSB[pb]])
                        D_(lambda e: e.reduce_max(out=w["gmax"][:], in_=lg[:, 0:4], axis=AX.X))
                        D_(lambda e: e.tensor_scalar(out=w["gex"][:], in0=lg[:, 0:4], scalar1=w["gmax"][:, 0:1], scalar2=None, op0=ALU.subtract))
                        S.op("act", lambda e: e.activation(out=w["gex"][:], in_=w["gex"][:], func=AF.Exp), reads=R, writes=R)
                        D_(lambda e: e.reduce_sum(out=w["gsum"][:], in_=w["gex"][:], axis=AX.X))
                        D_(lambda e: e.reciprocal(out=w["gw"][:], in_=w["gsum"][:]))
                        D_(lambda e: e.tensor_scalar(out=w["gm"][:], in0=lg[:, 0:4], scalar1=w["gmax"][:, 0:1], scalar2=None, op0=ALU.is_ge))
                        for g in range(4):
                            D_(lambda e, g=g: e.tensor_scalar(out=w["pen"][:, g * 8:(g + 1) * 8], in0=lg[:, 4 + g * 8:4 + (g + 1) * 8],
                                                              scalar1=0.0, scalar2=w["gm"][:, g:g + 1], op0=ALU.mult, op1=ALU.add))
                        D_(lambda e: e.tensor_scalar(out=w["pen"][:], in0=w["pen"][:], scalar1=-1.0, scalar2=1e9, op0=ALU.add, op1=ALU.mult))
                        D_(lambda e: e.tensor_tensor(out=w["el"][:], in0=lg[:, 4:36], in1=w["pen"][:], op=ALU.add))
                        D_(lambda e: e.reduce_max(out=w["m1"][:], in_=w["el"][:], axis=AX.X))
                        D_(lambda e: e.tensor_scalar(out=w["k1"][:], in0=w["el"][:], scalar1=w["m1"][:, 0:1], scalar2=None, op0=ALU.is_ge))
                        D_(lambda e: e.scalar_tensor_tensor(out=w["el2"][:], in0=w["k1"][:], scalar=-1e9, in1=w["el"][:], op0=ALU.mult, op1=ALU.add))
                        D_(lambda e: e.reduce_max(out=w["m2"][:], in_=w["el2"][:], axis=AX.X))
                        D_(lambda e: e.tensor_scalar(out=w["k2"][:], in0=w["el2"][:], scalar1=w["m2"][:, 0:1], scalar2=None, op0=ALU.is_ge))
                        D_(lambda e: e.tensor_tensor(out=w["dl"][:], in0=w["m1"][:], in1=w["m2"][:], op=ALU.subtract))
                        S.op("act", lambda e: e.activation(out=w["w1"][:], in_=w["dl"][:], func=AF.Sigmoid), reads=R, writes=R)
                        D_(lambda e: e.tensor_scalar(out=w["w2"][:], in0=w["w1"][:], scalar1=-1.0, scalar2=1.0, op0=ALU.mult, op1=ALU.add))
                        D_(lambda e: e.tensor_tensor(out=w["w1"][:], in0=w["w1"][:], in1=w["gw"][:], op=ALU.mult))
                        D_(lambda e: e.tensor_tensor(out=w["w2"][:], in0=w["w2"][:], in1=w["gw"][:], op=ALU.mult))
                        D_(lambda e: e.tensor_scalar(out=w["c"][:], in0=w["k1"][:], scalar1=w["w1"][:, 0:1], scalar2=None, op0=ALU.mult))
                        D_(lambda e: e.scalar_tensor_tensor(out=w["c2"][:], in0=w["k2"][:], scalar=w["w2"][:, 0:1], in1=w["c"][:], op0=ALU.mult, op1=ALU.add))
                        r0 = blk * 512 + s * 128
                        S.dma("pool", C_d[r0:r0 + 128, :], w["c2"][:], reads=R)
                        D_(lambda e: e.tensor_copy(out=w["rt"][:, 0:32], in_=w["k1"][:]))
                        D_(lambda e: e.tensor_copy(out=w["rt"][:, 32:64], in_=w["k2"][:]))
                        D_(lambda e: e.tensor_copy(out=w["rt"][:, 64:65], in_=w["w1"][:]))
                        D_(lambda e: e.tensor_copy(out=w["rt"][:, 65:66], in_=w["w2"][:]))
                        S.dma("pool", R_d[r0:r0 + 128, :], w["rt"][:], reads=R)
                S.barrier()

        if upto >= 10 and not sparse:
            with contextlib.ExitStack() as ls:
                Cs = sb("m_C", [128, 16, NE], F32, ls)
                cB = Buf()
                S.dma("sp", Cs[:], C_d.rearrange("(t p) e -> p t e", p=128), writes=[cB])
                acc = sb("m_acc", [128, 8, D], F32, ls)
                accB = [Buf() for _ in range(8)]
                hn = [sb(f"m_hn{i}", [128, KC, 512], BF16, ls) for i in range(2)]
                hnB = [Buf() for _ in range(2)]
                actT = sb("m_act", [128, 8, 1024], BF16, ls)
                actB = [Buf() for _ in range(8)]
                sgt = [sb(f"m_sg{i}", [128, 512], F32, ls) for i in range(2)]
                sgB = [Buf() for _ in range(2)]
                fst = sb("m_fst", [128, 4], F32, ls)
                fB = Buf()
                wst = [sb(f"m_wst{i}", [128, 4096], F32, ls) for i in range(2)]
                wstB = [Buf() for _ in range(2)]
                wbf = [sb(f"m_wbf{i}", [128, 4096], BF16, ls) for i in range(4)]
                wbfB = [Buf() for _ in range(4)]
                wi = [0]
                sgi = [0]
                pi = [0]

                def load_w(view, kc_n, wcols, gvec):
                    a = wi[0] % 2
                    b = wi[0] % 4
                    wi[0] += 1
                    wv = wst[a][:, 0:kc_n * wcols].rearrange("p (k c) -> p k c", k=kc_n)
                    wb = wbf[b][:, 0:kc_n * wcols].rearrange("p (k c) -> p k c", k=kc_n)
                    kq = max(1, kc_n // 4)
                    for k0 in range(0, kc_n, kq):
                        S.dma("sp", wv[:, k0:k0 + kq, :], view[:, k0:k0 + kq, :], writes=[wstB[a]])
                    ceng = "dve" if wi[0] % 2 == 0 else "act"
                    if gvec is None:
                        if ceng == "dve":
                            S.op("dve", lambda e: e.tensor_copy(out=wbf[b][:, 0:kc_n * wcols], in_=wst[a][:, 0:kc_n * wcols]),
                                 reads=[wstB[a]], writes=[wbfB[b]])
                        else:
                            S.op("act", lambda e: e.copy(out=wbf[b][:, 0:kc_n * wcols], in_=wst[a][:, 0:kc_n * wcols]),
                                 reads=[wstB[a]], writes=[wbfB[b]])
                    else:
                        def cast(e):
                            ins = None
                            for k in range(kc_n):
                                if ceng == "dve":
                                    ins = e.tensor_scalar(out=wb[:, k, :], in0=wv[:, k, :], scalar1=gvec[:, k:k + 1], scalar2=None, op0=ALU.mult)
                                else:
                                    ins = e.activation(out=wb[:, k, :], in_=wv[:, k, :], func=AF.Copy, scale=gvec[:, k:k + 1])
                            return ins
                        S.op(ceng, cast, reads=[wstB[a]], writes=[wbfB[b]])
                    return wb, wbfB[b]

                for tb in range(2):
                    for j in range(2):
                        S.dma("sp", hn[j][:], hnT_d[tb * 2 + j], writes=[hnB[j]])
                    for tt in range(8):
                        r0 = tb * 1024 + tt * 128
                        S.dma("sp", acc[:, tt, :], x2_d[r0:r0 + 128, :], writes=[accB[tt]])
                    for ex in range(NE):
                        wgv = w_gate[ex].rearrange("(k p) n -> p k n", p=128)
                        wuv = w_up[ex].rearrange("(k p) n -> p k n", p=128)
                        wdv = w_down[ex].rearrange("(k p) n -> p k n", p=128)
                        for f2 in range(4):
                            wg, wgB = load_w(wgv[:, :, f2 * 256:(f2 + 1) * 256], KC, 256, gffn_s)
                            wu, wuB = load_w(wuv[:, :, f2 * 256:(f2 + 1) * 256], KC, 256, gffn_s)
                            for j in range(2):
                                for ct in range(2):
                                    fc = f2 * 2 + ct
                                    pg = (pi[0] * 2) % 4
                                    pu = pg + 1
                                    pi[0] += 1

                                    def mmg(e, W=wg, P=pg, j=j, ct=ct):
                                        ins = None
                                        for k in range(KC):
                                            ins = e.matmul(PS[P][:], lhsT=W[:, k, ct * 128:(ct + 1) * 128], rhs=hn[j][:, k, :],
                                                           start=(k == 0), stop=(k == KC - 1))
                                        return ins
                                    S.op("pe", mmg, reads=[wgB, hnB[j]], writes=[PSB[pg]])
                                    S.op("pe", lambda e: mmg(e, W=wu, P=pu), reads=[wuB, hnB[j]], writes=[PSB[pu]])
                                    s2 = sgi[0] % 2
                                    sgi[0] += 1
                                    S.op("act", lambda e: e.activation(out=sgt[s2][:], in_=PS[pg][:], func=AF.Silu),
                                         reads=[PSB[pg]], writes=[sgB[s2]])
                                    S.op("dve", lambda e: e.tensor_tensor(out=actT[:, fc, j * 512:(j + 1) * 512], in0=PS[pu][:],
                                                                          in1=sgt[s2][:], op=ALU.mult),
                                         reads=[PSB[pu], sgB[s2]], writes=[actB[fc]])
                        for cg in range(4):
                            wd, wdB = load_w(wdv[:, :, cg * 512:(cg + 1) * 512], 8, 512, None)
                            for tt in range(8):
                                pd = 4 + (pi[0] % 4)
                                pi[0] += 1

                                def mmd(e, wd=wd, pd=pd, tt=tt):
                                    ins = None
                                    for k in range(8):
                                        ins = e.matmul(PS[pd][:], lhsT=actT[:, k, tt * 128:(tt + 1) * 128], rhs=wd[:, k, :],
                                                       start=(k == 0), stop=(k == 7))
                                    return ins
                                S.op("pe", mmd, reads=[wdB] + actB, writes=[PSB[pd]])
                                S.op("dve", lambda e: e.scalar_tensor_tensor(out=acc[:, tt, cg * 512:(cg + 1) * 512], in0=PS[pd][:],
                                                                             scalar=Cs[:, tb * 8 + tt, ex:ex + 1],
                                                                             in1=acc[:, tt, cg * 512:(cg + 1) * 512],
                                                                             op0=ALU.mult, op1=ALU.add),
                                     reads=[PSB[pd], cB, accB[tt]], writes=[accB[tt]])
                    gfin = wst[0][:, 0:D]
                    fjunk = actT[:, 0:2, :]
                    S.dma("sp", gfin, g_final.rearrange("(o t) -> o t", o=1).partition_broadcast(128), writes=[wstB[0]])
                    for tt in range(8):
                        r0 = tb * 1024 + tt * 128
                        S.op("act", lambda e: e.activation(out=fjunk, in_=acc[:, tt, :].rearrange("p (a b) -> p a b", a=2), func=AF.Square, accum_out=fst[:, 0:1]),
                             reads=[accB[tt]], writes=[fB, actB[0], actB[1]])
                        S.op("dve", lambda e: e.tensor_scalar(out=fst[:, 1:2], in0=fst[:, 0:1], scalar1=1.0 / D, scalar2=1e-6,
                                                              op0=ALU.mult, op1=ALU.add), reads=[fB], writes=[fB])
                        S.op("act", lambda e: e.activation(out=fst[:, 3:4], in_=fst[:, 1:2], func=AF.Sqrt), reads=[fB], writes=[fB])
                        S.op("dve", lambda e: e.reciprocal(out=fst[:, 2:3], in_=fst[:, 3:4]), reads=[fB], writes=[fB])
                        S.op("dve", lambda e: e.scalar_tensor_tensor(out=acc[:, tt, :], in0=acc[:, tt, :], scalar=fst[:, 2:3],
                                                                     in1=gfin, op0=ALU.mult, op1=ALU.mult),
                             reads=[accB[tt], fB, wstB[0]], writes=[accB[tt]])
                        S.dma("pool", out[r0:r0 + 128, :], acc[:, tt, :], reads=[accB[tt]])
                S.barrier()
        if upto >= 10 and sparse:
            with contextlib.ExitStack() as ls:
                Rs = sb("s_R", [128, 16, 66], F32, ls)
                rB = Buf()
                S.dma("sp", Rs[:], R_d.rearrange("(t p) e -> p t e", p=128), writes=[rB])
                lsf = sb("s_lsf", [128, 128], F32, ls)
                lsb = sb("s_lsb", [128, 128], BF16, ls)
                onb = sb("s_onb", [128, 128], BF16, ls)
                iog = sb("s_iog", [128, 16], F32, ls)
                iod = sb("s_iod", [128, 8], F32, ls)
                S.dma("sp", lsf[:], c_ls[:, :], writes=[rB])
                S.dma("sp", iog[:], c_iog[:, :], writes=[rB])
                S.dma("sp", iod[:], c_iod[:, :], writes=[rB])
                S.op("dve", lambda e: e.tensor_copy(out=lsb[:], in_=lsf[:]), reads=[rB], writes=[rB])
                S.op("dve", lambda e: e.memset(onb[:], 1.0), writes=[rB])
                Ab = sb("s_Ab", [128, 16, 32], BF16, ls)
                S.op("dve", lambda e: e.tensor_tensor(out=Ab[:], in0=Rs[:, :, 0:32], in1=Rs[:, :, 32:64], op=ALU.add), reads=[rB], writes=[rB])
                def mmc(e):
                    ins = None
                    for i in range(16):
                        ins = e.matmul(PS[0][:, 0:32], lhsT=onb[:], rhs=Ab[:, i, :], start=(i == 0), stop=(i == 15))
                    return ins
                S.op("pe", mmc, reads=[rB], writes=[PSB[0]])
                cnt = sb("s_cnt", [128, 32], F32, ls)
                nb = sb("s_nb", [128, 32], F32, ls)
                pend = sb("s_pend", [128, 32], F32, ls)
                pst = sb("s_pst", [128, 32], F32, ls)
                one32 = sb("s_one32", [128, 32], F32, ls)
                tmp32 = sb("s_tmp32", [128, 32], F32, ls)
                eb = sb("s_eb", [128, 64], F32, ls)
                cB = Buf()
                S.op("dve", lambda e: e.tensor_copy(out=cnt[:], in_=PS[0][:, 0:32]), reads=[PSB[0]], writes=[cB])
                S.op("dve", lambda e: e.memset(nb[:], 0.0), writes=[cB])
                S.op("dve", lambda e: e.memset(one32[:], 1.0), writes=[cB])
                for j in range(16):
                    S.op("dve", lambda e, j=j: e.scalar_tensor_tensor(out=nb[:], in0=cnt[:], scalar=128.0 * j, in1=nb[:],
                                                                      op0=ALU.is_gt, op1=ALU.add), reads=[cB], writes=[cB])
                S.op("dve", lambda e: e.tensor_scalar(out=nb[:], in0=nb[:], scalar1=128.0, scalar2=None, op0=ALU.mult), reads=[cB], writes=[cB])
                S.op("dve", lambda e: e.tensor_tensor_scan(out=pend[:], data0=one32[:], data1=nb[:], initial=0.0,
                                                           op0=ALU.mult, op1=ALU.add), reads=[cB], writes=[cB])
                S.op("dve", lambda e: e.tensor_tensor(out=pst[:], in0=pend[:], in1=nb[:], op=ALU.subtract), reads=[cB], writes=[cB])
                for b_ in range(64):
                    S.op("dve", lambda e, b_=b_: e.tensor_scalar(out=tmp32[:], in0=pend[:], scalar1=128.0 * b_, scalar2=0.0,
                                                                 op0=ALU.is_le, op1=ALU.add, accum_out=eb[:, b_:b_ + 1]),
                         reads=[cB], writes=[cB])
                S.op("dve", lambda e: e.tensor_scalar(out=eb[:], in0=eb[:], scalar1=31.0, scalar2=None, op0=ALU.min), reads=[cB], writes=[cB])
                ebg = sb("s_ebg", [128, 64], F32, ls)
                ebd = sb("s_ebd", [128, 64], F32, ls)
                S.op("dve", lambda e: e.tensor_scalar(out=ebg[:], in0=eb[:], scalar1=2048.0, scalar2=None, op0=ALU.mult), reads=[cB], writes=[cB])
                S.op("dve", lambda e: e.tensor_scalar(out=ebd[:], in0=eb[:], scalar1=1024.0, scalar2=None, op0=ALU.mult), reads=[cB], writes=[cB])
                igf = sb("s_igf", [128, 64, 16], F32, ls)
                idf = sb("s_idf", [128, 64, 8], F32, ls)
                igi = sb("s_igi", [128, 64, 16], I32, ls)
                idi = sb("s_idi", [128, 64, 8], I32, ls)
                for b_ in range(64):
                    S.op("dve", lambda e, b_=b_: e.tensor_scalar(out=igf[:, b_, :], in0=iog[:], scalar1=ebg[:, b_:b_ + 1], scalar2=None, op0=ALU.add),
                         reads=[cB, rB], writes=[cB])
                    S.op("dve", lambda e, b_=b_: e.tensor_scalar(out=idf[:, b_, :], in0=iod[:], scalar1=ebd[:, b_:b_ + 1], scalar2=None, op0=ALU.add),
                         reads=[cB, rB], writes=[cB])
                S.op("dve", lambda e: e.tensor_copy(out=igi[:], in_=igf[:]), reads=[cB], writes=[cB])
                S.op("dve", lambda e: e.tensor_copy(out=idi[:], in_=idf[:]), reads=[cB], writes=[cB])
                dsf = sb("s_dsf", [128, 16, 2], F32, ls)
                dsi = sb("s_dsi", [128, 16, 2], I32, ls)
                pos = sb("s_pos", [128, 32], F32, ls)
                dB = Buf()
                for i in range(16):
                    pb = 1 + (i % 3)

                    def mmr(e, i=i, pb=pb):
                        for i2 in range(i):
                            e.matmul(PS[pb][:, 0:32], lhsT=onb[:], rhs=Ab[:, i2, :], start=(i2 == 0), stop=False)
                        return e.matmul(PS[pb][:, 0:32], lhsT=lsb[:], rhs=Ab[:, i, :], start=(i == 0), stop=True)
                    S.op("pe", mmr, reads=[rB], writes=[PSB[pb]])
                    S.op("dve", lambda e: e.tensor_tensor(out=pos[:], in0=PS[pb][:, 0:32], in1=pst[:], op=ALU.add),
                         reads=[PSB[pb], cB, dB], writes=[dB])
                    for k_ in range(2):
                        S.op("dve", lambda e, k_=k_: e.tensor_tensor(out=tmp32[:], in0=pos[:], in1=Rs[:, i, k_ * 32:(k_ + 1) * 32], op=ALU.mult),
                             reads=[dB, rB, cB], writes=[cB])
                        S.op("dve", lambda e, k_=k_: e.reduce_sum(out=dsf[:, i, k_:k_ + 1], in_=tmp32[:], axis=AX.X), reads=[cB, dB], writes=[dB])
                S.op("dve", lambda e: e.tensor_copy(out=dsi[:], in_=dsf[:]), reads=[dB], writes=[dB])
                ls_d = contextlib.ExitStack()
                ht = [sb(f"s_ht{i}", [128, D], BF16, ls_d) for i in range(2)]
                htB = [Buf() for _ in range(2)]
                xsB = Buf()
                for i in range(16):
                    a = i % 2
                    S.dma("sp", ht[a][:], hn_d[i * 128:(i + 1) * 128, :], writes=[htB[a]])
                    for k_ in range(2):
                        S.idma(xs_d[:, :], bass.IndirectOffsetOnAxis(ap=dsi[:, i, k_:k_ + 1], axis=0), ht[a][:], None,
                               reads=[htB[a], dB], writes=[xsB])
                S.barrier()
                ls_d.close()
                ls_b = contextlib.ExitStack()
                wg_rows = w_gate.rearrange("e k n -> (e k) n")
                wu_rows = w_up.rearrange("e k n -> (e k) n")
                wd_rows = w_down.rearrange("e k n -> (e k) n")
                xb = [sb(f"s_xb{i}", [128, D], BF16, ls_b) for i in range(2)]
                xbB = [Buf() for _ in range(2)]
                xT = [sb(f"s_xT{i}", [128, KC, 128], BF16, ls_b) for i in range(2)]
                xTB = [Buf() for _ in range(2)]
                gst = [sb(f"s_gst{i}", [128, 1024], F32, ls_b) for i in range(12)]
                gstB = [Buf() for _ in range(12)]
                gbf = [sb(f"s_gbf{i}", [128, 1024], BF16, ls_b) for i in range(12)]
                gbfB = [Buf() for _ in range(12)]
                dst_ = [sb(f"s_dst{i}", [128, D], F32, ls_b) for i in range(6)]
                dstB = [Buf() for _ in range(6)]
                dbf = [sb(f"s_dbf{i}", [128, D], BF16, ls_b) for i in range(6)]
                dbfB = [Buf() for _ in range(6)]
                sgl = sb("s_sgl", [128, 1024], F32, ls_b)
                sglB = Buf()
                actb = sb("s_act", [128, 1024], BF16, ls_b)
                actB_ = Buf()
                aT = sb("s_aT", [128, 8, 128], BF16, ls_b)
                aTB = Buf()
                yb = [sb(f"s_yb{i}", [128, D], BF16, ls_b) for i in range(2)]
                ybB = [Buf() for _ in range(2)]
                ysB = Buf()
                gi = 0
                gbi = 0
                di = 0
                dbi = 0
                ce = 0
                for b_ in range(64):
                    x2i = b_ % 2
                    S.dma("sp", xb[x2i][:], xs_d[b_ * 128:(b_ + 1) * 128, :], writes=[xbB[x2i]])
                    for g4 in range(4):
                        pb = 4 + g4
                        pvw = PS[pb][:].bitcast(BF16)

                        def tr(e, g4=g4, pvw=pvw, x2i=x2i):
                            ins = None
                            for q in range(4):
                                kc = g4 * 4 + q
                                ins = e.transpose(out=pvw[:, q * 128:(q + 1) * 128], in_=xb[x2i][:, kc * 128:(kc + 1) * 128], identity=ident_b[:])
                            return ins
                        S.op("pe", tr, reads=[xbB[x2i]], writes=[PSB[pb]])
                        eng = "act" if g4 % 2 == 0 else "dve"
                        o_ = xT[x2i][:, g4 * 4:(g4 + 1) * 4, :]
                        i_ = pvw[:, 0:512].rearrange("p (k t) -> p k t", k=4)
                        if eng == "act":
                            S.op("act", lambda e: e.copy(out=o_, in_=i_), reads=[PSB[pb]], writes=[xTB[x2i]])
                        else:
                            S.op("dve", lambda e: e.tensor_copy(out=o_, in_=i_), reads=[PSB[pb]], writes=[xTB[x2i]])
                    for kc in range(KC):
                        wpair = []
                        for rows in (wg_rows, wu_rows):
                            a = gi % 12
                            gi += 1
                            bq = gbi % 12
                            gbi += 1
                            S.idma(gst[a][:], None, rows[:, :], bass.IndirectOffsetOnAxis(ap=igi[:, b_, kc:kc + 1], axis=0),
                                   reads=[cB], writes=[gstB[a]])
                            ceng = "dve" if ce % 2 == 0 else "act"
                            ce += 1
                            if ceng == "dve":
                                S.op("dve", lambda e: e.tensor_scalar(out=gbf[bq][:], in0=gst[a][:], scalar1=gffn_s[:, kc:kc + 1], scalar2=None, op0=ALU.mult),
                                     reads=[gstB[a]], writes=[gbfB[bq]])
                            else:
                                S.op("act", lambda e: e.activation(out=gbf[bq][:], in_=gst[a][:], func=AF.Copy, scale=gffn_s[:, kc:kc + 1]),
                                     reads=[gstB[a]], writes=[gbfB[bq]])
                            wpair.append(bq)

                        def mmgu(e, kc=kc, wpair=wpair, x2i=x2i):
                            ins = None
                            for wi_, bq in enumerate(wpair):
                                for hf in range(2):
                                    ins = e.matmul(PS[wi_ * 2 + hf][:], lhsT=xT[x2i][:, kc, :], rhs=gbf[bq][:, hf * 512:(hf + 1) * 512],
                                                   start=(kc == 0), stop=(kc == KC - 1))
                            return ins
                        S.op("pe", mmgu, reads=[xTB[x2i], gbfB[wpair[0]], gbfB[wpair[1]]], writes=[PSB[0], PSB[1], PSB[2], PSB[3]])
                    for hf in range(2):
                        S.op("act", lambda e, hf=hf: e.activation(out=sgl[:, hf * 512:(hf + 1) * 512], in_=PS[hf][:], func=AF.Silu),
                             reads=[PSB[hf]], writes=[sglB])
                        S.op("dve", lambda e, hf=hf: e.tensor_tensor(out=actb[:, hf * 512:(hf + 1) * 512], in0=PS[2 + hf][:],
                                                                     in1=sgl[:, hf * 512:(hf + 1) * 512], op=ALU.mult),
                             reads=[PSB[2 + hf], sglB], writes=[actB_])
                    for g2 in range(2):
                        pb = g2
                        pvw = PS[pb][:].bitcast(BF16)

                        def tr2(e, g2=g2, pvw=pvw):
                            ins = None
                            for q in range(4):
                                fc = g2 * 4 + q
                                ins = e.transpose(out=pvw[:, q * 128:(q + 1) * 128], in_=actb[:, fc * 128:(fc + 1) * 128], identity=ident_b[:])
                            return ins
                        S.op("pe", tr2, reads=[actB_], writes=[PSB[pb]])
                        o_ = aT[:, g2 * 4:(g2 + 1) * 4, :]
                        i_ = pvw[:, 0:512].rearrange("p (k t) -> p k t", k=4)
                        if g2 == 0:
                            S.op("act", lambda e: e.copy(out=o_, in_=i_), reads=[PSB[pb]], writes=[aTB])
                        else:
                            S.op("dve", lambda e: e.tensor_copy(out=o_, in_=i_), reads=[PSB[pb]], writes=[aTB])
                    for fc in range(8):
                        a = di % 6
                        di += 1
                        bq = dbi % 6
                        dbi += 1
                        S.idma(dst_[a][:], None, wd_rows[:, :], bass.IndirectOffsetOnAxis(ap=idi[:, b_, fc:fc + 1], axis=0),
                               reads=[cB], writes=[dstB[a]])
                        if fc % 2 == 0:
                            S.op("dve", lambda e: e.tensor_copy(out=dbf[bq][:], in_=dst_[a][:]), reads=[dstB[a]], writes=[dbfB[bq]])
                        else:
                            S.op("act", lambda e: e.copy(out=dbf[bq][:], in_=dst_[a][:]), reads=[dstB[a]], writes=[dbfB[bq]])

                        def mmd(e, fc=fc, bq=bq):
                            ins = None
                            for cg in range(4):
                                ins = e.matmul(PS[4 + cg][:], lhsT=aT[:, fc, :], rhs=dbf[bq][:, cg * 512:(cg + 1) * 512],
                                               start=(fc == 0), stop=(fc == 7))
                            return ins
                        S.op("pe", mmd, reads=[aTB, dbfB[bq]], writes=[PSB[4], PSB[5], PSB[6], PSB[7]])
                    for cg in range(4):
                        if cg % 2 == 0:
                            S.op("act", lambda e, cg=cg: e.copy(out=yb[x2i][:, cg * 512:(cg + 1) * 512], in_=PS[4 + cg][:]),
                                 reads=[PSB[4 + cg]], writes=[ybB[x2i]])
                        else:
                            S.op("dve", lambda e, cg=cg: e.tensor_copy(out=yb[x2i][:, cg * 512:(cg + 1) * 512], in_=PS[4 + cg][:]),
                                 reads=[PSB[4 + cg]], writes=[ybB[x2i]])
                    S.dma("sp", ys_d[b_ * 128:(b_ + 1) * 128, :], yb[x2i][:], reads=[ybB[x2i]], writes=[ysB])
                S.barrier()
                ls_b.close()
                gfin = sb("s_gf", [128, D], F32, ls)
                fB0 = Buf()
                S.dma("sp", gfin[:], g_final.rearrange("(o t) -> o t", o=1).partition_broadcast(128), writes=[fB0])
                g1 = [sb(f"s_g1{i}", [128, D], BF16, ls) for i in range(2)]
                g2_ = [sb(f"s_g2{i}", [128, D], BF16, ls) for i in range(2)]
                xr = [sb(f"s_xr{i}", [128, D], F32, ls) for i in range(2)]
                gB_ = [Buf() for _ in range(2)]
                xrB = [Buf() for _ in range(2)]
                fst = [sb(f"s_fst{i}", [128, 4], F32, ls) for i in range(2)]
                fj = sb("s_fj", [128, D], BF16, ls)
                fjB = Buf()
                for i in range(16):
                    a = i % 2
                    S.dma("sp", xr[a][:], x2_d[i * 128:(i + 1) * 128, :], writes=[xrB[a]])
                    S.idma(g1[a][:], None, ys_d[:, :], bass.IndirectOffsetOnAxis(ap=dsi[:, i, 0:1], axis=0), reads=[dB, ysB], writes=[gB_[a]])
                    S.idma(g2_[a][:], None, ys_d[:, :], bass.IndirectOffsetOnAxis(ap=dsi[:, i, 1:2], axis=0), reads=[dB, ysB], writes=[gB_[a]])
                    S.op("dve", lambda e: e.scalar_tensor_tensor(out=xr[a][:], in0=g1[a][:], scalar=Rs[:, i, 64:65], in1=xr[a][:],
                                                                 op0=ALU.mult, op1=ALU.add), reads=[gB_[a], rB, xrB[a]], writes=[xrB[a]])
                    S.op("dve", lambda e: e.scalar_tensor_tensor(out=xr[a][:], in0=g2_[a][:], scalar=Rs[:, i, 65:66], in1=xr[a][:],
                                                                 op0=ALU.mult, op1=ALU.add), reads=[gB_[a], rB, xrB[a]], writes=[xrB[a]])
                    S.op("act", lambda e: e.activation(out=fj[:], in_=xr[a][:], func=AF.Square, accum_out=fst[a][:, 0:1]),
                         reads=[xrB[a]], writes=[fjB, xrB[a]])
                    S.op("dve", lambda e: e.tensor_scalar(out=fst[a][:, 1:2], in0=fst[a][:, 0:1], scalar1=1.0 / D, scalar2=1e-6,
                                                          op0=ALU.mult, op1=ALU.add), reads=[xrB[a]], writes=[xrB[a]])
                    S.op("act", lambda e: e.activation(out=fst[a][:, 3:4], in_=fst[a][:, 1:2], func=AF.Sqrt), reads=[xrB[a]], writes=[xrB[a]])
                    S.op("dve", lambda e: e.reciprocal(out=fst[a][:, 2:3], in_=fst[a][:, 3:4]), reads=[xrB[a]], writes=[xrB[a]])
                    S.op("dve", lambda e: e.scalar_tensor_tensor(out=xr[a][:], in0=xr[a][:], scalar=fst[a][:, 2:3], in1=gfin[:],
                                                                 op0=ALU.mult, op1=ALU.mult), reads=[xrB[a], fB0], writes=[xrB[a]])
                    S.dma("sp", out[i * 128:(i + 1) * 128, :], xr[a][:], reads=[xrB[a]])
                S.barrier()
        S.barrier()
    return nc


def _consts():
    i = np.arange(64, dtype=np.float32)
    inv = (1.0 / (10000.0 ** (np.arange(0, 128, 2, dtype=np.float32) / 128.0))).astype(np.float32)
    c_rope = np.zeros((128, 2), np.float32)
    c_rope[:, 0] = np.concatenate([inv, inv])
    c_rope[:64, 1] = -1.0
    c_rope[64:, 1] = 1.0
    c_swap = np.zeros((128, 128), np.float32)
    for m in range(128):
        c_swap[(m + 64) % 128, m] = 1.0
    c_ident = np.eye(128, dtype=np.float32)
    kl = np.arange(128)[:, None, None] + 128 * np.arange(4)[None, :, None]
    ql = np.arange(512)[None, None, :]
    c_tri = (kl <= ql).astype(np.float32)
    c_ls = (np.arange(128)[:, None] < np.arange(128)[None, :]).astype(np.float32)
    c_iog = (np.arange(16)[None, :] * 128 + np.arange(128)[:, None]).astype(np.float32)
    c_iod = (np.arange(8)[None, :] * 128 + np.arange(128)[:, None]).astype(np.float32)
    return dict(c_rope=c_rope, c_swap=c_swap, c_ident=c_ident, c_tri=np.ascontiguousarray(c_tri),
                c_ls=c_ls, c_iog=np.ascontiguousarray(c_iog), c_iod=np.ascontiguousarray(c_iod))


def make_in_maps(inputs):
    x = np.asarray(inputs["x"], np.float32)
    pos = np.asarray(inputs["positions"], np.int32)
    f = lambda k: np.ascontiguousarray(np.asarray(inputs[k], np.float32)[0])
    pk = lambda v: np.ascontiguousarray(v.reshape(-1, 128).T)
    shared = dict(
        g_mix=pk(f("g_mix")), w_in=f("w_in"), conv_w=np.ascontiguousarray(f("conv_w").reshape(4, KC, 128).transpose(2, 0, 1)), conv_b=pk(f("conv_b")),
        w_rg_a=f("w_rg_a"), b_rg_a=pk(f("b_rg_a")), w_rg_x=f("w_rg_x"), b_rg_x=pk(f("b_rg_x")),
        lru_param=pk(f("lru_param")),
        lam_in=np.ascontiguousarray(np.stack([f("lambda_q1"), f("lambda_k1"), f("lambda_q2"), f("lambda_k2")])),
        subln_g=f("subln_g"), w_br_rnn=f("w_br_rnn"), w_br_attn=f("w_br_attn"), w_out=f("w_out"),
        g_ffn=pk(f("g_ffn")),
        w_router=np.ascontiguousarray(np.concatenate([f("w_grp_router"), f("w_exp_router")], axis=1)),
        w_gate=f("w_gate"), w_up=f("w_up"), w_down=f("w_down"),
        g_final=np.ascontiguousarray(np.asarray(inputs["g_final"], np.float32)),
    )
    shared.update(_consts())
    maps = []
    for b in range(2):
        for j in range(4):
            n = (j + 1) * 2048
            xfp = np.zeros((T, D), np.float32)
            xfp[T - n:] = x[b, :n]
            pp = np.zeros((T,), np.int32)
            pp[T - n:] = pos[b, :n]
            vv = np.zeros((T,), np.float32)
            vv[T - n:] = 1.0
            m = dict(shared)
            vp = np.zeros((T,), np.float32)
            vp[1:] = vv[:-1]
            m.update(xf=xfp, posf=pp, valid=vv, validp=vp, valid_pk=pk(vv))
            maps.append(m)
    return maps


def kernel(**inputs):
    nc = build()
    maps = make_in_maps(inputs)
    res = run_bass_kernel_spmd(nc, maps, core_ids=list(range(8)))
    outp = np.zeros((2, 8192, D), np.float32)
    for c in range(8):
        b, j = c // 4, c % 4
        outp[b, j * 2048:(j + 1) * 2048] = res.results[c]["out"]
    return outp
```
